# Optimizing a Trainium2 kernel written in Bass

```python
import jax, jax.numpy as jnp
from jax import lax
import numpy as np

D_MODEL = 1024
BATCH = 8
SEQ = 4096
DEPTH = 2

PL_DIM = 256
EPS = 1e-6
A_WIDTH = 512
A_GROUPS = 8
CONV_W = 3
SB_HEADS = 8
SB_HEAD_DIM = 64
SB_WIDTH = SB_HEADS * SB_HEAD_DIM
Q_BLOCK = 128
IN_EVEN = 3 * A_WIDTH + 3 * SB_WIDTH
MIX_EVEN = A_WIDTH + SB_WIDTH
C_WIDTH = 2048
C_GROUPS = 8
C_GROUP_DIM = C_WIDTH // C_GROUPS
CHUNK = 128
N_GROUPS = 4
EXP_PER_GROUP = 8
N_EXPERTS = N_GROUPS * EXP_PER_GROUP
TOP_K = 2
D_EXPERT = 256
N_EVEN = (DEPTH + 1) // 2
N_ODD = DEPTH // 2

kernel_name = "hybrid_conv_stickbreak_gmlp_hmoe"


def rmsnorm(x, g):
    xf = x.astype(jnp.float32)
    y = xf * lax.rsqrt(jnp.mean(xf * xf, axis=-1, keepdims=True) + EPS)
    return (y * g.astype(jnp.float32)).astype(x.dtype)


def short_conv_mixer(h, gb, gc, conv_w):
    z = gc * h
    kern = conv_w[:, None, :].astype(z.dtype)
    y = lax.conv_general_dilated(z, kern, window_strides=(1,), padding=[(CONV_W - 1, 0)],
                                 dimension_numbers=('NWC', 'WIO', 'NWC'),
                                 feature_group_count=A_WIDTH)
    return gb * y


def stick_breaking_attention(q, k, v):
    S = q.shape[1]
    scale = SB_HEAD_DIM ** -0.5
    outs = []
    for blk in range(S // Q_BLOCK):
        t0 = blk * Q_BLOCK
        L = t0 + Q_BLOCK
        z = jnp.einsum('bqhd,bkhd->bhqk', q[:, t0:L], k[:, :L],
                       preferred_element_type=jnp.float32) * scale
        t_idx = t0 + jnp.arange(Q_BLOCK)[:, None]
        s_idx = jnp.arange(L)[None, :]
        mask = s_idx < t_idx
        log_beta = jax.nn.log_sigmoid(z)
        log_rem = jnp.where(mask, jax.nn.log_sigmoid(-z), 0.0)
        later = lax.cumsum(log_rem, axis=3, reverse=True) - log_rem
        a = jnp.where(mask, jnp.exp(log_beta + later), 0.0)
        outs.append(jnp.einsum('bhqk,bkhd->bqhd', a.astype(v.dtype), v[:, :L]))
    return jnp.concatenate(outs, axis=1)


def even_mixer(xn, w_in, conv_w, w_out):
    B, S, _ = xn.shape
    proj = xn @ w_in
    cuts = [A_WIDTH, 2 * A_WIDTH, 3 * A_WIDTH, 3 * A_WIDTH + SB_WIDTH, 3 * A_WIDTH + 2 * SB_WIDTH]
    h, gb, gc, q, k, v = jnp.split(proj, cuts, axis=-1)
    y_a = short_conv_mixer(h, gb, gc, conv_w)
    shp = (B, S, SB_HEADS, SB_HEAD_DIM)
    y_b = stick_breaking_attention(q.reshape(shp), k.reshape(shp), v.reshape(shp))
    y = jnp.concatenate([y_a, y_b.reshape(B, S, SB_WIDTH)], axis=-1)
    return y @ w_out


def odd_mixer(xn, w_in, g_v, w_s, b_s, w_out):
    B, S, _ = xn.shape
    zc = jax.nn.gelu(xn @ w_in)
    u, v = jnp.split(zc, 2, axis=-1)
    v = rmsnorm(v, g_v)
    n_chunks = S // CHUNK
    v = v.reshape(B, n_chunks, CHUNK, C_GROUPS, C_GROUP_DIM)
    causal = jnp.tril(jnp.ones((CHUNK, CHUNK), dtype=w_s.dtype))
    w_m = w_s * causal
    gate = jnp.einsum('gts,bcsgd->bctgd', w_m, v) + b_s.T[None, None, :, :, None]
    y = u * gate.reshape(B, S, C_WIDTH)
    return y @ w_out


def hier_moe(xn, wc, bc, wf, bf, w1, w3, w2):
    B, S, D = xn.shape
    T = B * S
    xt = xn.reshape(T, D)
    pc = jax.nn.softmax((xt @ wc + bc).astype(jnp.float32), axis=-1)
    pg, gi = lax.top_k(pc, 1)
    lf = (xt @ wf + bf).astype(jnp.float32).reshape(T, N_GROUPS, EXP_PER_GROUP)
    idx = jnp.broadcast_to(gi[:, :, None], (T, 1, EXP_PER_GROUP))
    lf_sel = jnp.take_along_axis(lf, idx, axis=1)[:, 0]
    pf = jax.nn.softmax(lf_sel, axis=-1)
    wtop, ei = lax.top_k(pf, TOP_K)
    wtop = wtop / jnp.sum(wtop, axis=-1, keepdims=True)
    gates = pg * wtop
    eid = gi * EXP_PER_GROUP + ei
    comb = jnp.sum(jax.nn.one_hot(eid, N_EXPERTS, dtype=jnp.float32) * gates[..., None], axis=1)
    comb = comb.astype(xt.dtype)
    y = jnp.zeros_like(xt)
    for e in range(N_EXPERTS):
        he = jax.nn.silu(xt @ w1[e]) * (xt @ w3[e])
        y = y + comb[:, e:e + 1] * (he @ w2[e])
    return y.reshape(B, S, D)


def _nrm(k, shape, scale):
    return jax.random.normal(k, shape, jnp.float32) * scale


def _gain(k, shape):
    return 1.0 + 0.02 * jax.random.normal(k, shape, jnp.float32)


def setup_inputs(seed: int = 0) -> dict:
    key = jax.random.key(seed)
    ks = jax.random.split(key, 24)
    D = D_MODEL
    return {
        "x": _nrm(ks[0], (BATCH, SEQ, D), 1.0),
        "p": _nrm(ks[1], (DEPTH, BATCH, SEQ, PL_DIM), 1.0),
        "norm_mix": _gain(ks[2], (DEPTH, D)),
        "norm_ffn": _gain(ks[3], (DEPTH, D)),
        "norm_pl": _gain(ks[4], (DEPTH, D)),
        "final_norm": _gain(ks[5], (D,)),
        "w_in_even": _nrm(ks[6], (N_EVEN, D, IN_EVEN), D ** -0.5),
        "conv_w_even": _nrm(ks[7], (N_EVEN, CONV_W, A_WIDTH), CONV_W ** -0.5),
        "w_out_even": _nrm(ks[8], (N_EVEN, MIX_EVEN, D), MIX_EVEN ** -0.5),
        "w_in_odd": _nrm(ks[9], (N_ODD, D, 2 * C_WIDTH), D ** -0.5),
        "g_v_odd": _gain(ks[10], (N_ODD, C_WIDTH)),
        "w_s_odd": _nrm(ks[11], (N_ODD, C_GROUPS, CHUNK, CHUNK), CHUNK ** -0.5),
        "b_s_odd": 1.0 + _nrm(ks[12], (N_ODD, C_GROUPS, CHUNK), 0.02),
        "w_out_odd": _nrm(ks[13], (N_ODD, C_WIDTH, D), C_WIDTH ** -0.5),
        "router_c": _nrm(ks[14], (DEPTH, D, N_GROUPS), D ** -0.5),
        "router_c_b": _nrm(ks[15], (DEPTH, N_GROUPS), 0.01),
        "router_f": _nrm(ks[16], (DEPTH, D, N_EXPERTS), D ** -0.5),
        "router_f_b": _nrm(ks[17], (DEPTH, N_EXPERTS), 0.01),
        "moe_w1": _nrm(ks[18], (DEPTH, N_EXPERTS, D, D_EXPERT), D ** -0.5),
        "moe_w3": _nrm(ks[19], (DEPTH, N_EXPERTS, D, D_EXPERT), D ** -0.5),
        "moe_w2": _nrm(ks[20], (DEPTH, N_EXPERTS, D_EXPERT, D), D_EXPERT ** -0.5),
        "w_pe": _nrm(ks[21], (DEPTH, PL_DIM, D), PL_DIM ** -0.5),
        "w_pg": _nrm(ks[22], (DEPTH, D, D), D ** -0.5),
    }


def reference(x, p, norm_mix, norm_ffn, norm_pl, final_norm,
              w_in_even, conv_w_even, w_out_even,
              w_in_odd, g_v_odd, w_s_odd, b_s_odd, w_out_odd,
              router_c, router_c_b, router_f, router_f_b,
              moe_w1, moe_w3, moe_w2, w_pe, w_pg):
    h = x
    for i in range(DEPTH):
        j = i // 2
        xn = rmsnorm(h, norm_mix[i])
        if i % 2 == 0:
            h = h + even_mixer(xn, w_in_even[j], conv_w_even[j], w_out_even[j])
        else:
            h = h + odd_mixer(xn, w_in_odd[j], g_v_odd[j], w_s_odd[j], b_s_odd[j], w_out_odd[j])
        xn = rmsnorm(h, norm_ffn[i])
        h = h + hier_moe(xn, router_c[i], router_c_b[i], router_f[i], router_f_b[i],
                         moe_w1[i], moe_w3[i], moe_w2[i])
        gate = jax.nn.sigmoid(rmsnorm(h, norm_pl[i]) @ w_pg[i])
        h = h + gate * (p[i] @ w_pe[i])
    return rmsnorm(h, final_norm)
```

```python
import numpy as np
from contextlib import ExitStack
import concourse.bass as bass
import concourse.mybir as mybir
from concourse.bass_utils import run_bass_kernel_spmd

F32 = mybir.dt.float32
BF16 = mybir.dt.bfloat16
AF = mybir.ActivationFunctionType
ALU = mybir.AluOpType
AX = mybir.AxisListType

EPOCH = 30000
NDMASEM = 12
S = 4096
D = 1024
NB = S // 128
EPS = 1e-6


class Prog:
    ENG = ("pe", "act", "dve", "pool", "sp")

    def __init__(self, nc, stack):
        self.nc = nc
        self.stack = stack
        self.ops = []
        self.ccount = {e: 0 for e in self.ENG}
        self.dcount = {e: 0 for e in self.ENG}
        self.csem = {e: [] for e in self.ENG}
        self.dsem = {e: [] for e in self.ENG}
        self.dma_hist = {e: [] for e in self.ENG}
        self.waited = {e: {} for e in self.ENG}
        self.nops = 0

    def eng_obj(self, e):
        nc = self.nc
        return {"pe": nc.tensor, "act": nc.scalar, "dve": nc.vector,
                "pool": nc.gpsimd, "sp": nc.sync}[e]

    def op(self, eng, fn, reads=(), writes=()):
        self.ops.append(dict(eng=eng, fn=fn, reads=tuple(reads), writes=tuple(writes), dma=False))

    def dma(self, eng, fn, reads=(), writes=()):
        self.ops.append(dict(eng=eng, fn=fn, reads=tuple(reads), writes=tuple(writes), dma=True))

    def _csem(self, e, ep):
        while len(self.csem[e]) <= ep:
            self.csem[e].append(self.stack.enter_context(self.nc.semaphore(f"c_{e}_{len(self.csem[e])}")))
        return self.csem[e][ep]

    def _dsem(self, e, k):
        while len(self.dsem[e]) <= k:
            self.dsem[e].append(self.stack.enter_context(self.nc.semaphore(f"d_{e}_{len(self.dsem[e])}")))
        return self.dsem[e][k]

    def _wait(self, e, key, sem, val):
        w = self.waited[e]
        if key[0] == "c":
            for kk, v in w.items():
                if kk[0] == "c" and kk[1] == key[1] and (kk[2] > key[2] or (kk[2] == key[2] and v >= val)):
                    return
        elif w.get(key, 0) >= val:
            return
        self.eng_obj(e).wait_ge(sem, val)
        w[key] = max(w.get(key, 0), val)

    def flush(self):
        ops = self.ops
        n = len(ops)
        self.nops += n
        lw, rdc, rdd = {}, {}, {}
        deps = [None] * n
        dlist = {e: [] for e in self.ENG}
        for i, o in enumerate(ops):
            d = set()
            for k in o["reads"]:
                w = lw.get(k)
                if w is not None:
                    d.add(w)
            for k in o["writes"]:
                w = lw.get(k)
                if w is not None:
                    d.add(w)
                d.update(rdc.get(k, {}).values())
                d.update(rdd.get(k, ()))
            for k in o["reads"]:
                if o["dma"]:
                    rdd.setdefault(k, []).append(i)
                else:
                    rdc.setdefault(k, {})[o["eng"]] = i
            for k in o["writes"]:
                lw[k] = i
                rdc[k] = {}
                rdd[k] = []
            if o["dma"]:
                q = o["eng"]
                o["dn"] = self.dcount[q]
                self.dcount[q] += 1
                lst = dlist[q]
                if len(lst) >= NDMASEM:
                    d.add(lst[len(lst) - NDMASEM])
                lst.append(i)
            d.discard(i)
            if o["eng"] == "pe" and not o["dma"]:
                d = {j for j in d if not (ops[j]["eng"] == "pe" and not ops[j]["dma"])}
            deps[i] = d
        signaling = [False] * n
        for i in range(n):
            for j in deps[i]:
                signaling[j] = True
        last = {}
        for i, o in enumerate(ops):
            if not o["dma"]:
                last[o["eng"]] = i
        for e, i in last.items():
            signaling[i] = True
        for i, o in enumerate(ops):
            if not o["dma"] and signaling[i]:
                c = self.ccount[o["eng"]]
                o["sig"] = (c // EPOCH, c % EPOCH + 1)
                self.ccount[o["eng"]] = c + 1
        for i, o in enumerate(ops):
            e = o["eng"]
            need = {}
            for j in deps[i]:
                p = ops[j]
                if p["dma"]:
                    key = ("d", p["eng"], p["dn"] % NDMASEM)
                    val = 16 * (p["dn"] // NDMASEM + 1)
                    sem = self._dsem(p["eng"], p["dn"] % NDMASEM)
                else:
                    ep, cv = p["sig"]
                    key = ("c", p["eng"], ep)
                    val = cv
                    sem = self._csem(p["eng"], ep)
                if need.get(key, (None, 0))[1] < val:
                    need[key] = (sem, val)
            for key, (sem, val) in need.items():
                self._wait(e, key, sem, val)
            ins = o["fn"]()
            if o["dma"]:
                ins.then_inc(self._dsem(e, o["dn"] % NDMASEM), 16)
            elif signaling[i]:
                ep, cv = o["sig"]
                ins.then_inc(self._csem(e, ep), 1)
        for e in self.ENG:
            for e2, i in last.items():
                ep, cv = ops[i]["sig"]
                self._wait(e, ("c", e2, ep), self._csem(e2, ep), cv)
            for q in self.ENG:
                dc = self.dcount[q]
                for k in range(min(NDMASEM, dc)):
                    lastdn = ((dc - 1 - k) // NDMASEM) * NDMASEM + k
                    self._wait(e, ("d", q, k), self._dsem(q, k), 16 * (lastdn // NDMASEM + 1))
        self.ops = []


_UN = [0]


def _un(name):
    _UN[0] += 1
    return f"{name}_{_UN[0]}"


def _kl(k):
    return list(k) if isinstance(k, list) else [k]


def hk(blk):
    return [("hacc", blk, 0), ("hacc", blk, 1)]


class Rot:
    def __init__(self, tiles, name):
        self.tiles = tiles
        self.name = name
        self.i = 0

    def next(self):
        j = self.i % len(self.tiles)
        self.i += 1
        return self.tiles[j], (self.name, j)


def build(stop_after=99, dbg=False):
    nc = bass.Bass("TRN2", target_bir_lowering=False)

    def din(name, shape):
        return nc.dram_tensor(name, list(shape), F32, kind="ExternalInput").ap()

    x = din("x", [S, D])
    p = din("p", [2, S, 256])
    norm_mix = din("norm_mix", [2, D])
    norm_ffn = din("norm_ffn", [2, D])
    norm_pl = din("norm_pl", [2, D])
    final_norm = din("final_norm", [D])
    w_in_even = din("w_in_even", [1, D, 3072])
    conv_w_even = din("conv_w_even", [1, 3, 512])
    w_out_even = din("w_out_even", [1, 1024, D])
    w_in_odd = din("w_in_odd", [1, D, 4096])
    g_v_odd = din("g_v_odd", [1, 2048])
    w_s_odd = din("w_s_odd", [1, 8, 128, 128])
    b_s_odd = din("b_s_odd", [1, 8, 128])
    w_out_odd = din("w_out_odd", [1, 2048, D])
    router_c = din("router_c", [2, D, 4])
    router_c_b = din("router_c_b", [2, 4])
    router_f = din("router_f", [2, D, 32])
    router_f_b = din("router_f_b", [2, 32])
    moe_w1 = din("moe_w1", [2, 32, D, 256])
    moe_w3 = din("moe_w3", [2, 32, D, 256])
    moe_w2 = din("moe_w2", [2, 32, 256, D])
    w_pe = din("w_pe", [2, 256, D])
    w_pg = din("w_pg", [2, D, D])
    out = nc.dram_tensor("out", [S, D], F32, kind="ExternalOutput").ap()
    mixT = nc.dram_tensor("mixT", [8, 128, S], BF16, kind="ExternalOutput" if dbg else "Internal").ap()
    hA = nc.dram_tensor("hA", [S, D], F32, kind="ExternalOutput" if dbg else "Internal").ap()
    hB = nc.dram_tensor("hB", [S, D], F32, kind="ExternalOutput" if dbg else "Internal").ap()

    with ExitStack() as gst:
        P = Prog(nc, gst)

        def gsb(name, shape, dt):
            return gst.enter_context(nc.sbuf_tensor(_un(name), shape, dt))

        pb = [gst.enter_context(nc.psum_tensor(f"pb{i}", [128, 512], F32)) for i in range(7)]
        ptb = gst.enter_context(nc.psum_tensor("ptb", [128, 8, 128], BF16))

        identf = gsb("identf", [128, 128], F32)
        ident = gsb("ident", [128, 128], BF16)
        ntri = gsb("ntri", [128, 128], BF16)
        nones = gsb("nones", [128, 128], BF16)
        zeros = gsb("zeros", [128, 128], BF16)
        nmask = gsb("nmask", [128, 128], BF16)
        ones1 = gsb("ones1", [1, 128], BF16)
        tmpf = gsb("tmpf", [128, 128], F32)
        P.op("pool", lambda: nc.gpsimd.memset(identf[:], 1.0), writes=["identf"])
        P.op("pool", lambda: nc.gpsimd.affine_select(out=identf[:], in_=identf[:], pattern=[[-1, 128]],
                                                      compare_op=ALU.is_equal, fill=0.0, base=0, channel_multiplier=1),
             reads=["identf"], writes=["identf"])
        P.op("dve", lambda: nc.vector.tensor_copy(out=ident[:], in_=identf[:]), reads=["identf"], writes=["ident"])
        P.op("pool", lambda: nc.gpsimd.memset(tmpf[:], -1.0), writes=["tmpf"])
        P.op("dve", lambda: nc.vector.tensor_copy(out=nones[:], in_=tmpf[:]), reads=["tmpf"], writes=["nones"])
        P.op("pool", lambda: nc.gpsimd.affine_select(out=tmpf[:], in_=tmpf[:], pattern=[[-1, 128]],
                                                      compare_op=ALU.is_ge, fill=0.0, base=0, channel_multiplier=1),
             reads=["tmpf", "nones"], writes=["tmpf"])
        P.op("dve", lambda: nc.vector.tensor_copy(out=ntri[:], in_=tmpf[:]), reads=["tmpf"], writes=["ntri"])
        P.op("dve", lambda: nc.vector.tensor_scalar(out=nmask[:], in0=tmpf[:], scalar1=30000.0, scalar2=None, op0=ALU.mult),
             reads=["tmpf"], writes=["nmask"])
        P.op("pool", lambda: nc.gpsimd.memset(zeros[:], 0.0), writes=["zeros"])
        P.op("pool", lambda: nc.gpsimd.memset(ones1[:], 1.0), writes=["ones1"])
        P.flush()

        def norm_rstd(P, src, skey, width, junk, jkey, ss, sskey):
            P.op("act", lambda: nc.scalar.activation(out=junk, in_=src, func=AF.Square, accum_out=ss),
                 reads=_kl(skey), writes=[jkey, sskey])
            P.op("act", lambda: nc.scalar.activation(out=ss, in_=ss, func=AF.Ln, scale=1.0 / width, bias=epsb[:]),
                 reads=[sskey, "epsb"], writes=[sskey])
            P.op("act", lambda: nc.scalar.activation(out=ss, in_=ss, func=AF.Exp, scale=-0.5),
                 reads=[sskey], writes=[sskey])

        epsb = gsb("epsb", [128, 1], F32)
        P.op("pool", lambda: nc.gpsimd.memset(epsb[:], EPS), writes=["epsb"])

        njunk = Rot([gsb(f"njunk{i}", [128, 1024], BF16) for i in range(1)], "njunk")
        nss = Rot([gsb(f"nss{i}", [128, 1], F32) for i in range(4)], "nss")
        nxn = Rot([gsb(f"nxn{i}", [128, 1024], BF16) for i in range(2)], "nxn")

        def norm_T(P, src, skey, gB, gkey, dstT, dkey, cp_eng="act"):
            junk, jk = njunk.next()
            ss, ssk = nss.next()
            xn, xnk = nxn.next()
            norm_rstd(P, src, skey, 1024, junk[:], jk, ss[:], ssk)
            P.op("dve", lambda: nc.vector.scalar_tensor_tensor(out=xn[:], in0=src, scalar=ss[:], in1=gB,
                                                                 op0=ALU.mult, op1=ALU.mult),
                 reads=_kl(skey) + [ssk, gkey], writes=[xnk])
            for k in range(8):
                P.op("pe", lambda k=k: nc.tensor.transpose(out=ptb[:, k, :], in_=xn[:, k * 128:(k + 1) * 128], identity=ident[:]),
                     reads=[xnk, "ident"], writes=["ptb"])
            if cp_eng == "act":
                P.op("act", lambda: nc.scalar.copy(out=dstT, in_=ptb[:]), reads=["ptb"], writes=[dkey])
            else:
                P.op("dve", lambda: nc.vector.tensor_copy(out=dstT, in_=ptb[:]), reads=["ptb"], writes=[dkey])
            return ss, ssk

        def load_gB(P, dst, key, src_row):
            P.dma("sp", lambda: nc.sync.dma_start(out=dst, in_=src_row.partition_broadcast(128)), writes=[key])

        with ExitStack() as st:
            def sb(name, shape, dt):
                return st.enter_context(nc.sbuf_tensor(_un(name), shape, dt))
            xnT = sb("xnT", [128, 8, S], BF16)
            gB0 = sb("gB0", [128, 1024], F32)
            load_gB(P, gB0[:], "gB0", norm_mix[0])
            xt_rot = Rot([sb(f"xt{i}", [128, 1024], F32) for i in range(2)], "xt")
            for b in range(NB):
                xt, xk = xt_rot.next()
                P.dma("sp", lambda xt=xt, b=b: nc.sync.dma_start(out=xt[:], in_=x[b * 128:(b + 1) * 128, :]), writes=[xk])
                norm_T(P, xt[:], xk, gB0[:], "gB0", xnT[:, :, b * 128:(b + 1) * 128], ("xnT", b))
            xnT_keys = lambda tc: [("xnT", 4 * tc + i) for i in range(4)]

            P.flush()
            st2 = ExitStack()
            st2.__enter__()
            sb_outer = sb

            def sb(name, shape, dt):
                return st2.enter_context(nc.sbuf_tensor(_un(name), shape, dt))
            cw = sb("cw", [128, 4, 3], F32)
            for fc in range(4):
                P.dma("sp", lambda fc=fc: nc.sync.dma_start(
                    out=cw[:, fc, :], in_=conv_w_even[0, :, fc * 128:(fc + 1) * 128].rearrange("w f -> f w"),
                    allow_slow_non_contiguous=True), writes=["cw"])
            wc_rot = Rot([sb(f"wc{i}", [128, 3, 8, 128], BF16) for i in range(2)], "wc")
            z_rot = Rot([sb(f"z{i}", [128, S + 2], F32) for i in range(2)], "z")
            hs_rot = Rot([sb(f"hs{i}", [128, 512], F32) for i in range(2)], "hs")
            acc_rot = Rot([sb(f"acc{i}", [128, 512], F32) for i in range(2)], "acc")
            ya_rot = Rot([sb(f"ya{i}", [128, 512], BF16) for i in range(2)], "ya")
            w_in0 = w_in_even[0].rearrange("(k p) n -> p k n", p=128)
            bank = Rot(pb[0:6], "pb")
            for fc in range(4):
                wc, wck = wc_rot.next()
                for j in range(3):
                    P.dma("pool", lambda wc=wc, j=j, fc=fc: nc.gpsimd.dma_start(
                        out=wc[:, j, :, :], in_=w_in0[:, :, j * 512 + fc * 128: j * 512 + (fc + 1) * 128]),
                        writes=[(wck, j)])
                z, zk = z_rot.next()
                P.op("pool", lambda z=z: nc.gpsimd.memset(z[:, 0:2], 0.0), writes=[(zk, -1)])
                for tc in range(8):
                    pp = []
                    for j in range(3):
                        pt, pk = bank.next()
                        for k in range(8):
                            P.op("pe", lambda pt=pt, wc=wc, j=j, k=k, tc=tc: nc.tensor.matmul(
                                pt[:], lhsT=wc[:, j, k, :], rhs=xnT[:, k, tc * 512:(tc + 1) * 512],
                                start=(k == 0), stop=(k == 7)),
                                reads=[(wck, j)] + xnT_keys(tc), writes=[pk])
                        pp.append((pt, pk))
                    (ph, phk), (pgb, pgbk), (pgc, pgck) = pp
                    hs, hsk = hs_rot.next()
                    acc, acck = acc_rot.next()
                    ya, yak = ya_rot.next()
                    P.op("act", lambda hs=hs, ph=ph: nc.scalar.copy(out=hs[:], in_=ph[:]), reads=[phk], writes=[hsk])
                    P.op("dve", lambda z=z, tc=tc, pgc=pgc, hs=hs: nc.vector.tensor_tensor(
                        out=z[:, 2 + tc * 512: 2 + (tc + 1) * 512], in0=pgc[:], in1=hs[:], op=ALU.mult),
                        reads=[pgck, hsk], writes=[(zk, tc)])
                    zr = [(zk, tc), (zk, tc - 1)]
                    P.op("dve", lambda acc=acc, z=z, tc=tc, fc=fc: nc.vector.tensor_scalar(
                        out=acc[:], in0=z[:, tc * 512: tc * 512 + 512], scalar1=cw[:, fc, 0:1], scalar2=None, op0=ALU.mult),
                        reads=zr + ["cw"], writes=[acck])
                    for wv in (1, 2):
                        P.op("dve", lambda acc=acc, z=z, tc=tc, fc=fc, wv=wv: nc.vector.scalar_tensor_tensor(
                            out=acc[:], in0=z[:, tc * 512 + wv: tc * 512 + wv + 512], scalar=cw[:, fc, wv:wv + 1],
                            in1=acc[:], op0=ALU.mult, op1=ALU.add),
                            reads=zr + ["cw", acck], writes=[acck])
                    P.op("dve", lambda ya=ya, acc=acc, pgb=pgb: nc.vector.tensor_tensor(
                        out=ya[:], in0=pgb[:], in1=acc[:], op=ALU.mult), reads=[pgbk, acck], writes=[yak])
                    P.dma("sp", lambda ya=ya, fc=fc, tc=tc: nc.sync.dma_start(
                        out=mixT[fc, :, tc * 512:(tc + 1) * 512], in_=ya[:]), reads=[yak], writes=[("mixT", fc, tc)])
            P.flush()
            st2.__exit__(None, None, None)
            sb = sb_outer
            if stop_after >= 2:
                wq_rot = Rot([sb(f"wq{i}", [128, 3, 8, 128], BF16) for i in range(2)], "wq")
                qT_rot = Rot([sb(f"qT{i}", [128, S], BF16) for i in range(2)], "qT")
                kT_rot = Rot([sb(f"kT{i}", [128, S], BF16) for i in range(2)], "kT")
                v_rot = Rot([sb(f"v{i}", [128, NB, 128], BF16) for i in range(2)], "v")
                yb_rot = Rot([sb(f"yb{i}", [128, S], BF16) for i in range(2)], "yb")
                e_rot = Rot([sb(f"e{i}", [128, 512], F32) for i in range(3)], "e")
                sp_rot = Rot([sb(f"sp{i}", [128, 512], BF16) for i in range(3)], "sp")
                a_rot = Rot([sb(f"a{i}", [128, 512], BF16) for i in range(3)], "a")
                r32_rot = Rot([sb(f"r32{i}", [128, 512], F32) for i in range(2)], "r32")
                r16_rot = Rot([sb(f"r16{i}", [128, 512], BF16) for i in range(2)], "r16")
                zb = Rot(pb[0:3], "pb")
                ob = Rot(pb[3:7], "pbo")
                pjb = Rot(pb[3:7], "pbo")

                def proj(hp):
                    wq, wqk = wq_rot.next()
                    for j in range(3):
                        P.dma("pool", lambda wq=wq, j=j, hp=hp: nc.gpsimd.dma_start(
                            out=wq[:, j, :, :], in_=w_in0[:, :, 1536 + j * 512 + hp * 128: 1536 + j * 512 + (hp + 1) * 128]),
                            writes=[(wqk, j)])
                    qT, qk = qT_rot.next()
                    kT, kk = kT_rot.next()
                    v, vk = v_rot.next()
                    for tc in range(8):
                        for j, (dst, dk, sc) in enumerate(((qT, qk, 0.125), (kT, kk, 1.0))):
                            pt, pk = pjb.next()
                            for k in range(8):
                                P.op("pe", lambda pt=pt, wq=wq, j=j, k=k, tc=tc: nc.tensor.matmul(
                                    pt[:], lhsT=wq[:, j, k, :], rhs=xnT[:, k, tc * 512:(tc + 1) * 512],
                                    start=(k == 0), stop=(k == 7)),
                                    reads=[(wqk, j)] + xnT_keys(tc), writes=[pk])
                            P.op("dve", lambda dst=dst, pt=pt, tc=tc, sc=sc: nc.vector.tensor_scalar(
                                out=dst[:, tc * 512:(tc + 1) * 512], in0=pt[:], scalar1=sc, scalar2=None, op0=ALU.mult),
                                reads=[pk], writes=[(dk, tc)])
                        pt, pk = pjb.next()
                        for tb in range(4):
                            b = tc * 4 + tb
                            for k in range(8):
                                P.op("pe", lambda pt=pt, wq=wq, k=k, b=b, tb=tb: nc.tensor.matmul(
                                    pt[:, tb * 128:(tb + 1) * 128], lhsT=xnT[:, k, b * 128:(b + 1) * 128], rhs=wq[:, 2, k, :],
                                    start=(k == 0), stop=(k == 7)),
                                    reads=[(wqk, 2), ("xnT", b)], writes=[pk])
                        P.op("dve", lambda v=v, pt=pt, tc=tc: nc.vector.tensor_copy(
                            out=v[:, tc * 4:(tc + 1) * 4, :], in_=pt[:].rearrange("p (b d) -> p b d", b=4)),
                            reads=[pk], writes=[(vk, tc)])
                    return (qT, qk, kT, kk, v, vk)

                def attention(hp, qkv):
                    qT, qk, kT, kk, v, vk = qkv
                    yb, ybk = yb_rot.next()
                    tiles = []
                    for hh in range(2):
                        for qc in range(8):
                            nkb = 4 * qc + 4
                            for ii, kb in enumerate(range(nkb - 1, -1, -1)):
                                jd = kb - 4 * qc
                                c0 = jd * 128 if jd >= 0 else 0
                                tiles.append(dict(hh=hh, qc=qc, kb=kb, c0=c0, diag=(jd >= 0), first=(ii == 0),
                                                  last=(kb == 0)))
                    nt = len(tiles)
                    st_ = [dict() for _ in range(nt)]

                    def stage_qk(i):
                        t = tiles[i]
                        po = t["hh"] * 64
                        zt, zk_ = zb.next()
                        st_[i]["z"] = (zt, zk_)
                        c0 = t["c0"]
                        qc, kb = t["qc"], t["kb"]
                        P.op("pe", lambda: nc.tensor.matmul(
                            zt[:, c0:512], lhsT=kT[po:po + 64, kb * 128:(kb + 1) * 128],
                            rhs=qT[po:po + 64, qc * 512 + c0:(qc + 1) * 512], start=True, stop=False),
                            reads=[(kk, kb // 4), (qk, qc)], writes=[zk_])
                        if t["diag"]:
                            P.op("pe", lambda: nc.tensor.matmul(
                                zt[:, c0:c0 + 128], lhsT=ident[:], rhs=nmask[:], start=False, stop=False),
                                reads=["ident", "nmask"], writes=[zk_])
                        et, ek = e_rot.next()
                        spt, spk = sp_rot.next()
                        st_[i]["sp"] = (spt, spk)
                        P.op("act", lambda: nc.scalar.activation(out=et[:, c0:512], in_=zt[:, c0:512], func=AF.Exp),
                             reads=[zk_], writes=[ek])
                        P.op("act", lambda: nc.scalar.activation(out=spt[:, c0:512], in_=et[:, c0:512], func=AF.Ln, bias=1.0),
                             reads=[ek], writes=[spk])

                    def stage_cum(i):
                        t = tiles[i]
                        zt, zk_ = st_[i]["z"]
                        spt, spk = st_[i]["sp"]
                        c0 = t["c0"]
                        if t["first"]:
                            r32, r32k = r32_rot.next()
                            st_[i]["r32"] = (r32, r32k)
                            P.op("pool", lambda: nc.gpsimd.memset(r32[:], 0.0), writes=[r32k])
                        else:
                            st_[i]["r32"] = st_[i - 1]["r32"]
                            r32, r32k = st_[i]["r32"]
                        P.op("pe", lambda: nc.tensor.matmul(zt[:, c0:512], lhsT=ntri[:], rhs=spt[:, c0:512],
                                                            start=False, stop=t["first"]),
                             reads=["ntri", spk], writes=[zk_])
                        if not t["first"]:
                            r16, r16k = st_[i]["r16"]
                            P.op("pe", lambda: nc.tensor.matmul(zt[:, c0:512], lhsT=nones[:], rhs=r16[:, c0:512],
                                                                start=False, stop=True),
                                 reads=["nones", r16k], writes=[zk_])
                        if not t["last"]:
                            c1 = tiles[i + 1]["c0"]
                            P.op("dve", lambda: nc.vector.tensor_tensor(out=r32[:, c0:512], in0=r32[:, c0:512],
                                                                       in1=spt[:, c0:512], op=ALU.add),
                                 reads=[r32k, spk], writes=[r32k])
                            r16n, r16nk = r16_rot.next()
                            st_[i + 1]["r16"] = (r16n, r16nk)
                            P.op("pool", lambda: nc.gpsimd.tensor_copy(out=r16n[:, c1:512], in_=r32[:, c1:512]),
                                 reads=[r32k], writes=[r16nk])
                        at, ak = a_rot.next()
                        st_[i]["a"] = (at, ak)
                        P.op("act", lambda: nc.scalar.activation(out=at[:, c0:512], in_=zt[:, c0:512], func=AF.Exp),
                             reads=[zk_], writes=[ak])

                    def stage_av(i):
                        t = tiles[i]
                        at, ak = st_[i]["a"]
                        c0 = t["c0"]
                        kb = t["kb"]
                        po = t["hh"] * 64
                        if t["first"]:
                            ot, ok = ob.next()
                            st_[i]["o"] = (ot, ok)
                            P.op("pe", lambda: nc.tensor.matmul(ot[:], lhsT=zeros[:], rhs=qT[:, 0:512], start=True, stop=False),
                                 reads=["zeros", (qk, 0)], writes=[ok])
                        else:
                            st_[i]["o"] = st_[i - 1]["o"]
                            ot, ok = st_[i]["o"]
                        P.op("pe", lambda: nc.tensor.matmul(ot[:, c0:512], lhsT=v[:, kb, :], rhs=at[:, c0:512],
                                                            start=False, stop=t["last"]),
                             reads=[(vk, kb // 4), ak], writes=[ok])
                        if t["last"]:
                            qc = t["qc"]
                            P.op("dve", lambda: nc.vector.tensor_copy(out=yb[po:po + 64, qc * 512:(qc + 1) * 512],
                                                                      in_=ot[po:po + 64, :]),
                                 reads=[ok], writes=[(ybk, qc, t["hh"])])
                        st_[i].pop("z", None)

                    stage_qk(0)
                    for i in range(nt):
                        if i + 1 < nt:
                            stage_qk(i + 1)
                        stage_cum(i)
                        if i >= 1:
                            stage_av(i - 1)
                    stage_av(nt - 1)
                    for qc in range(8):
                        P.dma("sp", lambda qc=qc: nc.sync.dma_start(out=mixT[4 + hp, :, qc * 512:(qc + 1) * 512],
                                                                    in_=yb[:, qc * 512:(qc + 1) * 512]),
                              reads=[(ybk, qc, 0), (ybk, qc, 1)], writes=[("mixT", 4 + hp, qc)])

                nhp = 4 if stop_after >= 3 else 1
                qkvs = {0: proj(0)}
                for hp in range(nhp):
                    if hp + 1 < nhp:
                        qkvs[hp + 1] = proj(hp + 1)
                    attention(hp, qkvs.pop(hp))
            P.flush()

        if stop_after >= 4:
            with ExitStack() as st:
                def sb(name, shape, dt):
                    return st.enter_context(nc.sbuf_tensor(_un(name), shape, dt))
                wo = sb("wo", [128, 8, 1024], BF16)
                P.dma("pool", lambda: nc.gpsimd.dma_start(out=wo[:], in_=w_out_even[0].rearrange("(k p) n -> p k n", p=128)),
                      writes=["wo"])
                mx_rot = Rot([sb(f"mx{i}", [128, 8, 512], BF16) for i in range(2)], "mx")
                xt_rot = Rot([sb(f"xt{i}", [128, 1024], F32) for i in range(3)], "xt")
                bank = Rot(pb[0:6], "pb")
                for tc in range(8):
                    mx, mxk = mx_rot.next()
                    P.dma("sp", lambda mx=mx, tc=tc: nc.sync.dma_start(
                        out=mx[:], in_=mixT[:, :, tc * 512:(tc + 1) * 512].rearrange("c p t -> p c t")), writes=[mxk])
                    for tb in range(4):
                        b = tc * 4 + tb
                        xt, xk = xt_rot.next()
                        P.dma("sp", lambda xt=xt, b=b: nc.sync.dma_start(out=xt[:], in_=x[b * 128:(b + 1) * 128, :]), writes=[xk])
                        for half in range(2):
                            pt, pk = bank.next()
                            for c in range(8):
                                P.op("pe", lambda pt=pt, mx=mx, c=c, tb=tb, half=half: nc.tensor.matmul(
                                    pt[:], lhsT=mx[:, c, tb * 128:(tb + 1) * 128], rhs=wo[:, c, half * 512:(half + 1) * 512],
                                    start=(c == 0), stop=(c == 7)), reads=[mxk, "wo"], writes=[pk])
                            P.op("dve", lambda xt=xt, pt=pt, half=half: nc.vector.tensor_tensor(
                                out=xt[:, half * 512:(half + 1) * 512], in0=pt[:], in1=xt[:, half * 512:(half + 1) * 512], op=ALU.add),
                                reads=[pk, xk], writes=[xk])
                        P.dma("sp", lambda xt=xt, b=b: nc.sync.dma_start(out=hA[b * 128:(b + 1) * 128, :], in_=xt[:]),
                              reads=[xk], writes=[("hA", b)])
                P.flush()

        def moe_pl_phase(l, hin, hout, final):
            with ExitStack() as st:
                def sb(name, shape, dt):
                    return st.enter_context(nc.sbuf_tensor(_un(name), shape, dt))
                NBS = 16
                hacc = sb("hacc", [128, NBS, 1024], F32)
                xT = sb("xT", [128, 8, NBS * 128], BF16)
                gBf = sb("gBf", [128, 1024], F32)
                gBp = sb("gBp", [128, 1024], F32)
                load_gB(P, gBf[:], "gBf", norm_ffn[l])
                load_gB(P, gBp[:], "gBp", norm_pl[l])
                if final:
                    gBo = sb("gBo", [128, 1024], F32)
                    load_gB(P, gBo[:], "gBo", final_norm)
                wr = sb("wr", [128, 8, 36], BF16)
                P.dma("pool", lambda: nc.gpsimd.dma_start(out=wr[:, :, 0:4], in_=router_c[l].rearrange("(k p) n -> p k n", p=128)),
                      writes=["wr"])
                P.dma("pool", lambda: nc.gpsimd.dma_start(out=wr[:, :, 4:36], in_=router_f[l].rearrange("(k p) n -> p k n", p=128)),
                      writes=["wr"])
                rb = sb("rb", [128, 36], F32)
                P.dma("sp", lambda: nc.sync.dma_start(out=rb[:, 0:4], in_=router_c_b[l].partition_broadcast(128)), writes=["rb"])
                P.dma("sp", lambda: nc.sync.dma_start(out=rb[:, 4:36], in_=router_f_b[l].partition_broadcast(128)), writes=["rb"])
                wpg = sb("wpg", [128, 8, 1024], BF16)
                wpe = sb("wpe", [128, 2, 1024], BF16)
                P.dma("pool", lambda: nc.gpsimd.dma_start(out=wpg[:], in_=w_pg[l].rearrange("(k p) n -> p k n", p=128)), writes=["wpg"])
                P.dma("pool", lambda: nc.gpsimd.dma_start(out=wpe[:], in_=w_pe[l].rearrange("(k p) n -> p k n", p=128)), writes=["wpe"])
                w13_rot = Rot([sb(f"w13_{i}", [128, 2, 8, 256], BF16) for i in range(2)], "w13")
                w2_rot = Rot([sb(f"w2_{i}", [128, 2, 1024], BF16) for i in range(2)], "w2")
                lg = sb("lg", [128, NBS, 36], F32)
                comb = sb("comb", [128, NBS, 32], F32)
                r_mx = sb("r_mx", [128, NBS], F32)
                r_gm = sb("r_gm", [128, NBS, 4], F32)
                r_ec = sb("r_ec", [128, NBS, 4], F32)
                r_pg = sb("r_pg", [128, NBS], F32)
                r_t = sb("r_t", [128, NBS, 4, 8], F32)
                r_lfs = sb("r_lfs", [128, NBS, 8], F32)
                r_t8 = sb("r_t8", [128, NBS, 8], F32)
                r_sel = sb("r_sel", [128, NBS, 8], F32)
                r_ex = sb("r_ex", [128, NBS, 8], F32)
                r_d = sb("r_d", [128, NBS], F32)
                s_rot = Rot([sb(f"s{i}", [128, 512], F32) for i in range(2)], "s")
                he_rot = Rot([sb(f"he{i}", [128, 2, 512], BF16) for i in range(2)], "he")
                pblk_rot = Rot([sb(f"pblk{i}", [128, 256], F32) for i in range(2)], "pblk")
                pb16_rot = Rot([sb(f"pb16{i}", [128, 256], BF16) for i in range(2)], "pb16")
                pT_rot = Rot([sb(f"pT{i}", [128, 2, 128], BF16) for i in range(2)], "pT")
                hnT_rot = Rot([sb(f"hnT{i}", [128, 8, 128], BF16) for i in range(2)], "hnT")
                sg_rot = Rot([sb(f"sg{i}", [128, 1024], F32) for i in range(1)], "sg")
                for sc in range(2):
                    hbank = Rot(pb[0:4], "pb")
                    ybank = Rot(pb[4:7], "pby")
                    for blk in range(NBS):
                        b = sc * NBS + blk
                        P.dma("sp", lambda blk=blk, b=b: nc.sync.dma_start(out=hacc[:, blk, :], in_=hin[b * 128:(b + 1) * 128, :]),
                              writes=hk(blk))
                        norm_T(P, hacc[:, blk, :], hk(blk), gBf[:], "gBf", xT[:, :, blk * 128:(blk + 1) * 128], ("xT", blk))
                        pt, pk = ybank.next()
                        for k in range(8):
                            P.op("pe", lambda pt=pt, k=k, blk=blk: nc.tensor.matmul(
                                pt[:, 0:36], lhsT=xT[:, k, blk * 128:(blk + 1) * 128], rhs=wr[:, k, :],
                                start=(k == 0), stop=(k == 7)), reads=[("xT", blk), "wr"], writes=[pk])
                        P.op("dve", lambda pt=pt, blk=blk: nc.vector.tensor_tensor(out=lg[:, blk, :], in0=pt[:, 0:36], in1=rb[:], op=ALU.add),
                             reads=[pk, "rb"], writes=["lg"])
                    lc = lg[:, :, 0:4]
                    lf = lg[:, :, 4:36].rearrange("p b (g e) -> p b g e", g=4)
                    P.op("dve", lambda: nc.vector.tensor_reduce(out=r_mx[:], in_=lc, axis=AX.X, op=ALU.max), reads=["lg"], writes=["r_mx"])
                    P.op("dve", lambda: nc.vector.tensor_tensor(out=r_gm[:], in0=lc, in1=r_mx[:].unsqueeze(2).to_broadcast([128, NBS, 4]),
                                                                op=ALU.is_ge), reads=["lg", "r_mx"], writes=["r_gm"])
                    P.op("dve", lambda: nc.vector.tensor_tensor(out=r_ec[:], in0=lc, in1=r_mx[:].unsqueeze(2).to_broadcast([128, NBS, 4]),
                                                                op=ALU.subtract), reads=["lg", "r_mx"], writes=["r_ec"])
                    P.op("act", lambda: nc.scalar.activation(out=r_ec[:], in_=r_ec[:], func=AF.Exp), reads=["r_ec"], writes=["r_ec"])
                    P.op("dve", lambda: nc.vector.tensor_reduce(out=r_pg[:], in_=r_ec[:], axis=AX.X, op=ALU.add), reads=["r_ec"], writes=["r_pg"])
                    P.op("dve", lambda: nc.vector.tensor_tensor(out=r_t[:], in0=lf, in1=r_gm[:].unsqueeze(3).to_broadcast([128, NBS, 4, 8]),
                                                                op=ALU.mult), reads=["lg", "r_gm"], writes=["r_t"])
                    P.op("dve", lambda: nc.vector.tensor_reduce(out=r_lfs[:], in_=r_t[:].rearrange("p b g e -> p b e g"), axis=AX.X, op=ALU.add),
                         reads=["r_t"], writes=["r_lfs"])
                    for blk in range(NBS):
                        P.op("dve", lambda blk=blk: nc.vector.max(out=r_t8[:, blk, :], in_=r_lfs[:, blk, :]), reads=["r_lfs"], writes=["r_t8"])
                    l1b = r_t8[:, :, 0:1].to_broadcast([128, NBS, 8])
                    l2b = r_t8[:, :, 1:2].to_broadcast([128, NBS, 8])
                    P.op("dve", lambda: nc.vector.tensor_tensor(out=r_sel[:], in0=r_lfs[:], in1=l2b, op=ALU.is_ge), reads=["r_lfs", "r_t8"], writes=["r_sel"])
                    P.op("dve", lambda: nc.vector.tensor_tensor(out=r_ex[:], in0=r_lfs[:], in1=l1b, op=ALU.subtract), reads=["r_lfs", "r_t8"], writes=["r_ex"])
                    P.op("act", lambda: nc.scalar.activation(out=r_ex[:], in_=r_ex[:], func=AF.Exp), reads=["r_ex"], writes=["r_ex"])
                    P.op("dve", lambda: nc.vector.tensor_tensor(out=r_ex[:], in0=r_ex[:], in1=r_sel[:], op=ALU.mult), reads=["r_ex", "r_sel"], writes=["r_ex"])
                    P.op("dve", lambda: nc.vector.tensor_reduce(out=r_d[:], in_=r_ex[:], axis=AX.X, op=ALU.add), reads=["r_ex"], writes=["r_d"])
                    P.op("dve", lambda: nc.vector.tensor_tensor(out=r_d[:], in0=r_d[:], in1=r_pg[:], op=ALU.mult), reads=["r_d", "r_pg"], writes=["r_d"])
                    P.op("dve", lambda: nc.vector.reciprocal(out=r_d[:], in_=r_d[:]), reads=["r_d"], writes=["r_d"])
                    P.op("dve", lambda: nc.vector.tensor_tensor(out=r_ex[:], in0=r_ex[:], in1=r_d[:].unsqueeze(2).to_broadcast([128, NBS, 8]),
                                                                op=ALU.mult), reads=["r_ex", "r_d"], writes=["r_ex"])
                    P.op("dve", lambda: nc.vector.tensor_tensor(
                        out=comb[:].rearrange("p b (g e) -> p b g e", g=4),
                        in0=r_gm[:].unsqueeze(3).to_broadcast([128, NBS, 4, 8]),
                        in1=r_ex[:].unsqueeze(2).to_broadcast([128, NBS, 4, 8]), op=ALU.mult),
                        reads=["r_gm", "r_ex"], writes=["comb"])
                    wts = {}

                    def load_w(e):
                        w13, w13k = w13_rot.next()
                        w2, w2k = w2_rot.next()
                        P.dma("pool", lambda: nc.gpsimd.dma_start(out=w13[:, 0, :, :], in_=moe_w1[l, e].rearrange("(k p) f -> p k f", p=128)),
                              writes=[(w13k, 0)])
                        P.dma("pool", lambda: nc.gpsimd.dma_start(out=w13[:, 1, :, :], in_=moe_w3[l, e].rearrange("(k p) f -> p k f", p=128)),
                              writes=[(w13k, 1)])
                        P.dma("pool", lambda: nc.gpsimd.dma_start(out=w2[:], in_=moe_w2[l, e].rearrange("(c p) d -> p c d", p=128)),
                              writes=[w2k])
                        wts[e] = (w13, w13k, w2, w2k)

                    units = [(e, tch) for e in range(32) for tch in range(4)]
                    ust = {}

                    def stage_h(u):
                        e, tch = units[u]
                        w13, w13k, w2, w2k = wts[e]
                        he, hek = he_rot.next()
                        ust[u] = (he, hek)
                        for fch in range(2):
                            p1, p1k = hbank.next()
                            p3, p3k = hbank.next()
                            for j, (pt, pk) in enumerate(((p1, p1k), (p3, p3k))):
                                for k in range(8):
                                    P.op("pe", lambda pt=pt, j=j, k=k, fch=fch: nc.tensor.matmul(
                                        pt[:], lhsT=w13[:, j, k, fch * 128:(fch + 1) * 128], rhs=xT[:, k, tch * 512:(tch + 1) * 512],
                                        start=(k == 0), stop=(k == 7)),
                                        reads=[(w13k, j)] + [("xT", 4 * tch + i) for i in range(4)], writes=[pk])
                            s, sk = s_rot.next()
                            P.op("act", lambda s=s, p1=p1: nc.scalar.activation(out=s[:], in_=p1[:], func=AF.Silu), reads=[p1k], writes=[sk])
                            P.op("dve", lambda s=s, p3=p3, fch=fch: nc.vector.tensor_tensor(out=he[:, fch, :], in0=p3[:], in1=s[:], op=ALU.mult),
                                 reads=[p3k, sk], writes=[(hek, fch)])

                    def stage_y(u):
                        e, tch = units[u]
                        w13, w13k, w2, w2k = wts[e]
                        he, hek = ust.pop(u)
                        for tb in range(4):
                            blk = tch * 4 + tb
                            for half in range(2):
                                py, pyk = ybank.next()
                                for fch in range(2):
                                    P.op("pe", lambda py=py, fch=fch, tb=tb, half=half: nc.tensor.matmul(
                                        py[:], lhsT=he[:, fch, tb * 128:(tb + 1) * 128], rhs=w2[:, fch, half * 512:(half + 1) * 512],
                                        start=(fch == 0), stop=(fch == 1)), reads=[(hek, fch), w2k], writes=[pyk])
                                P.op("dve", lambda py=py, blk=blk, half=half: nc.vector.scalar_tensor_tensor(
                                    out=hacc[:, blk, half * 512:(half + 1) * 512], in0=py[:], scalar=comb[:, blk, e:e + 1],
                                    in1=hacc[:, blk, half * 512:(half + 1) * 512], op0=ALU.mult, op1=ALU.add),
                                    reads=[pyk, "comb", ("hacc", blk, half)], writes=[("hacc", blk, half)])
                        if tch == 3:
                            wts.pop(e)
                            if e + 2 < 32:
                                load_w(e + 2)

                    load_w(0)
                    load_w(1)
                    nu = len(units)
                    stage_h(0)
                    for u in range(nu):
                        if u + 1 < nu:
                            stage_h(u + 1)
                        stage_y(u)
                    gbank = Rot(pb[0:4], "pb")
                    for blk in range(NBS):
                        b = sc * NBS + blk
                        hnT, hnk = hnT_rot.next()
                        norm_T(P, hacc[:, blk, :], hk(blk), gBp[:], "gBp", hnT[:], hnk)
                        pblk, pblkk = pblk_rot.next()
                        p16, p16k = pb16_rot.next()
                        pT, pTk = pT_rot.next()
                        P.dma("sp", lambda pblk=pblk, b=b: nc.sync.dma_start(out=pblk[:], in_=p[l, b * 128:(b + 1) * 128, :]), writes=[pblkk])
                        P.op("pool", lambda p16=p16, pblk=pblk: nc.gpsimd.tensor_copy(out=p16[:], in_=pblk[:]), reads=[pblkk], writes=[p16k])
                        for k in range(2):
                            P.op("pe", lambda p16=p16, k=k: nc.tensor.transpose(out=ptb[:, k, :], in_=p16[:, k * 128:(k + 1) * 128], identity=ident[:]),
                                 reads=[p16k, "ident"], writes=["ptb"])
                        P.op("act", lambda pT=pT: nc.scalar.copy(out=pT[:], in_=ptb[:, 0:2, :]), reads=["ptb"], writes=[pTk])
                        sg, sgk = sg_rot.next()
                        for half in range(2):
                            pg_, pgk = gbank.next()
                            pe_, pek = gbank.next()
                            for k in range(8):
                                P.op("pe", lambda pg_=pg_, hnT=hnT, k=k, half=half: nc.tensor.matmul(
                                    pg_[:], lhsT=hnT[:, k, :], rhs=wpg[:, k, half * 512:(half + 1) * 512], start=(k == 0), stop=(k == 7)),
                                    reads=[hnk, "wpg"], writes=[pgk])
                            for k in range(2):
                                P.op("pe", lambda pe_=pe_, pT=pT, k=k, half=half: nc.tensor.matmul(
                                    pe_[:], lhsT=pT[:, k, :], rhs=wpe[:, k, half * 512:(half + 1) * 512], start=(k == 0), stop=(k == 1)),
                                    reads=[pTk, "wpe"], writes=[pek])
                            P.op("act", lambda sg=sg, pg_=pg_, half=half: nc.scalar.activation(out=sg[:, half * 512:(half + 1) * 512], in_=pg_[:], func=AF.Sigmoid),
                                 reads=[pgk], writes=[(sgk, half)])
                            P.op("dve", lambda sg=sg, pe_=pe_, half=half: nc.vector.tensor_tensor(
                                out=sg[:, half * 512:(half + 1) * 512], in0=pe_[:], in1=sg[:, half * 512:(half + 1) * 512], op=ALU.mult),
                                reads=[pek, (sgk, half)], writes=[(sgk, half)])
                        P.op("dve", lambda sg=sg, blk=blk: nc.vector.tensor_tensor(out=hacc[:, blk, :], in0=hacc[:, blk, :], in1=sg[:], op=ALU.add),
                             reads=[(sgk, 0), (sgk, 1)] + hk(blk), writes=hk(blk))
                        if final:
                            junk, jk = njunk.next()
                            ss, ssk = nss.next()
                            norm_rstd(P, hacc[:, blk, :], hk(blk), 1024, junk[:], jk, ss[:], ssk)
                            P.op("dve", lambda ss=ss, blk=blk: nc.vector.scalar_tensor_tensor(
                                out=hacc[:, blk, :], in0=hacc[:, blk, :], scalar=ss[:], in1=gBo[:], op0=ALU.mult, op1=ALU.mult),
                                reads=hk(blk) + [ssk, "gBo"], writes=hk(blk))
                        P.dma("sp", lambda blk=blk, b=b: nc.sync.dma_start(out=hout[b * 128:(b + 1) * 128, :], in_=hacc[:, blk, :]),
                              reads=hk(blk), writes=[("hout", b)])
                    P.flush()

        if stop_after >= 5:
            moe_pl_phase(0, hA, hB, False)

        if stop_after >= 6:
            with ExitStack() as st:
                def sb(name, shape, dt):
                    return st.enter_context(nc.sbuf_tensor(_un(name), shape, dt))
                wi = sb("wi", [128, 8, 4096], BF16)
                wi_src = w_in_odd[0].rearrange("(k p) n -> p k n", p=128)
                for k in range(8):
                    P.dma("pool", lambda k=k: nc.gpsimd.dma_start(out=wi[:, k, :], in_=wi_src[:, k, :]), writes=[("wi", k)])
                wi_keys = [("wi", k) for k in range(8)]
                wo1 = sb("wo1", [128, 16, 1024], BF16)
                wo_src = w_out_odd[0].rearrange("(c p) n -> p c n", p=128)
                for c in range(0, 16, 4):
                    P.dma("pool", lambda c=c: nc.gpsimd.dma_start(out=wo1[:, c:c + 4, :], in_=wo_src[:, c:c + 4, :]), writes=[("wo1", c)])
                wo_keys = [("wo1", c) for c in range(0, 16, 4)]
                gB1 = sb("gB1", [128, 1024], F32)
                load_gB(P, gB1[:], "gB1", norm_mix[1])
                gvB = sb("gvB", [128, 2048], F32)
                P.dma("sp", lambda: nc.sync.dma_start(out=gvB[:], in_=g_v_odd[0].partition_broadcast(128)), writes=["gvB"])
                bsf = sb("bsf", [1, 8, 128], F32)
                bs16 = sb("bs16", [1, 8, 128], BF16)
                P.dma("sp", lambda: nc.sync.dma_start(out=bsf[:], in_=b_s_odd[0:1]), writes=["bsf"])
                P.op("dve", lambda: nc.vector.tensor_copy(out=bs16[:], in_=bsf[:]), reads=["bsf"], writes=["bs16"])
                wsT = sb("wsT", [128, 8, 128], BF16)
                st3 = ExitStack()
                st3.__enter__()
                wsf = st3.enter_context(nc.sbuf_tensor(_un("wsf"), [128, 8, 128], F32))
                ws16 = st3.enter_context(nc.sbuf_tensor(_un("ws16"), [128, 8, 128], BF16))
                P.dma("sp", lambda: nc.sync.dma_start(out=wsf[:], in_=w_s_odd[0].rearrange("g t s -> t g s")), writes=["wsf"])
                for g in range(8):
                    P.op("pool", lambda g=g: nc.gpsimd.affine_select(out=wsf[:, g, :], in_=wsf[:, g, :], pattern=[[-1, 128]],
                                                                      compare_op=ALU.is_ge, fill=0.0, base=0, channel_multiplier=1),
                         reads=["wsf"], writes=["wsf"])
                P.op("dve", lambda: nc.vector.tensor_copy(out=ws16[:], in_=wsf[:]), reads=["wsf"], writes=["ws16"])
                for g in range(8):
                    P.op("pe", lambda g=g: nc.tensor.transpose(out=ptb[:, g, :], in_=ws16[:, g, :], identity=ident[:]),
                         reads=["ws16", "ident"], writes=["ptb"])
                P.op("dve", lambda: nc.vector.tensor_copy(out=wsT[:], in_=ptb[:]), reads=["ptb"], writes=["wsT"])
                P.flush()
                st3.__exit__(None, None, None)
                ht_rot = Rot([sb(f"ht{i}", [128, 1024], F32) for i in range(5)], "ht")
                xg_rot = Rot([sb(f"xg{i}", [128, 8, 512], BF16) for i in range(2)], "xg")
                uT_rot = Rot([sb(f"uT{i}", [128, 16, 512], BF16) for i in range(1)], "uT")
                vt_rot = Rot([sb(f"vt{i}", [128, 2048], F32) for i in range(1)], "vt")
                vn_rot = Rot([sb(f"vn{i}", [128, 2048], BF16) for i in range(1)], "vn")
                yT_rot = Rot([sb(f"yT{i}", [128, 16, 128], BF16) for i in range(2)], "yT")
                abank = Rot(pb[0:3], "pb")
                gbank = Rot(pb[3:5], "pbg")
                obank = Rot(pb[5:7], "pbo")
                for tc in range(8):
                    xg, xgk = xg_rot.next()
                    hts = []
                    for tb in range(4):
                        b = tc * 4 + tb
                        ht, htk = ht_rot.next()
                        hts.append((ht, htk))
                        P.dma("sp", lambda ht=ht, b=b: nc.sync.dma_start(out=ht[:], in_=hB[b * 128:(b + 1) * 128, :]),
                              reads=[("hout", b)], writes=[htk])
                        norm_T(P, ht[:], htk, gB1[:], "gB1", xg[:, :, tb * 128:(tb + 1) * 128], (xgk, tb))
                    xgkeys = [(xgk, tb) for tb in range(4)]
                    uT, uTk = uT_rot.next()
                    for fcu in range(16):
                        pt, pk = abank.next()
                        for k in range(8):
                            P.op("pe", lambda pt=pt, k=k, fcu=fcu, xg=xg: nc.tensor.matmul(
                                pt[:], lhsT=wi[:, k, fcu * 128:(fcu + 1) * 128], rhs=xg[:, k, :], start=(k == 0), stop=(k == 7)),
                                reads=[("wi", k)] + xgkeys, writes=[pk])
                        P.op("act", lambda pt=pt, uT=uT, fcu=fcu: nc.scalar.activation(out=uT[:, fcu, :], in_=pt[:], func=AF.Gelu_apprx_tanh),
                             reads=[pk], writes=[(uTk, fcu)])
                    for tb in range(4):
                        b = tc * 4 + tb
                        ht, htk = hts[tb]
                        vt, vtk = vt_rot.next()
                        for vg in range(4):
                            pt, pk = abank.next()
                            for k in range(8):
                                P.op("pe", lambda pt=pt, k=k, vg=vg, xg=xg, tb=tb: nc.tensor.matmul(
                                    pt[:], lhsT=xg[:, k, tb * 128:(tb + 1) * 128], rhs=wi[:, k, 2048 + vg * 512: 2048 + (vg + 1) * 512],
                                    start=(k == 0), stop=(k == 7)), reads=[("wi", k), (xgk, tb)], writes=[pk])
                            P.op("act", lambda pt=pt, vt=vt, vg=vg: nc.scalar.activation(out=vt[:, vg * 512:(vg + 1) * 512], in_=pt[:], func=AF.Gelu_apprx_tanh),
                                 reads=[pk], writes=[(vtk, vg)])
                        ss, ssk = nss.next()
                        vtkeys = [(vtk, vg) for vg in range(4)]
                        vn, vnk = vn_rot.next()
                        P.op("act", lambda vt=vt, ss=ss, vn=vn: nc.scalar.activation(out=vn[:], in_=vt[:], func=AF.Square, accum_out=ss[:]),
                             reads=vtkeys, writes=[vnk, ssk])
                        P.op("act", lambda ss=ss: nc.scalar.activation(out=ss[:], in_=ss[:], func=AF.Ln, scale=1.0 / 2048, bias=epsb[:]),
                             reads=[ssk, "epsb"], writes=[ssk])
                        P.op("act", lambda ss=ss: nc.scalar.activation(out=ss[:], in_=ss[:], func=AF.Exp, scale=-0.5), reads=[ssk], writes=[ssk])
                        P.op("dve", lambda vn=vn, vt=vt, ss=ss: nc.vector.scalar_tensor_tensor(out=vn[:], in0=vt[:], scalar=ss[:], in1=gvB[:],
                                                                                              op0=ALU.mult, op1=ALU.mult),
                             reads=vtkeys + [ssk, "gvB"], writes=[vnk])
                        yT, yTk = yT_rot.next()
                        for q4 in range(4):
                            pg_, pgk = gbank.next()
                            for i4 in range(4):
                                fcu = q4 * 4 + i4
                                g = fcu // 2
                                P.op("pe", lambda pg_=pg_, i4=i4, fcu=fcu, g=g, vn=vn: nc.tensor.matmul(
                                    pg_[:, i4 * 128:(i4 + 1) * 128], lhsT=vn[:, fcu * 128:(fcu + 1) * 128], rhs=wsT[:, g, :], start=True, stop=False),
                                    reads=[vnk, "wsT"], writes=[pgk])
                                P.op("pe", lambda pg_=pg_, i4=i4, g=g: nc.tensor.matmul(
                                    pg_[:, i4 * 128:(i4 + 1) * 128], lhsT=ones1[0:1, :], rhs=bs16[0:1, g, :], start=False, stop=True),
                                    reads=["ones1", "bs16"], writes=[pgk])
                            P.op("dve", lambda pg_=pg_, yT=yT, uT=uT, q4=q4, tb=tb: nc.vector.tensor_tensor(
                                out=yT[:, q4 * 4:(q4 + 1) * 4, :], in0=pg_[:].rearrange("p (i t) -> p i t", i=4),
                                in1=uT[:, q4 * 4:(q4 + 1) * 4, tb * 128:(tb + 1) * 128], op=ALU.mult),
                                reads=[pgk] + [(uTk, q4 * 4 + i) for i in range(4)], writes=[(yTk, q4)])
                        for half in range(2):
                            po_, pok = obank.next()
                            for fcu in range(16):
                                P.op("pe", lambda po_=po_, yT=yT, fcu=fcu, half=half: nc.tensor.matmul(
                                    po_[:], lhsT=yT[:, fcu, :], rhs=wo1[:, fcu, half * 512:(half + 1) * 512], start=(fcu == 0), stop=(fcu == 15)),
                                    reads=[(yTk, fcu // 4), ("wo1", (fcu // 4) * 4)], writes=[pok])
                            P.op("dve", lambda po_=po_, ht=ht, half=half: nc.vector.tensor_tensor(
                                out=ht[:, half * 512:(half + 1) * 512], in0=po_[:], in1=ht[:, half * 512:(half + 1) * 512], op=ALU.add),
                                reads=[pok, htk], writes=[htk])
                        P.dma("sp", lambda ht=ht, b=b: nc.sync.dma_start(out=hA[b * 128:(b + 1) * 128, :], in_=ht[:]),
                              reads=[htk], writes=[("hA", b)])
                P.flush()

        if stop_after >= 7:
            moe_pl_phase(1, hA, out, True)
        P.flush()
        nc._prog_stats = dict(nops=P.nops, ccount=dict(P.ccount), dcount=dict(P.dcount))
    return nc


_NC_CACHE = {}


def kernel(**inputs):
    n = 8
    if "nc" not in _NC_CACHE:
        _NC_CACHE["nc"] = build()
    nc = _NC_CACHE["nc"]
    in_maps = []
    for c in range(n):
        m = {}
        for k, v in inputs.items():
            v = np.asarray(v)
            if k == "x":
                m[k] = np.ascontiguousarray(v[c])
            elif k == "p":
                m[k] = np.ascontiguousarray(v[:, c])
            else:
                m[k] = np.ascontiguousarray(v)
        in_maps.append(m)
    res = run_bass_kernel_spmd(nc, in_maps, core_ids=list(range(n)))
    return np.stack([np.asarray(r["out"]) for r in res.results], axis=0).astype(np.float32)
```

```python
import numpy as np
from contextlib import ExitStack
import concourse.bass as bass
import concourse.mybir as mybir
from concourse.bass_utils import run_bass_kernel_spmd

F32 = mybir.dt.float32
BF16 = mybir.dt.bfloat16
AF = mybir.ActivationFunctionType
ALU = mybir.AluOpType
AX = mybir.AxisListType
I32 = mybir.dt.int32

EPOCH = 30000
NDMASEM = 12
S = 4096
D = 1024
NB = S // 128
EPS = 1e-6


class Prog:
    ENG = ("pe", "act", "dve", "pool", "sp")

    def __init__(self, nc, stack):
        self.nc = nc
        self.stack = stack
        self.ops = []
        self.ccount = {e: 0 for e in self.ENG}
        self.dcount = {e: 0 for e in self.ENG}
        self.csem = {e: [] for e in self.ENG}
        self.dsem = {e: [] for e in self.ENG}
        self.dma_hist = {e: [] for e in self.ENG}
        self.waited = {e: {} for e in self.ENG}
        self.nops = 0

    def eng_obj(self, e):
        nc = self.nc
        return {"pe": nc.tensor, "act": nc.scalar, "dve": nc.vector,
                "pool": nc.gpsimd, "sp": nc.sync}[e]

    def op(self, eng, fn, reads=(), writes=(), nosig=False):
        self.ops.append(dict(eng=eng, fn=fn, reads=tuple(reads), writes=tuple(writes), dma=False, nosig=nosig))

    def dma(self, eng, fn, reads=(), writes=()):
        self.ops.append(dict(eng=eng, fn=fn, reads=tuple(reads), writes=tuple(writes), dma=True))

    def _csem(self, e, ep):
        while len(self.csem[e]) <= ep:
            self.csem[e].append(self.stack.enter_context(self.nc.semaphore(f"c_{e}_{len(self.csem[e])}")))
        return self.csem[e][ep]

    def _dsem(self, e, k):
        while len(self.dsem[e]) <= k:
            self.dsem[e].append(self.stack.enter_context(self.nc.semaphore(f"d_{e}_{len(self.dsem[e])}")))
        return self.dsem[e][k]

    def _wait(self, e, key, sem, val):
        w = self.waited[e]
        if key[0] == "c":
            for kk, v in w.items():
                if kk[0] == "c" and kk[1] == key[1] and (kk[2] > key[2] or (kk[2] == key[2] and v >= val)):
                    return
        elif w.get(key, 0) >= val:
            return
        self.eng_obj(e).wait_ge(sem, val)
        w[key] = max(w.get(key, 0), val)

    def flush(self):
        ops = self.ops
        n = len(ops)
        self.nops += n
        lw, rdc, rdd = {}, {}, {}
        deps = [None] * n
        dlist = {e: [] for e in self.ENG}
        for i, o in enumerate(ops):
            d = set()
            for k in o["reads"]:
                w = lw.get(k)
                if w is not None:
                    d.add(w)
            for k in o["writes"]:
                w = lw.get(k)
                if w is not None:
                    d.add(w)
                d.update(rdc.get(k, {}).values())
                d.update(rdd.get(k, ()))
            for k in o["reads"]:
                if o["dma"]:
                    rdd.setdefault(k, []).append(i)
                else:
                    rdc.setdefault(k, {})[o["eng"]] = i
            for k in o["writes"]:
                lw[k] = i
                rdc[k] = {}
                rdd[k] = []
            if o["dma"]:
                q = o["eng"]
                o["dn"] = self.dcount[q]
                self.dcount[q] += 1
                lst = dlist[q]
                if len(lst) >= NDMASEM:
                    d.add(lst[len(lst) - NDMASEM])
                lst.append(i)
            d.discard(i)
            if o["eng"] == "pe" and not o["dma"]:
                d = {j for j in d if not (ops[j]["eng"] == "pe" and not ops[j]["dma"])}
            deps[i] = d
        signaling = [False] * n
        for i in range(n):
            for j in deps[i]:
                signaling[j] = True
        last = {}
        for i, o in enumerate(ops):
            if not o["dma"] and not o.get("nosig"):
                last[o["eng"]] = i
        for e, i in last.items():
            signaling[i] = True
        for i, o in enumerate(ops):
            if not o["dma"] and signaling[i]:
                c = self.ccount[o["eng"]]
                o["sig"] = (c // EPOCH, c % EPOCH + 1)
                self.ccount[o["eng"]] = c + 1
        for i, o in enumerate(ops):
            e = o["eng"]
            need = {}
            for j in deps[i]:
                p = ops[j]
                if p["dma"]:
                    key = ("d", p["eng"], p["dn"] % NDMASEM)
                    val = 16 * (p["dn"] // NDMASEM + 1)
                    sem = self._dsem(p["eng"], p["dn"] % NDMASEM)
                else:
                    ep, cv = p["sig"]
                    key = ("c", p["eng"], ep)
                    val = cv
                    sem = self._csem(p["eng"], ep)
                if need.get(key, (None, 0))[1] < val:
                    need[key] = (sem, val)
            for key, (sem, val) in need.items():
                self._wait(e, key, sem, val)
            ins = o["fn"]()
            if o["dma"]:
                ins.then_inc(self._dsem(e, o["dn"] % NDMASEM), 16)
            elif signaling[i]:
                ep, cv = o["sig"]
                ins.then_inc(self._csem(e, ep), 1)
        for e in self.ENG:
            for e2, i in last.items():
                ep, cv = ops[i]["sig"]
                self._wait(e, ("c", e2, ep), self._csem(e2, ep), cv)
            for q in self.ENG:
                dc = self.dcount[q]
                for k in range(min(NDMASEM, dc)):
                    lastdn = ((dc - 1 - k) // NDMASEM) * NDMASEM + k
                    self._wait(e, ("d", q, k), self._dsem(q, k), 16 * (lastdn // NDMASEM + 1))
        self.ops = []


_UN = [0]


def _un(name):
    _UN[0] += 1
    return f"{name}_{_UN[0]}"


def _kl(k):
    return list(k) if isinstance(k, list) else [k]


def hk(blk):
    return [("hacc", blk, 0), ("hacc", blk, 1)]


class Rot:
    def __init__(self, tiles, name):
        self.tiles = tiles
        self.name = name
        self.i = 0

    def next(self):
        j = self.i % len(self.tiles)
        self.i += 1
        return self.tiles[j], (self.name, j)


def build(stop_after=99, dbg=False):
    nc = bass.Bass("TRN2", target_bir_lowering=False)

    def din(name, shape):
        return nc.dram_tensor(name, list(shape), F32, kind="ExternalInput").ap()

    x = din("x", [S, D])
    p = din("p", [2, S, 256])
    norm_mix = din("norm_mix", [2, D])
    norm_ffn = din("norm_ffn", [2, D])
    norm_pl = din("norm_pl", [2, D])
    final_norm = din("final_norm", [D])
    w_in_even = din("w_in_even", [1, D, 3072])
    conv_w_even = din("conv_w_even", [1, 3, 512])
    w_out_even = din("w_out_even", [1, 1024, D])
    w_in_odd = din("w_in_odd", [1, D, 4096])
    g_v_odd = din("g_v_odd", [1, 2048])
    w_s_odd = din("w_s_odd", [1, 8, 128, 128])
    b_s_odd = din("b_s_odd", [1, 8, 128])
    w_out_odd = din("w_out_odd", [1, 2048, D])
    router_c = din("router_c", [2, D, 4])
    router_c_b = din("router_c_b", [2, 4])
    router_f = din("router_f", [2, D, 32])
    router_f_b = din("router_f_b", [2, 32])
    moe_w1 = din("moe_w1", [2, 32, D, 256])
    moe_w3 = din("moe_w3", [2, 32, D, 256])
    moe_w2 = din("moe_w2", [2, 32, 256, D])
    w_pe = din("w_pe", [2, 256, D])
    w_pg = din("w_pg", [2, D, D])
    out = nc.dram_tensor("out", [S, D], F32, kind="ExternalOutput").ap()
    mixT = nc.dram_tensor("mixT", [8, 128, S], BF16, kind="ExternalOutput" if dbg else "Internal").ap()
    XS = nc.dram_tensor("XS", [16384, 1024], BF16, kind="Internal").ap()
    YS = nc.dram_tensor("YS", [16384, 1024], F32, kind="Internal").ap()
    hA = nc.dram_tensor("hA", [S, D], F32, kind="ExternalOutput" if dbg else "Internal").ap()
    hB = nc.dram_tensor("hB", [S, D], F32, kind="ExternalOutput" if dbg else "Internal").ap()

    with ExitStack() as gst:
        P = Prog(nc, gst)

        def gsb(name, shape, dt):
            return gst.enter_context(nc.sbuf_tensor(_un(name), shape, dt))

        pb = [gst.enter_context(nc.psum_tensor(f"pb{i}", [128, 512], F32)) for i in range(7)]
        ptb = gst.enter_context(nc.psum_tensor("ptb", [128, 8, 128], BF16))

        identf = gsb("identf", [128, 128], F32)
        ident = gsb("ident", [128, 128], BF16)
        ntri = gsb("ntri", [128, 128], BF16)
        nones = gsb("nones", [128, 128], BF16)
        zeros = gsb("zeros", [128, 128], BF16)
        nmask = gsb("nmask", [128, 128], BF16)
        ones1 = gsb("ones1", [1, 128], BF16)
        pones = gsb("pones", [128, 128], BF16)
        lstrict = gsb("lstrict", [128, 128], BF16)
        lsf = gsb("lsf", [128, 128], F32)
        tmpf = gsb("tmpf", [128, 128], F32)
        P.op("pool", lambda: nc.gpsimd.memset(identf[:], 1.0), writes=["identf"])
        P.op("pool", lambda: nc.gpsimd.affine_select(out=identf[:], in_=identf[:], pattern=[[-1, 128]],
                                                      compare_op=ALU.is_equal, fill=0.0, base=0, channel_multiplier=1),
             reads=["identf"], writes=["identf"])
        P.op("dve", lambda: nc.vector.tensor_copy(out=ident[:], in_=identf[:]), reads=["identf"], writes=["ident"])
        P.op("pool", lambda: nc.gpsimd.memset(tmpf[:], -1.0), writes=["tmpf"])
        P.op("dve", lambda: nc.vector.tensor_copy(out=nones[:], in_=tmpf[:]), reads=["tmpf"], writes=["nones"])
        P.op("pool", lambda: nc.gpsimd.affine_select(out=tmpf[:], in_=tmpf[:], pattern=[[-1, 128]],
                                                      compare_op=ALU.is_ge, fill=0.0, base=0, channel_multiplier=1),
             reads=["tmpf", "nones"], writes=["tmpf"])
        P.op("dve", lambda: nc.vector.tensor_copy(out=ntri[:], in_=tmpf[:]), reads=["tmpf"], writes=["ntri"])
        P.op("dve", lambda: nc.vector.tensor_scalar(out=nmask[:], in0=tmpf[:], scalar1=30000.0, scalar2=None, op0=ALU.mult),
             reads=["tmpf"], writes=["nmask"])
        P.op("pool", lambda: nc.gpsimd.memset(zeros[:], 0.0), writes=["zeros"])
        P.op("pool", lambda: nc.gpsimd.memset(ones1[:], 1.0), writes=["ones1"])
        P.op("pool", lambda: nc.gpsimd.memset(pones[:], 1.0), writes=["pones"])
        P.op("pool", lambda: nc.gpsimd.memset(lsf[:], 1.0), writes=["lsf"])
        P.op("pool", lambda: nc.gpsimd.affine_select(out=lsf[:], in_=lsf[:], pattern=[[1, 128]],
                                                      compare_op=ALU.is_gt, fill=0.0, base=0, channel_multiplier=-1),
             reads=["lsf"], writes=["lsf"])
        P.op("dve", lambda: nc.vector.tensor_copy(out=lstrict[:], in_=lsf[:]), reads=["lsf"], writes=["lstrict"])
        zrow = gsb("zrow", [128, 1024], BF16)
        P.op("pool", lambda: nc.gpsimd.memset(zrow[:], 0.0), writes=["zrow"])
        for zi in range(16384 // 128):
            P.dma("sp", lambda zi=zi: nc.sync.dma_start(out=XS[zi * 128:(zi + 1) * 128, :], in_=zrow[:]), reads=["zrow"], writes=["XS"])
        P.flush()

        def norm_rstd(P, src, skey, width, junk, jkey, ss, sskey):
            P.op("act", lambda: nc.scalar.activation(out=junk, in_=src, func=AF.Square, accum_out=ss),
                 reads=_kl(skey), writes=[jkey, sskey])
            P.op("act", lambda: nc.scalar.activation(out=ss, in_=ss, func=AF.Ln, scale=1.0 / width, bias=epsb[:]),
                 reads=[sskey, "epsb"], writes=[sskey])
            P.op("act", lambda: nc.scalar.activation(out=ss, in_=ss, func=AF.Exp, scale=-0.5),
                 reads=[sskey], writes=[sskey])

        epsb = gsb("epsb", [128, 1], F32)
        P.op("pool", lambda: nc.gpsimd.memset(epsb[:], EPS), writes=["epsb"])

        njunk = Rot([gsb(f"njunk{i}", [128, 1024], BF16) for i in range(1)], "njunk")
        nss = Rot([gsb(f"nss{i}", [128, 1], F32) for i in range(4)], "nss")
        nxn = Rot([gsb(f"nxn{i}", [128, 1024], BF16) for i in range(2)], "nxn")

        def norm_T(P, src, skey, gB, gkey, dstT, dkey, cp_eng="act"):
            junk, jk = njunk.next()
            ss, ssk = nss.next()
            xn, xnk = nxn.next()
            norm_rstd(P, src, skey, 1024, junk[:], jk, ss[:], ssk)
            P.op("dve", lambda: nc.vector.scalar_tensor_tensor(out=xn[:], in0=src, scalar=ss[:], in1=gB,
                                                                 op0=ALU.mult, op1=ALU.mult),
                 reads=_kl(skey) + [ssk, gkey], writes=[xnk])
            for k in range(8):
                P.op("pe", lambda k=k: nc.tensor.transpose(out=ptb[:, k, :], in_=xn[:, k * 128:(k + 1) * 128], identity=ident[:]),
                     reads=[xnk, "ident"], writes=["ptb"])
            if cp_eng == "act":
                P.op("act", lambda: nc.scalar.copy(out=dstT, in_=ptb[:]), reads=["ptb"], writes=[dkey])
            else:
                P.op("dve", lambda: nc.vector.tensor_copy(out=dstT, in_=ptb[:]), reads=["ptb"], writes=[dkey])
            return ss, ssk

        def load_gB(P, dst, key, src_row):
            P.dma("sp", lambda: nc.sync.dma_start(out=dst, in_=src_row.partition_broadcast(128)), writes=[key])

        with ExitStack() as st:
            def sb(name, shape, dt):
                return st.enter_context(nc.sbuf_tensor(_un(name), shape, dt))
            xnT = sb("xnT", [128, 8, S], BF16)
            gB0 = sb("gB0", [128, 1024], F32)
            load_gB(P, gB0[:], "gB0", norm_mix[0])
            xt_rot = Rot([sb(f"xt{i}", [128, 1024], F32) for i in range(2)], "xt")
            for b in range(NB):
                xt, xk = xt_rot.next()
                P.dma("sp", lambda xt=xt, b=b: nc.sync.dma_start(out=xt[:], in_=x[b * 128:(b + 1) * 128, :]), writes=[xk])
                norm_T(P, xt[:], xk, gB0[:], "gB0", xnT[:, :, b * 128:(b + 1) * 128], ("xnT", b))
            xnT_keys = lambda tc: [("xnT", 4 * tc + i) for i in range(4)]

            P.flush()
            st2 = ExitStack()
            st2.__enter__()
            sb_outer = sb

            def sb(name, shape, dt):
                return st2.enter_context(nc.sbuf_tensor(_un(name), shape, dt))
            cw = sb("cw", [128, 4, 3], F32)
            for fc in range(4):
                P.dma("sp", lambda fc=fc: nc.sync.dma_start(
                    out=cw[:, fc, :], in_=conv_w_even[0, :, fc * 128:(fc + 1) * 128].rearrange("w f -> f w"),
                    allow_slow_non_contiguous=True), writes=["cw"])
            wc_rot = Rot([sb(f"wc{i}", [128, 3, 8, 128], BF16) for i in range(2)], "wc")
            z_rot = Rot([sb(f"z{i}", [128, S + 2], F32) for i in range(2)], "z")
            hs_rot = Rot([sb(f"hs{i}", [128, 512], F32) for i in range(2)], "hs")
            acc_rot = Rot([sb(f"acc{i}", [128, 512], F32) for i in range(2)], "acc")
            ya_rot = Rot([sb(f"ya{i}", [128, 512], BF16) for i in range(2)], "ya")
            w_in0 = w_in_even[0].rearrange("(k p) n -> p k n", p=128)
            bank = Rot(pb[0:6], "pb")
            for fc in range(4):
                wc, wck = wc_rot.next()
                for j in range(3):
                    P.dma("pool", lambda wc=wc, j=j, fc=fc: nc.gpsimd.dma_start(
                        out=wc[:, j, :, :], in_=w_in0[:, :, j * 512 + fc * 128: j * 512 + (fc + 1) * 128]),
                        writes=[(wck, j)])
                z, zk = z_rot.next()
                P.op("pool", lambda z=z: nc.gpsimd.memset(z[:, 0:2], 0.0), writes=[(zk, -1)])
                for tc in range(8):
                    pp = []
                    for j in range(3):
                        pt, pk = bank.next()
                        for k in range(8):
                            P.op("pe", lambda pt=pt, wc=wc, j=j, k=k, tc=tc: nc.tensor.matmul(
                                pt[:], lhsT=wc[:, j, k, :], rhs=xnT[:, k, tc * 512:(tc + 1) * 512],
                                start=(k == 0), stop=(k == 7)),
                                reads=[(wck, j)] + xnT_keys(tc), writes=[pk])
                        pp.append((pt, pk))
                    (ph, phk), (pgb, pgbk), (pgc, pgck) = pp
                    hs, hsk = hs_rot.next()
                    acc, acck = acc_rot.next()
                    ya, yak = ya_rot.next()
                    P.op("act", lambda hs=hs, ph=ph: nc.scalar.copy(out=hs[:], in_=ph[:]), reads=[phk], writes=[hsk])
                    P.op("dve", lambda z=z, tc=tc, pgc=pgc, hs=hs: nc.vector.tensor_tensor(
                        out=z[:, 2 + tc * 512: 2 + (tc + 1) * 512], in0=pgc[:], in1=hs[:], op=ALU.mult),
                        reads=[pgck, hsk], writes=[(zk, tc)])
                    zr = [(zk, tc), (zk, tc - 1)]
                    P.op("dve", lambda acc=acc, z=z, tc=tc, fc=fc: nc.vector.tensor_scalar(
                        out=acc[:], in0=z[:, tc * 512: tc * 512 + 512], scalar1=cw[:, fc, 0:1], scalar2=None, op0=ALU.mult),
                        reads=zr + ["cw"], writes=[acck])
                    for wv in (1, 2):
                        P.op("dve", lambda acc=acc, z=z, tc=tc, fc=fc, wv=wv: nc.vector.scalar_tensor_tensor(
                            out=acc[:], in0=z[:, tc * 512 + wv: tc * 512 + wv + 512], scalar=cw[:, fc, wv:wv + 1],
                            in1=acc[:], op0=ALU.mult, op1=ALU.add),
                            reads=zr + ["cw", acck], writes=[acck])
                    P.op("dve", lambda ya=ya, acc=acc, pgb=pgb: nc.vector.tensor_tensor(
                        out=ya[:], in0=pgb[:], in1=acc[:], op=ALU.mult), reads=[pgbk, acck], writes=[yak])
                    P.dma("sp", lambda ya=ya, fc=fc, tc=tc: nc.sync.dma_start(
                        out=mixT[fc, :, tc * 512:(tc + 1) * 512], in_=ya[:]), reads=[yak], writes=[("mixT", fc, tc)])
            P.flush()
            st2.__exit__(None, None, None)
            sb = sb_outer
            if stop_after >= 2:
                wq_rot = Rot([sb(f"wq{i}", [128, 3, 8, 128], BF16) for i in range(2)], "wq")
                qT_rot = Rot([sb(f"qT{i}", [128, S], BF16) for i in range(2)], "qT")
                kT_rot = Rot([sb(f"kT{i}", [128, S], BF16) for i in range(2)], "kT")
                v_rot = Rot([sb(f"v{i}", [128, NB, 128], BF16) for i in range(2)], "v")
                yb_rot = Rot([sb(f"yb{i}", [128, S], BF16) for i in range(2)], "yb")
                e_rot = Rot([sb(f"e{i}", [128, 512], F32) for i in range(3)], "e")
                sp_rot = Rot([sb(f"sp{i}", [128, 512], BF16) for i in range(3)], "sp")
                a_rot = Rot([sb(f"a{i}", [128, 512], BF16) for i in range(3)], "a")
                r32_rot = Rot([sb(f"r32{i}", [128, 512], F32) for i in range(2)], "r32")
                r16_rot = Rot([sb(f"r16{i}", [128, 512], BF16) for i in range(2)], "r16")
                zb = Rot(pb[0:3], "pb")
                ob = Rot(pb[3:7], "pbo")
                pjb = Rot(pb[3:7], "pbo")

                def proj(hp):
                    wq, wqk = wq_rot.next()
                    for j in range(3):
                        P.dma("pool", lambda wq=wq, j=j, hp=hp: nc.gpsimd.dma_start(
                            out=wq[:, j, :, :], in_=w_in0[:, :, 1536 + j * 512 + hp * 128: 1536 + j * 512 + (hp + 1) * 128]),
                            writes=[(wqk, j)])
                    qT, qk = qT_rot.next()
                    kT, kk = kT_rot.next()
                    v, vk = v_rot.next()
                    for tc in range(8):
                        for j, (dst, dk, sc) in enumerate(((qT, qk, 0.125), (kT, kk, 1.0))):
                            pt, pk = pjb.next()
                            for k in range(8):
                                P.op("pe", lambda pt=pt, wq=wq, j=j, k=k, tc=tc: nc.tensor.matmul(
                                    pt[:], lhsT=wq[:, j, k, :], rhs=xnT[:, k, tc * 512:(tc + 1) * 512],
                                    start=(k == 0), stop=(k == 7)),
                                    reads=[(wqk, j)] + xnT_keys(tc), writes=[pk])
                            P.op("dve", lambda dst=dst, pt=pt, tc=tc, sc=sc: nc.vector.tensor_scalar(
                                out=dst[:, tc * 512:(tc + 1) * 512], in0=pt[:], scalar1=sc, scalar2=None, op0=ALU.mult),
                                reads=[pk], writes=[(dk, tc)])
                        pt, pk = pjb.next()
                        for tb in range(4):
                            b = tc * 4 + tb
                            for k in range(8):
                                P.op("pe", lambda pt=pt, wq=wq, k=k, b=b, tb=tb: nc.tensor.matmul(
                                    pt[:, tb * 128:(tb + 1) * 128], lhsT=xnT[:, k, b * 128:(b + 1) * 128], rhs=wq[:, 2, k, :],
                                    start=(k == 0), stop=(k == 7)),
                                    reads=[(wqk, 2), ("xnT", b)], writes=[pk])
                        P.op("dve", lambda v=v, pt=pt, tc=tc: nc.vector.tensor_copy(
                            out=v[:, tc * 4:(tc + 1) * 4, :], in_=pt[:].rearrange("p (b d) -> p b d", b=4)),
                            reads=[pk], writes=[(vk, tc)])
                    return (qT, qk, kT, kk, v, vk)

                def attention(hp, qkv):
                    qT, qk, kT, kk, v, vk = qkv
                    yb, ybk = yb_rot.next()
                    tiles = []
                    for hh in range(2):
                        for qc in range(8):
                            nkb = 4 * qc + 4
                            for ii, kb in enumerate(range(nkb - 1, -1, -1)):
                                jd = kb - 4 * qc
                                c0 = jd * 128 if jd >= 0 else 0
                                tiles.append(dict(hh=hh, qc=qc, kb=kb, c0=c0, diag=(jd >= 0), first=(ii == 0),
                                                  last=(kb == 0)))
                    nt = len(tiles)
                    st_ = [dict() for _ in range(nt)]

                    def stage_qk(i):
                        t = tiles[i]
                        po = t["hh"] * 64
                        zt, zk_ = zb.next()
                        st_[i]["z"] = (zt, zk_)
                        c0 = t["c0"]
                        qc, kb = t["qc"], t["kb"]
                        P.op("pe", lambda: nc.tensor.matmul(
                            zt[:, c0:512], lhsT=kT[po:po + 64, kb * 128:(kb + 1) * 128],
                            rhs=qT[po:po + 64, qc * 512 + c0:(qc + 1) * 512], start=True, stop=False),
                            reads=[(kk, kb // 4), (qk, qc)], writes=[zk_])
                        if t["diag"]:
                            P.op("pe", lambda: nc.tensor.matmul(
                                zt[:, c0:c0 + 128], lhsT=ident[:], rhs=nmask[:], start=False, stop=False),
                                reads=["ident", "nmask"], writes=[zk_])
                        et, ek = e_rot.next()
                        spt, spk = sp_rot.next()
                        st_[i]["sp"] = (spt, spk)
                        P.op("act", lambda: nc.scalar.activation(out=et[:, c0:512], in_=zt[:, c0:512], func=AF.Exp),
                             reads=[zk_], writes=[ek])
                        P.op("act", lambda: nc.scalar.activation(out=spt[:, c0:512], in_=et[:, c0:512], func=AF.Ln, bias=1.0),
                             reads=[ek], writes=[spk])

                    def stage_cum(i):
                        t = tiles[i]
                        zt, zk_ = st_[i]["z"]
                        spt, spk = st_[i]["sp"]
                        c0 = t["c0"]
                        if t["first"]:
                            r32, r32k = r32_rot.next()
                            st_[i]["r32"] = (r32, r32k)
                            P.op("pool", lambda: nc.gpsimd.memset(r32[:], 0.0), writes=[r32k])
                        else:
                            st_[i]["r32"] = st_[i - 1]["r32"]
                            r32, r32k = st_[i]["r32"]
                        P.op("pe", lambda: nc.tensor.matmul(zt[:, c0:512], lhsT=ntri[:], rhs=spt[:, c0:512],
                                                            start=False, stop=t["first"]),
                             reads=["ntri", spk], writes=[zk_])
                        if not t["first"]:
                            r16, r16k = st_[i]["r16"]
                            P.op("pe", lambda: nc.tensor.matmul(zt[:, c0:512], lhsT=nones[:], rhs=r16[:, c0:512],
                                                                start=False, stop=True),
                                 reads=["nones", r16k], writes=[zk_])
                        if not t["last"]:
                            c1 = tiles[i + 1]["c0"]
                            P.op("dve", lambda: nc.vector.tensor_tensor(out=r32[:, c0:512], in0=r32[:, c0:512],
                                                                       in1=spt[:, c0:512], op=ALU.add),
                                 reads=[r32k, spk], writes=[r32k])
                            r16n, r16nk = r16_rot.next()
                            st_[i + 1]["r16"] = (r16n, r16nk)
                            P.op("pool", lambda: nc.gpsimd.tensor_copy(out=r16n[:, c1:512], in_=r32[:, c1:512]),
                                 reads=[r32k], writes=[r16nk])
                        at, ak = a_rot.next()
                        st_[i]["a"] = (at, ak)
                        P.op("act", lambda: nc.scalar.activation(out=at[:, c0:512], in_=zt[:, c0:512], func=AF.Exp),
                             reads=[zk_], writes=[ak])

                    def stage_av(i):
                        t = tiles[i]
                        at, ak = st_[i]["a"]
                        c0 = t["c0"]
                        kb = t["kb"]
                        po = t["hh"] * 64
                        if t["first"]:
                            ot, ok = ob.next()
                            st_[i]["o"] = (ot, ok)
                            P.op("pe", lambda: nc.tensor.matmul(ot[:], lhsT=zeros[:], rhs=qT[:, 0:512], start=True, stop=False),
                                 reads=["zeros", (qk, 0)], writes=[ok])
                        else:
                            st_[i]["o"] = st_[i - 1]["o"]
                            ot, ok = st_[i]["o"]
                        P.op("pe", lambda: nc.tensor.matmul(ot[:, c0:512], lhsT=v[:, kb, :], rhs=at[:, c0:512],
                                                            start=False, stop=t["last"]),
                             reads=[(vk, kb // 4), ak], writes=[ok])
                        if t["last"]:
                            qc = t["qc"]
                            P.op("dve", lambda: nc.vector.tensor_copy(out=yb[po:po + 64, qc * 512:(qc + 1) * 512],
                                                                      in_=ot[po:po + 64, :]),
                                 reads=[ok], writes=[(ybk, qc, t["hh"])])
                        st_[i].pop("z", None)

                    stage_qk(0)
                    for i in range(nt):
                        if i + 1 < nt:
                            stage_qk(i + 1)
                        stage_cum(i)
                        if i >= 1:
                            stage_av(i - 1)
                    stage_av(nt - 1)
                    for qc in range(8):
                        P.dma("sp", lambda qc=qc: nc.sync.dma_start(out=mixT[4 + hp, :, qc * 512:(qc + 1) * 512],
                                                                    in_=yb[:, qc * 512:(qc + 1) * 512]),
                              reads=[(ybk, qc, 0), (ybk, qc, 1)], writes=[("mixT", 4 + hp, qc)])

                nhp = 4 if stop_after >= 3 else 1
                qkvs = {0: proj(0)}
                for hp in range(nhp):
                    if hp + 1 < nhp:
                        qkvs[hp + 1] = proj(hp + 1)
                    attention(hp, qkvs.pop(hp))
            P.flush()

        if stop_after >= 4:
            with ExitStack() as st:
                def sb(name, shape, dt):
                    return st.enter_context(nc.sbuf_tensor(_un(name), shape, dt))
                wo = sb("wo", [128, 8, 1024], BF16)
                P.dma("pool", lambda: nc.gpsimd.dma_start(out=wo[:], in_=w_out_even[0].rearrange("(k p) n -> p k n", p=128)),
                      writes=["wo"])
                mx_rot = Rot([sb(f"mx{i}", [128, 8, 512], BF16) for i in range(2)], "mx")
                xt_rot = Rot([sb(f"xt{i}", [128, 1024], F32) for i in range(3)], "xt")
                bank = Rot(pb[0:6], "pb")
                for tc in range(8):
                    mx, mxk = mx_rot.next()
                    P.dma("sp", lambda mx=mx, tc=tc: nc.sync.dma_start(
                        out=mx[:], in_=mixT[:, :, tc * 512:(tc + 1) * 512].rearrange("c p t -> p c t")), writes=[mxk])
                    for tb in range(4):
                        b = tc * 4 + tb
                        xt, xk = xt_rot.next()
                        P.dma("sp", lambda xt=xt, b=b: nc.sync.dma_start(out=xt[:], in_=x[b * 128:(b + 1) * 128, :]), writes=[xk])
                        for half in range(2):
                            pt, pk = bank.next()
                            for c in range(8):
                                P.op("pe", lambda pt=pt, mx=mx, c=c, tb=tb, half=half: nc.tensor.matmul(
                                    pt[:], lhsT=mx[:, c, tb * 128:(tb + 1) * 128], rhs=wo[:, c, half * 512:(half + 1) * 512],
                                    start=(c == 0), stop=(c == 7)), reads=[mxk, "wo"], writes=[pk])
                            P.op("dve", lambda xt=xt, pt=pt, half=half: nc.vector.tensor_tensor(
                                out=xt[:, half * 512:(half + 1) * 512], in0=pt[:], in1=xt[:, half * 512:(half + 1) * 512], op=ALU.add),
                                reads=[pk, xk], writes=[xk])
                        P.dma("sp", lambda xt=xt, b=b: nc.sync.dma_start(out=hA[b * 128:(b + 1) * 128, :], in_=xt[:]),
                              reads=[xk], writes=[("hA", b)])
                P.flush()

        def moe_pl_phase(l, hin, hout, final):
            with ExitStack() as st:
                def sb(name, shape, dt):
                    return st.enter_context(nc.sbuf_tensor(_un(name), shape, dt))
                NBS = 16
                hacc = sb("hacc", [128, NBS, 1024], F32)
                xT = sb("xT", [128, 8, NBS * 128], BF16)
                gBf = sb("gBf", [128, 1024], F32)
                gBp = sb("gBp", [128, 1024], F32)
                load_gB(P, gBf[:], "gBf", norm_ffn[l])
                load_gB(P, gBp[:], "gBp", norm_pl[l])
                if final:
                    gBo = sb("gBo", [128, 1024], F32)
                    load_gB(P, gBo[:], "gBo", final_norm)
                wr = sb("wr", [128, 8, 36], BF16)
                P.dma("pool", lambda: nc.gpsimd.dma_start(out=wr[:, :, 0:4], in_=router_c[l].rearrange("(k p) n -> p k n", p=128)),
                      writes=["wr"])
                P.dma("pool", lambda: nc.gpsimd.dma_start(out=wr[:, :, 4:36], in_=router_f[l].rearrange("(k p) n -> p k n", p=128)),
                      writes=["wr"])
                rb = sb("rb", [128, 36], F32)
                P.dma("sp", lambda: nc.sync.dma_start(out=rb[:, 0:4], in_=router_c_b[l].partition_broadcast(128)), writes=["rb"])
                P.dma("sp", lambda: nc.sync.dma_start(out=rb[:, 4:36], in_=router_f_b[l].partition_broadcast(128)), writes=["rb"])
                wpg = sb("wpg", [128, 8, 1024], BF16)
                wpe = sb("wpe", [128, 2, 1024], BF16)
                P.dma("pool", lambda: nc.gpsimd.dma_start(out=wpg[:], in_=w_pg[l].rearrange("(k p) n -> p k n", p=128)), writes=["wpg"])
                P.dma("pool", lambda: nc.gpsimd.dma_start(out=wpe[:], in_=w_pe[l].rearrange("(k p) n -> p k n", p=128)), writes=["wpe"])
                w13_rot = Rot([sb(f"w13_{i}", [128, 2, 8, 256], BF16) for i in range(2)], "w13")
                w2_rot = Rot([sb(f"w2_{i}", [128, 2, 1024], BF16) for i in range(2)], "w2")
                lg = sb("lg", [128, NBS, 36], F32)
                comb = sb("comb", [128, NBS, 32], F32)
                r_mx = sb("r_mx", [128, NBS], F32)
                r_gm = sb("r_gm", [128, NBS, 4], F32)
                r_ec = sb("r_ec", [128, NBS, 4], F32)
                r_pg = sb("r_pg", [128, NBS], F32)
                r_t = sb("r_t", [128, NBS, 4, 8], F32)
                r_lfs = sb("r_lfs", [128, NBS, 8], F32)
                r_t8 = sb("r_t8", [128, NBS, 8], F32)
                r_sel = sb("r_sel", [128, NBS, 8], F32)
                r_ex = sb("r_ex", [128, NBS, 8], F32)
                r_d = sb("r_d", [128, NBS], F32)
                s_rot = Rot([sb(f"s{i}", [128, 512], F32) for i in range(2)], "s")
                he_rot = Rot([sb(f"he{i}", [128, 2, 512], BF16) for i in range(2)], "he")
                pblk_rot = Rot([sb(f"pblk{i}", [128, 256], F32) for i in range(2)], "pblk")
                pb16_rot = Rot([sb(f"pb16{i}", [128, 256], BF16) for i in range(2)], "pb16")
                pT_rot = Rot([sb(f"pT{i}", [128, 2, 128], BF16) for i in range(2)], "pT")
                hnT_rot = Rot([sb(f"hnT{i}", [128, 8, 128], BF16) for i in range(2)], "hnT")
                sg_rot = Rot([sb(f"sg{i}", [128, 1024], F32) for i in range(1)], "sg")
                for sc in range(2):
                    hbank = Rot(pb[0:4], "pb")
                    ybank = Rot(pb[4:7], "pby")
                    for blk in range(NBS):
                        b = sc * NBS + blk
                        P.dma("sp", lambda blk=blk, b=b: nc.sync.dma_start(out=hacc[:, blk, :], in_=hin[b * 128:(b + 1) * 128, :]),
                              writes=hk(blk))
                        norm_T(P, hacc[:, blk, :], hk(blk), gBf[:], "gBf", xT[:, :, blk * 128:(blk + 1) * 128], ("xT", blk))
                        pt, pk = ybank.next()
                        for k in range(8):
                            P.op("pe", lambda pt=pt, k=k, blk=blk: nc.tensor.matmul(
                                pt[:, 0:36], lhsT=xT[:, k, blk * 128:(blk + 1) * 128], rhs=wr[:, k, :],
                                start=(k == 0), stop=(k == 7)), reads=[("xT", blk), "wr"], writes=[pk])
                        P.op("dve", lambda pt=pt, blk=blk: nc.vector.tensor_tensor(out=lg[:, blk, :], in0=pt[:, 0:36], in1=rb[:], op=ALU.add),
                             reads=[pk, "rb"], writes=["lg"])
                    lc = lg[:, :, 0:4]
                    lf = lg[:, :, 4:36].rearrange("p b (g e) -> p b g e", g=4)
                    P.op("dve", lambda: nc.vector.tensor_reduce(out=r_mx[:], in_=lc, axis=AX.X, op=ALU.max), reads=["lg"], writes=["r_mx"])
                    P.op("dve", lambda: nc.vector.tensor_tensor(out=r_gm[:], in0=lc, in1=r_mx[:].unsqueeze(2).to_broadcast([128, NBS, 4]),
                                                                op=ALU.is_ge), reads=["lg", "r_mx"], writes=["r_gm"])
                    P.op("dve", lambda: nc.vector.tensor_tensor(out=r_ec[:], in0=lc, in1=r_mx[:].unsqueeze(2).to_broadcast([128, NBS, 4]),
                                                                op=ALU.subtract), reads=["lg", "r_mx"], writes=["r_ec"])
                    P.op("act", lambda: nc.scalar.activation(out=r_ec[:], in_=r_ec[:], func=AF.Exp), reads=["r_ec"], writes=["r_ec"])
                    P.op("dve", lambda: nc.vector.tensor_reduce(out=r_pg[:], in_=r_ec[:], axis=AX.X, op=ALU.add), reads=["r_ec"], writes=["r_pg"])
                    P.op("dve", lambda: nc.vector.tensor_tensor(out=r_t[:], in0=lf, in1=r_gm[:].unsqueeze(3).to_broadcast([128, NBS, 4, 8]),
                                                                op=ALU.mult), reads=["lg", "r_gm"], writes=["r_t"])
                    P.op("dve", lambda: nc.vector.tensor_reduce(out=r_lfs[:], in_=r_t[:].rearrange("p b g e -> p b e g"), axis=AX.X, op=ALU.add),
                         reads=["r_t"], writes=["r_lfs"])
                    for blk in range(NBS):
                        P.op("dve", lambda blk=blk: nc.vector.max(out=r_t8[:, blk, :], in_=r_lfs[:, blk, :]), reads=["r_lfs"], writes=["r_t8"])
                    l1b = r_t8[:, :, 0:1].to_broadcast([128, NBS, 8])
                    l2b = r_t8[:, :, 1:2].to_broadcast([128, NBS, 8])
                    P.op("dve", lambda: nc.vector.tensor_tensor(out=r_sel[:], in0=r_lfs[:], in1=l2b, op=ALU.is_ge), reads=["r_lfs", "r_t8"], writes=["r_sel"])
                    P.op("dve", lambda: nc.vector.tensor_tensor(out=r_ex[:], in0=r_lfs[:], in1=l1b, op=ALU.subtract), reads=["r_lfs", "r_t8"], writes=["r_ex"])
                    P.op("act", lambda: nc.scalar.activation(out=r_ex[:], in_=r_ex[:], func=AF.Exp), reads=["r_ex"], writes=["r_ex"])
                    P.op("dve", lambda: nc.vector.tensor_tensor(out=r_ex[:], in0=r_ex[:], in1=r_sel[:], op=ALU.mult), reads=["r_ex", "r_sel"], writes=["r_ex"])
                    P.op("dve", lambda: nc.vector.tensor_reduce(out=r_d[:], in_=r_ex[:], axis=AX.X, op=ALU.add), reads=["r_ex"], writes=["r_d"])
                    P.op("dve", lambda: nc.vector.tensor_tensor(out=r_d[:], in0=r_d[:], in1=r_pg[:], op=ALU.mult), reads=["r_d", "r_pg"], writes=["r_d"])
                    P.op("dve", lambda: nc.vector.reciprocal(out=r_d[:], in_=r_d[:]), reads=["r_d"], writes=["r_d"])
                    P.op("dve", lambda: nc.vector.tensor_tensor(out=r_ex[:], in0=r_ex[:], in1=r_d[:].unsqueeze(2).to_broadcast([128, NBS, 8]),
                                                                op=ALU.mult), reads=["r_ex", "r_d"], writes=["r_ex"])
                    P.op("dve", lambda: nc.vector.tensor_tensor(
                        out=comb[:].rearrange("p b (g e) -> p b g e", g=4),
                        in0=r_gm[:].unsqueeze(3).to_broadcast([128, NBS, 4, 8]),
                        in1=r_ex[:].unsqueeze(2).to_broadcast([128, NBS, 4, 8]), op=ALU.mult),
                        reads=["r_gm", "r_ex"], writes=["comb"])
                    wts = {}

                    def load_w(e):
                        w13, w13k = w13_rot.next()
                        w2, w2k = w2_rot.next()
                        P.dma("pool", lambda: nc.gpsimd.dma_start(out=w13[:, 0, :, :], in_=moe_w1[l, e].rearrange("(k p) f -> p k f", p=128)),
                              writes=[(w13k, 0)])
                        P.dma("pool", lambda: nc.gpsimd.dma_start(out=w13[:, 1, :, :], in_=moe_w3[l, e].rearrange("(k p) f -> p k f", p=128)),
                              writes=[(w13k, 1)])
                        P.dma("pool", lambda: nc.gpsimd.dma_start(out=w2[:], in_=moe_w2[l, e].rearrange("(c p) d -> p c d", p=128)),
                              writes=[w2k])
                        wts[e] = (w13, w13k, w2, w2k)

                    units = [(e, tch) for e in range(32) for tch in range(4)]
                    ust = {}

                    def stage_h(u):
                        e, tch = units[u]
                        w13, w13k, w2, w2k = wts[e]
                        he, hek = he_rot.next()
                        ust[u] = (he, hek)
                        for fch in range(2):
                            p1, p1k = hbank.next()
                            p3, p3k = hbank.next()
                            for j, (pt, pk) in enumerate(((p1, p1k), (p3, p3k))):
                                for k in range(8):
                                    P.op("pe", lambda pt=pt, j=j, k=k, fch=fch: nc.tensor.matmul(
                                        pt[:], lhsT=w13[:, j, k, fch * 128:(fch + 1) * 128], rhs=xT[:, k, tch * 512:(tch + 1) * 512],
                                        start=(k == 0), stop=(k == 7)),
                                        reads=[(w13k, j)] + [("xT", 4 * tch + i) for i in range(4)], writes=[pk])
                            s, sk = s_rot.next()
                            P.op("act", lambda s=s, p1=p1: nc.scalar.activation(out=s[:], in_=p1[:], func=AF.Silu), reads=[p1k], writes=[sk])
                            P.op("dve", lambda s=s, p3=p3, fch=fch: nc.vector.tensor_tensor(out=he[:, fch, :], in0=p3[:], in1=s[:], op=ALU.mult),
                                 reads=[p3k, sk], writes=[(hek, fch)])

                    def stage_y(u):
                        e, tch = units[u]
                        w13, w13k, w2, w2k = wts[e]
                        he, hek = ust.pop(u)
                        for tb in range(4):
                            blk = tch * 4 + tb
                            for half in range(2):
                                py, pyk = ybank.next()
                                for fch in range(2):
                                    P.op("pe", lambda py=py, fch=fch, tb=tb, half=half: nc.tensor.matmul(
                                        py[:], lhsT=he[:, fch, tb * 128:(tb + 1) * 128], rhs=w2[:, fch, half * 512:(half + 1) * 512],
                                        start=(fch == 0), stop=(fch == 1)), reads=[(hek, fch), w2k], writes=[pyk])
                                P.op("dve", lambda py=py, blk=blk, half=half: nc.vector.scalar_tensor_tensor(
                                    out=hacc[:, blk, half * 512:(half + 1) * 512], in0=py[:], scalar=comb[:, blk, e:e + 1],
                                    in1=hacc[:, blk, half * 512:(half + 1) * 512], op0=ALU.mult, op1=ALU.add),
                                    reads=[pyk, "comb", ("hacc", blk, half)], writes=[("hacc", blk, half)])
                        if tch == 3:
                            wts.pop(e)
                            if e + 2 < 32:
                                load_w(e + 2)

                    load_w(0)
                    load_w(1)
                    nu = len(units)
                    stage_h(0)
                    for u in range(nu):
                        if u + 1 < nu:
                            stage_h(u + 1)
                        stage_y(u)
                    gbank = Rot(pb[0:4], "pb")
                    for blk in range(NBS):
                        b = sc * NBS + blk
                        hnT, hnk = hnT_rot.next()
                        norm_T(P, hacc[:, blk, :], hk(blk), gBp[:], "gBp", hnT[:], hnk)
                        pblk, pblkk = pblk_rot.next()
                        p16, p16k = pb16_rot.next()
                        pT, pTk = pT_rot.next()
                        P.dma("sp", lambda pblk=pblk, b=b: nc.sync.dma_start(out=pblk[:], in_=p[l, b * 128:(b + 1) * 128, :]), writes=[pblkk])
                        P.op("pool", lambda p16=p16, pblk=pblk: nc.gpsimd.tensor_copy(out=p16[:], in_=pblk[:]), reads=[pblkk], writes=[p16k])
                        for k in range(2):
                            P.op("pe", lambda p16=p16, k=k: nc.tensor.transpose(out=ptb[:, k, :], in_=p16[:, k * 128:(k + 1) * 128], identity=ident[:]),
                                 reads=[p16k, "ident"], writes=["ptb"])
                        P.op("act", lambda pT=pT: nc.scalar.copy(out=pT[:], in_=ptb[:, 0:2, :]), reads=["ptb"], writes=[pTk])
                        sg, sgk = sg_rot.next()
                        for half in range(2):
                            pg_, pgk = gbank.next()
                            pe_, pek = gbank.next()
                            for k in range(8):
                                P.op("pe", lambda pg_=pg_, hnT=hnT, k=k, half=half: nc.tensor.matmul(
                                    pg_[:], lhsT=hnT[:, k, :], rhs=wpg[:, k, half * 512:(half + 1) * 512], start=(k == 0), stop=(k == 7)),
                                    reads=[hnk, "wpg"], writes=[pgk])
                            for k in range(2):
                                P.op("pe", lambda pe_=pe_, pT=pT, k=k, half=half: nc.tensor.matmul(
                                    pe_[:], lhsT=pT[:, k, :], rhs=wpe[:, k, half * 512:(half + 1) * 512], start=(k == 0), stop=(k == 1)),
                                    reads=[pTk, "wpe"], writes=[pek])
                            P.op("act", lambda sg=sg, pg_=pg_, half=half: nc.scalar.activation(out=sg[:, half * 512:(half + 1) * 512], in_=pg_[:], func=AF.Sigmoid),
                                 reads=[pgk], writes=[(sgk, half)])
                            P.op("dve", lambda sg=sg, pe_=pe_, half=half: nc.vector.tensor_tensor(
                                out=sg[:, half * 512:(half + 1) * 512], in0=pe_[:], in1=sg[:, half * 512:(half + 1) * 512], op=ALU.mult),
                                reads=[pek, (sgk, half)], writes=[(sgk, half)])
                        P.op("dve", lambda sg=sg, blk=blk: nc.vector.tensor_tensor(out=hacc[:, blk, :], in0=hacc[:, blk, :], in1=sg[:], op=ALU.add),
                             reads=[(sgk, 0), (sgk, 1)] + hk(blk), writes=hk(blk))
                        if final:
                            junk, jk = njunk.next()
                            ss, ssk = nss.next()
                            norm_rstd(P, hacc[:, blk, :], hk(blk), 1024, junk[:], jk, ss[:], ssk)
                            P.op("dve", lambda ss=ss, blk=blk: nc.vector.scalar_tensor_tensor(
                                out=hacc[:, blk, :], in0=hacc[:, blk, :], scalar=ss[:], in1=gBo[:], op0=ALU.mult, op1=ALU.mult),
                                reads=hk(blk) + [ssk, "gBo"], writes=hk(blk))
                        P.dma("sp", lambda blk=blk, b=b: nc.sync.dma_start(out=hout[b * 128:(b + 1) * 128, :], in_=hacc[:, blk, :]),
                              reads=hk(blk), writes=[("hout", b)])
                    P.flush()

        def sparse_moe_pl_phase(l, hin, hout, final):
            TS = 256
            NT = 64
            with ExitStack() as st:
                def sb(name, shape, dt):
                    return st.enter_context(nc.sbuf_tensor(_un(name), shape, dt))
                gBf = sb("gBf", [128, 1024], F32)
                gBp = sb("gBp", [128, 1024], F32)
                load_gB(P, gBf[:], "gBf", norm_ffn[l])
                load_gB(P, gBp[:], "gBp", norm_pl[l])
                if final:
                    gBo = sb("gBo", [128, 1024], F32)
                    load_gB(P, gBo[:], "gBo", final_norm)
                wr = sb("wr", [128, 8, 36], BF16)
                P.dma("pool", lambda: nc.gpsimd.dma_start(out=wr[:, :, 0:4], in_=router_c[l].rearrange("(k p) n -> p k n", p=128)),
                      writes=["wr"])
                P.dma("pool", lambda: nc.gpsimd.dma_start(out=wr[:, :, 4:36], in_=router_f[l].rearrange("(k p) n -> p k n", p=128)),
                      writes=["wr"])
                rb = sb("rb", [128, 36], F32)
                P.dma("sp", lambda: nc.sync.dma_start(out=rb[:, 0:4], in_=router_c_b[l].partition_broadcast(128)), writes=["rb"])
                P.dma("sp", lambda: nc.sync.dma_start(out=rb[:, 4:36], in_=router_f_b[l].partition_broadcast(128)), writes=["rb"])
                wpg = sb("wpg", [128, 8, 1024], BF16)
                wpe = sb("wpe", [128, 2, 1024], BF16)
                P.dma("pool", lambda: nc.gpsimd.dma_start(out=wpg[:], in_=w_pg[l].rearrange("(k p) n -> p k n", p=128)), writes=["wpg"])
                P.dma("pool", lambda: nc.gpsimd.dma_start(out=wpe[:], in_=w_pe[l].rearrange("(k p) n -> p k n", p=128)), writes=["wpe"])
                idx1_i = sb("idx1_i", [128, NB], I32)
                idx2_i = sb("idx2_i", [128, NB], I32)
                gate1 = sb("gate1", [128, NB], F32)
                gate2 = sb("gate2", [128, NB], F32)
                widx_i = sb("widx_i", [128, NT], I32)
                pio = sb("pio", [128, 1], F32)
                P.op("pool", lambda: nc.gpsimd.iota(pio[:], pattern=[[0, 1]], base=l * 32 * 128, channel_multiplier=1,
                                                    allow_small_or_imprecise_dtypes=True), writes=["pio"])
                with ExitStack() as st1:
                    def sb1(name, shape, dt):
                        return st1.enter_context(nc.sbuf_tensor(_un(name), shape, dt))
                    xnall = sb1("xnall", [128, NB, 1024], BF16)
                    ht_rot = Rot([sb1(f"ht{i}", [128, 1024], F32) for i in range(2)], "ht")
                    xTb_rot = Rot([sb1(f"xTb{i}", [128, 8, 128], BF16) for i in range(2)], "xTb")
                    lg = sb1("lg", [128, NB, 36], F32)
                    r_mx = sb1("r_mx", [128, NB], F32)
                    r_gm = sb1("r_gm", [128, NB, 4], F32)
                    r_ec = sb1("r_ec", [128, NB, 4], F32)
                    r_pg = sb1("r_pg", [128, NB], F32)
                    r_t = sb1("r_t", [128, NB, 4, 8], F32)
                    r_lfs = sb1("r_lfs", [128, NB, 8], F32)
                    r_t8 = sb1("r_t8", [128, NB, 8], F32)
                    r_sel = sb1("r_sel", [128, NB, 8], F32)
                    r_s1 = sb1("r_s1", [128, NB, 8], F32)
                    r_s2 = sb1("r_s2", [128, NB, 8], F32)
                    r_ex = sb1("r_ex", [128, NB, 8], F32)
                    r_d = sb1("r_d", [128, NB], F32)
                    r_tmp8 = sb1("r_tmp8", [128, NB, 8], F32)
                    M1 = sb1("M1", [128, NB, 32], F32)
                    M2 = sb1("M2", [128, NB, 32], F32)
                    Mb16 = sb1("Mb16", [128, NB, 32], BF16)
                    Rm16 = sb1("Rm16", [128, NB + 1, 32], BF16)
                    rank = sb1("rank", [128, NB, 32], F32)
                    cnt = sb1("cnt", [128, 32], F32)
                    thr16 = sb1("thr16", [128, 16], F32)
                    thr64 = sb1("thr64", [128, NT], F32)
                    cmpA = sb1("cmpA", [128, 32, 16], F32)
                    ntile = sb1("ntile", [128, 32], F32)
                    csA = sb1("csA", [128, 32], F32)
                    csB = sb1("csB", [128, 32], F32)
                    base = sb1("base", [128, 32], F32)
                    cmpB = sb1("cmpB", [128, NT, 32], F32)
                    te_f = sb1("te_f", [128, NT], F32)
                    idx_f = sb1("idx_f", [128, NB], F32)
                    tmp32 = sb1("tmp32", [128, NB, 32], F32)
                    ybank = Rot(pb[4:7], "pby")
                    for b in range(NB):
                        ht, htk = ht_rot.next()
                        P.dma("sp", lambda ht=ht, b=b: nc.sync.dma_start(out=ht[:], in_=hin[b * 128:(b + 1) * 128, :]), writes=[htk])
                        junk, jk = njunk.next()
                        ss, ssk = nss.next()
                        norm_rstd(P, ht[:], htk, 1024, junk[:], jk, ss[:], ssk)
                        P.op("dve", lambda ht=ht, ss=ss, b=b: nc.vector.scalar_tensor_tensor(
                            out=xnall[:, b, :], in0=ht[:], scalar=ss[:], in1=gBf[:], op0=ALU.mult, op1=ALU.mult),
                            reads=[htk, ssk, "gBf"], writes=[("xnall", b)])
                        for k in range(8):
                            P.op("pe", lambda k=k, b=b: nc.tensor.transpose(out=ptb[:, k, :], in_=xnall[:, b, k * 128:(k + 1) * 128], identity=ident[:]),
                                 reads=[("xnall", b), "ident"], writes=["ptb"])
                        xTb, xTbk = xTb_rot.next()
                        P.op("act", lambda xTb=xTb: nc.scalar.copy(out=xTb[:], in_=ptb[:]), reads=["ptb"], writes=[xTbk])
                        pt, pk = ybank.next()
                        for k in range(8):
                            P.op("pe", lambda pt=pt, k=k, xTb=xTb: nc.tensor.matmul(
                                pt[:, 0:36], lhsT=xTb[:, k, :], rhs=wr[:, k, :], start=(k == 0), stop=(k == 7)),
                                reads=[xTbk, "wr"], writes=[pk])
                        P.op("dve", lambda pt=pt, b=b: nc.vector.tensor_tensor(out=lg[:, b, :], in0=pt[:, 0:36], in1=rb[:], op=ALU.add),
                             reads=[pk, "rb"], writes=["lg"])
                    lc = lg[:, :, 0:4]
                    lf = lg[:, :, 4:36].rearrange("p b (g e) -> p b g e", g=4)

                    def bc(ap, shape):
                        return ap.to_broadcast(shape)

                    def dv(fn, reads, writes):
                        P.op("dve", fn, reads=reads, writes=writes)
                    dv(lambda: nc.vector.tensor_reduce(out=r_mx[:], in_=lc, axis=AX.X, op=ALU.max), ["lg"], ["r_mx"])
                    dv(lambda: nc.vector.tensor_tensor(out=r_gm[:], in0=lc, in1=bc(r_mx[:].unsqueeze(2), [128, NB, 4]), op=ALU.is_ge), ["lg", "r_mx"], ["r_gm"])
                    dv(lambda: nc.vector.tensor_tensor(out=r_ec[:], in0=lc, in1=bc(r_mx[:].unsqueeze(2), [128, NB, 4]), op=ALU.subtract), ["lg", "r_mx"], ["r_ec"])
                    P.op("act", lambda: nc.scalar.activation(out=r_ec[:], in_=r_ec[:], func=AF.Exp), reads=["r_ec"], writes=["r_ec"])
                    dv(lambda: nc.vector.tensor_reduce(out=r_pg[:], in_=r_ec[:], axis=AX.X, op=ALU.add), ["r_ec"], ["r_pg"])
                    dv(lambda: nc.vector.tensor_tensor(out=r_t[:], in0=lf, in1=bc(r_gm[:].unsqueeze(3), [128, NB, 4, 8]), op=ALU.mult), ["lg", "r_gm"], ["r_t"])
                    dv(lambda: nc.vector.tensor_reduce(out=r_lfs[:], in_=r_t[:].rearrange("p b g e -> p b e g"), axis=AX.X, op=ALU.add), ["r_t"], ["r_lfs"])
                    for b in range(NB):
                        dv(lambda b=b: nc.vector.max(out=r_t8[:, b, :], in_=r_lfs[:, b, :]), ["r_lfs"], ["r_t8"])
                    l1b = bc(r_t8[:, :, 0:1], [128, NB, 8])
                    l2b = bc(r_t8[:, :, 1:2], [128, NB, 8])
                    dv(lambda: nc.vector.tensor_tensor(out=r_sel[:], in0=r_lfs[:], in1=l2b, op=ALU.is_ge), ["r_lfs", "r_t8"], ["r_sel"])
                    dv(lambda: nc.vector.tensor_tensor(out=r_s1[:], in0=r_lfs[:], in1=l1b, op=ALU.is_ge), ["r_lfs", "r_t8"], ["r_s1"])
                    dv(lambda: nc.vector.tensor_tensor(out=r_s2[:], in0=r_sel[:], in1=r_s1[:], op=ALU.subtract), ["r_sel", "r_s1"], ["r_s2"])
                    dv(lambda: nc.vector.tensor_tensor(out=r_ex[:], in0=r_lfs[:], in1=l1b, op=ALU.subtract), ["r_lfs", "r_t8"], ["r_ex"])
                    P.op("act", lambda: nc.scalar.activation(out=r_ex[:], in_=r_ex[:], func=AF.Exp), reads=["r_ex"], writes=["r_ex"])
                    dv(lambda: nc.vector.tensor_tensor(out=r_ex[:], in0=r_ex[:], in1=r_sel[:], op=ALU.mult), ["r_ex", "r_sel"], ["r_ex"])
                    dv(lambda: nc.vector.tensor_reduce(out=r_d[:], in_=r_ex[:], axis=AX.X, op=ALU.add), ["r_ex"], ["r_d"])
                    dv(lambda: nc.vector.tensor_tensor(out=r_d[:], in0=r_d[:], in1=r_pg[:], op=ALU.mult), ["r_d", "r_pg"], ["r_d"])
                    dv(lambda: nc.vector.reciprocal(out=r_d[:], in_=r_d[:]), ["r_d"], ["r_d"])
                    dv(lambda: nc.vector.tensor_tensor(out=r_ex[:], in0=r_ex[:], in1=bc(r_d[:].unsqueeze(2), [128, NB, 8]), op=ALU.mult), ["r_ex", "r_d"], ["r_ex"])
                    dv(lambda: nc.vector.tensor_tensor(out=r_tmp8[:], in0=r_ex[:], in1=r_s1[:], op=ALU.mult), ["r_ex", "r_s1"], ["r_tmp8"])
                    dv(lambda: nc.vector.tensor_reduce(out=gate1[:], in_=r_tmp8[:], axis=AX.X, op=ALU.add), ["r_tmp8"], ["gate1"])
                    dv(lambda: nc.vector.tensor_tensor(out=r_tmp8[:], in0=r_ex[:], in1=r_s2[:], op=ALU.mult), ["r_ex", "r_s2", "gate1"], ["r_tmp8"])
                    dv(lambda: nc.vector.tensor_reduce(out=gate2[:], in_=r_tmp8[:], axis=AX.X, op=ALU.add), ["r_tmp8"], ["gate2"])
                    g4 = bc(r_gm[:].unsqueeze(3), [128, NB, 4, 8])
                    dv(lambda: nc.vector.tensor_tensor(out=M1[:].rearrange("p b (g e) -> p b g e", g=4), in0=g4,
                                                       in1=bc(r_s1[:].unsqueeze(2), [128, NB, 4, 8]), op=ALU.mult), ["r_gm", "r_s1"], ["M1"])
                    dv(lambda: nc.vector.tensor_tensor(out=M2[:].rearrange("p b (g e) -> p b g e", g=4), in0=g4,
                                                       in1=bc(r_s2[:].unsqueeze(2), [128, NB, 4, 8]), op=ALU.mult), ["r_gm", "r_s2"], ["M2"])
                    dv(lambda: nc.vector.tensor_tensor(out=Mb16[:], in0=M1[:], in1=M2[:], op=ALU.add), ["M1", "M2"], ["Mb16"])
                    P.op("pool", lambda: nc.gpsimd.memset(Rm16[:, 0, :], 0.0), writes=[("Rm", 0)])
                    for b in range(NB):
                        dv(lambda b=b: nc.vector.tensor_tensor(out=Rm16[:, b + 1, :], in0=Rm16[:, b, :], in1=Mb16[:, b, :], op=ALU.add),
                           [("Rm", b), "Mb16"], [("Rm", b + 1)])
                    rbank = [pb[0], pb[1]]
                    for b in range(NB):
                        pt = rbank[b // 16]
                        sl = slice((b % 16) * 32, (b % 16) * 32 + 32)
                        P.op("pe", lambda pt=pt, sl=sl, b=b: nc.tensor.matmul(pt[:, sl], lhsT=lstrict[:], rhs=Mb16[:, b, :], start=True, stop=False),
                             reads=["lstrict", "Mb16"], writes=[("pb", b // 16)])
                        P.op("pe", lambda pt=pt, sl=sl, b=b: nc.tensor.matmul(pt[:, sl], lhsT=pones[:], rhs=Rm16[:, b, :], start=False, stop=True),
                             reads=["pones", ("Rm", b)], writes=[("pb", b // 16)])
                    for hf in range(2):
                        dv(lambda hf=hf: nc.vector.tensor_copy(out=rank[:, hf * 16:(hf + 1) * 16, :],
                                                               in_=rbank[hf][:].rearrange("p (b e) -> p b e", b=16)),
                           [("pb", hf)], [("rank", hf)])
                    P.op("pe", lambda: nc.tensor.matmul(pb[2][:, 0:32], lhsT=pones[:], rhs=Rm16[:, NB, :], start=True, stop=True),
                         reads=["pones", ("Rm", NB)], writes=[("pb", 2)])
                    dv(lambda: nc.vector.tensor_copy(out=cnt[:], in_=pb[2][:, 0:32]), [("pb", 2)], ["cnt"])
                    P.op("pool", lambda: nc.gpsimd.iota(thr16[:], pattern=[[TS, 16]], base=0, channel_multiplier=0, allow_small_or_imprecise_dtypes=True), writes=["thr16"])
                    P.op("pool", lambda: nc.gpsimd.iota(thr64[:], pattern=[[TS, NT]], base=0, channel_multiplier=0, allow_small_or_imprecise_dtypes=True), writes=["thr64"])
                    dv(lambda: nc.vector.tensor_tensor(out=cmpA[:], in0=bc(cnt[:].unsqueeze(2), [128, 32, 16]), in1=bc(thr16[:].unsqueeze(1), [128, 32, 16]), op=ALU.is_gt),
                       ["cnt", "thr16"], ["cmpA"])
                    dv(lambda: nc.vector.tensor_reduce(out=ntile[:], in_=cmpA[:], axis=AX.X, op=ALU.add), ["cmpA"], ["ntile"])
                    src, srck = ntile, "ntile"
                    bufs = [(csA, "csA"), (csB, "csB")]
                    for si, sh in enumerate((1, 2, 4, 8, 16)):
                        dst, dstk = bufs[si % 2]
                        dv(lambda src=src, dst=dst, sh=sh: nc.vector.tensor_copy(out=dst[:, 0:sh], in_=src[:, 0:sh]), [srck], [(dstk, 0)])
                        dv(lambda src=src, dst=dst, sh=sh: nc.vector.tensor_tensor(out=dst[:, sh:32], in0=src[:, sh:32], in1=src[:, 0:32 - sh], op=ALU.add),
                           [srck], [(dstk, 1)])
                        src, srck = dst, dstk
                        srck_list = [(dstk, 0), (dstk, 1)]
                        srck = dstk
                        P.op("dve", lambda dst=dst: nc.vector.tensor_copy(out=dst[:, 0:1], in_=dst[:, 0:1]), reads=srck_list, writes=[dstk])
                    dv(lambda src=src: nc.vector.tensor_tensor(out=base[:], in0=src[:], in1=ntile[:], op=ALU.subtract), [srck, "ntile"], ["base"])
                    dv(lambda: nc.vector.tensor_scalar(out=base[:], in0=base[:], scalar1=float(TS), scalar2=None, op0=ALU.mult), ["base"], ["base"])
                    dv(lambda: nc.vector.tensor_tensor(out=cmpB[:], in0=bc(base[:].unsqueeze(1), [128, NT, 32]), in1=bc(thr64[:].unsqueeze(2), [128, NT, 32]), op=ALU.is_le),
                       ["base", "thr64"], ["cmpB"])
                    dv(lambda: nc.vector.tensor_reduce(out=te_f[:], in_=cmpB[:], axis=AX.X, op=ALU.add), ["cmpB"], ["te_f"])
                    dv(lambda: nc.vector.tensor_scalar(out=te_f[:], in0=te_f[:], scalar1=-1.0, scalar2=128.0, op0=ALU.add, op1=ALU.mult), ["te_f"], ["te_f"])
                    dv(lambda: nc.vector.tensor_scalar(out=te_f[:], in0=te_f[:], scalar1=pio[:, 0:1], scalar2=None, op0=ALU.add), ["te_f", "pio"], ["te_f"])
                    dv(lambda: nc.vector.tensor_copy(out=widx_i[:], in_=te_f[:]), ["te_f"], ["widx_i"])
                    dv(lambda: nc.vector.tensor_tensor(out=rank[:], in0=rank[:], in1=bc(base[:].unsqueeze(1), [128, NB, 32]), op=ALU.add),
                       [("rank", 0), ("rank", 1), "base"], ["rankp"])
                    for (Mk, Mkk, idxi, idxk) in ((M1, "M1", idx1_i, "idx1_i"), (M2, "M2", idx2_i, "idx2_i")):
                        dv(lambda Mk=Mk: nc.vector.tensor_tensor(out=tmp32[:], in0=rank[:], in1=Mk[:], op=ALU.mult), ["rankp", Mkk, "idx_f"], ["tmp32"])
                        dv(lambda: nc.vector.tensor_reduce(out=idx_f[:], in_=tmp32[:], axis=AX.X, op=ALU.add), ["tmp32"], ["idx_f"])
                        dv(lambda idxi=idxi: nc.vector.tensor_copy(out=idxi[:], in_=idx_f[:]), ["idx_f"], [idxk])
                    for b in range(NB):
                        for (idxi, idxk) in ((idx1_i, "idx1_i"), (idx2_i, "idx2_i")):
                            P.dma("pool", lambda b=b, idxi=idxi: nc.gpsimd.indirect_dma_start(
                                out=XS[:, :], out_offset=bass.IndirectOffsetOnAxis(ap=idxi[:, b:b + 1], axis=0),
                                in_=xnall[:, b, :], in_offset=None), reads=[("xnall", b), idxk], writes=["XS"])
                    P.flush()
                with ExitStack() as st2_:
                    def sb2(name, shape, dt):
                        return st2_.enter_context(nc.sbuf_tensor(_un(name), shape, dt))
                    w13_rot = Rot([sb2(f"w13_{i}", [128, 2, 8, 256], BF16) for i in range(3)], "w13")
                    w2_rot = Rot([sb2(f"w2_{i}", [128, 2, 1024], BF16) for i in range(3)], "w2")
                    xs_rot = Rot([sb2(f"xs{i}", [128, 2, 1024], BF16) for i in range(2)], "xs")
                    xT_rot = Rot([sb2(f"xTt{i}", [128, 8, TS], BF16) for i in range(2)], "xTt")
                    s_rot = Rot([sb2(f"s{i}", [128, TS], F32) for i in range(2)], "s")
                    he_rot = Rot([sb2(f"he{i}", [128, 2, TS], BF16) for i in range(2)], "he")
                    yt_rot = Rot([sb2(f"yt{i}", [128, 1024], F32) for i in range(3)], "yt")
                    hbank = Rot(pb[0:4], "pb")
                    ybank = Rot(pb[4:7], "pby")
                    wts = {}
                    tst = {}

                    def load_w(i):
                        w13, w13k = w13_rot.next()
                        w2, w2k = w2_rot.next()
                        ix = bass.IndirectOffsetOnAxis(ap=widx_i[:, i:i + 1], axis=0)
                        P.dma("pool", lambda: nc.gpsimd.indirect_dma_start(
                            out=w13[:, 0, :, :].rearrange("p k f -> p (k f)"), out_offset=None,
                            in_=moe_w1.rearrange("l e (p k) f -> (l e p) (k f)", k=8), in_offset=ix),
                            reads=["widx_i"], writes=[(w13k, 0)])
                        P.dma("pool", lambda: nc.gpsimd.indirect_dma_start(
                            out=w13[:, 1, :, :].rearrange("p k f -> p (k f)"), out_offset=None,
                            in_=moe_w3.rearrange("l e (p k) f -> (l e p) (k f)", k=8), in_offset=ix),
                            reads=["widx_i"], writes=[(w13k, 1)])
                        P.dma("pool", lambda: nc.gpsimd.indirect_dma_start(
                            out=w2[:].rearrange("p c d -> p (c d)"), out_offset=None,
                            in_=moe_w2.rearrange("l e (p c) d -> (l e p) (c d)", c=2), in_offset=ix),
                            reads=["widx_i"], writes=[w2k])
                        wts[i] = (w13, w13k, w2, w2k)

                    def stage_t(i):
                        xs, xsk = xs_rot.next()
                        P.dma("sp", lambda: nc.sync.dma_start(out=xs[:], in_=XS[i * TS:(i + 1) * TS, :].rearrange("(a p) d -> p a d", p=128)),
                              writes=[xsk])
                        xT, xTk = xT_rot.next()
                        for a in range(2):
                            for k in range(8):
                                P.op("pe", lambda a=a, k=k: nc.tensor.transpose(out=ptb[:, k, :], in_=xs[:, a, k:1024:8], identity=ident[:]),
                                     reads=[xsk, "ident"], writes=["ptb"])
                            if a == 0:
                                P.op("act", lambda a=a: nc.scalar.copy(out=xT[:, :, a * 128:(a + 1) * 128], in_=ptb[:]), reads=["ptb"], writes=[(xTk, a)])
                            else:
                                P.op("dve", lambda a=a: nc.vector.tensor_copy(out=xT[:, :, a * 128:(a + 1) * 128], in_=ptb[:]), reads=["ptb"], writes=[(xTk, a)])
                        w13, w13k, w2, w2k = wts[i]
                        he, hek = he_rot.next()
                        tst[i] = (he, hek)
                        for fch in range(2):
                            p1, p1k = hbank.next()
                            p3, p3k = hbank.next()
                            for j, (pt, pk) in enumerate(((p1, p1k), (p3, p3k))):
                                for k in range(8):
                                    P.op("pe", lambda pt=pt, j=j, k=k, fch=fch: nc.tensor.matmul(
                                        pt[:, 0:TS], lhsT=w13[:, j, k, fch:256:2], rhs=xT[:, k, :],
                                        start=(k == 0), stop=(k == 7)),
                                        reads=[(w13k, j), (xTk, 0), (xTk, 1)], writes=[pk])
                            s, sk = s_rot.next()
                            P.op("act", lambda s=s, p1=p1: nc.scalar.activation(out=s[:], in_=p1[:, 0:TS], func=AF.Silu), reads=[p1k], writes=[sk])
                            P.op("dve", lambda s=s, p3=p3, fch=fch: nc.vector.tensor_tensor(out=he[:, fch, :], in0=p3[:, 0:TS], in1=s[:], op=ALU.mult),
                                 reads=[p3k, sk], writes=[(hek, fch)])

                    def stage_y(i):
                        w13, w13k, w2, w2k = wts.pop(i)
                        he, hek = tst.pop(i)
                        for a in range(2):
                            yt, ytk = yt_rot.next()
                            for half in range(2):
                                py, pyk = ybank.next()
                                for fch in range(2):
                                    P.op("pe", lambda py=py, fch=fch, a=a, half=half: nc.tensor.matmul(
                                        py[:], lhsT=he[:, fch, a * 128:(a + 1) * 128], rhs=w2[:, fch, half * 512:(half + 1) * 512],
                                        start=(fch == 0), stop=(fch == 1)), reads=[(hek, fch), w2k], writes=[pyk])
                                if half == 0:
                                    P.op("act", lambda py=py, yt=yt, half=half: nc.scalar.copy(out=yt[:, half * 512:(half + 1) * 512], in_=py[:]),
                                         reads=[pyk], writes=[(ytk, half)])
                                else:
                                    P.op("dve", lambda py=py, yt=yt, half=half: nc.vector.tensor_copy(out=yt[:, half * 512:(half + 1) * 512], in_=py[:]),
                                         reads=[pyk], writes=[(ytk, half)])
                            r0 = i * TS + a * 128
                            P.dma("sp", lambda yt=yt, r0=r0: nc.sync.dma_start(out=YS[r0:r0 + 128, :], in_=yt[:]),
                                  reads=[(ytk, 0), (ytk, 1)], writes=[("YS", i, a)])
                        if i + 3 < NT:
                            load_w(i + 3)

                    load_w(0)
                    load_w(1)
                    load_w(2)
                    stage_t(0)
                    for i in range(NT):
                        if i + 1 < NT:
                            stage_t(i + 1)
                        stage_y(i)
                    P.flush()
                with ExitStack() as st3_:
                    def sb3(name, shape, dt):
                        return st3_.enter_context(nc.sbuf_tensor(_un(name), shape, dt))
                    ht_rot = Rot([sb3(f"htc{i}", [128, 1024], F32) for i in range(3)], "htc")
                    y1_rot = Rot([sb3(f"y1_{i}", [128, 1024], F32) for i in range(2)], "y1")
                    y2_rot = Rot([sb3(f"y2_{i}", [128, 1024], F32) for i in range(2)], "y2")
                    pblk_rot = Rot([sb3(f"pblk{i}", [128, 256], F32) for i in range(2)], "pblk")
                    pb16_rot = Rot([sb3(f"pb16{i}", [128, 256], BF16) for i in range(2)], "pb16")
                    pT_rot = Rot([sb3(f"pT{i}", [128, 2, 128], BF16) for i in range(2)], "pT")
                    hnT_rot = Rot([sb3(f"hnT{i}", [128, 8, 128], BF16) for i in range(2)], "hnT")
                    sg_rot = Rot([sb3(f"sg{i}", [128, 1024], F32) for i in range(2)], "sg")
                    gbank = Rot(pb[0:4], "pb")
                    for b in range(NB):
                        ht, htk = ht_rot.next()
                        y1, y1k = y1_rot.next()
                        y2, y2k = y2_rot.next()
                        P.dma("sp", lambda ht=ht, b=b: nc.sync.dma_start(out=ht[:], in_=hin[b * 128:(b + 1) * 128, :]), writes=[htk])
                        P.dma("pool", lambda y1=y1, b=b: nc.gpsimd.indirect_dma_start(
                            out=y1[:, :], out_offset=None, in_=YS[:, :], in_offset=bass.IndirectOffsetOnAxis(ap=idx1_i[:, b:b + 1], axis=0)),
                            reads=["idx1_i"], writes=[y1k])
                        P.dma("pool", lambda y2=y2, b=b: nc.gpsimd.indirect_dma_start(
                            out=y2[:, :], out_offset=None, in_=YS[:, :], in_offset=bass.IndirectOffsetOnAxis(ap=idx2_i[:, b:b + 1], axis=0)),
                            reads=["idx2_i"], writes=[y2k])
                        P.op("dve", lambda ht=ht, y1=y1, b=b: nc.vector.scalar_tensor_tensor(
                            out=ht[:], in0=y1[:], scalar=gate1[:, b:b + 1], in1=ht[:], op0=ALU.mult, op1=ALU.add),
                            reads=[htk, y1k, "gate1"], writes=[htk])
                        P.op("dve", lambda ht=ht, y2=y2, b=b: nc.vector.scalar_tensor_tensor(
                            out=ht[:], in0=y2[:], scalar=gate2[:, b:b + 1], in1=ht[:], op0=ALU.mult, op1=ALU.add),
                            reads=[htk, y2k, "gate2"], writes=[htk])
                        hnT, hnk = hnT_rot.next()
                        norm_T(P, ht[:], htk, gBp[:], "gBp", hnT[:], hnk)
                        pblk, pblkk = pblk_rot.next()
                        p16, p16k = pb16_rot.next()
                        pT, pTk = pT_rot.next()
                        P.dma("sp", lambda pblk=pblk, b=b: nc.sync.dma_start(out=pblk[:], in_=p[l, b * 128:(b + 1) * 128, :]), writes=[pblkk])
                        P.op("pool", lambda p16=p16, pblk=pblk: nc.gpsimd.tensor_copy(out=p16[:], in_=pblk[:]), reads=[pblkk], writes=[p16k])
                        for k in range(2):
                            P.op("pe", lambda p16=p16, k=k: nc.tensor.transpose(out=ptb[:, k, :], in_=p16[:, k * 128:(k + 1) * 128], identity=ident[:]),
                                 reads=[p16k, "ident"], writes=["ptb"])
                        P.op("act", lambda pT=pT: nc.scalar.copy(out=pT[:], in_=ptb[:, 0:2, :]), reads=["ptb"], writes=[pTk])
                        sg, sgk = sg_rot.next()
                        for half in range(2):
                            pg_, pgk = gbank.next()
                            pe_, pek = gbank.next()
                            for k in range(8):
                                P.op("pe", lambda pg_=pg_, hnT=hnT, k=k, half=half: nc.tensor.matmul(
                                    pg_[:], lhsT=hnT[:, k, :], rhs=wpg[:, k, half * 512:(half + 1) * 512], start=(k == 0), stop=(k == 7)),
                                    reads=[hnk, "wpg"], writes=[pgk])
                            for k in range(2):
                                P.op("pe", lambda pe_=pe_, pT=pT, k=k, half=half: nc.tensor.matmul(
                                    pe_[:], lhsT=pT[:, k, :], rhs=wpe[:, k, half * 512:(half + 1) * 512], start=(k == 0), stop=(k == 1)),
                                    reads=[pTk, "wpe"], writes=[pek])
                            P.op("act", lambda sg=sg, pg_=pg_, half=half: nc.scalar.activation(out=sg[:, half * 512:(half + 1) * 512], in_=pg_[:], func=AF.Sigmoid),
                                 reads=[pgk], writes=[(sgk, half)])
                            P.op("dve", lambda sg=sg, pe_=pe_, half=half: nc.vector.tensor_tensor(
                                out=sg[:, half * 512:(half + 1) * 512], in0=pe_[:], in1=sg[:, half * 512:(half + 1) * 512], op=ALU.mult),
                                reads=[pek, (sgk, half)], writes=[(sgk, half)])
                        P.op("pool", lambda sg=sg, ht=ht: nc.gpsimd.tensor_tensor(out=ht[:], in0=ht[:], in1=sg[:], op=ALU.add),
                             reads=[(sgk, 0), (sgk, 1), htk], writes=[htk])
                        if final:
                            junk, jk = njunk.next()
                            ss, ssk = nss.next()
                            norm_rstd(P, ht[:], htk, 1024, junk[:], jk, ss[:], ssk)
                            P.op("dve", lambda ss=ss, ht=ht: nc.vector.scalar_tensor_tensor(
                                out=ht[:], in0=ht[:], scalar=ss[:], in1=gBo[:], op0=ALU.mult, op1=ALU.mult),
                                reads=[htk, ssk, "gBo"], writes=[htk])
                        P.dma("sp", lambda ht=ht, b=b: nc.sync.dma_start(out=hout[b * 128:(b + 1) * 128, :], in_=ht[:]),
                              reads=[htk], writes=[("hout", b)])
                    P.flush()

        SPARSE = True
        mphase = sparse_moe_pl_phase if SPARSE else moe_pl_phase
        if stop_after >= 5:
            mphase(0, hA, hB, False)

        if stop_after >= 6:
            with ExitStack() as st:
                def sb(name, shape, dt):
                    return st.enter_context(nc.sbuf_tensor(_un(name), shape, dt))
                wi = sb("wi", [128, 8, 4096], BF16)
                wi_src = w_in_odd[0].rearrange("(k p) n -> p k n", p=128)
                for k in range(8):
                    P.dma("pool", lambda k=k: nc.gpsimd.dma_start(out=wi[:, k, :], in_=wi_src[:, k, :]), writes=[("wi", k)])
                wi_keys = [("wi", k) for k in range(8)]
                wo1 = sb("wo1", [128, 16, 1024], BF16)
                wo_src = w_out_odd[0].rearrange("(c p) n -> p c n", p=128)
                for c in range(0, 16, 4):
                    P.dma("pool", lambda c=c: nc.gpsimd.dma_start(out=wo1[:, c:c + 4, :], in_=wo_src[:, c:c + 4, :]), writes=[("wo1", c)])
                wo_keys = [("wo1", c) for c in range(0, 16, 4)]
                gB1 = sb("gB1", [128, 1024], F32)
                load_gB(P, gB1[:], "gB1", norm_mix[1])
                gvB = sb("gvB", [128, 2048], F32)
                P.dma("sp", lambda: nc.sync.dma_start(out=gvB[:], in_=g_v_odd[0].partition_broadcast(128)), writes=["gvB"])
                bsf = sb("bsf", [1, 8, 128], F32)
                bs16 = sb("bs16", [1, 8, 128], BF16)
                P.dma("sp", lambda: nc.sync.dma_start(out=bsf[:], in_=b_s_odd[0:1]), writes=["bsf"])
                P.op("dve", lambda: nc.vector.tensor_copy(out=bs16[:], in_=bsf[:]), reads=["bsf"], writes=["bs16"])
                wsT = sb("wsT", [128, 8, 128], BF16)
                st3 = ExitStack()
                st3.__enter__()
                wsf = st3.enter_context(nc.sbuf_tensor(_un("wsf"), [128, 8, 128], F32))
                ws16 = st3.enter_context(nc.sbuf_tensor(_un("ws16"), [128, 8, 128], BF16))
                P.dma("sp", lambda: nc.sync.dma_start(out=wsf[:], in_=w_s_odd[0].rearrange("g t s -> t g s")), writes=["wsf"])
                for g in range(8):
                    P.op("pool", lambda g=g: nc.gpsimd.affine_select(out=wsf[:, g, :], in_=wsf[:, g, :], pattern=[[-1, 128]],
                                                                      compare_op=ALU.is_ge, fill=0.0, base=0, channel_multiplier=1),
                         reads=["wsf"], writes=["wsf"])
                P.op("dve", lambda: nc.vector.tensor_copy(out=ws16[:], in_=wsf[:]), reads=["wsf"], writes=["ws16"])
                for g in range(8):
                    P.op("pe", lambda g=g: nc.tensor.transpose(out=ptb[:, g, :], in_=ws16[:, g, :], identity=ident[:]),
                         reads=["ws16", "ident"], writes=["ptb"])
                P.op("dve", lambda: nc.vector.tensor_copy(out=wsT[:], in_=ptb[:]), reads=["ptb"], writes=["wsT"])
                P.flush()
                st3.__exit__(None, None, None)
                ht_rot = Rot([sb(f"ht{i}", [128, 1024], F32) for i in range(5)], "ht")
                xg_rot = Rot([sb(f"xg{i}", [128, 8, 512], BF16) for i in range(2)], "xg")
                uT_rot = Rot([sb(f"uT{i}", [128, 16, 512], BF16) for i in range(1)], "uT")
                vt_rot = Rot([sb(f"vt{i}", [128, 2048], F32) for i in range(1)], "vt")
                vn_rot = Rot([sb(f"vn{i}", [128, 2048], BF16) for i in range(1)], "vn")
                yT_rot = Rot([sb(f"yT{i}", [128, 16, 128], BF16) for i in range(2)], "yT")
                abank = Rot(pb[0:3], "pb")
                gbank = Rot(pb[3:5], "pbg")
                obank = Rot(pb[5:7], "pbo")
                for tc in range(8):
                    xg, xgk = xg_rot.next()
                    hts = []
                    for tb in range(4):
                        b = tc * 4 + tb
                        ht, htk = ht_rot.next()
                        hts.append((ht, htk))
                        P.dma("sp", lambda ht=ht, b=b: nc.sync.dma_start(out=ht[:], in_=hB[b * 128:(b + 1) * 128, :]),
                              reads=[("hout", b)], writes=[htk])
                        norm_T(P, ht[:], htk, gB1[:], "gB1", xg[:, :, tb * 128:(tb + 1) * 128], (xgk, tb))
                    xgkeys = [(xgk, tb) for tb in range(4)]
                    uT, uTk = uT_rot.next()
                    for fcu in range(16):
                        pt, pk = abank.next()
                        for k in range(8):
                            P.op("pe", lambda pt=pt, k=k, fcu=fcu, xg=xg: nc.tensor.matmul(
                                pt[:], lhsT=wi[:, k, fcu * 128:(fcu + 1) * 128], rhs=xg[:, k, :], start=(k == 0), stop=(k == 7)),
                                reads=[("wi", k)] + xgkeys, writes=[pk])
                        P.op("act", lambda pt=pt, uT=uT, fcu=fcu: nc.scalar.activation(out=uT[:, fcu, :], in_=pt[:], func=AF.Gelu_apprx_tanh),
                             reads=[pk], writes=[(uTk, fcu)])
                    for tb in range(4):
                        b = tc * 4 + tb
                        ht, htk = hts[tb]
                        vt, vtk = vt_rot.next()
                        for vg in range(4):
                            pt, pk = abank.next()
                            for k in range(8):
                                P.op("pe", lambda pt=pt, k=k, vg=vg, xg=xg, tb=tb: nc.tensor.matmul(
                                    pt[:], lhsT=xg[:, k, tb * 128:(tb + 1) * 128], rhs=wi[:, k, 2048 + vg * 512: 2048 + (vg + 1) * 512],
                                    start=(k == 0), stop=(k == 7)), reads=[("wi", k), (xgk, tb)], writes=[pk])
                            P.op("act", lambda pt=pt, vt=vt, vg=vg: nc.scalar.activation(out=vt[:, vg * 512:(vg + 1) * 512], in_=pt[:], func=AF.Gelu_apprx_tanh),
                                 reads=[pk], writes=[(vtk, vg)])
                        ss, ssk = nss.next()
                        vtkeys = [(vtk, vg) for vg in range(4)]
                        vn, vnk = vn_rot.next()
                        P.op("act", lambda vt=vt, ss=ss, vn=vn: nc.scalar.activation(out=vn[:], in_=vt[:], func=AF.Square, accum_out=ss[:]),
                             reads=vtkeys, writes=[vnk, ssk])
                        P.op("act", lambda ss=ss: nc.scalar.activation(out=ss[:], in_=ss[:], func=AF.Ln, scale=1.0 / 2048, bias=epsb[:]),
                             reads=[ssk, "epsb"], writes=[ssk])
                        P.op("act", lambda ss=ss: nc.scalar.activation(out=ss[:], in_=ss[:], func=AF.Exp, scale=-0.5), reads=[ssk], writes=[ssk])
                        P.op("dve", lambda vn=vn, vt=vt, ss=ss: nc.vector.scalar_tensor_tensor(out=vn[:], in0=vt[:], scalar=ss[:], in1=gvB[:],
                                                                                              op0=ALU.mult, op1=ALU.mult),
                             reads=vtkeys + [ssk, "gvB"], writes=[vnk])
                        yT, yTk = yT_rot.next()
                        for q4 in range(4):
                            pg_, pgk = gbank.next()
                            for i4 in range(4):
                                fcu = q4 * 4 + i4
                                g = fcu // 2
                                P.op("pe", lambda pg_=pg_, i4=i4, fcu=fcu, g=g, vn=vn: nc.tensor.matmul(
                                    pg_[:, i4 * 128:(i4 + 1) * 128], lhsT=vn[:, fcu * 128:(fcu + 1) * 128], rhs=wsT[:, g, :], start=True, stop=False),
                                    reads=[vnk, "wsT"], writes=[pgk])
                                P.op("pe", lambda pg_=pg_, i4=i4, g=g: nc.tensor.matmul(
                                    pg_[:, i4 * 128:(i4 + 1) * 128], lhsT=ones1[0:1, :], rhs=bs16[0:1, g, :], start=False, stop=True),
                                    reads=["ones1", "bs16"], writes=[pgk])
                            P.op("dve", lambda pg_=pg_, yT=yT, uT=uT, q4=q4, tb=tb: nc.vector.tensor_tensor(
                                out=yT[:, q4 * 4:(q4 + 1) * 4, :], in0=pg_[:].rearrange("p (i t) -> p i t", i=4),
                                in1=uT[:, q4 * 4:(q4 + 1) * 4, tb * 128:(tb + 1) * 128], op=ALU.mult),
                                reads=[pgk] + [(uTk, q4 * 4 + i) for i in range(4)], writes=[(yTk, q4)])
                        for half in range(2):
                            po_, pok = obank.next()
                            for fcu in range(16):
                                P.op("pe", lambda po_=po_, yT=yT, fcu=fcu, half=half: nc.tensor.matmul(
                                    po_[:], lhsT=yT[:, fcu, :], rhs=wo1[:, fcu, half * 512:(half + 1) * 512], start=(fcu == 0), stop=(fcu == 15)),
                                    reads=[(yTk, fcu // 4), ("wo1", (fcu // 4) * 4)], writes=[pok])
                            P.op("dve", lambda po_=po_, ht=ht, half=half: nc.vector.tensor_tensor(
                                out=ht[:, half * 512:(half + 1) * 512], in0=po_[:], in1=ht[:, half * 512:(half + 1) * 512], op=ALU.add),
                                reads=[pok, htk], writes=[htk])
                        P.dma("sp", lambda ht=ht, b=b: nc.sync.dma_start(out=hA[b * 128:(b + 1) * 128, :], in_=ht[:]),
                              reads=[htk], writes=[("hA", b)])
                P.flush()

        if stop_after >= 7:
            mphase(1, hA, out, True)
        P.flush()
        nc._prog_stats = dict(nops=P.nops, ccount=dict(P.ccount), dcount=dict(P.dcount))
    return nc


_NC_CACHE = {}


def kernel(**inputs):
    n = 8
    if "nc" not in _NC_CACHE:
        _NC_CACHE["nc"] = build()
    nc = _NC_CACHE["nc"]
    in_maps = []
    for c in range(n):
        m = {}
        for k, v in inputs.items():
            v = np.asarray(v)
            if k == "x":
                m[k] = np.ascontiguousarray(v[c])
            elif k == "p":
                m[k] = np.ascontiguousarray(v[:, c])
            else:
                m[k] = np.ascontiguousarray(v)
        in_maps.append(m)
    res = run_bass_kernel_spmd(nc, in_maps, core_ids=list(range(n)))
    return np.stack([np.asarray(r["out"]) for r in res.results], axis=0).astype(np.float32)
```

```python
import numpy as np
from contextlib import ExitStack
import concourse.bass as bass
import concourse.mybir as mybir
from concourse.bass_utils import run_bass_kernel_spmd

F32 = mybir.dt.float32
BF16 = mybir.dt.bfloat16
AF = mybir.ActivationFunctionType
ALU = mybir.AluOpType
AX = mybir.AxisListType
I32 = mybir.dt.int32

EPOCH = 30000
NDMASEM = 12
S = 4096
D = 1024
NB = S // 128
EPS = 1e-6


class Prog:
    ENG = ("pe", "act", "dve", "pool", "sp")

    def __init__(self, nc, stack):
        self.nc = nc
        self.stack = stack
        self.ops = []
        self.ccount = {e: 0 for e in self.ENG}
        self.dcount = {e: 0 for e in self.ENG}
        self.csem = {e: [] for e in self.ENG}
        self.dsem = {e: [] for e in self.ENG}
        self.dma_hist = {e: [] for e in self.ENG}
        self.waited = {e: {} for e in self.ENG}
        self.nops = 0

    def eng_obj(self, e):
        nc = self.nc
        return {"pe": nc.tensor, "act": nc.scalar, "dve": nc.vector,
                "pool": nc.gpsimd, "sp": nc.sync}[e]

    def op(self, eng, fn, reads=(), writes=(), nosig=False):
        self.ops.append(dict(eng=eng, fn=fn, reads=tuple(reads), writes=tuple(writes), dma=False, nosig=nosig))

    def dma(self, eng, fn, reads=(), writes=()):
        self.ops.append(dict(eng=eng, fn=fn, reads=tuple(reads), writes=tuple(writes), dma=True))

    def _csem(self, e, ep):
        while len(self.csem[e]) <= ep:
            self.csem[e].append(self.stack.enter_context(self.nc.semaphore(f"c_{e}_{len(self.csem[e])}")))
        return self.csem[e][ep]

    def _dsem(self, e, k):
        while len(self.dsem[e]) <= k:
            self.dsem[e].append(self.stack.enter_context(self.nc.semaphore(f"d_{e}_{len(self.dsem[e])}")))
        return self.dsem[e][k]

    def _wait(self, e, key, sem, val):
        w = self.waited[e]
        if key[0] == "c":
            for kk, v in w.items():
                if kk[0] == "c" and kk[1] == key[1] and (kk[2] > key[2] or (kk[2] == key[2] and v >= val)):
                    return
        elif w.get(key, 0) >= val:
            return
        self.eng_obj(e).wait_ge(sem, val)
        w[key] = max(w.get(key, 0), val)

    def flush(self):
        ops = self.ops
        n = len(ops)
        self.nops += n
        lw, rdc, rdd = {}, {}, {}
        deps = [None] * n
        dlist = {e: [] for e in self.ENG}
        for i, o in enumerate(ops):
            d = set()
            for k in o["reads"]:
                w = lw.get(k)
                if w is not None:
                    d.add(w)
            for k in o["writes"]:
                w = lw.get(k)
                if w is not None:
                    d.add(w)
                d.update(rdc.get(k, {}).values())
                d.update(rdd.get(k, ()))
            for k in o["reads"]:
                if o["dma"]:
                    rdd.setdefault(k, []).append(i)
                else:
                    rdc.setdefault(k, {})[o["eng"]] = i
            for k in o["writes"]:
                lw[k] = i
                rdc[k] = {}
                rdd[k] = []
            if o["dma"]:
                q = o["eng"]
                o["dn"] = self.dcount[q]
                self.dcount[q] += 1
                lst = dlist[q]
                if len(lst) >= NDMASEM:
                    d.add(lst[len(lst) - NDMASEM])
                lst.append(i)
            d.discard(i)
            if o["eng"] == "pe" and not o["dma"]:
                d = {j for j in d if not (ops[j]["eng"] == "pe" and not ops[j]["dma"])}
            deps[i] = d
        signaling = [False] * n
        for i in range(n):
            for j in deps[i]:
                signaling[j] = True
        last = {}
        for i, o in enumerate(ops):
            if not o["dma"] and not o.get("nosig"):
                last[o["eng"]] = i
        for e, i in last.items():
            signaling[i] = True
        for i, o in enumerate(ops):
            if not o["dma"] and signaling[i]:
                c = self.ccount[o["eng"]]
                o["sig"] = (c // EPOCH, c % EPOCH + 1)
                self.ccount[o["eng"]] = c + 1
        for i, o in enumerate(ops):
            e = o["eng"]
            need = {}
            for j in deps[i]:
                p = ops[j]
                if p["dma"]:
                    key = ("d", p["eng"], p["dn"] % NDMASEM)
                    val = 16 * (p["dn"] // NDMASEM + 1)
                    sem = self._dsem(p["eng"], p["dn"] % NDMASEM)
                else:
                    ep, cv = p["sig"]
                    key = ("c", p["eng"], ep)
                    val = cv
                    sem = self._csem(p["eng"], ep)
                if need.get(key, (None, 0))[1] < val:
                    need[key] = (sem, val)
            for key, (sem, val) in need.items():
                self._wait(e, key, sem, val)
            ins = o["fn"]()
            if o["dma"]:
                ins.then_inc(self._dsem(e, o["dn"] % NDMASEM), 16)
            elif signaling[i]:
                ep, cv = o["sig"]
                ins.then_inc(self._csem(e, ep), 1)
        for e in self.ENG:
            for e2, i in last.items():
                ep, cv = ops[i]["sig"]
                self._wait(e, ("c", e2, ep), self._csem(e2, ep), cv)
            for q in self.ENG:
                dc = self.dcount[q]
                for k in range(min(NDMASEM, dc)):
                    lastdn = ((dc - 1 - k) // NDMASEM) * NDMASEM + k
                    self._wait(e, ("d", q, k), self._dsem(q, k), 16 * (lastdn // NDMASEM + 1))
        self.ops = []


_UN = [0]


def _un(name):
    _UN[0] += 1
    return f"{name}_{_UN[0]}"


def _kl(k):
    return list(k) if isinstance(k, list) else [k]


def hk(blk):
    return [("hacc", blk, 0), ("hacc", blk, 1)]


class Rot:
    def __init__(self, tiles, name):
        self.tiles = tiles
        self.name = name
        self.i = 0

    def next(self):
        j = self.i % len(self.tiles)
        self.i += 1
        return self.tiles[j], (self.name, j)


def build(stop_after=99, dbg=False, sub=9):
    nc = bass.Bass("TRN2", target_bir_lowering=False)

    def din(name, shape):
        return nc.dram_tensor(name, list(shape), F32, kind="ExternalInput").ap()

    x = din("x", [S, D])
    p = din("p", [2, S, 256])
    norm_mix = din("norm_mix", [2, D])
    norm_ffn = din("norm_ffn", [2, D])
    norm_pl = din("norm_pl", [2, D])
    final_norm = din("final_norm", [D])
    w_in_even = din("w_in_even", [1, D, 3072])
    conv_w_even = din("conv_w_even", [1, 3, 512])
    w_out_even = din("w_out_even", [1, 1024, D])
    w_in_odd = din("w_in_odd", [1, D, 4096])
    g_v_odd = din("g_v_odd", [1, 2048])
    w_s_odd = din("w_s_odd", [1, 8, 128, 128])
    b_s_odd = din("b_s_odd", [1, 8, 128])
    w_out_odd = din("w_out_odd", [1, 2048, D])
    router_c = din("router_c", [2, D, 4])
    router_c_b = din("router_c_b", [2, 4])
    router_f = din("router_f", [2, D, 32])
    router_f_b = din("router_f_b", [2, 32])
    moe_w1 = din("moe_w1", [2, 32, D, 256])
    moe_w3 = din("moe_w3", [2, 32, D, 256])
    moe_w2 = din("moe_w2", [2, 32, 256, D])
    w_pe = din("w_pe", [2, 256, D])
    w_pg = din("w_pg", [2, D, D])
    out = nc.dram_tensor("out", [S, D], F32, kind="ExternalOutput").ap()
    mixT = nc.dram_tensor("mixT", [8, 128, S], BF16, kind="ExternalOutput" if dbg else "Internal").ap()
    XS = nc.dram_tensor("XS", [24576, 1024], BF16, kind="Internal").ap()
    YS = nc.dram_tensor("YS", [24576, 1024], F32, kind="Internal").ap()
    hA = nc.dram_tensor("hA", [S, D], F32, kind="ExternalOutput" if dbg else "Internal").ap()
    hB = nc.dram_tensor("hB", [S, D], F32, kind="ExternalOutput" if dbg else "Internal").ap()

    with ExitStack() as gst:
        P = Prog(nc, gst)

        def gsb(name, shape, dt):
            return gst.enter_context(nc.sbuf_tensor(_un(name), shape, dt))

        pb = [gst.enter_context(nc.psum_tensor(f"pb{i}", [128, 512], F32)) for i in range(7)]
        ptb = gst.enter_context(nc.psum_tensor("ptb", [128, 8, 128], BF16))

        identf = gsb("identf", [128, 128], F32)
        ident = gsb("ident", [128, 128], BF16)
        ntri = gsb("ntri", [128, 128], BF16)
        nones = gsb("nones", [128, 128], BF16)
        zeros = gsb("zeros", [128, 128], BF16)
        nmask = gsb("nmask", [128, 128], BF16)
        ones1 = gsb("ones1", [1, 128], BF16)
        pones = gsb("pones", [128, 128], BF16)
        lstrict = gsb("lstrict", [128, 128], BF16)
        lsf = gsb("lsf", [128, 128], F32)
        tmpf = gsb("tmpf", [128, 128], F32)
        P.op("pool", lambda: nc.gpsimd.memset(identf[:], 1.0), writes=["identf"])
        P.op("pool", lambda: nc.gpsimd.affine_select(out=identf[:], in_=identf[:], pattern=[[-1, 128]],
                                                      compare_op=ALU.is_equal, fill=0.0, base=0, channel_multiplier=1),
             reads=["identf"], writes=["identf"])
        P.op("dve", lambda: nc.vector.tensor_copy(out=ident[:], in_=identf[:]), reads=["identf"], writes=["ident"])
        P.op("pool", lambda: nc.gpsimd.memset(tmpf[:], -1.0), writes=["tmpf"])
        P.op("dve", lambda: nc.vector.tensor_copy(out=nones[:], in_=tmpf[:]), reads=["tmpf"], writes=["nones"])
        P.op("pool", lambda: nc.gpsimd.affine_select(out=tmpf[:], in_=tmpf[:], pattern=[[-1, 128]],
                                                      compare_op=ALU.is_ge, fill=0.0, base=0, channel_multiplier=1),
             reads=["tmpf", "nones"], writes=["tmpf"])
        P.op("dve", lambda: nc.vector.tensor_copy(out=ntri[:], in_=tmpf[:]), reads=["tmpf"], writes=["ntri"])
        P.op("dve", lambda: nc.vector.tensor_scalar(out=nmask[:], in0=tmpf[:], scalar1=30000.0, scalar2=None, op0=ALU.mult),
             reads=["tmpf"], writes=["nmask"])
        P.op("pool", lambda: nc.gpsimd.memset(zeros[:], 0.0), writes=["zeros"])
        P.op("pool", lambda: nc.gpsimd.memset(ones1[:], 1.0), writes=["ones1"])
        P.op("pool", lambda: nc.gpsimd.memset(pones[:], 1.0), writes=["pones"])
        P.op("pool", lambda: nc.gpsimd.memset(lsf[:], 1.0), writes=["lsf"])
        P.op("pool", lambda: nc.gpsimd.affine_select(out=lsf[:], in_=lsf[:], pattern=[[1, 128]],
                                                      compare_op=ALU.is_gt, fill=0.0, base=0, channel_multiplier=-1),
             reads=["lsf"], writes=["lsf"])
        P.op("dve", lambda: nc.vector.tensor_copy(out=lstrict[:], in_=lsf[:]), reads=["lsf"], writes=["lstrict"])
        zrow = gsb("zrow", [128, 4, 1024], BF16)
        P.op("pool", lambda: nc.gpsimd.memset(zrow[:], 0.0), writes=["zrow"])
        P.flush()
        for zi in range(24576 // 512):
            P.dma("pool", lambda zi=zi: nc.gpsimd.dma_start(out=XS[zi * 512:(zi + 1) * 512, :].rearrange("(p a) d -> p a d", a=4), in_=zrow[:]),
                  reads=["zrow"], writes=["XS"])

        def norm_rstd(P, src, skey, width, junk, jkey, ss, sskey):
            P.op("act", lambda: nc.scalar.activation(out=junk, in_=src, func=AF.Square, accum_out=ss),
                 reads=_kl(skey), writes=[jkey, sskey])
            P.op("act", lambda: nc.scalar.activation(out=ss, in_=ss, func=AF.Ln, scale=1.0 / width, bias=epsb[:]),
                 reads=[sskey, "epsb"], writes=[sskey])
            P.op("act", lambda: nc.scalar.activation(out=ss, in_=ss, func=AF.Exp, scale=-0.5),
                 reads=[sskey], writes=[sskey])

        epsb = gsb("epsb", [128, 1], F32)
        P.op("pool", lambda: nc.gpsimd.memset(epsb[:], EPS), writes=["epsb"])

        njunk = Rot([gsb(f"njunk{i}", [128, 1024], BF16) for i in range(1)], "njunk")
        nss = Rot([gsb(f"nss{i}", [128, 1], F32) for i in range(4)], "nss")
        nxn = Rot([gsb(f"nxn{i}", [128, 1024], BF16) for i in range(2)], "nxn")

        def norm_T(P, src, skey, gB, gkey, dstT, dkey, cp_eng="act"):
            junk, jk = njunk.next()
            ss, ssk = nss.next()
            xn, xnk = nxn.next()
            norm_rstd(P, src, skey, 1024, junk[:], jk, ss[:], ssk)
            P.op("dve", lambda: nc.vector.scalar_tensor_tensor(out=xn[:], in0=src, scalar=ss[:], in1=gB,
                                                                 op0=ALU.mult, op1=ALU.mult),
                 reads=_kl(skey) + [ssk, gkey], writes=[xnk])
            for k in range(8):
                P.op("pe", lambda k=k: nc.tensor.transpose(out=ptb[:, k, :], in_=xn[:, k * 128:(k + 1) * 128], identity=ident[:]),
                     reads=[xnk, "ident"], writes=["ptb"])
            if cp_eng == "act":
                P.op("act", lambda: nc.scalar.copy(out=dstT, in_=ptb[:]), reads=["ptb"], writes=[dkey])
            else:
                P.op("dve", lambda: nc.vector.tensor_copy(out=dstT, in_=ptb[:]), reads=["ptb"], writes=[dkey])
            return ss, ssk

        def load_gB(P, dst, key, src_row):
            P.dma("sp", lambda: nc.sync.dma_start(out=dst, in_=src_row.partition_broadcast(128)), writes=[key])

        with ExitStack() as st:
            def sb(name, shape, dt):
                return st.enter_context(nc.sbuf_tensor(_un(name), shape, dt))
            xnT = sb("xnT", [128, 8, S], BF16)
            gB0 = sb("gB0", [128, 1024], F32)
            load_gB(P, gB0[:], "gB0", norm_mix[0])
            xt_rot = Rot([sb(f"xt{i}", [128, 1024], F32) for i in range(2)], "xt")
            for b in range(NB):
                xt, xk = xt_rot.next()
                P.dma("sp", lambda xt=xt, b=b: nc.sync.dma_start(out=xt[:], in_=x[b * 128:(b + 1) * 128, :]), writes=[xk])
                norm_T(P, xt[:], xk, gB0[:], "gB0", xnT[:, :, b * 128:(b + 1) * 128], ("xnT", b))
            xnT_keys = lambda tc: [("xnT", 4 * tc + i) for i in range(4)]

            P.flush()
            st2 = ExitStack()
            st2.__enter__()
            sb_outer = sb

            def sb(name, shape, dt):
                return st2.enter_context(nc.sbuf_tensor(_un(name), shape, dt))
            cw = sb("cw", [128, 4, 3], F32)
            for fc in range(4):
                P.dma("sp", lambda fc=fc: nc.sync.dma_start(
                    out=cw[:, fc, :], in_=conv_w_even[0, :, fc * 128:(fc + 1) * 128].rearrange("w f -> f w"),
                    allow_slow_non_contiguous=True), writes=["cw"])
            wc_rot = Rot([sb(f"wc{i}", [128, 3, 8, 128], BF16) for i in range(2)], "wc")
            z_rot = Rot([sb(f"z{i}", [128, S + 2], F32) for i in range(2)], "z")
            hs_rot = Rot([sb(f"hs{i}", [128, 512], F32) for i in range(2)], "hs")
            acc_rot = Rot([sb(f"acc{i}", [128, 512], F32) for i in range(2)], "acc")
            ya_rot = Rot([sb(f"ya{i}", [128, 512], BF16) for i in range(2)], "ya")
            w_in0 = w_in_even[0].rearrange("(k p) n -> p k n", p=128)
            bank = Rot(pb[0:6], "pb")
            for fc in range(4):
                wc, wck = wc_rot.next()
                for j in range(3):
                    P.dma("pool", lambda wc=wc, j=j, fc=fc: nc.gpsimd.dma_start(
                        out=wc[:, j, :, :], in_=w_in0[:, :, j * 512 + fc * 128: j * 512 + (fc + 1) * 128]),
                        writes=[(wck, j)])
                z, zk = z_rot.next()
                P.op("pool", lambda z=z: nc.gpsimd.memset(z[:, 0:2], 0.0), writes=[(zk, -1)])
                for tc in range(8):
                    pp = []
                    for j in range(3):
                        pt, pk = bank.next()
                        for k in range(8):
                            P.op("pe", lambda pt=pt, wc=wc, j=j, k=k, tc=tc: nc.tensor.matmul(
                                pt[:], lhsT=wc[:, j, k, :], rhs=xnT[:, k, tc * 512:(tc + 1) * 512],
                                start=(k == 0), stop=(k == 7)),
                                reads=[(wck, j)] + xnT_keys(tc), writes=[pk])
                        pp.append((pt, pk))
                    (ph, phk), (pgb, pgbk), (pgc, pgck) = pp
                    hs, hsk = hs_rot.next()
                    acc, acck = acc_rot.next()
                    ya, yak = ya_rot.next()
                    P.op("act", lambda hs=hs, ph=ph: nc.scalar.copy(out=hs[:], in_=ph[:]), reads=[phk], writes=[hsk])
                    P.op("dve", lambda z=z, tc=tc, pgc=pgc, hs=hs: nc.vector.tensor_tensor(
                        out=z[:, 2 + tc * 512: 2 + (tc + 1) * 512], in0=pgc[:], in1=hs[:], op=ALU.mult),
                        reads=[pgck, hsk], writes=[(zk, tc)])
                    zr = [(zk, tc), (zk, tc - 1)]
                    P.op("dve", lambda acc=acc, z=z, tc=tc, fc=fc: nc.vector.tensor_scalar(
                        out=acc[:], in0=z[:, tc * 512: tc * 512 + 512], scalar1=cw[:, fc, 0:1], scalar2=None, op0=ALU.mult),
                        reads=zr + ["cw"], writes=[acck])
                    for wv in (1, 2):
                        P.op("dve", lambda acc=acc, z=z, tc=tc, fc=fc, wv=wv: nc.vector.scalar_tensor_tensor(
                            out=acc[:], in0=z[:, tc * 512 + wv: tc * 512 + wv + 512], scalar=cw[:, fc, wv:wv + 1],
                            in1=acc[:], op0=ALU.mult, op1=ALU.add),
                            reads=zr + ["cw", acck], writes=[acck])
                    P.op("dve", lambda ya=ya, acc=acc, pgb=pgb: nc.vector.tensor_tensor(
                        out=ya[:], in0=pgb[:], in1=acc[:], op=ALU.mult), reads=[pgbk, acck], writes=[yak])
                    P.dma("sp", lambda ya=ya, fc=fc, tc=tc: nc.sync.dma_start(
                        out=mixT[fc, :, tc * 512:(tc + 1) * 512], in_=ya[:]), reads=[yak], writes=[("mixT", fc, tc)])
            P.flush()
            st2.__exit__(None, None, None)
            sb = sb_outer
            if stop_after >= 2:
                wq_rot = Rot([sb(f"wq{i}", [128, 3, 8, 128], BF16) for i in range(2)], "wq")
                qT_rot = Rot([sb(f"qT{i}", [128, S], BF16) for i in range(2)], "qT")
                kT_rot = Rot([sb(f"kT{i}", [128, S], BF16) for i in range(2)], "kT")
                v_rot = Rot([sb(f"v{i}", [128, NB, 128], BF16) for i in range(2)], "v")
                yb_rot = Rot([sb(f"yb{i}", [128, S], BF16) for i in range(2)], "yb")
                e_rot = Rot([sb(f"e{i}", [128, 512], F32) for i in range(3)], "e")
                sp_rot = Rot([sb(f"sp{i}", [128, 512], BF16) for i in range(3)], "sp")
                a_rot = Rot([sb(f"a{i}", [128, 512], BF16) for i in range(3)], "a")
                r32_rot = Rot([sb(f"r32{i}", [128, 512], F32) for i in range(2)], "r32")
                r16_rot = Rot([sb(f"r16{i}", [128, 512], BF16) for i in range(2)], "r16")
                zb = Rot(pb[0:3], "pb")
                ob = Rot(pb[3:7], "pbo")
                pjb = Rot(pb[3:7], "pbo")

                def proj(hp):
                    wq, wqk = wq_rot.next()
                    for j in range(3):
                        P.dma("pool", lambda wq=wq, j=j, hp=hp: nc.gpsimd.dma_start(
                            out=wq[:, j, :, :], in_=w_in0[:, :, 1536 + j * 512 + hp * 128: 1536 + j * 512 + (hp + 1) * 128]),
                            writes=[(wqk, j)])
                    qT, qk = qT_rot.next()
                    kT, kk = kT_rot.next()
                    v, vk = v_rot.next()
                    for tc in range(8):
                        for j, (dst, dk, sc) in enumerate(((qT, qk, 0.125), (kT, kk, 1.0))):
                            pt, pk = pjb.next()
                            for k in range(8):
                                P.op("pe", lambda pt=pt, wq=wq, j=j, k=k, tc=tc: nc.tensor.matmul(
                                    pt[:], lhsT=wq[:, j, k, :], rhs=xnT[:, k, tc * 512:(tc + 1) * 512],
                                    start=(k == 0), stop=(k == 7)),
                                    reads=[(wqk, j)] + xnT_keys(tc), writes=[pk])
                            P.op("dve", lambda dst=dst, pt=pt, tc=tc, sc=sc: nc.vector.tensor_scalar(
                                out=dst[:, tc * 512:(tc + 1) * 512], in0=pt[:], scalar1=sc, scalar2=None, op0=ALU.mult),
                                reads=[pk], writes=[(dk, tc)])
                        pt, pk = pjb.next()
                        for tb in range(4):
                            b = tc * 4 + tb
                            for k in range(8):
                                P.op("pe", lambda pt=pt, wq=wq, k=k, b=b, tb=tb: nc.tensor.matmul(
                                    pt[:, tb * 128:(tb + 1) * 128], lhsT=xnT[:, k, b * 128:(b + 1) * 128], rhs=wq[:, 2, k, :],
                                    start=(k == 0), stop=(k == 7)),
                                    reads=[(wqk, 2), ("xnT", b)], writes=[pk])
                        P.op("dve", lambda v=v, pt=pt, tc=tc: nc.vector.tensor_copy(
                            out=v[:, tc * 4:(tc + 1) * 4, :], in_=pt[:].rearrange("p (b d) -> p b d", b=4)),
                            reads=[pk], writes=[(vk, tc)])
                    return (qT, qk, kT, kk, v, vk)

                def attention(hp, qkv):
                    qT, qk, kT, kk, v, vk = qkv
                    yb, ybk = yb_rot.next()
                    tiles = []
                    for hh in range(2):
                        for qc in range(8):
                            nkb = 4 * qc + 4
                            for ii, kb in enumerate(range(nkb - 1, -1, -1)):
                                jd = kb - 4 * qc
                                c0 = jd * 128 if jd >= 0 else 0
                                tiles.append(dict(hh=hh, qc=qc, kb=kb, c0=c0, diag=(jd >= 0), first=(ii == 0),
                                                  last=(kb == 0)))
                    nt = len(tiles)
                    st_ = [dict() for _ in range(nt)]

                    def stage_qk(i):
                        t = tiles[i]
                        po = t["hh"] * 64
                        zt, zk_ = zb.next()
                        st_[i]["z"] = (zt, zk_)
                        c0 = t["c0"]
                        qc, kb = t["qc"], t["kb"]
                        P.op("pe", lambda: nc.tensor.matmul(
                            zt[:, c0:512], lhsT=kT[po:po + 64, kb * 128:(kb + 1) * 128],
                            rhs=qT[po:po + 64, qc * 512 + c0:(qc + 1) * 512], start=True, stop=False),
                            reads=[(kk, kb // 4), (qk, qc)], writes=[zk_])
                        if t["diag"]:
                            P.op("pe", lambda: nc.tensor.matmul(
                                zt[:, c0:c0 + 128], lhsT=ident[:], rhs=nmask[:], start=False, stop=False),
                                reads=["ident", "nmask"], writes=[zk_])
                        et, ek = e_rot.next()
                        spt, spk = sp_rot.next()
                        st_[i]["sp"] = (spt, spk)
                        P.op("act", lambda: nc.scalar.activation(out=et[:, c0:512], in_=zt[:, c0:512], func=AF.Exp),
                             reads=[zk_], writes=[ek])
                        P.op("act", lambda: nc.scalar.activation(out=spt[:, c0:512], in_=et[:, c0:512], func=AF.Ln, bias=1.0),
                             reads=[ek], writes=[spk])

                    def stage_cum(i):
                        t = tiles[i]
                        zt, zk_ = st_[i]["z"]
                        spt, spk = st_[i]["sp"]
                        c0 = t["c0"]
                        if t["first"]:
                            r32, r32k = r32_rot.next()
                            st_[i]["r32"] = (r32, r32k)
                            P.op("pool", lambda: nc.gpsimd.memset(r32[:], 0.0), writes=[r32k])
                        else:
                            st_[i]["r32"] = st_[i - 1]["r32"]
                            r32, r32k = st_[i]["r32"]
                        P.op("pe", lambda: nc.tensor.matmul(zt[:, c0:512], lhsT=ntri[:], rhs=spt[:, c0:512],
                                                            start=False, stop=t["first"]),
                             reads=["ntri", spk], writes=[zk_])
                        if not t["first"]:
                            r16, r16k = st_[i]["r16"]
                            P.op("pe", lambda: nc.tensor.matmul(zt[:, c0:512], lhsT=nones[:], rhs=r16[:, c0:512],
                                                                start=False, stop=True),
                                 reads=["nones", r16k], writes=[zk_])
                        if not t["last"]:
                            c1 = tiles[i + 1]["c0"]
                            P.op("dve", lambda: nc.vector.tensor_tensor(out=r32[:, c0:512], in0=r32[:, c0:512],
                                                                       in1=spt[:, c0:512], op=ALU.add),
                                 reads=[r32k, spk], writes=[r32k])
                            r16n, r16nk = r16_rot.next()
                            st_[i + 1]["r16"] = (r16n, r16nk)
                            P.op("pool", lambda: nc.gpsimd.tensor_copy(out=r16n[:, c1:512], in_=r32[:, c1:512]),
                                 reads=[r32k], writes=[r16nk])
                        at, ak = a_rot.next()
                        st_[i]["a"] = (at, ak)
                        P.op("act", lambda: nc.scalar.activation(out=at[:, c0:512], in_=zt[:, c0:512], func=AF.Exp),
                             reads=[zk_], writes=[ak])

                    def stage_av(i):
                        t = tiles[i]
                        at, ak = st_[i]["a"]
                        c0 = t["c0"]
                        kb = t["kb"]
                        po = t["hh"] * 64
                        if t["first"]:
                            ot, ok = ob.next()
                            st_[i]["o"] = (ot, ok)
                            P.op("pe", lambda: nc.tensor.matmul(ot[:], lhsT=zeros[:], rhs=qT[:, 0:512], start=True, stop=False),
                                 reads=["zeros", (qk, 0)], writes=[ok])
                        else:
                            st_[i]["o"] = st_[i - 1]["o"]
                            ot, ok = st_[i]["o"]
                        P.op("pe", lambda: nc.tensor.matmul(ot[:, c0:512], lhsT=v[:, kb, :], rhs=at[:, c0:512],
                                                            start=False, stop=t["last"]),
                             reads=[(vk, kb // 4), ak], writes=[ok])
                        if t["last"]:
                            qc = t["qc"]
                            P.op("dve", lambda: nc.vector.tensor_copy(out=yb[po:po + 64, qc * 512:(qc + 1) * 512],
                                                                      in_=ot[po:po + 64, :]),
                                 reads=[ok], writes=[(ybk, qc, t["hh"])])
                        st_[i].pop("z", None)

                    stage_qk(0)
                    for i in range(nt):
                        if i + 1 < nt:
                            stage_qk(i + 1)
                        stage_cum(i)
                        if i >= 1:
                            stage_av(i - 1)
                    stage_av(nt - 1)
                    for qc in range(8):
                        P.dma("sp", lambda qc=qc: nc.sync.dma_start(out=mixT[4 + hp, :, qc * 512:(qc + 1) * 512],
                                                                    in_=yb[:, qc * 512:(qc + 1) * 512]),
                              reads=[(ybk, qc, 0), (ybk, qc, 1)], writes=[("mixT", 4 + hp, qc)])

                nhp = 4 if stop_after >= 3 else 1
                qkvs = {0: proj(0)}
                for hp in range(nhp):
                    if hp + 1 < nhp:
                        qkvs[hp + 1] = proj(hp + 1)
                    attention(hp, qkvs.pop(hp))
            P.flush()

        if stop_after >= 4:
            with ExitStack() as st:
                def sb(name, shape, dt):
                    return st.enter_context(nc.sbuf_tensor(_un(name), shape, dt))
                wo = sb("wo", [128, 8, 1024], BF16)
                P.dma("pool", lambda: nc.gpsimd.dma_start(out=wo[:], in_=w_out_even[0].rearrange("(k p) n -> p k n", p=128)),
                      writes=["wo"])
                mx_rot = Rot([sb(f"mx{i}", [128, 8, 512], BF16) for i in range(2)], "mx")
                xt_rot = Rot([sb(f"xt{i}", [128, 1024], F32) for i in range(3)], "xt")
                bank = Rot(pb[0:6], "pb")
                for tc in range(8):
                    mx, mxk = mx_rot.next()
                    P.dma("sp", lambda mx=mx, tc=tc: nc.sync.dma_start(
                        out=mx[:], in_=mixT[:, :, tc * 512:(tc + 1) * 512].rearrange("c p t -> p c t")), writes=[mxk])
                    for tb in range(4):
                        b = tc * 4 + tb
                        xt, xk = xt_rot.next()
                        P.dma("sp", lambda xt=xt, b=b: nc.sync.dma_start(out=xt[:], in_=x[b * 128:(b + 1) * 128, :]), writes=[xk])
                        for half in range(2):
                            pt, pk = bank.next()
                            for c in range(8):
                                P.op("pe", lambda pt=pt, mx=mx, c=c, tb=tb, half=half: nc.tensor.matmul(
                                    pt[:], lhsT=mx[:, c, tb * 128:(tb + 1) * 128], rhs=wo[:, c, half * 512:(half + 1) * 512],
                                    start=(c == 0), stop=(c == 7)), reads=[mxk, "wo"], writes=[pk])
                            P.op("dve", lambda xt=xt, pt=pt, half=half: nc.vector.tensor_tensor(
                                out=xt[:, half * 512:(half + 1) * 512], in0=pt[:], in1=xt[:, half * 512:(half + 1) * 512], op=ALU.add),
                                reads=[pk, xk], writes=[xk])
                        P.dma("sp", lambda xt=xt, b=b: nc.sync.dma_start(out=hA[b * 128:(b + 1) * 128, :], in_=xt[:]),
                              reads=[xk], writes=[("hA", b)])
                P.flush()

        def moe_pl_phase(l, hin, hout, final):
            with ExitStack() as st:
                def sb(name, shape, dt):
                    return st.enter_context(nc.sbuf_tensor(_un(name), shape, dt))
                NBS = 16
                hacc = sb("hacc", [128, NBS, 1024], F32)
                xT = sb("xT", [128, 8, NBS * 128], BF16)
                gBf = sb("gBf", [128, 1024], F32)
                gBp = sb("gBp", [128, 1024], F32)
                load_gB(P, gBf[:], "gBf", norm_ffn[l])
                load_gB(P, gBp[:], "gBp", norm_pl[l])
                if final:
                    gBo = sb("gBo", [128, 1024], F32)
                    load_gB(P, gBo[:], "gBo", final_norm)
                wr = sb("wr", [128, 8, 36], BF16)
                P.dma("pool", lambda: nc.gpsimd.dma_start(out=wr[:, :, 0:4], in_=router_c[l].rearrange("(k p) n -> p k n", p=128)),
                      writes=["wr"])
                P.dma("pool", lambda: nc.gpsimd.dma_start(out=wr[:, :, 4:36], in_=router_f[l].rearrange("(k p) n -> p k n", p=128)),
                      writes=["wr"])
                rb = sb("rb", [128, 36], F32)
                P.dma("sp", lambda: nc.sync.dma_start(out=rb[:, 0:4], in_=router_c_b[l].partition_broadcast(128)), writes=["rb"])
                P.dma("sp", lambda: nc.sync.dma_start(out=rb[:, 4:36], in_=router_f_b[l].partition_broadcast(128)), writes=["rb"])
                wpg = sb("wpg", [128, 8, 1024], BF16)
                wpe = sb("wpe", [128, 2, 1024], BF16)
                P.dma("pool", lambda: nc.gpsimd.dma_start(out=wpg[:], in_=w_pg[l].rearrange("(k p) n -> p k n", p=128)), writes=["wpg"])
                P.dma("pool", lambda: nc.gpsimd.dma_start(out=wpe[:], in_=w_pe[l].rearrange("(k p) n -> p k n", p=128)), writes=["wpe"])
                w13_rot = Rot([sb(f"w13_{i}", [128, 2, 8, 256], BF16) for i in range(2)], "w13")
                w2_rot = Rot([sb(f"w2_{i}", [128, 2, 1024], BF16) for i in range(2)], "w2")
                lg = sb("lg", [128, NBS, 36], F32)
                comb = sb("comb", [128, NBS, 32], F32)
                r_mx = sb("r_mx", [128, NBS], F32)
                r_gm = sb("r_gm", [128, NBS, 4], F32)
                r_ec = sb("r_ec", [128, NBS, 4], F32)
                r_pg = sb("r_pg", [128, NBS], F32)
                r_t = sb("r_t", [128, NBS, 4, 8], F32)
                r_lfs = sb("r_lfs", [128, NBS, 8], F32)
                r_t8 = sb("r_t8", [128, NBS, 8], F32)
                r_sel = sb("r_sel", [128, NBS, 8], F32)
                r_ex = sb("r_ex", [128, NBS, 8], F32)
                r_d = sb("r_d", [128, NBS], F32)
                s_rot = Rot([sb(f"s{i}", [128, 512], F32) for i in range(2)], "s")
                he_rot = Rot([sb(f"he{i}", [128, 2, 512], BF16) for i in range(2)], "he")
                pblk_rot = Rot([sb(f"pblk{i}", [128, 256], F32) for i in range(2)], "pblk")
                pb16_rot = Rot([sb(f"pb16{i}", [128, 256], BF16) for i in range(2)], "pb16")
                pT_rot = Rot([sb(f"pT{i}", [128, 2, 128], BF16) for i in range(2)], "pT")
                hnT_rot = Rot([sb(f"hnT{i}", [128, 8, 128], BF16) for i in range(2)], "hnT")
                sg_rot = Rot([sb(f"sg{i}", [128, 1024], F32) for i in range(1)], "sg")
                for sc in range(2):
                    hbank = Rot(pb[0:4], "pb")
                    ybank = Rot(pb[4:7], "pby")
                    for blk in range(NBS):
                        b = sc * NBS + blk
                        P.dma("sp", lambda blk=blk, b=b: nc.sync.dma_start(out=hacc[:, blk, :], in_=hin[b * 128:(b + 1) * 128, :]),
                              writes=hk(blk))
                        norm_T(P, hacc[:, blk, :], hk(blk), gBf[:], "gBf", xT[:, :, blk * 128:(blk + 1) * 128], ("xT", blk))
                        pt, pk = ybank.next()
                        for k in range(8):
                            P.op("pe", lambda pt=pt, k=k, blk=blk: nc.tensor.matmul(
                                pt[:, 0:36], lhsT=xT[:, k, blk * 128:(blk + 1) * 128], rhs=wr[:, k, :],
                                start=(k == 0), stop=(k == 7)), reads=[("xT", blk), "wr"], writes=[pk])
                        P.op("dve", lambda pt=pt, blk=blk: nc.vector.tensor_tensor(out=lg[:, blk, :], in0=pt[:, 0:36], in1=rb[:], op=ALU.add),
                             reads=[pk, "rb"], writes=["lg"])
                    lc = lg[:, :, 0:4]
                    lf = lg[:, :, 4:36].rearrange("p b (g e) -> p b g e", g=4)
                    P.op("dve", lambda: nc.vector.tensor_reduce(out=r_mx[:], in_=lc, axis=AX.X, op=ALU.max), reads=["lg"], writes=["r_mx"])
                    P.op("dve", lambda: nc.vector.tensor_tensor(out=r_gm[:], in0=lc, in1=r_mx[:].unsqueeze(2).to_broadcast([128, NBS, 4]),
                                                                op=ALU.is_ge), reads=["lg", "r_mx"], writes=["r_gm"])
                    P.op("dve", lambda: nc.vector.tensor_tensor(out=r_ec[:], in0=lc, in1=r_mx[:].unsqueeze(2).to_broadcast([128, NBS, 4]),
                                                                op=ALU.subtract), reads=["lg", "r_mx"], writes=["r_ec"])
                    P.op("act", lambda: nc.scalar.activation(out=r_ec[:], in_=r_ec[:], func=AF.Exp), reads=["r_ec"], writes=["r_ec"])
                    P.op("dve", lambda: nc.vector.tensor_reduce(out=r_pg[:], in_=r_ec[:], axis=AX.X, op=ALU.add), reads=["r_ec"], writes=["r_pg"])
                    P.op("dve", lambda: nc.vector.tensor_tensor(out=r_t[:], in0=lf, in1=r_gm[:].unsqueeze(3).to_broadcast([128, NBS, 4, 8]),
                                                                op=ALU.mult), reads=["lg", "r_gm"], writes=["r_t"])
                    P.op("dve", lambda: nc.vector.tensor_reduce(out=r_lfs[:], in_=r_t[:].rearrange("p b g e -> p b e g"), axis=AX.X, op=ALU.add),
                         reads=["r_t"], writes=["r_lfs"])
                    for blk in range(NBS):
                        P.op("dve", lambda blk=blk: nc.vector.max(out=r_t8[:, blk, :], in_=r_lfs[:, blk, :]), reads=["r_lfs"], writes=["r_t8"])
                    l1b = r_t8[:, :, 0:1].to_broadcast([128, NBS, 8])
                    l2b = r_t8[:, :, 1:2].to_broadcast([128, NBS, 8])
                    P.op("dve", lambda: nc.vector.tensor_tensor(out=r_sel[:], in0=r_lfs[:], in1=l2b, op=ALU.is_ge), reads=["r_lfs", "r_t8"], writes=["r_sel"])
                    P.op("dve", lambda: nc.vector.tensor_tensor(out=r_ex[:], in0=r_lfs[:], in1=l1b, op=ALU.subtract), reads=["r_lfs", "r_t8"], writes=["r_ex"])
                    P.op("act", lambda: nc.scalar.activation(out=r_ex[:], in_=r_ex[:], func=AF.Exp), reads=["r_ex"], writes=["r_ex"])
                    P.op("dve", lambda: nc.vector.tensor_tensor(out=r_ex[:], in0=r_ex[:], in1=r_sel[:], op=ALU.mult), reads=["r_ex", "r_sel"], writes=["r_ex"])
                    P.op("dve", lambda: nc.vector.tensor_reduce(out=r_d[:], in_=r_ex[:], axis=AX.X, op=ALU.add), reads=["r_ex"], writes=["r_d"])
                    P.op("dve", lambda: nc.vector.tensor_tensor(out=r_d[:], in0=r_d[:], in1=r_pg[:], op=ALU.mult), reads=["r_d", "r_pg"], writes=["r_d"])
                    P.op("dve", lambda: nc.vector.reciprocal(out=r_d[:], in_=r_d[:]), reads=["r_d"], writes=["r_d"])
                    P.op("dve", lambda: nc.vector.tensor_tensor(out=r_ex[:], in0=r_ex[:], in1=r_d[:].unsqueeze(2).to_broadcast([128, NBS, 8]),
                                                                op=ALU.mult), reads=["r_ex", "r_d"], writes=["r_ex"])
                    P.op("dve", lambda: nc.vector.tensor_tensor(
                        out=comb[:].rearrange("p b (g e) -> p b g e", g=4),
                        in0=r_gm[:].unsqueeze(3).to_broadcast([128, NBS, 4, 8]),
                        in1=r_ex[:].unsqueeze(2).to_broadcast([128, NBS, 4, 8]), op=ALU.mult),
                        reads=["r_gm", "r_ex"], writes=["comb"])
                    wts = {}

                    def load_w(e):
                        w13, w13k = w13_rot.next()
                        w2, w2k = w2_rot.next()
                        P.dma("pool", lambda: nc.gpsimd.dma_start(out=w13[:, 0, :, :], in_=moe_w1[l, e].rearrange("(k p) f -> p k f", p=128)),
                              writes=[(w13k, 0)])
                        P.dma("pool", lambda: nc.gpsimd.dma_start(out=w13[:, 1, :, :], in_=moe_w3[l, e].rearrange("(k p) f -> p k f", p=128)),
                              writes=[(w13k, 1)])
                        P.dma("pool", lambda: nc.gpsimd.dma_start(out=w2[:], in_=moe_w2[l, e].rearrange("(c p) d -> p c d", p=128)),
                              writes=[w2k])
                        wts[e] = (w13, w13k, w2, w2k)

                    units = [(e, tch) for e in range(32) for tch in range(4)]
                    ust = {}

                    def stage_h(u):
                        e, tch = units[u]
                        w13, w13k, w2, w2k = wts[e]
                        he, hek = he_rot.next()
                        ust[u] = (he, hek)
                        for fch in range(2):
                            p1, p1k = hbank.next()
                            p3, p3k = hbank.next()
                            for j, (pt, pk) in enumerate(((p1, p1k), (p3, p3k))):
                                for k in range(8):
                                    P.op("pe", lambda pt=pt, j=j, k=k, fch=fch: nc.tensor.matmul(
                                        pt[:], lhsT=w13[:, j, k, fch * 128:(fch + 1) * 128], rhs=xT[:, k, tch * 512:(tch + 1) * 512],
                                        start=(k == 0), stop=(k == 7)),
                                        reads=[(w13k, j)] + [("xT", 4 * tch + i) for i in range(4)], writes=[pk])
                            s, sk = s_rot.next()
                            P.op("act", lambda s=s, p1=p1: nc.scalar.activation(out=s[:], in_=p1[:], func=AF.Silu), reads=[p1k], writes=[sk])
                            P.op("dve", lambda s=s, p3=p3, fch=fch: nc.vector.tensor_tensor(out=he[:, fch, :], in0=p3[:], in1=s[:], op=ALU.mult),
                                 reads=[p3k, sk], writes=[(hek, fch)])

                    def stage_y(u):
                        e, tch = units[u]
                        w13, w13k, w2, w2k = wts[e]
                        he, hek = ust.pop(u)
                        for tb in range(4):
                            blk = tch * 4 + tb
                            for half in range(2):
                                py, pyk = ybank.next()
                                for fch in range(2):
                                    P.op("pe", lambda py=py, fch=fch, tb=tb, half=half: nc.tensor.matmul(
                                        py[:], lhsT=he[:, fch, tb * 128:(tb + 1) * 128], rhs=w2[:, fch, half * 512:(half + 1) * 512],
                                        start=(fch == 0), stop=(fch == 1)), reads=[(hek, fch), (w2k, fch)], writes=[pyk])
                                P.op("dve", lambda py=py, blk=blk, half=half: nc.vector.scalar_tensor_tensor(
                                    out=hacc[:, blk, half * 512:(half + 1) * 512], in0=py[:], scalar=comb[:, blk, e:e + 1],
                                    in1=hacc[:, blk, half * 512:(half + 1) * 512], op0=ALU.mult, op1=ALU.add),
                                    reads=[pyk, "comb", ("hacc", blk, half)], writes=[("hacc", blk, half)])
                        if tch == 3:
                            wts.pop(e)
                            if e + 2 < 32:
                                load_w(e + 2)

                    load_w(0)
                    load_w(1)
                    nu = len(units)
                    stage_h(0)
                    for u in range(nu):
                        if u + 1 < nu:
                            stage_h(u + 1)
                        stage_y(u)
                    gbank = Rot(pb[0:4], "pb")
                    for blk in range(NBS):
                        b = sc * NBS + blk
                        hnT, hnk = hnT_rot.next()
                        norm_T(P, hacc[:, blk, :], hk(blk), gBp[:], "gBp", hnT[:], hnk)
                        pblk, pblkk = pblk_rot.next()
                        p16, p16k = pb16_rot.next()
                        pT, pTk = pT_rot.next()
                        P.dma("sp", lambda pblk=pblk, b=b: nc.sync.dma_start(out=pblk[:], in_=p[l, b * 128:(b + 1) * 128, :]), writes=[pblkk])
                        P.op("pool", lambda p16=p16, pblk=pblk: nc.gpsimd.tensor_copy(out=p16[:], in_=pblk[:]), reads=[pblkk], writes=[p16k])
                        for k in range(2):
                            P.op("pe", lambda p16=p16, k=k: nc.tensor.transpose(out=ptb[:, k, :], in_=p16[:, k * 128:(k + 1) * 128], identity=ident[:]),
                                 reads=[p16k, "ident"], writes=["ptb"])
                        P.op("act", lambda pT=pT: nc.scalar.copy(out=pT[:], in_=ptb[:, 0:2, :]), reads=["ptb"], writes=[pTk])
                        sg, sgk = sg_rot.next()
                        for half in range(2):
                            pg_, pgk = gbank.next()
                            pe_, pek = gbank.next()
                            for k in range(8):
                                P.op("pe", lambda pg_=pg_, hnT=hnT, k=k, half=half: nc.tensor.matmul(
                                    pg_[:], lhsT=hnT[:, k, :], rhs=wpg[:, k, half * 512:(half + 1) * 512], start=(k == 0), stop=(k == 7)),
                                    reads=[hnk, "wpg"], writes=[pgk])
                            for k in range(2):
                                P.op("pe", lambda pe_=pe_, pT=pT, k=k, half=half: nc.tensor.matmul(
                                    pe_[:], lhsT=pT[:, k, :], rhs=wpe[:, k, half * 512:(half + 1) * 512], start=(k == 0), stop=(k == 1)),
                                    reads=[pTk, "wpe"], writes=[pek])
                            P.op("act", lambda sg=sg, pg_=pg_, half=half: nc.scalar.activation(out=sg[:, half * 512:(half + 1) * 512], in_=pg_[:], func=AF.Sigmoid),
                                 reads=[pgk], writes=[(sgk, half)])
                            P.op("dve", lambda sg=sg, pe_=pe_, half=half: nc.vector.tensor_tensor(
                                out=sg[:, half * 512:(half + 1) * 512], in0=pe_[:], in1=sg[:, half * 512:(half + 1) * 512], op=ALU.mult),
                                reads=[pek, (sgk, half)], writes=[(sgk, half)])
                        P.op("dve", lambda sg=sg, blk=blk: nc.vector.tensor_tensor(out=hacc[:, blk, :], in0=hacc[:, blk, :], in1=sg[:], op=ALU.add),
                             reads=[(sgk, 0), (sgk, 1)] + hk(blk), writes=hk(blk))
                        if final:
                            junk, jk = njunk.next()
                            ss, ssk = nss.next()
                            norm_rstd(P, hacc[:, blk, :], hk(blk), 1024, junk[:], jk, ss[:], ssk)
                            P.op("dve", lambda ss=ss, blk=blk: nc.vector.scalar_tensor_tensor(
                                out=hacc[:, blk, :], in0=hacc[:, blk, :], scalar=ss[:], in1=gBo[:], op0=ALU.mult, op1=ALU.mult),
                                reads=hk(blk) + [ssk, "gBo"], writes=hk(blk))
                        P.dma("sp", lambda blk=blk, b=b: nc.sync.dma_start(out=hout[b * 128:(b + 1) * 128, :], in_=hacc[:, blk, :]),
                              reads=hk(blk), writes=[("hout", b)])
                    P.flush()

        def sparse_moe_pl_phase(l, hin, hout, final):
            TS = 512
            NT = 48
            NA = TS // 128
            with ExitStack() as st:
                def sb(name, shape, dt):
                    return st.enter_context(nc.sbuf_tensor(_un(name), shape, dt))
                gBf = sb("gBf", [128, 1024], F32)
                gBp = sb("gBp", [128, 1024], F32)
                load_gB(P, gBf[:], "gBf", norm_ffn[l])
                load_gB(P, gBp[:], "gBp", norm_pl[l])
                if final:
                    gBo = sb("gBo", [128, 1024], F32)
                    load_gB(P, gBo[:], "gBo", final_norm)
                wr = sb("wr", [128, 8, 36], BF16)
                P.dma("pool", lambda: nc.gpsimd.dma_start(out=wr[:, :, 0:4], in_=router_c[l].rearrange("(k p) n -> p k n", p=128)),
                      writes=["wr"])
                P.dma("pool", lambda: nc.gpsimd.dma_start(out=wr[:, :, 4:36], in_=router_f[l].rearrange("(k p) n -> p k n", p=128)),
                      writes=["wr"])
                rb = sb("rb", [128, 36], F32)
                P.dma("sp", lambda: nc.sync.dma_start(out=rb[:, 0:4], in_=router_c_b[l].partition_broadcast(128)), writes=["rb"])
                P.dma("sp", lambda: nc.sync.dma_start(out=rb[:, 4:36], in_=router_f_b[l].partition_broadcast(128)), writes=["rb"])
                wpg = sb("wpg", [128, 8, 1024], BF16)
                wpe = sb("wpe", [128, 2, 1024], BF16)
                P.dma("pool", lambda: nc.gpsimd.dma_start(out=wpg[:], in_=w_pg[l].rearrange("(k p) n -> p k n", p=128)), writes=["wpg"])
                P.dma("pool", lambda: nc.gpsimd.dma_start(out=wpe[:], in_=w_pe[l].rearrange("(k p) n -> p k n", p=128)), writes=["wpe"])
                idx1_i = sb("idx1_i", [128, NB], I32)
                idx2_i = sb("idx2_i", [128, NB], I32)
                gate1 = sb("gate1", [128, NB], F32)
                gate2 = sb("gate2", [128, NB], F32)
                widx_i = sb("widx_i", [128, NT], I32)
                pio = sb("pio", [128, 1], F32)
                P.op("pool", lambda: nc.gpsimd.iota(pio[:], pattern=[[0, 1]], base=l * 32 * 128, channel_multiplier=1,
                                                    allow_small_or_imprecise_dtypes=True), writes=["pio"])
                with ExitStack() as st1:
                    def sb1(name, shape, dt):
                        return st1.enter_context(nc.sbuf_tensor(_un(name), shape, dt))
                    xnall = sb1("xnall", [128, NB, 1024], BF16)
                    ht_rot = Rot([sb1(f"ht{i}", [128, 1024], F32) for i in range(2)], "ht")
                    xTb_rot = Rot([sb1(f"xTb{i}", [128, 8, 128], BF16) for i in range(2)], "xTb")
                    lg = sb1("lg", [128, NB, 36], F32)
                    r_mx = sb1("r_mx", [128, NB], F32)
                    r_gm = sb1("r_gm", [128, NB, 4], F32)
                    r_ec = sb1("r_ec", [128, NB, 4], F32)
                    r_pg = sb1("r_pg", [128, NB], F32)
                    r_t = sb1("r_t", [128, NB, 4, 8], F32)
                    r_lfs = sb1("r_lfs", [128, NB, 8], F32)
                    r_t8 = sb1("r_t8", [128, NB, 8], F32)
                    r_sel = sb1("r_sel", [128, NB, 8], F32)
                    r_s1 = sb1("r_s1", [128, NB, 8], F32)
                    r_s2 = sb1("r_s2", [128, NB, 8], F32)
                    r_ex = sb1("r_ex", [128, NB, 8], F32)
                    r_d = sb1("r_d", [128, NB], F32)
                    r_tmp8 = sb1("r_tmp8", [128, NB, 8], F32)
                    M1 = sb1("M1", [128, NB, 32], F32)
                    M2 = sb1("M2", [128, NB, 32], F32)
                    Mb16 = sb1("Mb16", [128, NB, 32], BF16)
                    Rm16 = sb1("Rm16", [128, NB + 1, 32], BF16)
                    rank = sb1("rank", [128, NB, 32], F32)
                    cnt = sb1("cnt", [128, 32], F32)
                    thr16 = sb1("thr16", [128, 16], F32)
                    thr64 = sb1("thr64", [128, NT], F32)
                    cmpA = sb1("cmpA", [128, 32, 16], F32)
                    ntile = sb1("ntile", [128, 32], F32)
                    csA = sb1("csA", [128, 32], F32)
                    csB = sb1("csB", [128, 32], F32)
                    base = sb1("base", [128, 32], F32)
                    cmpB = sb1("cmpB", [128, NT, 32], F32)
                    te_f = sb1("te_f", [128, NT], F32)
                    tail = sb1("tail", [128, NT], F32)
                    tot = sb1("tot", [128, 1], F32)
                    idx_f = sb1("idx_f", [128, NB], F32)
                    tmp32 = sb1("tmp32", [128, NB, 32], F32)
                    ybank = Rot(pb[4:7], "pby")
                    for b in range(NB):
                        ht, htk = ht_rot.next()
                        P.dma("sp", lambda ht=ht, b=b: nc.sync.dma_start(out=ht[:], in_=hin[b * 128:(b + 1) * 128, :]), writes=[htk])
                        junk, jk = njunk.next()
                        ss, ssk = nss.next()
                        norm_rstd(P, ht[:], htk, 1024, junk[:], jk, ss[:], ssk)
                        P.op("dve", lambda ht=ht, ss=ss, b=b: nc.vector.scalar_tensor_tensor(
                            out=xnall[:, b, :], in0=ht[:], scalar=ss[:], in1=gBf[:], op0=ALU.mult, op1=ALU.mult),
                            reads=[htk, ssk, "gBf"], writes=[("xnall", b)])
                        for k in range(8):
                            P.op("pe", lambda k=k, b=b: nc.tensor.transpose(out=ptb[:, k, :], in_=xnall[:, b, k * 128:(k + 1) * 128], identity=ident[:]),
                                 reads=[("xnall", b), "ident"], writes=["ptb"])
                        xTb, xTbk = xTb_rot.next()
                        P.op("act", lambda xTb=xTb: nc.scalar.copy(out=xTb[:], in_=ptb[:]), reads=["ptb"], writes=[xTbk])
                        pt, pk = ybank.next()
                        for k in range(8):
                            P.op("pe", lambda pt=pt, k=k, xTb=xTb: nc.tensor.matmul(
                                pt[:, 0:36], lhsT=xTb[:, k, :], rhs=wr[:, k, :], start=(k == 0), stop=(k == 7)),
                                reads=[xTbk, "wr"], writes=[pk])
                        P.op("dve", lambda pt=pt, b=b: nc.vector.tensor_tensor(out=lg[:, b, :], in0=pt[:, 0:36], in1=rb[:], op=ALU.add),
                             reads=[pk, "rb"], writes=["lg"])
                    lc = lg[:, :, 0:4]
                    lf = lg[:, :, 4:36].rearrange("p b (g e) -> p b g e", g=4)

                    def bc(ap, shape):
                        return ap.to_broadcast(shape)

                    def dv(fn, reads, writes):
                        P.op("dve", fn, reads=reads, writes=writes)
                    dv(lambda: nc.vector.tensor_reduce(out=r_mx[:], in_=lc, axis=AX.X, op=ALU.max), ["lg"], ["r_mx"])
                    dv(lambda: nc.vector.tensor_tensor(out=r_gm[:], in0=lc, in1=bc(r_mx[:].unsqueeze(2), [128, NB, 4]), op=ALU.is_ge), ["lg", "r_mx"], ["r_gm"])
                    dv(lambda: nc.vector.tensor_tensor(out=r_ec[:], in0=lc, in1=bc(r_mx[:].unsqueeze(2), [128, NB, 4]), op=ALU.subtract), ["lg", "r_mx"], ["r_ec"])
                    P.op("act", lambda: nc.scalar.activation(out=r_ec[:], in_=r_ec[:], func=AF.Exp), reads=["r_ec"], writes=["r_ec"])
                    dv(lambda: nc.vector.tensor_reduce(out=r_pg[:], in_=r_ec[:], axis=AX.X, op=ALU.add), ["r_ec"], ["r_pg"])
                    dv(lambda: nc.vector.tensor_tensor(out=r_t[:], in0=lf, in1=bc(r_gm[:].unsqueeze(3), [128, NB, 4, 8]), op=ALU.mult), ["lg", "r_gm"], ["r_t"])
                    dv(lambda: nc.vector.tensor_reduce(out=r_lfs[:], in_=r_t[:].rearrange("p b g e -> p b e g"), axis=AX.X, op=ALU.add), ["r_t"], ["r_lfs"])
                    for b in range(NB):
                        dv(lambda b=b: nc.vector.max(out=r_t8[:, b, :], in_=r_lfs[:, b, :]), ["r_lfs"], ["r_t8"])
                    l1b = bc(r_t8[:, :, 0:1], [128, NB, 8])
                    l2b = bc(r_t8[:, :, 1:2], [128, NB, 8])
                    dv(lambda: nc.vector.tensor_tensor(out=r_sel[:], in0=r_lfs[:], in1=l2b, op=ALU.is_ge), ["r_lfs", "r_t8"], ["r_sel"])
                    dv(lambda: nc.vector.tensor_tensor(out=r_s1[:], in0=r_lfs[:], in1=l1b, op=ALU.is_ge), ["r_lfs", "r_t8"], ["r_s1"])
                    dv(lambda: nc.vector.tensor_tensor(out=r_s2[:], in0=r_sel[:], in1=r_s1[:], op=ALU.subtract), ["r_sel", "r_s1"], ["r_s2"])
                    dv(lambda: nc.vector.tensor_tensor(out=r_ex[:], in0=r_lfs[:], in1=l1b, op=ALU.subtract), ["r_lfs", "r_t8"], ["r_ex"])
                    P.op("act", lambda: nc.scalar.activation(out=r_ex[:], in_=r_ex[:], func=AF.Exp), reads=["r_ex"], writes=["r_ex"])
                    dv(lambda: nc.vector.tensor_tensor(out=r_ex[:], in0=r_ex[:], in1=r_sel[:], op=ALU.mult), ["r_ex", "r_sel"], ["r_ex"])
                    dv(lambda: nc.vector.tensor_reduce(out=r_d[:], in_=r_ex[:], axis=AX.X, op=ALU.add), ["r_ex"], ["r_d"])
                    dv(lambda: nc.vector.tensor_tensor(out=r_d[:], in0=r_d[:], in1=r_pg[:], op=ALU.mult), ["r_d", "r_pg"], ["r_d"])
                    dv(lambda: nc.vector.reciprocal(out=r_d[:], in_=r_d[:]), ["r_d"], ["r_d"])
                    dv(lambda: nc.vector.tensor_tensor(out=r_ex[:], in0=r_ex[:], in1=bc(r_d[:].unsqueeze(2), [128, NB, 8]), op=ALU.mult), ["r_ex", "r_d"], ["r_ex"])
                    dv(lambda: nc.vector.tensor_tensor(out=r_tmp8[:], in0=r_ex[:], in1=r_s1[:], op=ALU.mult), ["r_ex", "r_s1"], ["r_tmp8"])
                    dv(lambda: nc.vector.tensor_reduce(out=gate1[:], in_=r_tmp8[:], axis=AX.X, op=ALU.add), ["r_tmp8"], ["gate1"])
                    dv(lambda: nc.vector.tensor_tensor(out=r_tmp8[:], in0=r_ex[:], in1=r_s2[:], op=ALU.mult), ["r_ex", "r_s2", "gate1"], ["r_tmp8"])
                    dv(lambda: nc.vector.tensor_reduce(out=gate2[:], in_=r_tmp8[:], axis=AX.X, op=ALU.add), ["r_tmp8"], ["gate2"])
                    g4 = bc(r_gm[:].unsqueeze(3), [128, NB, 4, 8])
                    dv(lambda: nc.vector.tensor_tensor(out=M1[:].rearrange("p b (g e) -> p b g e", g=4), in0=g4,
                                                       in1=bc(r_s1[:].unsqueeze(2), [128, NB, 4, 8]), op=ALU.mult), ["r_gm", "r_s1"], ["M1"])
                    dv(lambda: nc.vector.tensor_tensor(out=M2[:].rearrange("p b (g e) -> p b g e", g=4), in0=g4,
                                                       in1=bc(r_s2[:].unsqueeze(2), [128, NB, 4, 8]), op=ALU.mult), ["r_gm", "r_s2"], ["M2"])
                    dv(lambda: nc.vector.tensor_tensor(out=Mb16[:], in0=M1[:], in1=M2[:], op=ALU.add), ["M1", "M2"], ["Mb16"])
                    P.op("pool", lambda: nc.gpsimd.memset(Rm16[:, 0, :], 0.0), writes=[("Rm", 0)])
                    for b in range(NB):
                        dv(lambda b=b: nc.vector.tensor_tensor(out=Rm16[:, b + 1, :], in0=Rm16[:, b, :], in1=Mb16[:, b, :], op=ALU.add),
                           [("Rm", b), "Mb16"], [("Rm", b + 1)])
                    rbank = [pb[0], pb[1]]
                    for b in range(NB):
                        pt = rbank[b // 16]
                        sl = slice((b % 16) * 32, (b % 16) * 32 + 32)
                        P.op("pe", lambda pt=pt, sl=sl, b=b: nc.tensor.matmul(pt[:, sl], lhsT=lstrict[:], rhs=Mb16[:, b, :], start=True, stop=False),
                             reads=["lstrict", "Mb16"], writes=[("pb", b // 16)])
                        P.op("pe", lambda pt=pt, sl=sl, b=b: nc.tensor.matmul(pt[:, sl], lhsT=pones[:], rhs=Rm16[:, b, :], start=False, stop=True),
                             reads=["pones", ("Rm", b)], writes=[("pb", b // 16)])
                    for hf in range(2):
                        dv(lambda hf=hf: nc.vector.tensor_copy(out=rank[:, hf * 16:(hf + 1) * 16, :],
                                                               in_=rbank[hf][:].rearrange("p (b e) -> p b e", b=16)),
                           [("pb", hf)], [("rank", hf)])
                    P.op("pe", lambda: nc.tensor.matmul(pb[2][:, 0:32], lhsT=pones[:], rhs=Rm16[:, NB, :], start=True, stop=True),
                         reads=["pones", ("Rm", NB)], writes=[("pb", 2)])
                    dv(lambda: nc.vector.tensor_copy(out=cnt[:], in_=pb[2][:, 0:32]), [("pb", 2)], ["cnt"])
                    P.op("pool", lambda: nc.gpsimd.iota(thr16[:], pattern=[[TS, 16]], base=0, channel_multiplier=0, allow_small_or_imprecise_dtypes=True), writes=["thr16"])
                    P.op("pool", lambda: nc.gpsimd.iota(thr64[:], pattern=[[TS, NT]], base=0, channel_multiplier=0, allow_small_or_imprecise_dtypes=True), writes=["thr64"])
                    dv(lambda: nc.vector.tensor_tensor(out=cmpA[:], in0=bc(cnt[:].unsqueeze(2), [128, 32, 16]), in1=bc(thr16[:].unsqueeze(1), [128, 32, 16]), op=ALU.is_gt),
                       ["cnt", "thr16"], ["cmpA"])
                    dv(lambda: nc.vector.tensor_reduce(out=ntile[:], in_=cmpA[:], axis=AX.X, op=ALU.add), ["cmpA"], ["ntile"])
                    src, srck = ntile, "ntile"
                    bufs = [(csA, "csA"), (csB, "csB")]
                    for si, sh in enumerate((1, 2, 4, 8, 16)):
                        dst, dstk = bufs[si % 2]
                        dv(lambda src=src, dst=dst, sh=sh: nc.vector.tensor_copy(out=dst[:, 0:sh], in_=src[:, 0:sh]), [srck], [(dstk, 0)])
                        dv(lambda src=src, dst=dst, sh=sh: nc.vector.tensor_tensor(out=dst[:, sh:32], in0=src[:, sh:32], in1=src[:, 0:32 - sh], op=ALU.add),
                           [srck], [(dstk, 1)])
                        src, srck = dst, dstk
                        srck_list = [(dstk, 0), (dstk, 1)]
                        srck = dstk
                        P.op("dve", lambda dst=dst: nc.vector.tensor_copy(out=dst[:, 0:1], in_=dst[:, 0:1]), reads=srck_list, writes=[dstk])
                    dv(lambda src=src: nc.vector.tensor_tensor(out=base[:], in0=src[:], in1=ntile[:], op=ALU.subtract), [srck, "ntile"], ["base"])
                    dv(lambda: nc.vector.tensor_scalar(out=base[:], in0=base[:], scalar1=float(TS), scalar2=None, op0=ALU.mult), ["base"], ["base"])
                    dv(lambda: nc.vector.tensor_tensor(out=cmpB[:], in0=bc(base[:].unsqueeze(1), [128, NT, 32]), in1=bc(thr64[:].unsqueeze(2), [128, NT, 32]), op=ALU.is_le),
                       ["base", "thr64"], ["cmpB"])
                    dv(lambda: nc.vector.tensor_reduce(out=te_f[:], in_=cmpB[:], axis=AX.X, op=ALU.add), ["cmpB"], ["te_f"])
                    dv(lambda: nc.vector.tensor_scalar(out=te_f[:], in0=te_f[:], scalar1=-1.0, scalar2=128.0, op0=ALU.add, op1=ALU.mult), ["te_f"], ["te_f"])
                    dv(lambda: nc.vector.tensor_scalar(out=te_f[:], in0=te_f[:], scalar1=pio[:, 0:1], scalar2=None, op0=ALU.add), ["te_f", "pio"], ["te_f"])
                    dv(lambda src=src: nc.vector.tensor_scalar(out=tot[:], in0=src[:, 31:32], scalar1=float(TS), scalar2=None, op0=ALU.mult), [srck], ["tot"])
                    dv(lambda: nc.vector.tensor_scalar(out=tail[:], in0=thr64[:], scalar1=tot[:, 0:1], scalar2=1.0e6, op0=ALU.is_ge, op1=ALU.mult),
                       ["thr64", "tot"], ["tail"])
                    dv(lambda: nc.vector.tensor_tensor(out=te_f[:], in0=te_f[:], in1=tail[:], op=ALU.add), ["te_f", "tail"], ["te_f"])
                    dv(lambda: nc.vector.tensor_copy(out=widx_i[:], in_=te_f[:]), ["te_f"], ["widx_i"])
                    dv(lambda: nc.vector.tensor_tensor(out=rank[:], in0=rank[:], in1=bc(base[:].unsqueeze(1), [128, NB, 32]), op=ALU.add),
                       [("rank", 0), ("rank", 1), "base"], ["rankp"])
                    for (Mk, Mkk, idxi, idxk) in ((M1, "M1", idx1_i, "idx1_i"), (M2, "M2", idx2_i, "idx2_i")):
                        dv(lambda Mk=Mk: nc.vector.tensor_tensor(out=tmp32[:], in0=rank[:], in1=Mk[:], op=ALU.mult), ["rankp", Mkk, "idx_f"], ["tmp32"])
                        dv(lambda: nc.vector.tensor_reduce(out=idx_f[:], in_=tmp32[:], axis=AX.X, op=ALU.add), ["tmp32"], ["idx_f"])
                        dv(lambda idxi=idxi: nc.vector.tensor_copy(out=idxi[:], in_=idx_f[:]), ["idx_f"], [idxk])
                    for b in range(NB):
                        for (idxi, idxk) in ((idx1_i, "idx1_i"), (idx2_i, "idx2_i")):
                            P.dma("pool", lambda b=b, idxi=idxi: nc.gpsimd.indirect_dma_start(
                                out=XS[:, :], out_offset=bass.IndirectOffsetOnAxis(ap=idxi[:, b:b + 1], axis=0),
                                in_=xnall[:, b, :], in_offset=None), reads=[("xnall", b), idxk], writes=["XS"])
                    P.flush()
                if sub < 2:
                    return
                with ExitStack() as st2_:
                    def sb2(name, shape, dt):
                        return st2_.enter_context(nc.sbuf_tensor(_un(name), shape, dt))
                    w13_rot = Rot([sb2(f"w13_{i}", [128, 2, 8, 256], BF16) for i in range(2)], "w13")
                    w2_rot = Rot([sb2(f"w2_{i}", [128, 2, 1024], BF16) for i in range(2)], "w2")
                    stg_rot = Rot([sb2(f"stg{i}", [128, 3, 2048], F32) for i in range(3)], "stg")
                    xs_rot = Rot([sb2(f"xs{i}", [128, NA, 1024], BF16) for i in range(2)], "xs")
                    xT_rot = Rot([sb2(f"xTt{i}", [128, 8, TS], BF16) for i in range(2)], "xTt")
                    s_rot = Rot([sb2(f"s{i}", [128, TS], F32) for i in range(2)], "s")
                    he_rot = Rot([sb2(f"he{i}", [128, 2, TS], BF16) for i in range(2)], "he")
                    yt_rot = Rot([sb2(f"yt{i}", [128, 1024], F32) for i in range(3)], "yt")
                    hbank = Rot(pb[0:4], "pb")
                    ybank = Rot(pb[4:7], "pby")
                    wts = {}
                    tst = {}
                    bcreg = {}

                    def mkreg():
                        bcreg["r"] = nc.gpsimd.to_reg(2 * 32 * 128 - 1)
                        return None
                    P.op("pool", mkreg, nosig=True)

                    wstg = {}

                    def cast_w(i):
                        stg, stgk = wstg.pop(i)
                        w13, w13k = w13_rot.next()
                        w2, w2k = w2_rot.next()
                        wts[i] = (w13, w13k, w2, w2k)
                        P.op("act", lambda: nc.scalar.copy(out=w13[:, 0, :, :].rearrange("p k f -> p (k f)"), in_=stg[:, 0, :]),
                             reads=[(stgk, 0)], writes=[(w13k, 0)])
                        P.op("dve", lambda: nc.vector.tensor_copy(out=w13[:, 1, :, :].rearrange("p k f -> p (k f)"), in_=stg[:, 1, :]),
                             reads=[(stgk, 1)], writes=[(w13k, 1)])
                        P.op("act", lambda: nc.scalar.copy(out=w2[:, 0, :], in_=stg[:, 2, 0:1024]),
                             reads=[(stgk, 2)], writes=[(w2k, 0)])
                        P.op("dve", lambda: nc.vector.tensor_copy(out=w2[:, 1, :], in_=stg[:, 2, 1024:2048]),
                             reads=[(stgk, 2)], writes=[(w2k, 1)])

                    def load_w(i):
                        stg, stgk = stg_rot.next()
                        ix = bass.IndirectOffsetOnAxis(ap=widx_i[:, i:i + 1], axis=0)
                        for j, (src_ap, pat, kw) in enumerate(((moe_w1, "l e (p k) f -> (l e p) (k f)", dict(k=8)),
                                                              (moe_w3, "l e (p k) f -> (l e p) (k f)", dict(k=8)),
                                                              (moe_w2, "l e (p c) d -> (l e p) (c d)", dict(c=2)))):
                            P.dma("pool", lambda j=j, src_ap=src_ap, pat=pat, kw=kw: nc.gpsimd.indirect_dma_start(
                                out=stg[:, j, :], out_offset=None, in_=src_ap.rearrange(pat, **kw), in_offset=ix,
                                bounds_check=bcreg["r"], oob_is_err=False),
                                reads=["widx_i"], writes=[(stgk, j)])
                        wstg[i] = (stg, stgk)

                    xsl = {}

                    def load_xs(i):
                        xs, xsk = xs_rot.next()
                        P.dma("sp", lambda: nc.sync.dma_start(out=xs[:], in_=XS[i * TS:(i + 1) * TS, :].rearrange("(a p) d -> p a d", p=128)),
                              writes=[xsk])
                        xsl[i] = (xs, xsk)

                    def stage_t(i):
                        cast_w(i)
                        if i + 3 < NT:
                            load_w(i + 3)
                        if i + 1 < NT:
                            load_xs(i + 1)
                        xs, xsk = xsl.pop(i)
                        xT, xTk = xT_rot.next()
                        for a in range(NA):
                            for k in range(8):
                                P.op("pe", lambda a=a, k=k: nc.tensor.transpose(out=ptb[:, k, :], in_=xs[:, a, k:1024:8], identity=ident[:]),
                                     reads=[xsk, "ident"], writes=["ptb"])
                            if a % 2 == 0:
                                P.op("act", lambda a=a: nc.scalar.copy(out=xT[:, :, a * 128:(a + 1) * 128], in_=ptb[:]), reads=["ptb"], writes=[(xTk, a)])
                            else:
                                P.op("dve", lambda a=a: nc.vector.tensor_copy(out=xT[:, :, a * 128:(a + 1) * 128], in_=ptb[:]), reads=["ptb"], writes=[(xTk, a)])
                        w13, w13k, w2, w2k = wts[i]
                        he, hek = he_rot.next()
                        tst[i] = (he, hek)
                        for fch in range(2):
                            p1, p1k = hbank.next()
                            p3, p3k = hbank.next()
                            for j, (pt, pk) in enumerate(((p1, p1k), (p3, p3k))):
                                for k in range(8):
                                    P.op("pe", lambda pt=pt, j=j, k=k, fch=fch: nc.tensor.matmul(
                                        pt[:, 0:TS], lhsT=w13[:, j, k, fch:256:2], rhs=xT[:, k, :],
                                        start=(k == 0), stop=(k == 7)),
                                        reads=[(w13k, j)] + [(xTk, a_) for a_ in range(NA)], writes=[pk])
                            s, sk = s_rot.next()
                            P.op("act", lambda s=s, p1=p1: nc.scalar.activation(out=s[:], in_=p1[:, 0:TS], func=AF.Silu), reads=[p1k], writes=[sk])
                            P.op("dve", lambda s=s, p3=p3, fch=fch: nc.vector.tensor_tensor(out=he[:, fch, :], in0=p3[:, 0:TS], in1=s[:], op=ALU.mult),
                                 reads=[p3k, sk], writes=[(hek, fch)])

                    def stage_y(i):
                        w13, w13k, w2, w2k = wts.pop(i)
                        he, hek = tst.pop(i)
                        for a in range(NA):
                            yt, ytk = yt_rot.next()
                            for half in range(2):
                                py, pyk = ybank.next()
                                for fch in range(2):
                                    P.op("pe", lambda py=py, fch=fch, a=a, half=half: nc.tensor.matmul(
                                        py[:], lhsT=he[:, fch, a * 128:(a + 1) * 128], rhs=w2[:, fch, half * 512:(half + 1) * 512],
                                        start=(fch == 0), stop=(fch == 1)), reads=[(hek, fch), (w2k, fch)], writes=[pyk])
                                if half == 0:
                                    P.op("act", lambda py=py, yt=yt, half=half: nc.scalar.copy(out=yt[:, half * 512:(half + 1) * 512], in_=py[:]),
                                         reads=[pyk], writes=[(ytk, half)])
                                else:
                                    P.op("dve", lambda py=py, yt=yt, half=half: nc.vector.tensor_copy(out=yt[:, half * 512:(half + 1) * 512], in_=py[:]),
                                         reads=[pyk], writes=[(ytk, half)])
                            r0 = i * TS + a * 128
                            P.dma("sp", lambda yt=yt, r0=r0: nc.sync.dma_start(out=YS[r0:r0 + 128, :], in_=yt[:]),
                                  reads=[(ytk, 0), (ytk, 1)], writes=[("YS", i, a)])

                    load_w(0)
                    load_w(1)
                    load_w(2)
                    load_xs(0)
                    stage_t(0)
                    for i in range(NT):
                        if i + 1 < NT:
                            stage_t(i + 1)
                        stage_y(i)
                    P.flush()
                if sub < 3:
                    return
                with ExitStack() as st3_:
                    def sb3(name, shape, dt):
                        return st3_.enter_context(nc.sbuf_tensor(_un(name), shape, dt))
                    ht_rot = Rot([sb3(f"htc{i}", [128, 1024], F32) for i in range(5)], "htc")
                    y1_rot = Rot([sb3(f"y1_{i}", [128, 1024], F32) for i in range(4)], "y1")
                    y2_rot = Rot([sb3(f"y2_{i}", [128, 1024], F32) for i in range(4)], "y2")
                    pblk_rot = Rot([sb3(f"pblk{i}", [128, 256], F32) for i in range(4)], "pblk")
                    pb16_rot = Rot([sb3(f"pb16{i}", [128, 256], BF16) for i in range(2)], "pb16")
                    pT_rot = Rot([sb3(f"pT{i}", [128, 2, 128], BF16) for i in range(2)], "pT")
                    hnT_rot = Rot([sb3(f"hnT{i}", [128, 8, 128], BF16) for i in range(2)], "hnT")
                    sg_rot = Rot([sb3(f"sg{i}", [128, 1024], F32) for i in range(2)], "sg")
                    gbank = Rot(pb[0:4], "pb")
                    ld = {}

                    def loads7(b):
                        ht, htk = ht_rot.next()
                        y1, y1k = y1_rot.next()
                        y2, y2k = y2_rot.next()
                        pblk, pblkk = pblk_rot.next()
                        P.dma("sp", lambda: nc.sync.dma_start(out=ht[:], in_=hin[b * 128:(b + 1) * 128, :]), writes=[htk])
                        P.dma("sp", lambda: nc.sync.dma_start(out=pblk[:], in_=p[l, b * 128:(b + 1) * 128, :]), writes=[pblkk])
                        P.dma("pool", lambda: nc.gpsimd.indirect_dma_start(
                            out=y1[:, :], out_offset=None, in_=YS[:, :], in_offset=bass.IndirectOffsetOnAxis(ap=idx1_i[:, b:b + 1], axis=0)),
                            reads=["idx1_i"], writes=[y1k])
                        P.dma("pool", lambda: nc.gpsimd.indirect_dma_start(
                            out=y2[:, :], out_offset=None, in_=YS[:, :], in_offset=bass.IndirectOffsetOnAxis(ap=idx2_i[:, b:b + 1], axis=0)),
                            reads=["idx2_i"], writes=[y2k])
                        ld[b] = (ht, htk, y1, y1k, y2, y2k, pblk, pblkk)

                    loads7(0)
                    loads7(1)
                    for b in range(NB):
                        if b + 2 < NB:
                            loads7(b + 2)
                        ht, htk, y1, y1k, y2, y2k, pblk, pblkk = ld.pop(b)
                        P.op("dve", lambda ht=ht, y1=y1, b=b: nc.vector.scalar_tensor_tensor(
                            out=ht[:], in0=y1[:], scalar=gate1[:, b:b + 1], in1=ht[:], op0=ALU.mult, op1=ALU.add),
                            reads=[htk, y1k, "gate1"], writes=[htk])
                        P.op("dve", lambda ht=ht, y2=y2, b=b: nc.vector.scalar_tensor_tensor(
                            out=ht[:], in0=y2[:], scalar=gate2[:, b:b + 1], in1=ht[:], op0=ALU.mult, op1=ALU.add),
                            reads=[htk, y2k, "gate2"], writes=[htk])
                        hnT, hnk = hnT_rot.next()
                        norm_T(P, ht[:], htk, gBp[:], "gBp", hnT[:], hnk)
                        p16, p16k = pb16_rot.next()
                        pT, pTk = pT_rot.next()
                        P.op("pool", lambda p16=p16, pblk=pblk: nc.gpsimd.tensor_copy(out=p16[:], in_=pblk[:]), reads=[pblkk], writes=[p16k])
                        for k in range(2):
                            P.op("pe", lambda p16=p16, k=k: nc.tensor.transpose(out=ptb[:, k, :], in_=p16[:, k * 128:(k + 1) * 128], identity=ident[:]),
                                 reads=[p16k, "ident"], writes=["ptb"])
                        P.op("act", lambda pT=pT: nc.scalar.copy(out=pT[:], in_=ptb[:, 0:2, :]), reads=["ptb"], writes=[pTk])
                        sg, sgk = sg_rot.next()
                        for half in range(2):
                            pg_, pgk = gbank.next()
                            pe_, pek = gbank.next()
                            for k in range(8):
                                P.op("pe", lambda pg_=pg_, hnT=hnT, k=k, half=half: nc.tensor.matmul(
                                    pg_[:], lhsT=hnT[:, k, :], rhs=wpg[:, k, half * 512:(half + 1) * 512], start=(k == 0), stop=(k == 7)),
                                    reads=[hnk, "wpg"], writes=[pgk])
                            for k in range(2):
                                P.op("pe", lambda pe_=pe_, pT=pT, k=k, half=half: nc.tensor.matmul(
                                    pe_[:], lhsT=pT[:, k, :], rhs=wpe[:, k, half * 512:(half + 1) * 512], start=(k == 0), stop=(k == 1)),
                                    reads=[pTk, "wpe"], writes=[pek])
                            P.op("act", lambda sg=sg, pg_=pg_, half=half: nc.scalar.activation(out=sg[:, half * 512:(half + 1) * 512], in_=pg_[:], func=AF.Sigmoid),
                                 reads=[pgk], writes=[(sgk, half)])
                            P.op("dve", lambda sg=sg, pe_=pe_, half=half: nc.vector.tensor_tensor(
                                out=sg[:, half * 512:(half + 1) * 512], in0=pe_[:], in1=sg[:, half * 512:(half + 1) * 512], op=ALU.mult),
                                reads=[pek, (sgk, half)], writes=[(sgk, half)])
                        P.op("pool", lambda sg=sg, ht=ht: nc.gpsimd.tensor_tensor(out=ht[:], in0=ht[:], in1=sg[:], op=ALU.add),
                             reads=[(sgk, 0), (sgk, 1), htk], writes=[htk])
                        if final:
                            junk, jk = njunk.next()
                            ss, ssk = nss.next()
                            norm_rstd(P, ht[:], htk, 1024, junk[:], jk, ss[:], ssk)
                            P.op("dve", lambda ss=ss, ht=ht: nc.vector.scalar_tensor_tensor(
                                out=ht[:], in0=ht[:], scalar=ss[:], in1=gBo[:], op0=ALU.mult, op1=ALU.mult),
                                reads=[htk, ssk, "gBo"], writes=[htk])
                        P.dma("sp", lambda ht=ht, b=b: nc.sync.dma_start(out=hout[b * 128:(b + 1) * 128, :], in_=ht[:]),
                              reads=[htk], writes=[("hout", b)])
                    P.flush()

        SPARSE = True
        mphase = sparse_moe_pl_phase if SPARSE else moe_pl_phase
        if stop_after >= 5:
            mphase(0, hA, hB, False)

        if stop_after >= 6:
            with ExitStack() as st:
                def sb(name, shape, dt):
                    return st.enter_context(nc.sbuf_tensor(_un(name), shape, dt))
                wi = sb("wi", [128, 8, 4096], BF16)
                wi_src = w_in_odd[0].rearrange("(k p) n -> p k n", p=128)
                for k in range(8):
                    P.dma("pool", lambda k=k: nc.gpsimd.dma_start(out=wi[:, k, :], in_=wi_src[:, k, :]), writes=[("wi", k)])
                wi_keys = [("wi", k) for k in range(8)]
                wo1 = sb("wo1", [128, 16, 1024], BF16)
                wo_src = w_out_odd[0].rearrange("(c p) n -> p c n", p=128)
                for c in range(0, 16, 4):
                    P.dma("pool", lambda c=c: nc.gpsimd.dma_start(out=wo1[:, c:c + 4, :], in_=wo_src[:, c:c + 4, :]), writes=[("wo1", c)])
                wo_keys = [("wo1", c) for c in range(0, 16, 4)]
                gB1 = sb("gB1", [128, 1024], F32)
                load_gB(P, gB1[:], "gB1", norm_mix[1])
                gvB = sb("gvB", [128, 2048], F32)
                P.dma("sp", lambda: nc.sync.dma_start(out=gvB[:], in_=g_v_odd[0].partition_broadcast(128)), writes=["gvB"])
                bsf = sb("bsf", [1, 8, 128], F32)
                bs16 = sb("bs16", [1, 8, 128], BF16)
                P.dma("sp", lambda: nc.sync.dma_start(out=bsf[:], in_=b_s_odd[0:1]), writes=["bsf"])
                P.op("dve", lambda: nc.vector.tensor_copy(out=bs16[:], in_=bsf[:]), reads=["bsf"], writes=["bs16"])
                wsT = sb("wsT", [128, 8, 128], BF16)
                st3 = ExitStack()
                st3.__enter__()
                wsf = st3.enter_context(nc.sbuf_tensor(_un("wsf"), [128, 8, 128], F32))
                ws16 = st3.enter_context(nc.sbuf_tensor(_un("ws16"), [128, 8, 128], BF16))
                P.dma("sp", lambda: nc.sync.dma_start(out=wsf[:], in_=w_s_odd[0].rearrange("g t s -> t g s")), writes=["wsf"])
                for g in range(8):
                    P.op("pool", lambda g=g: nc.gpsimd.affine_select(out=wsf[:, g, :], in_=wsf[:, g, :], pattern=[[-1, 128]],
                                                                      compare_op=ALU.is_ge, fill=0.0, base=0, channel_multiplier=1),
                         reads=["wsf"], writes=["wsf"])
                P.op("dve", lambda: nc.vector.tensor_copy(out=ws16[:], in_=wsf[:]), reads=["wsf"], writes=["ws16"])
                for g in range(8):
                    P.op("pe", lambda g=g: nc.tensor.transpose(out=ptb[:, g, :], in_=ws16[:, g, :], identity=ident[:]),
                         reads=["ws16", "ident"], writes=["ptb"])
                P.op("dve", lambda: nc.vector.tensor_copy(out=wsT[:], in_=ptb[:]), reads=["ptb"], writes=["wsT"])
                P.flush()
                st3.__exit__(None, None, None)
                ht_rot = Rot([sb(f"ht{i}", [128, 1024], F32) for i in range(5)], "ht")
                xg_rot = Rot([sb(f"xg{i}", [128, 8, 512], BF16) for i in range(2)], "xg")
                uT_rot = Rot([sb(f"uT{i}", [128, 16, 512], BF16) for i in range(1)], "uT")
                vt_rot = Rot([sb(f"vt{i}", [128, 2048], F32) for i in range(1)], "vt")
                vn_rot = Rot([sb(f"vn{i}", [128, 2048], BF16) for i in range(1)], "vn")
                yT_rot = Rot([sb(f"yT{i}", [128, 16, 128], BF16) for i in range(2)], "yT")
                abank = Rot(pb[0:3], "pb")
                gbank = Rot(pb[3:5], "pbg")
                obank = Rot(pb[5:7], "pbo")
                for tc in range(8):
                    xg, xgk = xg_rot.next()
                    hts = []
                    for tb in range(4):
                        b = tc * 4 + tb
                        ht, htk = ht_rot.next()
                        hts.append((ht, htk))
                        P.dma("sp", lambda ht=ht, b=b: nc.sync.dma_start(out=ht[:], in_=hB[b * 128:(b + 1) * 128, :]),
                              reads=[("hout", b)], writes=[htk])
                        norm_T(P, ht[:], htk, gB1[:], "gB1", xg[:, :, tb * 128:(tb + 1) * 128], (xgk, tb))
                    xgkeys = [(xgk, tb) for tb in range(4)]
                    uT, uTk = uT_rot.next()
                    for fcu in range(16):
                        pt, pk = abank.next()
                        for k in range(8):
                            P.op("pe", lambda pt=pt, k=k, fcu=fcu, xg=xg: nc.tensor.matmul(
                                pt[:], lhsT=wi[:, k, fcu * 128:(fcu + 1) * 128], rhs=xg[:, k, :], start=(k == 0), stop=(k == 7)),
                                reads=[("wi", k)] + xgkeys, writes=[pk])
                        P.op("act", lambda pt=pt, uT=uT, fcu=fcu: nc.scalar.activation(out=uT[:, fcu, :], in_=pt[:], func=AF.Gelu_apprx_tanh),
                             reads=[pk], writes=[(uTk, fcu)])
                    for tb in range(4):
                        b = tc * 4 + tb
                        ht, htk = hts[tb]
                        vt, vtk = vt_rot.next()
                        for vg in range(4):
                            pt, pk = abank.next()
                            for k in range(8):
                                P.op("pe", lambda pt=pt, k=k, vg=vg, xg=xg, tb=tb: nc.tensor.matmul(
                                    pt[:], lhsT=xg[:, k, tb * 128:(tb + 1) * 128], rhs=wi[:, k, 2048 + vg * 512: 2048 + (vg + 1) * 512],
                                    start=(k == 0), stop=(k == 7)), reads=[("wi", k), (xgk, tb)], writes=[pk])
                            P.op("act", lambda pt=pt, vt=vt, vg=vg: nc.scalar.activation(out=vt[:, vg * 512:(vg + 1) * 512], in_=pt[:], func=AF.Gelu_apprx_tanh),
                                 reads=[pk], writes=[(vtk, vg)])
                        ss, ssk = nss.next()
                        vtkeys = [(vtk, vg) for vg in range(4)]
                        vn, vnk = vn_rot.next()
                        P.op("act", lambda vt=vt, ss=ss, vn=vn: nc.scalar.activation(out=vn[:], in_=vt[:], func=AF.Square, accum_out=ss[:]),
                             reads=vtkeys, writes=[vnk, ssk])
                        P.op("act", lambda ss=ss: nc.scalar.activation(out=ss[:], in_=ss[:], func=AF.Ln, scale=1.0 / 2048, bias=epsb[:]),
                             reads=[ssk, "epsb"], writes=[ssk])
                        P.op("act", lambda ss=ss: nc.scalar.activation(out=ss[:], in_=ss[:], func=AF.Exp, scale=-0.5), reads=[ssk], writes=[ssk])
                        P.op("dve", lambda vn=vn, vt=vt, ss=ss: nc.vector.scalar_tensor_tensor(out=vn[:], in0=vt[:], scalar=ss[:], in1=gvB[:],
                                                                                              op0=ALU.mult, op1=ALU.mult),
                             reads=vtkeys + [ssk, "gvB"], writes=[vnk])
                        yT, yTk = yT_rot.next()
                        for q4 in range(4):
                            pg_, pgk = gbank.next()
                            for i4 in range(4):
                                fcu = q4 * 4 + i4
                                g = fcu // 2
                                P.op("pe", lambda pg_=pg_, i4=i4, fcu=fcu, g=g, vn=vn: nc.tensor.matmul(
                                    pg_[:, i4 * 128:(i4 + 1) * 128], lhsT=vn[:, fcu * 128:(fcu + 1) * 128], rhs=wsT[:, g, :], start=True, stop=False),
                                    reads=[vnk, "wsT"], writes=[pgk])
                                P.op("pe", lambda pg_=pg_, i4=i4, g=g: nc.tensor.matmul(
                                    pg_[:, i4 * 128:(i4 + 1) * 128], lhsT=ones1[0:1, :], rhs=bs16[0:1, g, :], start=False, stop=True),
                                    reads=["ones1", "bs16"], writes=[pgk])
                            P.op("dve", lambda pg_=pg_, yT=yT, uT=uT, q4=q4, tb=tb: nc.vector.tensor_tensor(
                                out=yT[:, q4 * 4:(q4 + 1) * 4, :], in0=pg_[:].rearrange("p (i t) -> p i t", i=4),
                                in1=uT[:, q4 * 4:(q4 + 1) * 4, tb * 128:(tb + 1) * 128], op=ALU.mult),
                                reads=[pgk] + [(uTk, q4 * 4 + i) for i in range(4)], writes=[(yTk, q4)])
                        for half in range(2):
                            po_, pok = obank.next()
                            for fcu in range(16):
                                P.op("pe", lambda po_=po_, yT=yT, fcu=fcu, half=half: nc.tensor.matmul(
                                    po_[:], lhsT=yT[:, fcu, :], rhs=wo1[:, fcu, half * 512:(half + 1) * 512], start=(fcu == 0), stop=(fcu == 15)),
                                    reads=[(yTk, fcu // 4), ("wo1", (fcu // 4) * 4)], writes=[pok])
                            P.op("dve", lambda po_=po_, ht=ht, half=half: nc.vector.tensor_tensor(
                                out=ht[:, half * 512:(half + 1) * 512], in0=po_[:], in1=ht[:, half * 512:(half + 1) * 512], op=ALU.add),
                                reads=[pok, htk], writes=[htk])
                        P.dma("sp", lambda ht=ht, b=b: nc.sync.dma_start(out=hA[b * 128:(b + 1) * 128, :], in_=ht[:]),
                              reads=[htk], writes=[("hA", b)])
                P.flush()

        if stop_after >= 7:
            mphase(1, hA, out, True)
        P.flush()
        nc._prog_stats = dict(nops=P.nops, ccount=dict(P.ccount), dcount=dict(P.dcount))
    return nc


_NC_CACHE = {}


def kernel(**inputs):
    n = 8
    if "nc" not in _NC_CACHE:
        _NC_CACHE["nc"] = build()
    nc = _NC_CACHE["nc"]
    in_maps = []
    for c in range(n):
        m = {}
        for k, v in inputs.items():
            v = np.asarray(v)
            if k == "x":
                m[k] = np.ascontiguousarray(v[c])
            elif k == "p":
                m[k] = np.ascontiguousarray(v[:, c])
            else:
                m[k] = np.ascontiguousarray(v)
        in_maps.append(m)
    res = run_bass_kernel_spmd(nc, in_maps, core_ids=list(range(n)))
    return np.stack([np.asarray(r["out"]) for r in res.results], axis=0).astype(np.float32)
```

```python
import numpy as np
from contextlib import ExitStack
import concourse.bass as bass
import concourse.mybir as mybir
from concourse.bass_utils import run_bass_kernel_spmd

F32 = mybir.dt.float32
BF16 = mybir.dt.bfloat16
AF = mybir.ActivationFunctionType
ALU = mybir.AluOpType
AX = mybir.AxisListType
I32 = mybir.dt.int32

EPOCH = 30000
NDMASEM = 12
S = 4096
D = 1024
NB = S // 128
EPS = 1e-6


class Prog:
    ENG = ("pe", "act", "dve", "pool", "sp")

    def __init__(self, nc, stack):
        self.nc = nc
        self.stack = stack
        self.ops = []
        self.ccount = {e: 0 for e in self.ENG}
        self.dcount = {e: 0 for e in self.ENG}
        self.csem = {e: [] for e in self.ENG}
        self.dsem = {e: [] for e in self.ENG}
        self.dma_hist = {e: [] for e in self.ENG}
        self.waited = {e: {} for e in self.ENG}
        self.nops = 0

    def eng_obj(self, e):
        nc = self.nc
        return {"pe": nc.tensor, "act": nc.scalar, "dve": nc.vector,
                "pool": nc.gpsimd, "sp": nc.sync}[e]

    def op(self, eng, fn, reads=(), writes=(), nosig=False):
        self.ops.append(dict(eng=eng, fn=fn, reads=tuple(reads), writes=tuple(writes), dma=False, nosig=nosig))

    def dma(self, eng, fn, reads=(), writes=()):
        self.ops.append(dict(eng=eng, fn=fn, reads=tuple(reads), writes=tuple(writes), dma=True))

    def _csem(self, e, ep):
        while len(self.csem[e]) <= ep:
            self.csem[e].append(self.stack.enter_context(self.nc.semaphore(f"c_{e}_{len(self.csem[e])}")))
        return self.csem[e][ep]

    def _dsem(self, e, k):
        while len(self.dsem[e]) <= k:
            self.dsem[e].append(self.stack.enter_context(self.nc.semaphore(f"d_{e}_{len(self.dsem[e])}")))
        return self.dsem[e][k]

    def _wait(self, e, key, sem, val):
        w = self.waited[e]
        if key[0] == "c":
            for kk, v in w.items():
                if kk[0] == "c" and kk[1] == key[1] and (kk[2] > key[2] or (kk[2] == key[2] and v >= val)):
                    return
        elif w.get(key, 0) >= val:
            return
        self.eng_obj(e).wait_ge(sem, val)
        w[key] = max(w.get(key, 0), val)

    def flush(self):
        ops = self.ops
        n = len(ops)
        self.nops += n
        lw, rdc, rdd = {}, {}, {}
        deps = [None] * n
        dlist = {e: [] for e in self.ENG}
        for i, o in enumerate(ops):
            d = set()
            for k in o["reads"]:
                w = lw.get(k)
                if w is not None:
                    d.add(w)
            for k in o["writes"]:
                w = lw.get(k)
                if w is not None:
                    d.add(w)
                d.update(rdc.get(k, {}).values())
                d.update(rdd.get(k, ()))
            for k in o["reads"]:
                if o["dma"]:
                    rdd.setdefault(k, []).append(i)
                else:
                    rdc.setdefault(k, {})[o["eng"]] = i
            for k in o["writes"]:
                lw[k] = i
                rdc[k] = {}
                rdd[k] = []
            if o["dma"]:
                q = o["eng"]
                o["dn"] = self.dcount[q]
                self.dcount[q] += 1
                lst = dlist[q]
                if len(lst) >= NDMASEM:
                    d.add(lst[len(lst) - NDMASEM])
                lst.append(i)
            d.discard(i)
            if o["eng"] == "pe" and not o["dma"]:
                d = {j for j in d if not (ops[j]["eng"] == "pe" and not ops[j]["dma"])}
            deps[i] = d
        signaling = [False] * n
        for i in range(n):
            for j in deps[i]:
                signaling[j] = True
        last = {}
        for i, o in enumerate(ops):
            if not o["dma"] and not o.get("nosig"):
                last[o["eng"]] = i
        for e, i in last.items():
            signaling[i] = True
        for i, o in enumerate(ops):
            if not o["dma"] and signaling[i]:
                c = self.ccount[o["eng"]]
                o["sig"] = (c // EPOCH, c % EPOCH + 1)
                self.ccount[o["eng"]] = c + 1
        for i, o in enumerate(ops):
            e = o["eng"]
            need = {}
            for j in deps[i]:
                p = ops[j]
                if p["dma"]:
                    key = ("d", p["eng"], p["dn"] % NDMASEM)
                    val = 16 * (p["dn"] // NDMASEM + 1)
                    sem = self._dsem(p["eng"], p["dn"] % NDMASEM)
                else:
                    ep, cv = p["sig"]
                    key = ("c", p["eng"], ep)
                    val = cv
                    sem = self._csem(p["eng"], ep)
                if need.get(key, (None, 0))[1] < val:
                    need[key] = (sem, val)
            for key, (sem, val) in need.items():
                self._wait(e, key, sem, val)
            ins = o["fn"]()
            if o["dma"]:
                ins.then_inc(self._dsem(e, o["dn"] % NDMASEM), 16)
            elif signaling[i]:
                ep, cv = o["sig"]
                ins.then_inc(self._csem(e, ep), 1)
        for e in self.ENG:
            for e2, i in last.items():
                ep, cv = ops[i]["sig"]
                self._wait(e, ("c", e2, ep), self._csem(e2, ep), cv)
            for q in self.ENG:
                dc = self.dcount[q]
                for k in range(min(NDMASEM, dc)):
                    lastdn = ((dc - 1 - k) // NDMASEM) * NDMASEM + k
                    self._wait(e, ("d", q, k), self._dsem(q, k), 16 * (lastdn // NDMASEM + 1))
        self.ops = []


_UN = [0]


def _un(name):
    _UN[0] += 1
    return f"{name}_{_UN[0]}"


def _kl(k):
    return list(k) if isinstance(k, list) else [k]


def hk(blk):
    return [("hacc", blk, 0), ("hacc", blk, 1)]


class Rot:
    def __init__(self, tiles, name):
        self.tiles = tiles
        self.name = name
        self.i = 0

    def next(self):
        j = self.i % len(self.tiles)
        self.i += 1
        return self.tiles[j], (self.name, j)


def build(stop_after=99, dbg=False, sub=9):
    nc = bass.Bass("TRN2", target_bir_lowering=False)

    def din(name, shape):
        return nc.dram_tensor(name, list(shape), F32, kind="ExternalInput").ap()

    x = din("x", [S, D])
    p = din("p", [2, S, 256])
    norm_mix = din("norm_mix", [2, D])
    norm_ffn = din("norm_ffn", [2, D])
    norm_pl = din("norm_pl", [2, D])
    final_norm = din("final_norm", [D])
    w_in_even = din("w_in_even", [1, D, 3072])
    conv_w_even = din("conv_w_even", [1, 3, 512])
    w_out_even = din("w_out_even", [1, 1024, D])
    w_in_odd = din("w_in_odd", [1, D, 4096])
    g_v_odd = din("g_v_odd", [1, 2048])
    w_s_odd = din("w_s_odd", [1, 8, 128, 128])
    b_s_odd = din("b_s_odd", [1, 8, 128])
    w_out_odd = din("w_out_odd", [1, 2048, D])
    router_c = din("router_c", [2, D, 4])
    router_c_b = din("router_c_b", [2, 4])
    router_f = din("router_f", [2, D, 32])
    router_f_b = din("router_f_b", [2, 32])
    moe_w1 = din("moe_w1", [2, 32, D, 256])
    moe_w3 = din("moe_w3", [2, 32, D, 256])
    moe_w2 = din("moe_w2", [2, 32, 256, D])
    w_pe = din("w_pe", [2, 256, D])
    w_pg = din("w_pg", [2, D, D])
    out = nc.dram_tensor("out", [S, D], F32, kind="ExternalOutput").ap()
    mixT = nc.dram_tensor("mixT", [8, 128, S], BF16, kind="ExternalOutput" if dbg else "Internal").ap()
    XS = nc.dram_tensor("XS", [24576, 1024], BF16, kind="Internal").ap()
    YS = nc.dram_tensor("YS", [24576, 1024], F32, kind="Internal").ap()
    hA = nc.dram_tensor("hA", [S, D], F32, kind="ExternalOutput" if dbg else "Internal").ap()
    hB = nc.dram_tensor("hB", [S, D], F32, kind="ExternalOutput" if dbg else "Internal").ap()

    with ExitStack() as gst:
        P = Prog(nc, gst)

        def gsb(name, shape, dt):
            return gst.enter_context(nc.sbuf_tensor(_un(name), shape, dt))

        pb = [gst.enter_context(nc.psum_tensor(f"pb{i}", [128, 512], F32)) for i in range(7)]
        ptb = gst.enter_context(nc.psum_tensor("ptb", [128, 8, 128], BF16))

        identf = gsb("identf", [128, 128], F32)
        ident = gsb("ident", [128, 128], BF16)
        ntri = gsb("ntri", [128, 128], BF16)
        nones = gsb("nones", [128, 128], BF16)
        zeros = gsb("zeros", [128, 128], BF16)
        nmask = gsb("nmask", [128, 128], BF16)
        ones1 = gsb("ones1", [1, 128], BF16)
        pones = gsb("pones", [128, 128], BF16)
        lstrict = gsb("lstrict", [128, 128], BF16)
        lsf = gsb("lsf", [128, 128], F32)
        tmpf = gsb("tmpf", [128, 128], F32)
        P.op("pool", lambda: nc.gpsimd.memset(identf[:], 1.0), writes=["identf"])
        P.op("pool", lambda: nc.gpsimd.affine_select(out=identf[:], in_=identf[:], pattern=[[-1, 128]],
                                                      compare_op=ALU.is_equal, fill=0.0, base=0, channel_multiplier=1),
             reads=["identf"], writes=["identf"])
        P.op("dve", lambda: nc.vector.tensor_copy(out=ident[:], in_=identf[:]), reads=["identf"], writes=["ident"])
        P.op("pool", lambda: nc.gpsimd.memset(tmpf[:], -1.0), writes=["tmpf"])
        P.op("dve", lambda: nc.vector.tensor_copy(out=nones[:], in_=tmpf[:]), reads=["tmpf"], writes=["nones"])
        P.op("pool", lambda: nc.gpsimd.affine_select(out=tmpf[:], in_=tmpf[:], pattern=[[-1, 128]],
                                                      compare_op=ALU.is_ge, fill=0.0, base=0, channel_multiplier=1),
             reads=["tmpf", "nones"], writes=["tmpf"])
        P.op("dve", lambda: nc.vector.tensor_copy(out=ntri[:], in_=tmpf[:]), reads=["tmpf"], writes=["ntri"])
        P.op("dve", lambda: nc.vector.tensor_scalar(out=nmask[:], in0=tmpf[:], scalar1=30000.0, scalar2=None, op0=ALU.mult),
             reads=["tmpf"], writes=["nmask"])
        P.op("pool", lambda: nc.gpsimd.memset(zeros[:], 0.0), writes=["zeros"])
        P.op("pool", lambda: nc.gpsimd.memset(ones1[:], 1.0), writes=["ones1"])
        P.op("pool", lambda: nc.gpsimd.memset(pones[:], 1.0), writes=["pones"])
        P.op("pool", lambda: nc.gpsimd.memset(lsf[:], 1.0), writes=["lsf"])
        P.op("pool", lambda: nc.gpsimd.affine_select(out=lsf[:], in_=lsf[:], pattern=[[1, 128]],
                                                      compare_op=ALU.is_gt, fill=0.0, base=0, channel_multiplier=-1),
             reads=["lsf"], writes=["lsf"])
        P.op("dve", lambda: nc.vector.tensor_copy(out=lstrict[:], in_=lsf[:]), reads=["lsf"], writes=["lstrict"])
        zrow = gsb("zrow", [128, 4, 1024], BF16)
        P.op("pool", lambda: nc.gpsimd.memset(zrow[:], 0.0), writes=["zrow"])
        P.flush()
        for zi in range(24576 // 512):
            P.dma("pool", lambda zi=zi: nc.gpsimd.dma_start(out=XS[zi * 512:(zi + 1) * 512, :].rearrange("(p a) d -> p a d", a=4), in_=zrow[:]),
                  reads=["zrow"], writes=["XS"])

        def norm_rstd(P, src, skey, width, junk, jkey, ss, sskey):
            P.op("act", lambda: nc.scalar.activation(out=junk, in_=src, func=AF.Square, accum_out=ss),
                 reads=_kl(skey), writes=[jkey, sskey])
            P.op("act", lambda: nc.scalar.activation(out=ss, in_=ss, func=AF.Ln, scale=1.0 / width, bias=epsb[:]),
                 reads=[sskey, "epsb"], writes=[sskey])
            P.op("act", lambda: nc.scalar.activation(out=ss, in_=ss, func=AF.Exp, scale=-0.5),
                 reads=[sskey], writes=[sskey])

        epsb = gsb("epsb", [128, 1], F32)
        P.op("pool", lambda: nc.gpsimd.memset(epsb[:], EPS), writes=["epsb"])

        njunk = Rot([gsb(f"njunk{i}", [128, 1024], BF16) for i in range(1)], "njunk")
        nss = Rot([gsb(f"nss{i}", [128, 1], F32) for i in range(4)], "nss")
        nxn = Rot([gsb(f"nxn{i}", [128, 1024], BF16) for i in range(2)], "nxn")

        def norm_T(P, src, skey, gB, gkey, dstT, dkey, cp_eng="act"):
            junk, jk = njunk.next()
            ss, ssk = nss.next()
            xn, xnk = nxn.next()
            norm_rstd(P, src, skey, 1024, junk[:], jk, ss[:], ssk)
            P.op("dve", lambda: nc.vector.scalar_tensor_tensor(out=xn[:], in0=src, scalar=ss[:], in1=gB,
                                                                 op0=ALU.mult, op1=ALU.mult),
                 reads=_kl(skey) + [ssk, gkey], writes=[xnk])
            for k in range(8):
                P.op("pe", lambda k=k: nc.tensor.transpose(out=ptb[:, k, :], in_=xn[:, k * 128:(k + 1) * 128], identity=ident[:]),
                     reads=[xnk, "ident"], writes=["ptb"])
            if cp_eng == "act":
                P.op("act", lambda: nc.scalar.copy(out=dstT, in_=ptb[:]), reads=["ptb"], writes=[dkey])
            else:
                P.op("dve", lambda: nc.vector.tensor_copy(out=dstT, in_=ptb[:]), reads=["ptb"], writes=[dkey])
            return ss, ssk

        def load_gB(P, dst, key, src_row):
            P.dma("sp", lambda: nc.sync.dma_start(out=dst, in_=src_row.partition_broadcast(128)), writes=[key])

        with ExitStack() as st:
            def sb(name, shape, dt):
                return st.enter_context(nc.sbuf_tensor(_un(name), shape, dt))
            xnT = sb("xnT", [128, 8, S], BF16)
            gB0 = sb("gB0", [128, 1024], F32)
            load_gB(P, gB0[:], "gB0", norm_mix[0])
            xt_rot = Rot([sb(f"xt{i}", [128, 1024], F32) for i in range(2)], "xt")
            for b in range(NB):
                xt, xk = xt_rot.next()
                P.dma("sp", lambda xt=xt, b=b: nc.sync.dma_start(out=xt[:], in_=x[b * 128:(b + 1) * 128, :]), writes=[xk])
                norm_T(P, xt[:], xk, gB0[:], "gB0", xnT[:, :, b * 128:(b + 1) * 128], ("xnT", b))
            xnT_keys = lambda tc: [("xnT", 4 * tc + i) for i in range(4)]

            P.flush()
            st2 = ExitStack()
            st2.__enter__()
            sb_outer = sb

            def sb(name, shape, dt):
                return st2.enter_context(nc.sbuf_tensor(_un(name), shape, dt))
            cw = sb("cw", [128, 4, 3], F32)
            for fc in range(4):
                P.dma("sp", lambda fc=fc: nc.sync.dma_start(
                    out=cw[:, fc, :], in_=conv_w_even[0, :, fc * 128:(fc + 1) * 128].rearrange("w f -> f w"),
                    allow_slow_non_contiguous=True), writes=["cw"])
            wc_rot = Rot([sb(f"wc{i}", [128, 3, 8, 128], BF16) for i in range(2)], "wc")
            z_rot = Rot([sb(f"z{i}", [128, S + 2], F32) for i in range(2)], "z")
            hs_rot = Rot([sb(f"hs{i}", [128, 512], F32) for i in range(2)], "hs")
            acc_rot = Rot([sb(f"acc{i}", [128, 512], F32) for i in range(2)], "acc")
            ya_rot = Rot([sb(f"ya{i}", [128, 512], BF16) for i in range(2)], "ya")
            w_in0 = w_in_even[0].rearrange("(k p) n -> p k n", p=128)
            bank = Rot(pb[0:6], "pb")
            for fc in range(4):
                wc, wck = wc_rot.next()
                for j in range(3):
                    P.dma("pool", lambda wc=wc, j=j, fc=fc: nc.gpsimd.dma_start(
                        out=wc[:, j, :, :], in_=w_in0[:, :, j * 512 + fc * 128: j * 512 + (fc + 1) * 128]),
                        writes=[(wck, j)])
                z, zk = z_rot.next()
                P.op("pool", lambda z=z: nc.gpsimd.memset(z[:, 0:2], 0.0), writes=[(zk, -1)])
                for tc in range(8):
                    pp = []
                    for j in range(3):
                        pt, pk = bank.next()
                        for k in range(8):
                            P.op("pe", lambda pt=pt, wc=wc, j=j, k=k, tc=tc: nc.tensor.matmul(
                                pt[:], lhsT=wc[:, j, k, :], rhs=xnT[:, k, tc * 512:(tc + 1) * 512],
                                start=(k == 0), stop=(k == 7)),
                                reads=[(wck, j)] + xnT_keys(tc), writes=[pk])
                        pp.append((pt, pk))
                    (ph, phk), (pgb, pgbk), (pgc, pgck) = pp
                    hs, hsk = hs_rot.next()
                    acc, acck = acc_rot.next()
                    ya, yak = ya_rot.next()
                    P.op("act", lambda hs=hs, ph=ph: nc.scalar.copy(out=hs[:], in_=ph[:]), reads=[phk], writes=[hsk])
                    P.op("dve", lambda z=z, tc=tc, pgc=pgc, hs=hs: nc.vector.tensor_tensor(
                        out=z[:, 2 + tc * 512: 2 + (tc + 1) * 512], in0=pgc[:], in1=hs[:], op=ALU.mult),
                        reads=[pgck, hsk], writes=[(zk, tc)])
                    zr = [(zk, tc), (zk, tc - 1)]
                    P.op("dve", lambda acc=acc, z=z, tc=tc, fc=fc: nc.vector.tensor_scalar(
                        out=acc[:], in0=z[:, tc * 512: tc * 512 + 512], scalar1=cw[:, fc, 0:1], scalar2=None, op0=ALU.mult),
                        reads=zr + ["cw"], writes=[acck])
                    for wv in (1, 2):
                        P.op("dve", lambda acc=acc, z=z, tc=tc, fc=fc, wv=wv: nc.vector.scalar_tensor_tensor(
                            out=acc[:], in0=z[:, tc * 512 + wv: tc * 512 + wv + 512], scalar=cw[:, fc, wv:wv + 1],
                            in1=acc[:], op0=ALU.mult, op1=ALU.add),
                            reads=zr + ["cw", acck], writes=[acck])
                    P.op("dve", lambda ya=ya, acc=acc, pgb=pgb: nc.vector.tensor_tensor(
                        out=ya[:], in0=pgb[:], in1=acc[:], op=ALU.mult), reads=[pgbk, acck], writes=[yak])
                    P.dma("sp", lambda ya=ya, fc=fc, tc=tc: nc.sync.dma_start(
                        out=mixT[fc, :, tc * 512:(tc + 1) * 512], in_=ya[:]), reads=[yak], writes=[("mixT", fc, tc)])
            P.flush()
            st2.__exit__(None, None, None)
            sb = sb_outer
            if stop_after >= 2:
                wq_rot = Rot([sb(f"wq{i}", [128, 3, 8, 128], BF16) for i in range(2)], "wq")
                qT_rot = Rot([sb(f"qT{i}", [128, S], BF16) for i in range(2)], "qT")
                kT_rot = Rot([sb(f"kT{i}", [128, S], BF16) for i in range(2)], "kT")
                v_rot = Rot([sb(f"v{i}", [128, NB, 128], BF16) for i in range(2)], "v")
                yb_rot = Rot([sb(f"yb{i}", [128, S], BF16) for i in range(2)], "yb")
                e_rot = Rot([sb(f"e{i}", [128, 512], F32) for i in range(3)], "e")
                sp_rot = Rot([sb(f"sp{i}", [128, 512], BF16) for i in range(3)], "sp")
                a_rot = Rot([sb(f"a{i}", [128, 512], BF16) for i in range(3)], "a")
                r32_rot = Rot([sb(f"r32{i}", [128, 512], F32) for i in range(2)], "r32")
                r16_rot = Rot([sb(f"r16{i}", [128, 512], BF16) for i in range(3)], "r16")
                zb = Rot(pb[0:3], "pb")
                ob = Rot(pb[3:7], "pbo")
                pjb = Rot(pb[3:7], "pbo")

                def proj(hp):
                    wq, wqk = wq_rot.next()
                    for j in range(3):
                        P.dma("pool", lambda wq=wq, j=j, hp=hp: nc.gpsimd.dma_start(
                            out=wq[:, j, :, :], in_=w_in0[:, :, 1536 + j * 512 + hp * 128: 1536 + j * 512 + (hp + 1) * 128]),
                            writes=[(wqk, j)])
                    qT, qk = qT_rot.next()
                    kT, kk = kT_rot.next()
                    v, vk = v_rot.next()
                    for tc in range(8):
                        for j, (dst, dk, sc) in enumerate(((qT, qk, 0.125), (kT, kk, 1.0))):
                            pt, pk = pjb.next()
                            for k in range(8):
                                P.op("pe", lambda pt=pt, wq=wq, j=j, k=k, tc=tc: nc.tensor.matmul(
                                    pt[:], lhsT=wq[:, j, k, :], rhs=xnT[:, k, tc * 512:(tc + 1) * 512],
                                    start=(k == 0), stop=(k == 7)),
                                    reads=[(wqk, j)] + xnT_keys(tc), writes=[pk])
                            P.op("dve", lambda dst=dst, pt=pt, tc=tc, sc=sc: nc.vector.tensor_scalar(
                                out=dst[:, tc * 512:(tc + 1) * 512], in0=pt[:], scalar1=sc, scalar2=None, op0=ALU.mult),
                                reads=[pk], writes=[(dk, tc)])
                        pt, pk = pjb.next()
                        for tb in range(4):
                            b = tc * 4 + tb
                            for k in range(8):
                                P.op("pe", lambda pt=pt, wq=wq, k=k, b=b, tb=tb: nc.tensor.matmul(
                                    pt[:, tb * 128:(tb + 1) * 128], lhsT=xnT[:, k, b * 128:(b + 1) * 128], rhs=wq[:, 2, k, :],
                                    start=(k == 0), stop=(k == 7)),
                                    reads=[(wqk, 2), ("xnT", b)], writes=[pk])
                        P.op("dve", lambda v=v, pt=pt, tc=tc: nc.vector.tensor_copy(
                            out=v[:, tc * 4:(tc + 1) * 4, :], in_=pt[:].rearrange("p (b d) -> p b d", b=4)),
                            reads=[pk], writes=[(vk, tc)])
                    return (qT, qk, kT, kk, v, vk)

                def attention(hp, qkv):
                    qT, qk, kT, kk, v, vk = qkv
                    yb, ybk = yb_rot.next()
                    tiles = []
                    for hh in range(2):
                        for qc in range(8):
                            nkb = 4 * qc + 4
                            for ii, kb in enumerate(range(nkb - 1, -1, -1)):
                                jd = kb - 4 * qc
                                c0 = jd * 128 if jd >= 0 else 0
                                tiles.append(dict(hh=hh, qc=qc, kb=kb, c0=c0, diag=(jd >= 0), first=(ii == 0),
                                                  last=(kb == 0)))
                    nt = len(tiles)
                    st_ = [dict() for _ in range(nt)]

                    def stage_qk(i):
                        t = tiles[i]
                        po = t["hh"] * 64
                        zt, zk_ = zb.next()
                        st_[i]["z"] = (zt, zk_)
                        c0 = t["c0"]
                        qc, kb = t["qc"], t["kb"]
                        P.op("pe", lambda: nc.tensor.matmul(
                            zt[:, c0:512], lhsT=kT[po:po + 64, kb * 128:(kb + 1) * 128],
                            rhs=qT[po:po + 64, qc * 512 + c0:(qc + 1) * 512], start=True, stop=False),
                            reads=[(kk, kb // 4), (qk, qc)], writes=[zk_])
                        if t["diag"]:
                            P.op("pe", lambda: nc.tensor.matmul(
                                zt[:, c0:c0 + 128], lhsT=ident[:], rhs=nmask[:], start=False, stop=False),
                                reads=["ident", "nmask"], writes=[zk_])
                        et, ek = e_rot.next()
                        spt, spk = sp_rot.next()
                        st_[i]["sp"] = (spt, spk)
                        P.op("act", lambda: nc.scalar.activation(out=et[:, c0:512], in_=zt[:, c0:512], func=AF.Exp),
                             reads=[zk_], writes=[ek])
                        P.op("act", lambda: nc.scalar.activation(out=spt[:, c0:512], in_=et[:, c0:512], func=AF.Ln, bias=1.0),
                             reads=[ek], writes=[spk])

                    def stage_cum(i):
                        t = tiles[i]
                        zt, zk_ = st_[i]["z"]
                        spt, spk = st_[i]["sp"]
                        c0 = t["c0"]
                        if t["first"]:
                            r32, r32k = r32_rot.next()
                            st_[i]["r32"] = (r32, r32k)
                            P.op("pool", lambda: nc.gpsimd.memset(r32[:], 0.0), writes=[r32k])
                        else:
                            st_[i]["r32"] = st_[i - 1]["r32"]
                            r32, r32k = st_[i]["r32"]
                        P.op("pe", lambda: nc.tensor.matmul(zt[:, c0:512], lhsT=ntri[:], rhs=spt[:, c0:512],
                                                            start=False, stop=t["first"]),
                             reads=["ntri", spk], writes=[zk_])
                        if not t["first"]:
                            r16, r16k = st_[i]["r16"]
                            P.op("pe", lambda: nc.tensor.matmul(zt[:, c0:512], lhsT=nones[:], rhs=r16[:, c0:512],
                                                                start=False, stop=True),
                                 reads=["nones"] + st_[i]["r16keys"], writes=[zk_])
                        if not t["last"]:
                            c1 = tiles[i + 1]["c0"]
                            r16n, r16nk = r16_rot.next()
                            st_[i + 1]["r16"] = (r16n, r16nk)
                            wk = [r16nk]
                            if c1 < c0:
                                P.op("pool", lambda: nc.gpsimd.memset(r16n[:, c1:c0], 0.0), writes=[r16nk])
                            P.op("dve", lambda: nc.vector.tensor_tensor(out=r16n[:, c0:512], in0=r32[:, c0:512],
                                                                       in1=spt[:, c0:512], op=ALU.add),
                                 reads=[r32k, spk], writes=[r16nk])
                            P.op("dve", lambda: nc.vector.tensor_tensor(out=r32[:, c0:512], in0=r32[:, c0:512],
                                                                       in1=spt[:, c0:512], op=ALU.add),
                                 reads=[r32k, spk], writes=[r32k])
                            st_[i + 1]["r16keys"] = wk
                        at, ak = a_rot.next()
                        st_[i]["a"] = (at, ak)
                        P.op("act", lambda: nc.scalar.activation(out=at[:, c0:512], in_=zt[:, c0:512], func=AF.Exp),
                             reads=[zk_], writes=[ak])

                    def stage_av(i):
                        t = tiles[i]
                        at, ak = st_[i]["a"]
                        c0 = t["c0"]
                        kb = t["kb"]
                        po = t["hh"] * 64
                        if t["first"]:
                            ot, ok = ob.next()
                            st_[i]["o"] = (ot, ok)
                            P.op("pe", lambda: nc.tensor.matmul(ot[:], lhsT=zeros[:], rhs=qT[:, 0:512], start=True, stop=False),
                                 reads=["zeros", (qk, 0)], writes=[ok])
                        else:
                            st_[i]["o"] = st_[i - 1]["o"]
                            ot, ok = st_[i]["o"]
                        P.op("pe", lambda: nc.tensor.matmul(ot[:, c0:512], lhsT=v[:, kb, :], rhs=at[:, c0:512],
                                                            start=False, stop=t["last"]),
                             reads=[(vk, kb // 4), ak], writes=[ok])
                        if t["last"]:
                            qc = t["qc"]
                            P.op("dve", lambda: nc.vector.tensor_copy(out=yb[po:po + 64, qc * 512:(qc + 1) * 512],
                                                                      in_=ot[po:po + 64, :]),
                                 reads=[ok], writes=[(ybk, qc, t["hh"])])
                        st_[i].pop("z", None)

                    stage_qk(0)
                    for i in range(nt):
                        if i + 1 < nt:
                            stage_qk(i + 1)
                        stage_cum(i)
                        if i >= 1:
                            stage_av(i - 1)
                    stage_av(nt - 1)
                    for qc in range(8):
                        P.dma("sp", lambda qc=qc: nc.sync.dma_start(out=mixT[4 + hp, :, qc * 512:(qc + 1) * 512],
                                                                    in_=yb[:, qc * 512:(qc + 1) * 512]),
                              reads=[(ybk, qc, 0), (ybk, qc, 1)], writes=[("mixT", 4 + hp, qc)])

                nhp = 4 if stop_after >= 3 else 1
                qkvs = {0: proj(0)}
                for hp in range(nhp):
                    if hp + 1 < nhp:
                        qkvs[hp + 1] = proj(hp + 1)
                    attention(hp, qkvs.pop(hp))
            P.flush()

        if stop_after >= 4:
            with ExitStack() as st:
                def sb(name, shape, dt):
                    return st.enter_context(nc.sbuf_tensor(_un(name), shape, dt))
                wo = sb("wo", [128, 8, 1024], BF16)
                P.dma("pool", lambda: nc.gpsimd.dma_start(out=wo[:], in_=w_out_even[0].rearrange("(k p) n -> p k n", p=128)),
                      writes=["wo"])
                mx_rot = Rot([sb(f"mx{i}", [128, 8, 512], BF16) for i in range(2)], "mx")
                xt_rot = Rot([sb(f"xt{i}", [128, 1024], F32) for i in range(3)], "xt")
                bank = Rot(pb[0:6], "pb")
                for tc in range(8):
                    mx, mxk = mx_rot.next()
                    P.dma("sp", lambda mx=mx, tc=tc: nc.sync.dma_start(
                        out=mx[:], in_=mixT[:, :, tc * 512:(tc + 1) * 512].rearrange("c p t -> p c t")), writes=[mxk])
                    for tb in range(4):
                        b = tc * 4 + tb
                        xt, xk = xt_rot.next()
                        P.dma("sp", lambda xt=xt, b=b: nc.sync.dma_start(out=xt[:], in_=x[b * 128:(b + 1) * 128, :]), writes=[xk])
                        for half in range(2):
                            pt, pk = bank.next()
                            for c in range(8):
                                P.op("pe", lambda pt=pt, mx=mx, c=c, tb=tb, half=half: nc.tensor.matmul(
                                    pt[:], lhsT=mx[:, c, tb * 128:(tb + 1) * 128], rhs=wo[:, c, half * 512:(half + 1) * 512],
                                    start=(c == 0), stop=(c == 7)), reads=[mxk, "wo"], writes=[pk])
                            P.op("dve", lambda xt=xt, pt=pt, half=half: nc.vector.tensor_tensor(
                                out=xt[:, half * 512:(half + 1) * 512], in0=pt[:], in1=xt[:, half * 512:(half + 1) * 512], op=ALU.add),
                                reads=[pk, xk], writes=[xk])
                        P.dma("sp", lambda xt=xt, b=b: nc.sync.dma_start(out=hA[b * 128:(b + 1) * 128, :], in_=xt[:]),
                              reads=[xk], writes=[("hA", b)])
                P.flush()

        def moe_pl_phase(l, hin, hout, final):
            with ExitStack() as st:
                def sb(name, shape, dt):
                    return st.enter_context(nc.sbuf_tensor(_un(name), shape, dt))
                NBS = 16
                hacc = sb("hacc", [128, NBS, 1024], F32)
                xT = sb("xT", [128, 8, NBS * 128], BF16)
                gBf = sb("gBf", [128, 1024], F32)
                gBp = sb("gBp", [128, 1024], F32)
                load_gB(P, gBf[:], "gBf", norm_ffn[l])
                load_gB(P, gBp[:], "gBp", norm_pl[l])
                if final:
                    gBo = sb("gBo", [128, 1024], F32)
                    load_gB(P, gBo[:], "gBo", final_norm)
                wr = sb("wr", [128, 8, 36], BF16)
                P.dma("pool", lambda: nc.gpsimd.dma_start(out=wr[:, :, 0:4], in_=router_c[l].rearrange("(k p) n -> p k n", p=128)),
                      writes=["wr"])
                P.dma("pool", lambda: nc.gpsimd.dma_start(out=wr[:, :, 4:36], in_=router_f[l].rearrange("(k p) n -> p k n", p=128)),
                      writes=["wr"])
                rb = sb("rb", [128, 36], F32)
                P.dma("sp", lambda: nc.sync.dma_start(out=rb[:, 0:4], in_=router_c_b[l].partition_broadcast(128)), writes=["rb"])
                P.dma("sp", lambda: nc.sync.dma_start(out=rb[:, 4:36], in_=router_f_b[l].partition_broadcast(128)), writes=["rb"])
                wpg = sb("wpg", [128, 8, 1024], BF16)
                wpe = sb("wpe", [128, 2, 1024], BF16)
                P.dma("pool", lambda: nc.gpsimd.dma_start(out=wpg[:], in_=w_pg[l].rearrange("(k p) n -> p k n", p=128)), writes=["wpg"])
                P.dma("pool", lambda: nc.gpsimd.dma_start(out=wpe[:], in_=w_pe[l].rearrange("(k p) n -> p k n", p=128)), writes=["wpe"])
                w13_rot = Rot([sb(f"w13_{i}", [128, 2, 8, 256], BF16) for i in range(2)], "w13")
                w2_rot = Rot([sb(f"w2_{i}", [128, 2, 1024], BF16) for i in range(2)], "w2")
                lg = sb("lg", [128, NBS, 36], F32)
                comb = sb("comb", [128, NBS, 32], F32)
                r_mx = sb("r_mx", [128, NBS], F32)
                r_gm = sb("r_gm", [128, NBS, 4], F32)
                r_ec = sb("r_ec", [128, NBS, 4], F32)
                r_pg = sb("r_pg", [128, NBS], F32)
                r_t = sb("r_t", [128, NBS, 4, 8], F32)
                r_lfs = sb("r_lfs", [128, NBS, 8], F32)
                r_t8 = sb("r_t8", [128, NBS, 8], F32)
                r_sel = sb("r_sel", [128, NBS, 8], F32)
                r_ex = sb("r_ex", [128, NBS, 8], F32)
                r_d = sb("r_d", [128, NBS], F32)
                s_rot = Rot([sb(f"s{i}", [128, 512], F32) for i in range(2)], "s")
                he_rot = Rot([sb(f"he{i}", [128, 2, 512], BF16) for i in range(2)], "he")
                pblk_rot = Rot([sb(f"pblk{i}", [128, 256], F32) for i in range(2)], "pblk")
                pb16_rot = Rot([sb(f"pb16{i}", [128, 256], BF16) for i in range(2)], "pb16")
                pT_rot = Rot([sb(f"pT{i}", [128, 2, 128], BF16) for i in range(2)], "pT")
                hnT_rot = Rot([sb(f"hnT{i}", [128, 8, 128], BF16) for i in range(2)], "hnT")
                sg_rot = Rot([sb(f"sg{i}", [128, 1024], F32) for i in range(1)], "sg")
                for sc in range(2):
                    hbank = Rot(pb[0:4], "pb")
                    ybank = Rot(pb[4:7], "pby")
                    for blk in range(NBS):
                        b = sc * NBS + blk
                        P.dma("sp", lambda blk=blk, b=b: nc.sync.dma_start(out=hacc[:, blk, :], in_=hin[b * 128:(b + 1) * 128, :]),
                              writes=hk(blk))
                        norm_T(P, hacc[:, blk, :], hk(blk), gBf[:], "gBf", xT[:, :, blk * 128:(blk + 1) * 128], ("xT", blk))
                        pt, pk = ybank.next()
                        for k in range(8):
                            P.op("pe", lambda pt=pt, k=k, blk=blk: nc.tensor.matmul(
                                pt[:, 0:36], lhsT=xT[:, k, blk * 128:(blk + 1) * 128], rhs=wr[:, k, :],
                                start=(k == 0), stop=(k == 7)), reads=[("xT", blk), "wr"], writes=[pk])
                        P.op("dve", lambda pt=pt, blk=blk: nc.vector.tensor_tensor(out=lg[:, blk, :], in0=pt[:, 0:36], in1=rb[:], op=ALU.add),
                             reads=[pk, "rb"], writes=["lg"])
                    lc = lg[:, :, 0:4]
                    lf = lg[:, :, 4:36].rearrange("p b (g e) -> p b g e", g=4)
                    P.op("dve", lambda: nc.vector.tensor_reduce(out=r_mx[:], in_=lc, axis=AX.X, op=ALU.max), reads=["lg"], writes=["r_mx"])
                    P.op("dve", lambda: nc.vector.tensor_tensor(out=r_gm[:], in0=lc, in1=r_mx[:].unsqueeze(2).to_broadcast([128, NBS, 4]),
                                                                op=ALU.is_ge), reads=["lg", "r_mx"], writes=["r_gm"])
                    P.op("dve", lambda: nc.vector.tensor_tensor(out=r_ec[:], in0=lc, in1=r_mx[:].unsqueeze(2).to_broadcast([128, NBS, 4]),
                                                                op=ALU.subtract), reads=["lg", "r_mx"], writes=["r_ec"])
                    P.op("act", lambda: nc.scalar.activation(out=r_ec[:], in_=r_ec[:], func=AF.Exp), reads=["r_ec"], writes=["r_ec"])
                    P.op("dve", lambda: nc.vector.tensor_reduce(out=r_pg[:], in_=r_ec[:], axis=AX.X, op=ALU.add), reads=["r_ec"], writes=["r_pg"])
                    P.op("dve", lambda: nc.vector.tensor_tensor(out=r_t[:], in0=lf, in1=r_gm[:].unsqueeze(3).to_broadcast([128, NBS, 4, 8]),
                                                                op=ALU.mult), reads=["lg", "r_gm"], writes=["r_t"])
                    P.op("dve", lambda: nc.vector.tensor_reduce(out=r_lfs[:], in_=r_t[:].rearrange("p b g e -> p b e g"), axis=AX.X, op=ALU.add),
                         reads=["r_t"], writes=["r_lfs"])
                    for blk in range(NBS):
                        P.op("dve", lambda blk=blk: nc.vector.max(out=r_t8[:, blk, :], in_=r_lfs[:, blk, :]), reads=["r_lfs"], writes=["r_t8"])
                    l1b = r_t8[:, :, 0:1].to_broadcast([128, NBS, 8])
                    l2b = r_t8[:, :, 1:2].to_broadcast([128, NBS, 8])
                    P.op("dve", lambda: nc.vector.tensor_tensor(out=r_sel[:], in0=r_lfs[:], in1=l2b, op=ALU.is_ge), reads=["r_lfs", "r_t8"], writes=["r_sel"])
                    P.op("dve", lambda: nc.vector.tensor_tensor(out=r_ex[:], in0=r_lfs[:], in1=l1b, op=ALU.subtract), reads=["r_lfs", "r_t8"], writes=["r_ex"])
                    P.op("act", lambda: nc.scalar.activation(out=r_ex[:], in_=r_ex[:], func=AF.Exp), reads=["r_ex"], writes=["r_ex"])
                    P.op("dve", lambda: nc.vector.tensor_tensor(out=r_ex[:], in0=r_ex[:], in1=r_sel[:], op=ALU.mult), reads=["r_ex", "r_sel"], writes=["r_ex"])
                    P.op("dve", lambda: nc.vector.tensor_reduce(out=r_d[:], in_=r_ex[:], axis=AX.X, op=ALU.add), reads=["r_ex"], writes=["r_d"])
                    P.op("dve", lambda: nc.vector.tensor_tensor(out=r_d[:], in0=r_d[:], in1=r_pg[:], op=ALU.mult), reads=["r_d", "r_pg"], writes=["r_d"])
                    P.op("dve", lambda: nc.vector.reciprocal(out=r_d[:], in_=r_d[:]), reads=["r_d"], writes=["r_d"])
                    P.op("dve", lambda: nc.vector.tensor_tensor(out=r_ex[:], in0=r_ex[:], in1=r_d[:].unsqueeze(2).to_broadcast([128, NBS, 8]),
                                                                op=ALU.mult), reads=["r_ex", "r_d"], writes=["r_ex"])
                    P.op("dve", lambda: nc.vector.tensor_tensor(
                        out=comb[:].rearrange("p b (g e) -> p b g e", g=4),
                        in0=r_gm[:].unsqueeze(3).to_broadcast([128, NBS, 4, 8]),
                        in1=r_ex[:].unsqueeze(2).to_broadcast([128, NBS, 4, 8]), op=ALU.mult),
                        reads=["r_gm", "r_ex"], writes=["comb"])
                    wts = {}

                    def load_w(e):
                        w13, w13k = w13_rot.next()
                        w2, w2k = w2_rot.next()
                        P.dma("pool", lambda: nc.gpsimd.dma_start(out=w13[:, 0, :, :], in_=moe_w1[l, e].rearrange("(k p) f -> p k f", p=128)),
                              writes=[(w13k, 0)])
                        P.dma("pool", lambda: nc.gpsimd.dma_start(out=w13[:, 1, :, :], in_=moe_w3[l, e].rearrange("(k p) f -> p k f", p=128)),
                              writes=[(w13k, 1)])
                        P.dma("pool", lambda: nc.gpsimd.dma_start(out=w2[:], in_=moe_w2[l, e].rearrange("(c p) d -> p c d", p=128)),
                              writes=[w2k])
                        wts[e] = (w13, w13k, w2, w2k)

                    units = [(e, tch) for e in range(32) for tch in range(4)]
                    ust = {}

                    def stage_h(u):
                        e, tch = units[u]
                        w13, w13k, w2, w2k = wts[e]
                        he, hek = he_rot.next()
                        ust[u] = (he, hek)
                        for fch in range(2):
                            p1, p1k = hbank.next()
                            p3, p3k = hbank.next()
                            for j, (pt, pk) in enumerate(((p1, p1k), (p3, p3k))):
                                for k in range(8):
                                    P.op("pe", lambda pt=pt, j=j, k=k, fch=fch: nc.tensor.matmul(
                                        pt[:], lhsT=w13[:, j, k, fch * 128:(fch + 1) * 128], rhs=xT[:, k, tch * 512:(tch + 1) * 512],
                                        start=(k == 0), stop=(k == 7)),
                                        reads=[(w13k, j)] + [("xT", 4 * tch + i) for i in range(4)], writes=[pk])
                            s, sk = s_rot.next()
                            P.op("act", lambda s=s, p1=p1: nc.scalar.activation(out=s[:], in_=p1[:], func=AF.Silu), reads=[p1k], writes=[sk])
                            P.op("dve", lambda s=s, p3=p3, fch=fch: nc.vector.tensor_tensor(out=he[:, fch, :], in0=p3[:], in1=s[:], op=ALU.mult),
                                 reads=[p3k, sk], writes=[(hek, fch)])

                    def stage_y(u):
                        e, tch = units[u]
                        w13, w13k, w2, w2k = wts[e]
                        he, hek = ust.pop(u)
                        for tb in range(4):
                            blk = tch * 4 + tb
                            for half in range(2):
                                py, pyk = ybank.next()
                                for fch in range(2):
                                    P.op("pe", lambda py=py, fch=fch, tb=tb, half=half: nc.tensor.matmul(
                                        py[:], lhsT=he[:, fch, tb * 128:(tb + 1) * 128], rhs=w2[:, fch, half * 512:(half + 1) * 512],
                                        start=(fch == 0), stop=(fch == 1)), reads=[(hek, fch), (w2k, fch)], writes=[pyk])
                                P.op("dve", lambda py=py, blk=blk, half=half: nc.vector.scalar_tensor_tensor(
                                    out=hacc[:, blk, half * 512:(half + 1) * 512], in0=py[:], scalar=comb[:, blk, e:e + 1],
                                    in1=hacc[:, blk, half * 512:(half + 1) * 512], op0=ALU.mult, op1=ALU.add),
                                    reads=[pyk, "comb", ("hacc", blk, half)], writes=[("hacc", blk, half)])
                        if tch == 3:
                            wts.pop(e)
                            if e + 2 < 32:
                                load_w(e + 2)

                    load_w(0)
                    load_w(1)
                    nu = len(units)
                    stage_h(0)
                    for u in range(nu):
                        if u + 1 < nu:
                            stage_h(u + 1)
                        stage_y(u)
                    gbank = Rot(pb[0:4], "pb")
                    for blk in range(NBS):
                        b = sc * NBS + blk
                        hnT, hnk = hnT_rot.next()
                        norm_T(P, hacc[:, blk, :], hk(blk), gBp[:], "gBp", hnT[:], hnk)
                        pblk, pblkk = pblk_rot.next()
                        p16, p16k = pb16_rot.next()
                        pT, pTk = pT_rot.next()
                        P.dma("sp", lambda pblk=pblk, b=b: nc.sync.dma_start(out=pblk[:], in_=p[l, b * 128:(b + 1) * 128, :]), writes=[pblkk])
                        P.op("pool", lambda p16=p16, pblk=pblk: nc.gpsimd.tensor_copy(out=p16[:], in_=pblk[:]), reads=[pblkk], writes=[p16k])
                        for k in range(2):
                            P.op("pe", lambda p16=p16, k=k: nc.tensor.transpose(out=ptb[:, k, :], in_=p16[:, k * 128:(k + 1) * 128], identity=ident[:]),
                                 reads=[p16k, "ident"], writes=["ptb"])
                        P.op("act", lambda pT=pT: nc.scalar.copy(out=pT[:], in_=ptb[:, 0:2, :]), reads=["ptb"], writes=[pTk])
                        sg, sgk = sg_rot.next()
                        for half in range(2):
                            pg_, pgk = gbank.next()
                            pe_, pek = gbank.next()
                            for k in range(8):
                                P.op("pe", lambda pg_=pg_, hnT=hnT, k=k, half=half: nc.tensor.matmul(
                                    pg_[:], lhsT=hnT[:, k, :], rhs=wpg[:, k, half * 512:(half + 1) * 512], start=(k == 0), stop=(k == 7)),
                                    reads=[hnk, "wpg"], writes=[pgk])
                            for k in range(2):
                                P.op("pe", lambda pe_=pe_, pT=pT, k=k, half=half: nc.tensor.matmul(
                                    pe_[:], lhsT=pT[:, k, :], rhs=wpe[:, k, half * 512:(half + 1) * 512], start=(k == 0), stop=(k == 1)),
                                    reads=[pTk, "wpe"], writes=[pek])
                            P.op("act", lambda sg=sg, pg_=pg_, half=half: nc.scalar.activation(out=sg[:, half * 512:(half + 1) * 512], in_=pg_[:], func=AF.Sigmoid),
                                 reads=[pgk], writes=[(sgk, half)])
                            P.op("dve", lambda sg=sg, pe_=pe_, half=half: nc.vector.tensor_tensor(
                                out=sg[:, half * 512:(half + 1) * 512], in0=pe_[:], in1=sg[:, half * 512:(half + 1) * 512], op=ALU.mult),
                                reads=[pek, (sgk, half)], writes=[(sgk, half)])
                        P.op("dve", lambda sg=sg, blk=blk: nc.vector.tensor_tensor(out=hacc[:, blk, :], in0=hacc[:, blk, :], in1=sg[:], op=ALU.add),
                             reads=[(sgk, 0), (sgk, 1)] + hk(blk), writes=hk(blk))
                        if final:
                            junk, jk = njunk.next()
                            ss, ssk = nss.next()
                            norm_rstd(P, hacc[:, blk, :], hk(blk), 1024, junk[:], jk, ss[:], ssk)
                            P.op("dve", lambda ss=ss, blk=blk: nc.vector.scalar_tensor_tensor(
                                out=hacc[:, blk, :], in0=hacc[:, blk, :], scalar=ss[:], in1=gBo[:], op0=ALU.mult, op1=ALU.mult),
                                reads=hk(blk) + [ssk, "gBo"], writes=hk(blk))
                        P.dma("sp", lambda blk=blk, b=b: nc.sync.dma_start(out=hout[b * 128:(b + 1) * 128, :], in_=hacc[:, blk, :]),
                              reads=hk(blk), writes=[("hout", b)])
                    P.flush()

        def sparse_moe_pl_phase(l, hin, hout, final):
            TS = 512
            NT = 48
            NA = TS // 128
            with ExitStack() as st:
                def sb(name, shape, dt):
                    return st.enter_context(nc.sbuf_tensor(_un(name), shape, dt))
                gBf = sb("gBf", [128, 1024], F32)
                gBp = sb("gBp", [128, 1024], F32)
                load_gB(P, gBf[:], "gBf", norm_ffn[l])
                load_gB(P, gBp[:], "gBp", norm_pl[l])
                if final:
                    gBo = sb("gBo", [128, 1024], F32)
                    load_gB(P, gBo[:], "gBo", final_norm)
                wr = sb("wr", [128, 8, 36], BF16)
                P.dma("pool", lambda: nc.gpsimd.dma_start(out=wr[:, :, 0:4], in_=router_c[l].rearrange("(k p) n -> p k n", p=128)),
                      writes=["wr"])
                P.dma("pool", lambda: nc.gpsimd.dma_start(out=wr[:, :, 4:36], in_=router_f[l].rearrange("(k p) n -> p k n", p=128)),
                      writes=["wr"])
                rb = sb("rb", [128, 36], F32)
                P.dma("sp", lambda: nc.sync.dma_start(out=rb[:, 0:4], in_=router_c_b[l].partition_broadcast(128)), writes=["rb"])
                P.dma("sp", lambda: nc.sync.dma_start(out=rb[:, 4:36], in_=router_f_b[l].partition_broadcast(128)), writes=["rb"])
                wpg = sb("wpg", [128, 8, 1024], BF16)
                wpe = sb("wpe", [128, 2, 1024], BF16)
                P.dma("pool", lambda: nc.gpsimd.dma_start(out=wpg[:], in_=w_pg[l].rearrange("(k p) n -> p k n", p=128)), writes=["wpg"])
                P.dma("pool", lambda: nc.gpsimd.dma_start(out=wpe[:], in_=w_pe[l].rearrange("(k p) n -> p k n", p=128)), writes=["wpe"])
                idx1_i = sb("idx1_i", [128, NB], I32)
                idx2_i = sb("idx2_i", [128, NB], I32)
                gate1 = sb("gate1", [128, NB], F32)
                gate2 = sb("gate2", [128, NB], F32)
                widx_i = sb("widx_i", [128, NT], I32)
                pio = sb("pio", [128, 1], F32)
                P.op("pool", lambda: nc.gpsimd.iota(pio[:], pattern=[[0, 1]], base=l * 32 * 128, channel_multiplier=1,
                                                    allow_small_or_imprecise_dtypes=True), writes=["pio"])
                with ExitStack() as st1:
                    def sb1(name, shape, dt):
                        return st1.enter_context(nc.sbuf_tensor(_un(name), shape, dt))
                    xnall = sb1("xnall", [128, NB, 1024], BF16)
                    ht_rot = Rot([sb1(f"ht{i}", [128, 1024], F32) for i in range(2)], "ht")
                    xTb_rot = Rot([sb1(f"xTb{i}", [128, 8, 128], BF16) for i in range(2)], "xTb")
                    lg = sb1("lg", [128, NB, 36], F32)
                    r_mx = sb1("r_mx", [128, NB], F32)
                    r_gm = sb1("r_gm", [128, NB, 4], F32)
                    r_ec = sb1("r_ec", [128, NB, 4], F32)
                    r_pg = sb1("r_pg", [128, NB], F32)
                    r_t = sb1("r_t", [128, NB, 4, 8], F32)
                    r_lfs = sb1("r_lfs", [128, NB, 8], F32)
                    r_t8 = sb1("r_t8", [128, NB, 8], F32)
                    r_sel = sb1("r_sel", [128, NB, 8], F32)
                    r_s1 = sb1("r_s1", [128, NB, 8], F32)
                    r_s2 = sb1("r_s2", [128, NB, 8], F32)
                    r_ex = sb1("r_ex", [128, NB, 8], F32)
                    r_d = sb1("r_d", [128, NB], F32)
                    r_tmp8 = sb1("r_tmp8", [128, NB, 8], F32)
                    M1 = sb1("M1", [128, NB, 32], F32)
                    M2 = sb1("M2", [128, NB, 32], F32)
                    Mb16 = sb1("Mb16", [128, NB, 32], BF16)
                    Rm16 = sb1("Rm16", [128, NB + 1, 32], BF16)
                    rank = sb1("rank", [128, NB, 32], F32)
                    cnt = sb1("cnt", [128, 32], F32)
                    thr16 = sb1("thr16", [128, 16], F32)
                    thr64 = sb1("thr64", [128, NT], F32)
                    cmpA = sb1("cmpA", [128, 32, 16], F32)
                    ntile = sb1("ntile", [128, 32], F32)
                    csA = sb1("csA", [128, 32], F32)
                    csB = sb1("csB", [128, 32], F32)
                    base = sb1("base", [128, 32], F32)
                    cmpB = sb1("cmpB", [128, NT, 32], F32)
                    te_f = sb1("te_f", [128, NT], F32)
                    tail = sb1("tail", [128, NT], F32)
                    tot = sb1("tot", [128, 1], F32)
                    idx_f = sb1("idx_f", [128, NB], F32)
                    tmp32 = sb1("tmp32", [128, NB, 32], F32)
                    ybank = Rot(pb[4:7], "pby")
                    for b in range(NB):
                        ht, htk = ht_rot.next()
                        P.dma("sp", lambda ht=ht, b=b: nc.sync.dma_start(out=ht[:], in_=hin[b * 128:(b + 1) * 128, :]), writes=[htk])
                        junk, jk = njunk.next()
                        ss, ssk = nss.next()
                        norm_rstd(P, ht[:], htk, 1024, junk[:], jk, ss[:], ssk)
                        P.op("dve", lambda ht=ht, ss=ss, b=b: nc.vector.scalar_tensor_tensor(
                            out=xnall[:, b, :], in0=ht[:], scalar=ss[:], in1=gBf[:], op0=ALU.mult, op1=ALU.mult),
                            reads=[htk, ssk, "gBf"], writes=[("xnall", b)])
                        for k in range(8):
                            P.op("pe", lambda k=k, b=b: nc.tensor.transpose(out=ptb[:, k, :], in_=xnall[:, b, k * 128:(k + 1) * 128], identity=ident[:]),
                                 reads=[("xnall", b), "ident"], writes=["ptb"])
                        xTb, xTbk = xTb_rot.next()
                        P.op("act", lambda xTb=xTb: nc.scalar.copy(out=xTb[:], in_=ptb[:]), reads=["ptb"], writes=[xTbk])
                        pt, pk = ybank.next()
                        for k in range(8):
                            P.op("pe", lambda pt=pt, k=k, xTb=xTb: nc.tensor.matmul(
                                pt[:, 0:36], lhsT=xTb[:, k, :], rhs=wr[:, k, :], start=(k == 0), stop=(k == 7)),
                                reads=[xTbk, "wr"], writes=[pk])
                        P.op("dve", lambda pt=pt, b=b: nc.vector.tensor_tensor(out=lg[:, b, :], in0=pt[:, 0:36], in1=rb[:], op=ALU.add),
                             reads=[pk, "rb"], writes=["lg"])
                    lc = lg[:, :, 0:4]
                    lf = lg[:, :, 4:36].rearrange("p b (g e) -> p b g e", g=4)

                    def bc(ap, shape):
                        return ap.to_broadcast(shape)

                    def dv(fn, reads, writes):
                        P.op("dve", fn, reads=reads, writes=writes)
                    dv(lambda: nc.vector.tensor_reduce(out=r_mx[:], in_=lc, axis=AX.X, op=ALU.max), ["lg"], ["r_mx"])
                    dv(lambda: nc.vector.tensor_tensor(out=r_gm[:], in0=lc, in1=bc(r_mx[:].unsqueeze(2), [128, NB, 4]), op=ALU.is_ge), ["lg", "r_mx"], ["r_gm"])
                    dv(lambda: nc.vector.tensor_tensor(out=r_ec[:], in0=lc, in1=bc(r_mx[:].unsqueeze(2), [128, NB, 4]), op=ALU.subtract), ["lg", "r_mx"], ["r_ec"])
                    P.op("act", lambda: nc.scalar.activation(out=r_ec[:], in_=r_ec[:], func=AF.Exp), reads=["r_ec"], writes=["r_ec"])
                    dv(lambda: nc.vector.tensor_reduce(out=r_pg[:], in_=r_ec[:], axis=AX.X, op=ALU.add), ["r_ec"], ["r_pg"])
                    dv(lambda: nc.vector.tensor_tensor(out=r_t[:], in0=lf, in1=bc(r_gm[:].unsqueeze(3), [128, NB, 4, 8]), op=ALU.mult), ["lg", "r_gm"], ["r_t"])
                    dv(lambda: nc.vector.tensor_reduce(out=r_lfs[:], in_=r_t[:].rearrange("p b g e -> p b e g"), axis=AX.X, op=ALU.add), ["r_t"], ["r_lfs"])
                    for b in range(NB):
                        dv(lambda b=b: nc.vector.max(out=r_t8[:, b, :], in_=r_lfs[:, b, :]), ["r_lfs"], ["r_t8"])
                    l1b = bc(r_t8[:, :, 0:1], [128, NB, 8])
                    l2b = bc(r_t8[:, :, 1:2], [128, NB, 8])
                    dv(lambda: nc.vector.tensor_tensor(out=r_sel[:], in0=r_lfs[:], in1=l2b, op=ALU.is_ge), ["r_lfs", "r_t8"], ["r_sel"])
                    dv(lambda: nc.vector.tensor_tensor(out=r_s1[:], in0=r_lfs[:], in1=l1b, op=ALU.is_ge), ["r_lfs", "r_t8"], ["r_s1"])
                    dv(lambda: nc.vector.tensor_tensor(out=r_s2[:], in0=r_sel[:], in1=r_s1[:], op=ALU.subtract), ["r_sel", "r_s1"], ["r_s2"])
                    dv(lambda: nc.vector.tensor_tensor(out=r_ex[:], in0=r_lfs[:], in1=l1b, op=ALU.subtract), ["r_lfs", "r_t8"], ["r_ex"])
                    P.op("act", lambda: nc.scalar.activation(out=r_ex[:], in_=r_ex[:], func=AF.Exp), reads=["r_ex"], writes=["r_ex"])
                    dv(lambda: nc.vector.tensor_tensor(out=r_ex[:], in0=r_ex[:], in1=r_sel[:], op=ALU.mult), ["r_ex", "r_sel"], ["r_ex"])
                    dv(lambda: nc.vector.tensor_reduce(out=r_d[:], in_=r_ex[:], axis=AX.X, op=ALU.add), ["r_ex"], ["r_d"])
                    dv(lambda: nc.vector.tensor_tensor(out=r_d[:], in0=r_d[:], in1=r_pg[:], op=ALU.mult), ["r_d", "r_pg"], ["r_d"])
                    dv(lambda: nc.vector.reciprocal(out=r_d[:], in_=r_d[:]), ["r_d"], ["r_d"])
                    dv(lambda: nc.vector.tensor_tensor(out=r_ex[:], in0=r_ex[:], in1=bc(r_d[:].unsqueeze(2), [128, NB, 8]), op=ALU.mult), ["r_ex", "r_d"], ["r_ex"])
                    dv(lambda: nc.vector.tensor_tensor(out=r_tmp8[:], in0=r_ex[:], in1=r_s1[:], op=ALU.mult), ["r_ex", "r_s1"], ["r_tmp8"])
                    dv(lambda: nc.vector.tensor_reduce(out=gate1[:], in_=r_tmp8[:], axis=AX.X, op=ALU.add), ["r_tmp8"], ["gate1"])
                    dv(lambda: nc.vector.tensor_tensor(out=r_tmp8[:], in0=r_ex[:], in1=r_s2[:], op=ALU.mult), ["r_ex", "r_s2", "gate1"], ["r_tmp8"])
                    dv(lambda: nc.vector.tensor_reduce(out=gate2[:], in_=r_tmp8[:], axis=AX.X, op=ALU.add), ["r_tmp8"], ["gate2"])
                    g4 = bc(r_gm[:].unsqueeze(3), [128, NB, 4, 8])
                    dv(lambda: nc.vector.tensor_tensor(out=M1[:].rearrange("p b (g e) -> p b g e", g=4), in0=g4,
                                                       in1=bc(r_s1[:].unsqueeze(2), [128, NB, 4, 8]), op=ALU.mult), ["r_gm", "r_s1"], ["M1"])
                    dv(lambda: nc.vector.tensor_tensor(out=M2[:].rearrange("p b (g e) -> p b g e", g=4), in0=g4,
                                                       in1=bc(r_s2[:].unsqueeze(2), [128, NB, 4, 8]), op=ALU.mult), ["r_gm", "r_s2"], ["M2"])
                    dv(lambda: nc.vector.tensor_tensor(out=Mb16[:], in0=M1[:], in1=M2[:], op=ALU.add), ["M1", "M2"], ["Mb16"])
                    P.op("pool", lambda: nc.gpsimd.memset(Rm16[:, 0, :], 0.0), writes=[("Rm", 0)])
                    for b in range(NB):
                        dv(lambda b=b: nc.vector.tensor_tensor(out=Rm16[:, b + 1, :], in0=Rm16[:, b, :], in1=Mb16[:, b, :], op=ALU.add),
                           [("Rm", b), "Mb16"], [("Rm", b + 1)])
                    rbank = [pb[0], pb[1]]
                    for b in range(NB):
                        pt = rbank[b // 16]
                        sl = slice((b % 16) * 32, (b % 16) * 32 + 32)
                        P.op("pe", lambda pt=pt, sl=sl, b=b: nc.tensor.matmul(pt[:, sl], lhsT=lstrict[:], rhs=Mb16[:, b, :], start=True, stop=False),
                             reads=["lstrict", "Mb16"], writes=[("pb", b // 16)])
                        P.op("pe", lambda pt=pt, sl=sl, b=b: nc.tensor.matmul(pt[:, sl], lhsT=pones[:], rhs=Rm16[:, b, :], start=False, stop=True),
                             reads=["pones", ("Rm", b)], writes=[("pb", b // 16)])
                    for hf in range(2):
                        dv(lambda hf=hf: nc.vector.tensor_copy(out=rank[:, hf * 16:(hf + 1) * 16, :],
                                                               in_=rbank[hf][:].rearrange("p (b e) -> p b e", b=16)),
                           [("pb", hf)], [("rank", hf)])
                    P.op("pe", lambda: nc.tensor.matmul(pb[2][:, 0:32], lhsT=pones[:], rhs=Rm16[:, NB, :], start=True, stop=True),
                         reads=["pones", ("Rm", NB)], writes=[("pb", 2)])
                    dv(lambda: nc.vector.tensor_copy(out=cnt[:], in_=pb[2][:, 0:32]), [("pb", 2)], ["cnt"])
                    P.op("pool", lambda: nc.gpsimd.iota(thr16[:], pattern=[[TS, 16]], base=0, channel_multiplier=0, allow_small_or_imprecise_dtypes=True), writes=["thr16"])
                    P.op("pool", lambda: nc.gpsimd.iota(thr64[:], pattern=[[TS, NT]], base=0, channel_multiplier=0, allow_small_or_imprecise_dtypes=True), writes=["thr64"])
                    dv(lambda: nc.vector.tensor_tensor(out=cmpA[:], in0=bc(cnt[:].unsqueeze(2), [128, 32, 16]), in1=bc(thr16[:].unsqueeze(1), [128, 32, 16]), op=ALU.is_gt),
                       ["cnt", "thr16"], ["cmpA"])
                    dv(lambda: nc.vector.tensor_reduce(out=ntile[:], in_=cmpA[:], axis=AX.X, op=ALU.add), ["cmpA"], ["ntile"])
                    src, srck = ntile, "ntile"
                    bufs = [(csA, "csA"), (csB, "csB")]
                    for si, sh in enumerate((1, 2, 4, 8, 16)):
                        dst, dstk = bufs[si % 2]
                        dv(lambda src=src, dst=dst, sh=sh: nc.vector.tensor_copy(out=dst[:, 0:sh], in_=src[:, 0:sh]), [srck], [(dstk, 0)])
                        dv(lambda src=src, dst=dst, sh=sh: nc.vector.tensor_tensor(out=dst[:, sh:32], in0=src[:, sh:32], in1=src[:, 0:32 - sh], op=ALU.add),
                           [srck], [(dstk, 1)])
                        src, srck = dst, dstk
                        srck_list = [(dstk, 0), (dstk, 1)]
                        srck = dstk
                        P.op("dve", lambda dst=dst: nc.vector.tensor_copy(out=dst[:, 0:1], in_=dst[:, 0:1]), reads=srck_list, writes=[dstk])
                    dv(lambda src=src: nc.vector.tensor_tensor(out=base[:], in0=src[:], in1=ntile[:], op=ALU.subtract), [srck, "ntile"], ["base"])
                    dv(lambda: nc.vector.tensor_scalar(out=base[:], in0=base[:], scalar1=float(TS), scalar2=None, op0=ALU.mult), ["base"], ["base"])
                    dv(lambda: nc.vector.tensor_tensor(out=cmpB[:], in0=bc(base[:].unsqueeze(1), [128, NT, 32]), in1=bc(thr64[:].unsqueeze(2), [128, NT, 32]), op=ALU.is_le),
                       ["base", "thr64"], ["cmpB"])
                    dv(lambda: nc.vector.tensor_reduce(out=te_f[:], in_=cmpB[:], axis=AX.X, op=ALU.add), ["cmpB"], ["te_f"])
                    dv(lambda: nc.vector.tensor_scalar(out=te_f[:], in0=te_f[:], scalar1=-1.0, scalar2=128.0, op0=ALU.add, op1=ALU.mult), ["te_f"], ["te_f"])
                    dv(lambda: nc.vector.tensor_scalar(out=te_f[:], in0=te_f[:], scalar1=pio[:, 0:1], scalar2=None, op0=ALU.add), ["te_f", "pio"], ["te_f"])
                    dv(lambda src=src: nc.vector.tensor_scalar(out=tot[:], in0=src[:, 31:32], scalar1=float(TS), scalar2=None, op0=ALU.mult), [srck], ["tot"])
                    dv(lambda: nc.vector.tensor_scalar(out=tail[:], in0=thr64[:], scalar1=tot[:, 0:1], scalar2=1.0e6, op0=ALU.is_ge, op1=ALU.mult),
                       ["thr64", "tot"], ["tail"])
                    dv(lambda: nc.vector.tensor_tensor(out=te_f[:], in0=te_f[:], in1=tail[:], op=ALU.add), ["te_f", "tail"], ["te_f"])
                    dv(lambda: nc.vector.tensor_copy(out=widx_i[:], in_=te_f[:]), ["te_f"], ["widx_i"])
                    dv(lambda: nc.vector.tensor_tensor(out=rank[:], in0=rank[:], in1=bc(base[:].unsqueeze(1), [128, NB, 32]), op=ALU.add),
                       [("rank", 0), ("rank", 1), "base"], ["rankp"])
                    for (Mk, Mkk, idxi, idxk) in ((M1, "M1", idx1_i, "idx1_i"), (M2, "M2", idx2_i, "idx2_i")):
                        dv(lambda Mk=Mk: nc.vector.tensor_tensor(out=tmp32[:], in0=rank[:], in1=Mk[:], op=ALU.mult), ["rankp", Mkk, "idx_f"], ["tmp32"])
                        dv(lambda: nc.vector.tensor_reduce(out=idx_f[:], in_=tmp32[:], axis=AX.X, op=ALU.add), ["tmp32"], ["idx_f"])
                        dv(lambda idxi=idxi: nc.vector.tensor_copy(out=idxi[:], in_=idx_f[:]), ["idx_f"], [idxk])
                    for b in range(NB):
                        for (idxi, idxk) in ((idx1_i, "idx1_i"), (idx2_i, "idx2_i")):
                            P.dma("pool", lambda b=b, idxi=idxi: nc.gpsimd.indirect_dma_start(
                                out=XS[:, :], out_offset=bass.IndirectOffsetOnAxis(ap=idxi[:, b:b + 1], axis=0),
                                in_=xnall[:, b, :], in_offset=None), reads=[("xnall", b), idxk], writes=["XS"])
                    P.flush()
                if sub < 2:
                    return
                with ExitStack() as st2_:
                    def sb2(name, shape, dt):
                        return st2_.enter_context(nc.sbuf_tensor(_un(name), shape, dt))
                    w13_rot = Rot([sb2(f"w13_{i}", [128, 2, 8, 256], BF16) for i in range(2)], "w13")
                    w2_rot = Rot([sb2(f"w2_{i}", [128, 2, 1024], BF16) for i in range(2)], "w2")
                    stg_rot = Rot([sb2(f"stg{i}", [128, 3, 2048], F32) for i in range(3)], "stg")
                    xs_rot = Rot([sb2(f"xs{i}", [128, NA, 1024], BF16) for i in range(2)], "xs")
                    xT_rot = Rot([sb2(f"xTt{i}", [128, 8, TS], BF16) for i in range(2)], "xTt")
                    s_rot = Rot([sb2(f"s{i}", [128, TS], F32) for i in range(2)], "s")
                    he_rot = Rot([sb2(f"he{i}", [128, 2, TS], BF16) for i in range(2)], "he")
                    yt_rot = Rot([sb2(f"yt{i}", [128, 1024], F32) for i in range(3)], "yt")
                    hbank = Rot(pb[0:4], "pb")
                    ybank = Rot(pb[4:7], "pby")
                    wts = {}
                    tst = {}
                    bcreg = {}

                    def mkreg():
                        bcreg["r"] = nc.gpsimd.to_reg(2 * 32 * 128 - 1)
                        return None
                    P.op("pool", mkreg, nosig=True)

                    wstg = {}

                    def cast_w(i):
                        stg, stgk = wstg.pop(i)
                        w13, w13k = w13_rot.next()
                        w2, w2k = w2_rot.next()
                        wts[i] = (w13, w13k, w2, w2k)
                        P.op("act", lambda: nc.scalar.copy(out=w13[:, 0, :, :].rearrange("p k f -> p (k f)"), in_=stg[:, 0, :]),
                             reads=[(stgk, 0)], writes=[(w13k, 0)])
                        P.op("dve", lambda: nc.vector.tensor_copy(out=w13[:, 1, :, :].rearrange("p k f -> p (k f)"), in_=stg[:, 1, :]),
                             reads=[(stgk, 1)], writes=[(w13k, 1)])
                        P.op("act", lambda: nc.scalar.copy(out=w2[:, 0, :], in_=stg[:, 2, 0:1024]),
                             reads=[(stgk, 2)], writes=[(w2k, 0)])
                        P.op("dve", lambda: nc.vector.tensor_copy(out=w2[:, 1, :], in_=stg[:, 2, 1024:2048]),
                             reads=[(stgk, 2)], writes=[(w2k, 1)])

                    def load_w(i):
                        stg, stgk = stg_rot.next()
                        ix = bass.IndirectOffsetOnAxis(ap=widx_i[:, i:i + 1], axis=0)
                        for j, (src_ap, pat, kw) in enumerate(((moe_w1, "l e (p k) f -> (l e p) (k f)", dict(k=8)),
                                                              (moe_w3, "l e (p k) f -> (l e p) (k f)", dict(k=8)),
                                                              (moe_w2, "l e (p c) d -> (l e p) (c d)", dict(c=2)))):
                            P.dma("pool", lambda j=j, src_ap=src_ap, pat=pat, kw=kw: nc.gpsimd.indirect_dma_start(
                                out=stg[:, j, :], out_offset=None, in_=src_ap.rearrange(pat, **kw), in_offset=ix,
                                bounds_check=bcreg["r"], oob_is_err=False),
                                reads=["widx_i"], writes=[(stgk, j)])
                        wstg[i] = (stg, stgk)

                    xsl = {}

                    def load_xs(i):
                        xs, xsk = xs_rot.next()
                        P.dma("sp", lambda: nc.sync.dma_start(out=xs[:], in_=XS[i * TS:(i + 1) * TS, :].rearrange("(a p) d -> p a d", p=128)),
                              writes=[xsk])
                        xsl[i] = (xs, xsk)

                    def stage_t(i):
                        cast_w(i)
                        if i + 3 < NT:
                            load_w(i + 3)
                        if i + 1 < NT:
                            load_xs(i + 1)
                        xs, xsk = xsl.pop(i)
                        xT, xTk = xT_rot.next()
                        for a in range(NA):
                            for k in range(8):
                                P.op("pe", lambda a=a, k=k: nc.tensor.transpose(out=ptb[:, k, :], in_=xs[:, a, k:1024:8], identity=ident[:]),
                                     reads=[xsk, "ident"], writes=["ptb"])
                            if a % 2 == 0:
                                P.op("act", lambda a=a: nc.scalar.copy(out=xT[:, :, a * 128:(a + 1) * 128], in_=ptb[:]), reads=["ptb"], writes=[(xTk, a)])
                            else:
                                P.op("dve", lambda a=a: nc.vector.tensor_copy(out=xT[:, :, a * 128:(a + 1) * 128], in_=ptb[:]), reads=["ptb"], writes=[(xTk, a)])
                        w13, w13k, w2, w2k = wts[i]
                        he, hek = he_rot.next()
                        tst[i] = (he, hek)
                        for fch in range(2):
                            p1, p1k = hbank.next()
                            p3, p3k = hbank.next()
                            for j, (pt, pk) in enumerate(((p1, p1k), (p3, p3k))):
                                for k in range(8):
                                    P.op("pe", lambda pt=pt, j=j, k=k, fch=fch: nc.tensor.matmul(
                                        pt[:, 0:TS], lhsT=w13[:, j, k, fch:256:2], rhs=xT[:, k, :],
                                        start=(k == 0), stop=(k == 7)),
                                        reads=[(w13k, j)] + [(xTk, a_) for a_ in range(NA)], writes=[pk])
                            s, sk = s_rot.next()
                            P.op("act", lambda s=s, p1=p1: nc.scalar.activation(out=s[:], in_=p1[:, 0:TS], func=AF.Silu), reads=[p1k], writes=[sk])
                            P.op("dve", lambda s=s, p3=p3, fch=fch: nc.vector.tensor_tensor(out=he[:, fch, :], in0=p3[:, 0:TS], in1=s[:], op=ALU.mult),
                                 reads=[p3k, sk], writes=[(hek, fch)])

                    def stage_y(i):
                        w13, w13k, w2, w2k = wts.pop(i)
                        he, hek = tst.pop(i)
                        for a in range(NA):
                            yt, ytk = yt_rot.next()
                            for half in range(2):
                                py, pyk = ybank.next()
                                for fch in range(2):
                                    P.op("pe", lambda py=py, fch=fch, a=a, half=half: nc.tensor.matmul(
                                        py[:], lhsT=he[:, fch, a * 128:(a + 1) * 128], rhs=w2[:, fch, half * 512:(half + 1) * 512],
                                        start=(fch == 0), stop=(fch == 1)), reads=[(hek, fch), (w2k, fch)], writes=[pyk])
                                if half == 0:
                                    P.op("act", lambda py=py, yt=yt, half=half: nc.scalar.copy(out=yt[:, half * 512:(half + 1) * 512], in_=py[:]),
                                         reads=[pyk], writes=[(ytk, half)])
                                else:
                                    P.op("dve", lambda py=py, yt=yt, half=half: nc.vector.tensor_copy(out=yt[:, half * 512:(half + 1) * 512], in_=py[:]),
                                         reads=[pyk], writes=[(ytk, half)])
                            r0 = i * TS + a * 128
                            P.dma("sp", lambda yt=yt, r0=r0: nc.sync.dma_start(out=YS[r0:r0 + 128, :], in_=yt[:]),
                                  reads=[(ytk, 0), (ytk, 1)], writes=[("YS", i, a)])

                    load_w(0)
                    load_w(1)
                    load_w(2)
                    load_xs(0)
                    stage_t(0)
                    for i in range(NT):
                        if i + 1 < NT:
                            stage_t(i + 1)
                        stage_y(i)
                    P.flush()
                if sub < 3:
                    return
                with ExitStack() as st3_:
                    def sb3(name, shape, dt):
                        return st3_.enter_context(nc.sbuf_tensor(_un(name), shape, dt))
                    ht_rot = Rot([sb3(f"htc{i}", [128, 1024], F32) for i in range(5)], "htc")
                    y1_rot = Rot([sb3(f"y1_{i}", [128, 1024], F32) for i in range(4)], "y1")
                    y2_rot = Rot([sb3(f"y2_{i}", [128, 1024], F32) for i in range(4)], "y2")
                    pblk_rot = Rot([sb3(f"pblk{i}", [128, 256], F32) for i in range(4)], "pblk")
                    pb16_rot = Rot([sb3(f"pb16{i}", [128, 256], BF16) for i in range(2)], "pb16")
                    pT_rot = Rot([sb3(f"pT{i}", [128, 2, 128], BF16) for i in range(2)], "pT")
                    hnT_rot = Rot([sb3(f"hnT{i}", [128, 8, 128], BF16) for i in range(2)], "hnT")
                    sg_rot = Rot([sb3(f"sg{i}", [128, 1024], F32) for i in range(2)], "sg")
                    gbank = Rot(pb[0:4], "pb")
                    ld = {}

                    def loads7(b):
                        ht, htk = ht_rot.next()
                        y1, y1k = y1_rot.next()
                        y2, y2k = y2_rot.next()
                        pblk, pblkk = pblk_rot.next()
                        P.dma("sp", lambda: nc.sync.dma_start(out=ht[:], in_=hin[b * 128:(b + 1) * 128, :]), writes=[htk])
                        P.dma("sp", lambda: nc.sync.dma_start(out=pblk[:], in_=p[l, b * 128:(b + 1) * 128, :]), writes=[pblkk])
                        P.dma("pool", lambda: nc.gpsimd.indirect_dma_start(
                            out=y1[:, :], out_offset=None, in_=YS[:, :], in_offset=bass.IndirectOffsetOnAxis(ap=idx1_i[:, b:b + 1], axis=0)),
                            reads=["idx1_i"], writes=[y1k])
                        P.dma("pool", lambda: nc.gpsimd.indirect_dma_start(
                            out=y2[:, :], out_offset=None, in_=YS[:, :], in_offset=bass.IndirectOffsetOnAxis(ap=idx2_i[:, b:b + 1], axis=0)),
                            reads=["idx2_i"], writes=[y2k])
                        ld[b] = (ht, htk, y1, y1k, y2, y2k, pblk, pblkk)

                    loads7(0)
                    loads7(1)
                    for b in range(NB):
                        if b + 2 < NB:
                            loads7(b + 2)
                        ht, htk, y1, y1k, y2, y2k, pblk, pblkk = ld.pop(b)
                        P.op("dve", lambda ht=ht, y1=y1, b=b: nc.vector.scalar_tensor_tensor(
                            out=ht[:], in0=y1[:], scalar=gate1[:, b:b + 1], in1=ht[:], op0=ALU.mult, op1=ALU.add),
                            reads=[htk, y1k, "gate1"], writes=[htk])
                        P.op("dve", lambda ht=ht, y2=y2, b=b: nc.vector.scalar_tensor_tensor(
                            out=ht[:], in0=y2[:], scalar=gate2[:, b:b + 1], in1=ht[:], op0=ALU.mult, op1=ALU.add),
                            reads=[htk, y2k, "gate2"], writes=[htk])
                        hnT, hnk = hnT_rot.next()
                        norm_T(P, ht[:], htk, gBp[:], "gBp", hnT[:], hnk)
                        p16, p16k = pb16_rot.next()
                        pT, pTk = pT_rot.next()
                        P.op("pool", lambda p16=p16, pblk=pblk: nc.gpsimd.tensor_copy(out=p16[:], in_=pblk[:]), reads=[pblkk], writes=[p16k])
                        for k in range(2):
                            P.op("pe", lambda p16=p16, k=k: nc.tensor.transpose(out=ptb[:, k, :], in_=p16[:, k * 128:(k + 1) * 128], identity=ident[:]),
                                 reads=[p16k, "ident"], writes=["ptb"])
                        P.op("act", lambda pT=pT: nc.scalar.copy(out=pT[:], in_=ptb[:, 0:2, :]), reads=["ptb"], writes=[pTk])
                        sg, sgk = sg_rot.next()
                        for half in range(2):
                            pg_, pgk = gbank.next()
                            pe_, pek = gbank.next()
                            for k in range(8):
                                P.op("pe", lambda pg_=pg_, hnT=hnT, k=k, half=half: nc.tensor.matmul(
                                    pg_[:], lhsT=hnT[:, k, :], rhs=wpg[:, k, half * 512:(half + 1) * 512], start=(k == 0), stop=(k == 7)),
                                    reads=[hnk, "wpg"], writes=[pgk])
                            for k in range(2):
                                P.op("pe", lambda pe_=pe_, pT=pT, k=k, half=half: nc.tensor.matmul(
                                    pe_[:], lhsT=pT[:, k, :], rhs=wpe[:, k, half * 512:(half + 1) * 512], start=(k == 0), stop=(k == 1)),
                                    reads=[pTk, "wpe"], writes=[pek])
                            P.op("act", lambda sg=sg, pg_=pg_, half=half: nc.scalar.activation(out=sg[:, half * 512:(half + 1) * 512], in_=pg_[:], func=AF.Sigmoid),
                                 reads=[pgk], writes=[(sgk, half)])
                            P.op("dve", lambda sg=sg, pe_=pe_, half=half: nc.vector.tensor_tensor(
                                out=sg[:, half * 512:(half + 1) * 512], in0=pe_[:], in1=sg[:, half * 512:(half + 1) * 512], op=ALU.mult),
                                reads=[pek, (sgk, half)], writes=[(sgk, half)])
                        P.op("pool", lambda sg=sg, ht=ht: nc.gpsimd.tensor_tensor(out=ht[:], in0=ht[:], in1=sg[:], op=ALU.add),
                             reads=[(sgk, 0), (sgk, 1), htk], writes=[htk])
                        if final:
                            junk, jk = njunk.next()
                            ss, ssk = nss.next()
                            norm_rstd(P, ht[:], htk, 1024, junk[:], jk, ss[:], ssk)
                            P.op("dve", lambda ss=ss, ht=ht: nc.vector.scalar_tensor_tensor(
                                out=ht[:], in0=ht[:], scalar=ss[:], in1=gBo[:], op0=ALU.mult, op1=ALU.mult),
                                reads=[htk, ssk, "gBo"], writes=[htk])
                        P.dma("sp", lambda ht=ht, b=b: nc.sync.dma_start(out=hout[b * 128:(b + 1) * 128, :], in_=ht[:]),
                              reads=[htk], writes=[("hout", b)])
                    P.flush()

        SPARSE = True
        mphase = sparse_moe_pl_phase if SPARSE else moe_pl_phase
        if stop_after >= 5:
            mphase(0, hA, hB, False)

        if stop_after >= 6:
            with ExitStack() as st:
                def sb(name, shape, dt):
                    return st.enter_context(nc.sbuf_tensor(_un(name), shape, dt))
                wi = sb("wi", [128, 8, 4096], BF16)
                wi_src = w_in_odd[0].rearrange("(k p) n -> p k n", p=128)
                for k in range(8):
                    P.dma("pool", lambda k=k: nc.gpsimd.dma_start(out=wi[:, k, :], in_=wi_src[:, k, :]), writes=[("wi", k)])
                wi_keys = [("wi", k) for k in range(8)]
                wo1 = sb("wo1", [128, 16, 1024], BF16)
                wo_src = w_out_odd[0].rearrange("(c p) n -> p c n", p=128)
                for c in range(0, 16, 4):
                    P.dma("pool", lambda c=c: nc.gpsimd.dma_start(out=wo1[:, c:c + 4, :], in_=wo_src[:, c:c + 4, :]), writes=[("wo1", c)])
                wo_keys = [("wo1", c) for c in range(0, 16, 4)]
                gB1 = sb("gB1", [128, 1024], F32)
                load_gB(P, gB1[:], "gB1", norm_mix[1])
                gvB = sb("gvB", [128, 2048], F32)
                P.dma("sp", lambda: nc.sync.dma_start(out=gvB[:], in_=g_v_odd[0].partition_broadcast(128)), writes=["gvB"])
                bsf = sb("bsf", [1, 8, 128], F32)
                bs16 = sb("bs16", [1, 8, 128], BF16)
                P.dma("sp", lambda: nc.sync.dma_start(out=bsf[:], in_=b_s_odd[0:1]), writes=["bsf"])
                P.op("dve", lambda: nc.vector.tensor_copy(out=bs16[:], in_=bsf[:]), reads=["bsf"], writes=["bs16"])
                wsT = sb("wsT", [128, 8, 128], BF16)
                st3 = ExitStack()
                st3.__enter__()
                wsf = st3.enter_context(nc.sbuf_tensor(_un("wsf"), [128, 8, 128], F32))
                ws16 = st3.enter_context(nc.sbuf_tensor(_un("ws16"), [128, 8, 128], BF16))
                P.dma("sp", lambda: nc.sync.dma_start(out=wsf[:], in_=w_s_odd[0].rearrange("g t s -> t g s")), writes=["wsf"])
                for g in range(8):
                    P.op("pool", lambda g=g: nc.gpsimd.affine_select(out=wsf[:, g, :], in_=wsf[:, g, :], pattern=[[-1, 128]],
                                                                      compare_op=ALU.is_ge, fill=0.0, base=0, channel_multiplier=1),
                         reads=["wsf"], writes=["wsf"])
                P.op("dve", lambda: nc.vector.tensor_copy(out=ws16[:], in_=wsf[:]), reads=["wsf"], writes=["ws16"])
                for g in range(8):
                    P.op("pe", lambda g=g: nc.tensor.transpose(out=ptb[:, g, :], in_=ws16[:, g, :], identity=ident[:]),
                         reads=["ws16", "ident"], writes=["ptb"])
                P.op("dve", lambda: nc.vector.tensor_copy(out=wsT[:], in_=ptb[:]), reads=["ptb"], writes=["wsT"])
                P.flush()
                st3.__exit__(None, None, None)
                ht_rot = Rot([sb(f"ht{i}", [128, 1024], F32) for i in range(5)], "ht")
                xg_rot = Rot([sb(f"xg{i}", [128, 8, 512], BF16) for i in range(2)], "xg")
                uT_rot = Rot([sb(f"uT{i}", [128, 16, 512], BF16) for i in range(1)], "uT")
                vt_rot = Rot([sb(f"vt{i}", [128, 2048], F32) for i in range(1)], "vt")
                vn_rot = Rot([sb(f"vn{i}", [128, 2048], BF16) for i in range(1)], "vn")
                yT_rot = Rot([sb(f"yT{i}", [128, 16, 128], BF16) for i in range(2)], "yT")
                abank = Rot(pb[0:3], "pb")
                gbank = Rot(pb[3:5], "pbg")
                obank = Rot(pb[5:7], "pbo")
                for tc in range(8):
                    xg, xgk = xg_rot.next()
                    hts = []
                    for tb in range(4):
                        b = tc * 4 + tb
                        ht, htk = ht_rot.next()
                        hts.append((ht, htk))
                        P.dma("sp", lambda ht=ht, b=b: nc.sync.dma_start(out=ht[:], in_=hB[b * 128:(b + 1) * 128, :]),
                              reads=[("hout", b)], writes=[htk])
                        norm_T(P, ht[:], htk, gB1[:], "gB1", xg[:, :, tb * 128:(tb + 1) * 128], (xgk, tb))
                    xgkeys = [(xgk, tb) for tb in range(4)]
                    uT, uTk = uT_rot.next()
                    for fcu in range(16):
                        pt, pk = abank.next()
                        for k in range(8):
                            P.op("pe", lambda pt=pt, k=k, fcu=fcu, xg=xg: nc.tensor.matmul(
                                pt[:], lhsT=wi[:, k, fcu * 128:(fcu + 1) * 128], rhs=xg[:, k, :], start=(k == 0), stop=(k == 7)),
                                reads=[("wi", k)] + xgkeys, writes=[pk])
                        P.op("act", lambda pt=pt, uT=uT, fcu=fcu: nc.scalar.activation(out=uT[:, fcu, :], in_=pt[:], func=AF.Gelu_apprx_tanh),
                             reads=[pk], writes=[(uTk, fcu)])
                    for tb in range(4):
                        b = tc * 4 + tb
                        ht, htk = hts[tb]
                        vt, vtk = vt_rot.next()
                        for vg in range(4):
                            pt, pk = abank.next()
                            for k in range(8):
                                P.op("pe", lambda pt=pt, k=k, vg=vg, xg=xg, tb=tb: nc.tensor.matmul(
                                    pt[:], lhsT=xg[:, k, tb * 128:(tb + 1) * 128], rhs=wi[:, k, 2048 + vg * 512: 2048 + (vg + 1) * 512],
                                    start=(k == 0), stop=(k == 7)), reads=[("wi", k), (xgk, tb)], writes=[pk])
                            P.op("act", lambda pt=pt, vt=vt, vg=vg: nc.scalar.activation(out=vt[:, vg * 512:(vg + 1) * 512], in_=pt[:], func=AF.Gelu_apprx_tanh),
                                 reads=[pk], writes=[(vtk, vg)])
                        ss, ssk = nss.next()
                        vtkeys = [(vtk, vg) for vg in range(4)]
                        vn, vnk = vn_rot.next()
                        P.op("act", lambda vt=vt, ss=ss, vn=vn: nc.scalar.activation(out=vn[:], in_=vt[:], func=AF.Square, accum_out=ss[:]),
                             reads=vtkeys, writes=[vnk, ssk])
                        P.op("act", lambda ss=ss: nc.scalar.activation(out=ss[:], in_=ss[:], func=AF.Ln, scale=1.0 / 2048, bias=epsb[:]),
                             reads=[ssk, "epsb"], writes=[ssk])
                        P.op("act", lambda ss=ss: nc.scalar.activation(out=ss[:], in_=ss[:], func=AF.Exp, scale=-0.5), reads=[ssk], writes=[ssk])
                        P.op("dve", lambda vn=vn, vt=vt, ss=ss: nc.vector.scalar_tensor_tensor(out=vn[:], in0=vt[:], scalar=ss[:], in1=gvB[:],
                                                                                              op0=ALU.mult, op1=ALU.mult),
                             reads=vtkeys + [ssk, "gvB"], writes=[vnk])
                        yT, yTk = yT_rot.next()
                        for q4 in range(4):
                            pg_, pgk = gbank.next()
                            for i4 in range(4):
                                fcu = q4 * 4 + i4
                                g = fcu // 2
                                P.op("pe", lambda pg_=pg_, i4=i4, fcu=fcu, g=g, vn=vn: nc.tensor.matmul(
                                    pg_[:, i4 * 128:(i4 + 1) * 128], lhsT=vn[:, fcu * 128:(fcu + 1) * 128], rhs=wsT[:, g, :], start=True, stop=False),
                                    reads=[vnk, "wsT"], writes=[pgk])
                                P.op("pe", lambda pg_=pg_, i4=i4, g=g: nc.tensor.matmul(
                                    pg_[:, i4 * 128:(i4 + 1) * 128], lhsT=ones1[0:1, :], rhs=bs16[0:1, g, :], start=False, stop=True),
                                    reads=["ones1", "bs16"], writes=[pgk])
                            P.op("dve", lambda pg_=pg_, yT=yT, uT=uT, q4=q4, tb=tb: nc.vector.tensor_tensor(
                                out=yT[:, q4 * 4:(q4 + 1) * 4, :], in0=pg_[:].rearrange("p (i t) -> p i t", i=4),
                                in1=uT[:, q4 * 4:(q4 + 1) * 4, tb * 128:(tb + 1) * 128], op=ALU.mult),
                                reads=[pgk] + [(uTk, q4 * 4 + i) for i in range(4)], writes=[(yTk, q4)])
                        for half in range(2):
                            po_, pok = obank.next()
                            for fcu in range(16):
                                P.op("pe", lambda po_=po_, yT=yT, fcu=fcu, half=half: nc.tensor.matmul(
                                    po_[:], lhsT=yT[:, fcu, :], rhs=wo1[:, fcu, half * 512:(half + 1) * 512], start=(fcu == 0), stop=(fcu == 15)),
                                    reads=[(yTk, fcu // 4), ("wo1", (fcu // 4) * 4)], writes=[pok])
                            P.op("dve", lambda po_=po_, ht=ht, half=half: nc.vector.tensor_tensor(
                                out=ht[:, half * 512:(half + 1) * 512], in0=po_[:], in1=ht[:, half * 512:(half + 1) * 512], op=ALU.add),
                                reads=[pok, htk], writes=[htk])
                        P.dma("sp", lambda ht=ht, b=b: nc.sync.dma_start(out=hA[b * 128:(b + 1) * 128, :], in_=ht[:]),
                              reads=[htk], writes=[("hA", b)])
                P.flush()

        if stop_after >= 7:
            mphase(1, hA, out, True)
        P.flush()
        nc._prog_stats = dict(nops=P.nops, ccount=dict(P.ccount), dcount=dict(P.dcount))
    return nc


_NC_CACHE = {}


def kernel(**inputs):
    n = 8
    if "nc" not in _NC_CACHE:
        _NC_CACHE["nc"] = build()
    nc = _NC_CACHE["nc"]
    in_maps = []
    for c in range(n):
        m = {}
        for k, v in inputs.items():
            v = np.asarray(v)
            if k == "x":
                m[k] = np.ascontiguousarray(v[c])
            elif k == "p":
                m[k] = np.ascontiguousarray(v[:, c])
            else:
                m[k] = np.ascontiguousarray(v)
        in_maps.append(m)
    res = run_bass_kernel_spmd(nc, in_maps, core_ids=list(range(n)))
    return np.stack([np.asarray(r["out"]) for r in res.results], axis=0).astype(np.float32)
```

```python
import numpy as np
from contextlib import ExitStack
import concourse.bass as bass
import concourse.mybir as mybir
from concourse.bass_utils import run_bass_kernel_spmd

F32 = mybir.dt.float32
BF16 = mybir.dt.bfloat16
AF = mybir.ActivationFunctionType
ALU = mybir.AluOpType
AX = mybir.AxisListType
I32 = mybir.dt.int32

EPOCH = 30000
NDMASEM = 12
S = 4096
D = 1024
NB = S // 128
EPS = 1e-6


class Prog:
    ENG = ("pe", "act", "dve", "pool", "sp")

    def __init__(self, nc, stack):
        self.nc = nc
        self.stack = stack
        self.ops = []
        self.ccount = {e: 0 for e in self.ENG}
        self.dcount = {e: 0 for e in self.ENG}
        self.csem = {e: [] for e in self.ENG}
        self.dsem = {e: [] for e in self.ENG}
        self.dma_hist = {e: [] for e in self.ENG}
        self.waited = {e: {} for e in self.ENG}
        self.nops = 0

    def eng_obj(self, e):
        nc = self.nc
        return {"pe": nc.tensor, "act": nc.scalar, "dve": nc.vector,
                "pool": nc.gpsimd, "sp": nc.sync}[e]

    def op(self, eng, fn, reads=(), writes=(), nosig=False):
        self.ops.append(dict(eng=eng, fn=fn, reads=tuple(reads), writes=tuple(writes), dma=False, nosig=nosig))

    def dma(self, eng, fn, reads=(), writes=()):
        self.ops.append(dict(eng=eng, fn=fn, reads=tuple(reads), writes=tuple(writes), dma=True))

    def _csem(self, e, ep):
        while len(self.csem[e]) <= ep:
            self.csem[e].append(self.stack.enter_context(self.nc.semaphore(f"c_{e}_{len(self.csem[e])}")))
        return self.csem[e][ep]

    def _dsem(self, e, k):
        while len(self.dsem[e]) <= k:
            self.dsem[e].append(self.stack.enter_context(self.nc.semaphore(f"d_{e}_{len(self.dsem[e])}")))
        return self.dsem[e][k]

    def _wait(self, e, key, sem, val):
        w = self.waited[e]
        if key[0] == "c":
            for kk, v in w.items():
                if kk[0] == "c" and kk[1] == key[1] and (kk[2] > key[2] or (kk[2] == key[2] and v >= val)):
                    return
        elif w.get(key, 0) >= val:
            return
        self.eng_obj(e).wait_ge(sem, val)
        w[key] = max(w.get(key, 0), val)

    def flush(self):
        ops = self.ops
        n = len(ops)
        self.nops += n
        lw, rdc, rdd = {}, {}, {}
        deps = [None] * n
        dlist = {e: [] for e in self.ENG}
        for i, o in enumerate(ops):
            d = set()
            for k in o["reads"]:
                w = lw.get(k)
                if w is not None:
                    d.add(w)
            for k in o["writes"]:
                w = lw.get(k)
                if w is not None:
                    d.add(w)
                d.update(rdc.get(k, {}).values())
                d.update(rdd.get(k, ()))
            for k in o["reads"]:
                if o["dma"]:
                    rdd.setdefault(k, []).append(i)
                else:
                    rdc.setdefault(k, {})[o["eng"]] = i
            for k in o["writes"]:
                lw[k] = i
                rdc[k] = {}
                rdd[k] = []
            if o["dma"]:
                q = o["eng"]
                o["dn"] = self.dcount[q]
                self.dcount[q] += 1
                lst = dlist[q]
                if len(lst) >= NDMASEM:
                    d.add(lst[len(lst) - NDMASEM])
                lst.append(i)
            d.discard(i)
            if o["eng"] == "pe" and not o["dma"]:
                d = {j for j in d if not (ops[j]["eng"] == "pe" and not ops[j]["dma"])}
            deps[i] = d
        signaling = [False] * n
        for i in range(n):
            for j in deps[i]:
                signaling[j] = True
        last = {}
        for i, o in enumerate(ops):
            if not o["dma"] and not o.get("nosig"):
                last[o["eng"]] = i
        for e, i in last.items():
            signaling[i] = True
        for i, o in enumerate(ops):
            if not o["dma"] and signaling[i]:
                c = self.ccount[o["eng"]]
                o["sig"] = (c // EPOCH, c % EPOCH + 1)
                self.ccount[o["eng"]] = c + 1
        for i, o in enumerate(ops):
            e = o["eng"]
            need = {}
            for j in deps[i]:
                p = ops[j]
                if p["dma"]:
                    key = ("d", p["eng"], p["dn"] % NDMASEM)
                    val = 16 * (p["dn"] // NDMASEM + 1)
                    sem = self._dsem(p["eng"], p["dn"] % NDMASEM)
                else:
                    ep, cv = p["sig"]
                    key = ("c", p["eng"], ep)
                    val = cv
                    sem = self._csem(p["eng"], ep)
                if need.get(key, (None, 0))[1] < val:
                    need[key] = (sem, val)
            for key, (sem, val) in need.items():
                self._wait(e, key, sem, val)
            ins = o["fn"]()
            if o["dma"]:
                ins.then_inc(self._dsem(e, o["dn"] % NDMASEM), 16)
            elif signaling[i]:
                ep, cv = o["sig"]
                ins.then_inc(self._csem(e, ep), 1)
        for e in self.ENG:
            for e2, i in last.items():
                ep, cv = ops[i]["sig"]
                self._wait(e, ("c", e2, ep), self._csem(e2, ep), cv)
            for q in self.ENG:
                dc = self.dcount[q]
                for k in range(min(NDMASEM, dc)):
                    lastdn = ((dc - 1 - k) // NDMASEM) * NDMASEM + k
                    self._wait(e, ("d", q, k), self._dsem(q, k), 16 * (lastdn // NDMASEM + 1))
        self.ops = []


_UN = [0]


def _un(name):
    _UN[0] += 1
    return f"{name}_{_UN[0]}"


def _kl(k):
    return list(k) if isinstance(k, list) else [k]


def hk(blk):
    return [("hacc", blk, 0), ("hacc", blk, 1)]


class Rot:
    def __init__(self, tiles, name):
        self.tiles = tiles
        self.name = name
        self.i = 0

    def next(self):
        j = self.i % len(self.tiles)
        self.i += 1
        return self.tiles[j], (self.name, j)


def build(stop_after=99, dbg=False, sub=9):
    nc = bass.Bass("TRN2", target_bir_lowering=False)

    def din(name, shape):
        return nc.dram_tensor(name, list(shape), F32, kind="ExternalInput").ap()

    x = din("x", [S, D])
    p = din("p", [2, S, 256])
    norm_mix = din("norm_mix", [2, D])
    norm_ffn = din("norm_ffn", [2, D])
    norm_pl = din("norm_pl", [2, D])
    final_norm = din("final_norm", [D])
    w_in_even = din("w_in_even", [1, D, 3072])
    conv_w_even = din("conv_w_even", [1, 3, 512])
    w_out_even = din("w_out_even", [1, 1024, D])
    w_in_odd = din("w_in_odd", [1, D, 4096])
    g_v_odd = din("g_v_odd", [1, 2048])
    w_s_odd = din("w_s_odd", [1, 8, 128, 128])
    b_s_odd = din("b_s_odd", [1, 8, 128])
    w_out_odd = din("w_out_odd", [1, 2048, D])
    router_c = din("router_c", [2, D, 4])
    router_c_b = din("router_c_b", [2, 4])
    router_f = din("router_f", [2, D, 32])
    router_f_b = din("router_f_b", [2, 32])
    moe_w1 = din("moe_w1", [2, 32, D, 256])
    moe_w3 = din("moe_w3", [2, 32, D, 256])
    moe_w2 = din("moe_w2", [2, 32, 256, D])
    w_pe = din("w_pe", [2, 256, D])
    w_pg = din("w_pg", [2, D, D])
    out = nc.dram_tensor("out", [S, D], F32, kind="ExternalOutput").ap()
    mixT = nc.dram_tensor("mixT", [8, 128, S], BF16, kind="ExternalOutput" if dbg else "Internal").ap()
    XS = nc.dram_tensor("XS", [24576, 1024], BF16, kind="Internal").ap()
    YS = nc.dram_tensor("YS", [24576, 1024], F32, kind="Internal").ap()
    hA = nc.dram_tensor("hA", [S, D], F32, kind="ExternalOutput" if dbg else "Internal").ap()
    hB = nc.dram_tensor("hB", [S, D], F32, kind="ExternalOutput" if dbg else "Internal").ap()

    with ExitStack() as gst:
        P = Prog(nc, gst)

        def gsb(name, shape, dt):
            return gst.enter_context(nc.sbuf_tensor(_un(name), shape, dt))

        pb = [gst.enter_context(nc.psum_tensor(f"pb{i}", [128, 512], F32)) for i in range(7)]
        ptb = gst.enter_context(nc.psum_tensor("ptb", [128, 8, 128], BF16))

        identf = gsb("identf", [128, 128], F32)
        ident = gsb("ident", [128, 128], BF16)
        ntri = gsb("ntri", [128, 128], BF16)
        nones = gsb("nones", [128, 128], BF16)
        zeros = gsb("zeros", [128, 128], BF16)
        nmask = gsb("nmask", [128, 128], BF16)
        ones1 = gsb("ones1", [1, 128], BF16)
        pones = gsb("pones", [128, 128], BF16)
        lstrict = gsb("lstrict", [128, 128], BF16)
        lsf = gsb("lsf", [128, 128], F32)
        tmpf = gsb("tmpf", [128, 128], F32)
        P.op("pool", lambda: nc.gpsimd.memset(identf[:], 1.0), writes=["identf"])
        P.op("pool", lambda: nc.gpsimd.affine_select(out=identf[:], in_=identf[:], pattern=[[-1, 128]],
                                                      compare_op=ALU.is_equal, fill=0.0, base=0, channel_multiplier=1),
             reads=["identf"], writes=["identf"])
        P.op("dve", lambda: nc.vector.tensor_copy(out=ident[:], in_=identf[:]), reads=["identf"], writes=["ident"])
        P.op("pool", lambda: nc.gpsimd.memset(tmpf[:], -1.0), writes=["tmpf"])
        P.op("dve", lambda: nc.vector.tensor_copy(out=nones[:], in_=tmpf[:]), reads=["tmpf"], writes=["nones"])
        P.op("pool", lambda: nc.gpsimd.affine_select(out=tmpf[:], in_=tmpf[:], pattern=[[-1, 128]],
                                                      compare_op=ALU.is_ge, fill=0.0, base=0, channel_multiplier=1),
             reads=["tmpf", "nones"], writes=["tmpf"])
        P.op("dve", lambda: nc.vector.tensor_copy(out=ntri[:], in_=tmpf[:]), reads=["tmpf"], writes=["ntri"])
        P.op("dve", lambda: nc.vector.tensor_scalar(out=nmask[:], in0=tmpf[:], scalar1=30000.0, scalar2=None, op0=ALU.mult),
             reads=["tmpf"], writes=["nmask"])
        P.op("pool", lambda: nc.gpsimd.memset(zeros[:], 0.0), writes=["zeros"])
        P.op("pool", lambda: nc.gpsimd.memset(ones1[:], 1.0), writes=["ones1"])
        P.op("pool", lambda: nc.gpsimd.memset(pones[:], 1.0), writes=["pones"])
        P.op("pool", lambda: nc.gpsimd.memset(lsf[:], 1.0), writes=["lsf"])
        P.op("pool", lambda: nc.gpsimd.affine_select(out=lsf[:], in_=lsf[:], pattern=[[1, 128]],
                                                      compare_op=ALU.is_gt, fill=0.0, base=0, channel_multiplier=-1),
             reads=["lsf"], writes=["lsf"])
        P.op("dve", lambda: nc.vector.tensor_copy(out=lstrict[:], in_=lsf[:]), reads=["lsf"], writes=["lstrict"])
        P.flush()

        def norm_rstd(P, src, skey, width, junk, jkey, ss, sskey):
            P.op("act", lambda: nc.scalar.activation(out=junk, in_=src, func=AF.Square, accum_out=ss),
                 reads=_kl(skey), writes=[jkey, sskey])
            P.op("act", lambda: nc.scalar.activation(out=ss, in_=ss, func=AF.Ln, scale=1.0 / width, bias=epsb[:]),
                 reads=[sskey, "epsb"], writes=[sskey])
            P.op("act", lambda: nc.scalar.activation(out=ss, in_=ss, func=AF.Exp, scale=-0.5),
                 reads=[sskey], writes=[sskey])

        epsb = gsb("epsb", [128, 1], F32)
        P.op("pool", lambda: nc.gpsimd.memset(epsb[:], EPS), writes=["epsb"])

        njunk = Rot([gsb(f"njunk{i}", [128, 1024], BF16) for i in range(1)], "njunk")
        nss = Rot([gsb(f"nss{i}", [128, 1], F32) for i in range(4)], "nss")
        nxn = Rot([gsb(f"nxn{i}", [128, 1024], BF16) for i in range(2)], "nxn")

        def norm_T(P, src, skey, gB, gkey, dstT, dkey, cp_eng="act"):
            junk, jk = njunk.next()
            ss, ssk = nss.next()
            xn, xnk = nxn.next()
            norm_rstd(P, src, skey, 1024, junk[:], jk, ss[:], ssk)
            P.op("dve", lambda: nc.vector.scalar_tensor_tensor(out=xn[:], in0=src, scalar=ss[:], in1=gB,
                                                                 op0=ALU.mult, op1=ALU.mult),
                 reads=_kl(skey) + [ssk, gkey], writes=[xnk])
            for k in range(8):
                P.op("pe", lambda k=k: nc.tensor.transpose(out=ptb[:, k, :], in_=xn[:, k * 128:(k + 1) * 128], identity=ident[:]),
                     reads=[xnk, "ident"], writes=["ptb"])
            if cp_eng == "act":
                P.op("act", lambda: nc.scalar.copy(out=dstT, in_=ptb[:]), reads=["ptb"], writes=[dkey])
            else:
                P.op("dve", lambda: nc.vector.tensor_copy(out=dstT, in_=ptb[:]), reads=["ptb"], writes=[dkey])
            return ss, ssk

        def load_gB(P, dst, key, src_row):
            P.dma("sp", lambda: nc.sync.dma_start(out=dst, in_=src_row.partition_broadcast(128)), writes=[key])

        with ExitStack() as st:
            def sb(name, shape, dt):
                return st.enter_context(nc.sbuf_tensor(_un(name), shape, dt))
            xnT = sb("xnT", [128, 8, S], BF16)
            zrow = sb("zrow", [128, 4, 1024], BF16)
            P.op("pool", lambda: nc.gpsimd.memset(zrow[:], 0.0), writes=["zrow"])
            for zi in range(24576 // 512):
                P.dma("pool", lambda zi=zi: nc.gpsimd.dma_start(out=XS[zi * 512:(zi + 1) * 512, :].rearrange("(p a) d -> p a d", a=4), in_=zrow[:]),
                      reads=["zrow"], writes=["XS"])
            gB0 = sb("gB0", [128, 1024], F32)
            load_gB(P, gB0[:], "gB0", norm_mix[0])
            xt_rot = Rot([sb(f"xt{i}", [128, 1024], F32) for i in range(2)], "xt")
            for b in range(NB):
                xt, xk = xt_rot.next()
                P.dma("sp", lambda xt=xt, b=b: nc.sync.dma_start(out=xt[:], in_=x[b * 128:(b + 1) * 128, :]), writes=[xk])
                norm_T(P, xt[:], xk, gB0[:], "gB0", xnT[:, :, b * 128:(b + 1) * 128], ("xnT", b))
            xnT_keys = lambda tc: [("xnT", 4 * tc + i) for i in range(4)]

            P.flush()
            st2 = ExitStack()
            st2.__enter__()
            sb_outer = sb

            def sb(name, shape, dt):
                return st2.enter_context(nc.sbuf_tensor(_un(name), shape, dt))
            cw = sb("cw", [128, 4, 3], F32)
            for fc in range(4):
                P.dma("sp", lambda fc=fc: nc.sync.dma_start(
                    out=cw[:, fc, :], in_=conv_w_even[0, :, fc * 128:(fc + 1) * 128].rearrange("w f -> f w"),
                    allow_slow_non_contiguous=True), writes=["cw"])
            wc_rot = Rot([sb(f"wc{i}", [128, 3, 8, 128], BF16) for i in range(2)], "wc")
            z_rot = Rot([sb(f"z{i}", [128, S + 2], F32) for i in range(2)], "z")
            hs_rot = Rot([sb(f"hs{i}", [128, 512], F32) for i in range(2)], "hs")
            acc_rot = Rot([sb(f"acc{i}", [128, 512], F32) for i in range(2)], "acc")
            ya_rot = Rot([sb(f"ya{i}", [128, 512], BF16) for i in range(2)], "ya")
            w_in0 = w_in_even[0].rearrange("(k p) n -> p k n", p=128)
            bank = Rot(pb[0:6], "pb")
            for fc in range(4):
                wc, wck = wc_rot.next()
                for j in range(3):
                    P.dma("pool", lambda wc=wc, j=j, fc=fc: nc.gpsimd.dma_start(
                        out=wc[:, j, :, :], in_=w_in0[:, :, j * 512 + fc * 128: j * 512 + (fc + 1) * 128]),
                        writes=[(wck, j)])
                z, zk = z_rot.next()
                P.op("pool", lambda z=z: nc.gpsimd.memset(z[:, 0:2], 0.0), writes=[(zk, -1)])
                for tc in range(8):
                    pp = []
                    for j in range(3):
                        pt, pk = bank.next()
                        for k in range(8):
                            P.op("pe", lambda pt=pt, wc=wc, j=j, k=k, tc=tc: nc.tensor.matmul(
                                pt[:], lhsT=wc[:, j, k, :], rhs=xnT[:, k, tc * 512:(tc + 1) * 512],
                                start=(k == 0), stop=(k == 7)),
                                reads=[(wck, j)] + xnT_keys(tc), writes=[pk])
                        pp.append((pt, pk))
                    (ph, phk), (pgb, pgbk), (pgc, pgck) = pp
                    hs, hsk = hs_rot.next()
                    acc, acck = acc_rot.next()
                    ya, yak = ya_rot.next()
                    P.op("act", lambda hs=hs, ph=ph: nc.scalar.copy(out=hs[:], in_=ph[:]), reads=[phk], writes=[hsk])
                    P.op("dve", lambda z=z, tc=tc, pgc=pgc, hs=hs: nc.vector.tensor_tensor(
                        out=z[:, 2 + tc * 512: 2 + (tc + 1) * 512], in0=pgc[:], in1=hs[:], op=ALU.mult),
                        reads=[pgck, hsk], writes=[(zk, tc)])
                    zr = [(zk, tc), (zk, tc - 1)]
                    P.op("dve", lambda acc=acc, z=z, tc=tc, fc=fc: nc.vector.tensor_scalar(
                        out=acc[:], in0=z[:, tc * 512: tc * 512 + 512], scalar1=cw[:, fc, 0:1], scalar2=None, op0=ALU.mult),
                        reads=zr + ["cw"], writes=[acck])
                    for wv in (1, 2):
                        P.op("dve", lambda acc=acc, z=z, tc=tc, fc=fc, wv=wv: nc.vector.scalar_tensor_tensor(
                            out=acc[:], in0=z[:, tc * 512 + wv: tc * 512 + wv + 512], scalar=cw[:, fc, wv:wv + 1],
                            in1=acc[:], op0=ALU.mult, op1=ALU.add),
                            reads=zr + ["cw", acck], writes=[acck])
                    P.op("dve", lambda ya=ya, acc=acc, pgb=pgb: nc.vector.tensor_tensor(
                        out=ya[:], in0=pgb[:], in1=acc[:], op=ALU.mult), reads=[pgbk, acck], writes=[yak])
                    P.dma("sp", lambda ya=ya, fc=fc, tc=tc: nc.sync.dma_start(
                        out=mixT[fc, :, tc * 512:(tc + 1) * 512], in_=ya[:]), reads=[yak], writes=[("mixT", fc, tc)])
            P.flush()
            st2.__exit__(None, None, None)
            sb = sb_outer
            if stop_after >= 2:
                wq_rot = Rot([sb(f"wq{i}", [128, 3, 8, 128], BF16) for i in range(2)], "wq")
                qT_rot = Rot([sb(f"qT{i}", [128, S], BF16) for i in range(2)], "qT")
                kT_rot = Rot([sb(f"kT{i}", [128, S], BF16) for i in range(2)], "kT")
                v_rot = Rot([sb(f"v{i}", [128, NB, 128], BF16) for i in range(2)], "v")
                yb_rot = Rot([sb(f"yb{i}", [128, S], BF16) for i in range(2)], "yb")
                e_rot = Rot([sb(f"e{i}", [128, 512], F32) for i in range(3)], "e")
                sp_rot = Rot([sb(f"sp{i}", [128, 512], BF16) for i in range(3)], "sp")
                a_rot = Rot([sb(f"a{i}", [128, 512], BF16) for i in range(3)], "a")
                r32_rot = Rot([sb(f"r32{i}", [128, 512], F32) for i in range(2)], "r32")
                r16_rot = Rot([sb(f"r16{i}", [128, 512], BF16) for i in range(3)], "r16")
                zb = Rot(pb[0:3], "pb")
                ob = Rot(pb[3:7], "pbo")
                pjb = Rot(pb[3:7], "pbo")

                def proj(hp):
                    wq, wqk = wq_rot.next()
                    for j in range(3):
                        P.dma("pool", lambda wq=wq, j=j, hp=hp: nc.gpsimd.dma_start(
                            out=wq[:, j, :, :], in_=w_in0[:, :, 1536 + j * 512 + hp * 128: 1536 + j * 512 + (hp + 1) * 128]),
                            writes=[(wqk, j)])
                    qT, qk = qT_rot.next()
                    kT, kk = kT_rot.next()
                    v, vk = v_rot.next()
                    for tc in range(8):
                        for j, (dst, dk, sc) in enumerate(((qT, qk, 0.125), (kT, kk, 1.0))):
                            pt, pk = pjb.next()
                            for k in range(8):
                                P.op("pe", lambda pt=pt, wq=wq, j=j, k=k, tc=tc: nc.tensor.matmul(
                                    pt[:], lhsT=wq[:, j, k, :], rhs=xnT[:, k, tc * 512:(tc + 1) * 512],
                                    start=(k == 0), stop=(k == 7)),
                                    reads=[(wqk, j)] + xnT_keys(tc), writes=[pk])
                            P.op("dve", lambda dst=dst, pt=pt, tc=tc, sc=sc: nc.vector.tensor_scalar(
                                out=dst[:, tc * 512:(tc + 1) * 512], in0=pt[:], scalar1=sc, scalar2=None, op0=ALU.mult),
                                reads=[pk], writes=[(dk, tc)])
                        pt, pk = pjb.next()
                        for tb in range(4):
                            b = tc * 4 + tb
                            for k in range(8):
                                P.op("pe", lambda pt=pt, wq=wq, k=k, b=b, tb=tb: nc.tensor.matmul(
                                    pt[:, tb * 128:(tb + 1) * 128], lhsT=xnT[:, k, b * 128:(b + 1) * 128], rhs=wq[:, 2, k, :],
                                    start=(k == 0), stop=(k == 7)),
                                    reads=[(wqk, 2), ("xnT", b)], writes=[pk])
                        P.op("dve", lambda v=v, pt=pt, tc=tc: nc.vector.tensor_copy(
                            out=v[:, tc * 4:(tc + 1) * 4, :], in_=pt[:].rearrange("p (b d) -> p b d", b=4)),
                            reads=[pk], writes=[(vk, tc)])
                    return (qT, qk, kT, kk, v, vk)

                def attention(hp, qkv):
                    qT, qk, kT, kk, v, vk = qkv
                    yb, ybk = yb_rot.next()
                    tiles = []
                    for hh in range(2):
                        for qc in range(8):
                            nkb = 4 * qc + 4
                            for ii, kb in enumerate(range(nkb - 1, -1, -1)):
                                jd = kb - 4 * qc
                                c0 = jd * 128 if jd >= 0 else 0
                                tiles.append(dict(hh=hh, qc=qc, kb=kb, c0=c0, diag=(jd >= 0), first=(ii == 0),
                                                  last=(kb == 0)))
                    nt = len(tiles)
                    st_ = [dict() for _ in range(nt)]

                    def stage_qk(i):
                        t = tiles[i]
                        po = t["hh"] * 64
                        zt, zk_ = zb.next()
                        st_[i]["z"] = (zt, zk_)
                        c0 = t["c0"]
                        qc, kb = t["qc"], t["kb"]
                        P.op("pe", lambda: nc.tensor.matmul(
                            zt[:, c0:512], lhsT=kT[po:po + 64, kb * 128:(kb + 1) * 128],
                            rhs=qT[po:po + 64, qc * 512 + c0:(qc + 1) * 512], start=True, stop=False),
                            reads=[(kk, kb // 4), (qk, qc)], writes=[zk_])
                        if t["diag"]:
                            P.op("pe", lambda: nc.tensor.matmul(
                                zt[:, c0:c0 + 128], lhsT=ident[:], rhs=nmask[:], start=False, stop=False),
                                reads=["ident", "nmask"], writes=[zk_])
                        et, ek = e_rot.next()
                        spt, spk = sp_rot.next()
                        st_[i]["sp"] = (spt, spk)
                        P.op("act", lambda: nc.scalar.activation(out=et[:, c0:512], in_=zt[:, c0:512], func=AF.Exp),
                             reads=[zk_], writes=[ek])
                        P.op("act", lambda: nc.scalar.activation(out=spt[:, c0:512], in_=et[:, c0:512], func=AF.Ln, bias=1.0),
                             reads=[ek], writes=[spk])

                    def stage_cum(i):
                        t = tiles[i]
                        zt, zk_ = st_[i]["z"]
                        spt, spk = st_[i]["sp"]
                        c0 = t["c0"]
                        if t["first"]:
                            r32, r32k = r32_rot.next()
                            st_[i]["r32"] = (r32, r32k)
                            P.op("pool", lambda: nc.gpsimd.memset(r32[:], 0.0), writes=[r32k])
                        else:
                            st_[i]["r32"] = st_[i - 1]["r32"]
                            r32, r32k = st_[i]["r32"]
                        P.op("pe", lambda: nc.tensor.matmul(zt[:, c0:512], lhsT=ntri[:], rhs=spt[:, c0:512],
                                                            start=False, stop=t["first"]),
                             reads=["ntri", spk], writes=[zk_])
                        if not t["first"]:
                            r16, r16k = st_[i]["r16"]
                            P.op("pe", lambda: nc.tensor.matmul(zt[:, c0:512], lhsT=nones[:], rhs=r16[:, c0:512],
                                                                start=False, stop=True),
                                 reads=["nones"] + st_[i]["r16keys"], writes=[zk_])
                        if not t["last"]:
                            c1 = tiles[i + 1]["c0"]
                            r16n, r16nk = r16_rot.next()
                            st_[i + 1]["r16"] = (r16n, r16nk)
                            wk = [r16nk]
                            if c1 < c0:
                                P.op("pool", lambda: nc.gpsimd.memset(r16n[:, c1:c0], 0.0), writes=[r16nk])
                            P.op("dve", lambda: nc.vector.tensor_tensor(out=r16n[:, c0:512], in0=r32[:, c0:512],
                                                                       in1=spt[:, c0:512], op=ALU.add),
                                 reads=[r32k, spk], writes=[r16nk])
                            P.op("dve", lambda: nc.vector.tensor_tensor(out=r32[:, c0:512], in0=r32[:, c0:512],
                                                                       in1=spt[:, c0:512], op=ALU.add),
                                 reads=[r32k, spk], writes=[r32k])
                            st_[i + 1]["r16keys"] = wk
                        at, ak = a_rot.next()
                        st_[i]["a"] = (at, ak)
                        P.op("act", lambda: nc.scalar.activation(out=at[:, c0:512], in_=zt[:, c0:512], func=AF.Exp),
                             reads=[zk_], writes=[ak])

                    def stage_av(i):
                        t = tiles[i]
                        at, ak = st_[i]["a"]
                        c0 = t["c0"]
                        kb = t["kb"]
                        po = t["hh"] * 64
                        if t["first"]:
                            ot, ok = ob.next()
                            st_[i]["o"] = (ot, ok)
                            P.op("pe", lambda: nc.tensor.matmul(ot[:], lhsT=zeros[:], rhs=qT[:, 0:512], start=True, stop=False),
                                 reads=["zeros", (qk, 0)], writes=[ok])
                        else:
                            st_[i]["o"] = st_[i - 1]["o"]
                            ot, ok = st_[i]["o"]
                        P.op("pe", lambda: nc.tensor.matmul(ot[:, c0:512], lhsT=v[:, kb, :], rhs=at[:, c0:512],
                                                            start=False, stop=t["last"]),
                             reads=[(vk, kb // 4), ak], writes=[ok])
                        if t["last"]:
                            qc = t["qc"]
                            P.op("dve", lambda: nc.vector.tensor_copy(out=yb[po:po + 64, qc * 512:(qc + 1) * 512],
                                                                      in_=ot[po:po + 64, :]),
                                 reads=[ok], writes=[(ybk, qc, t["hh"])])
                        st_[i].pop("z", None)

                    stage_qk(0)
                    for i in range(nt):
                        if i + 1 < nt:
                            stage_qk(i + 1)
                        stage_cum(i)
                        if i >= 1:
                            stage_av(i - 1)
                    stage_av(nt - 1)
                    for qc in range(8):
                        P.dma("sp", lambda qc=qc: nc.sync.dma_start(out=mixT[4 + hp, :, qc * 512:(qc + 1) * 512],
                                                                    in_=yb[:, qc * 512:(qc + 1) * 512]),
                              reads=[(ybk, qc, 0), (ybk, qc, 1)], writes=[("mixT", 4 + hp, qc)])

                nhp = 4 if stop_after >= 3 else 1
                qkvs = {0: proj(0)}
                for hp in range(nhp):
                    if hp + 1 < nhp:
                        qkvs[hp + 1] = proj(hp + 1)
                    attention(hp, qkvs.pop(hp))
            P.flush()

        if stop_after >= 4:
            with ExitStack() as st:
                def sb(name, shape, dt):
                    return st.enter_context(nc.sbuf_tensor(_un(name), shape, dt))
                wo = sb("wo", [128, 8, 1024], BF16)
                P.dma("pool", lambda: nc.gpsimd.dma_start(out=wo[:], in_=w_out_even[0].rearrange("(k p) n -> p k n", p=128)),
                      writes=["wo"])
                mx_rot = Rot([sb(f"mx{i}", [128, 8, 512], BF16) for i in range(2)], "mx")
                xt_rot = Rot([sb(f"xt{i}", [128, 1024], F32) for i in range(3)], "xt")
                bank = Rot(pb[0:6], "pb")
                for tc in range(8):
                    mx, mxk = mx_rot.next()
                    P.dma("sp", lambda mx=mx, tc=tc: nc.sync.dma_start(
                        out=mx[:], in_=mixT[:, :, tc * 512:(tc + 1) * 512].rearrange("c p t -> p c t")), writes=[mxk])
                    for tb in range(4):
                        b = tc * 4 + tb
                        xt, xk = xt_rot.next()
                        P.dma("sp", lambda xt=xt, b=b: nc.sync.dma_start(out=xt[:], in_=x[b * 128:(b + 1) * 128, :]), writes=[xk])
                        for half in range(2):
                            pt, pk = bank.next()
                            for c in range(8):
                                P.op("pe", lambda pt=pt, mx=mx, c=c, tb=tb, half=half: nc.tensor.matmul(
                                    pt[:], lhsT=mx[:, c, tb * 128:(tb + 1) * 128], rhs=wo[:, c, half * 512:(half + 1) * 512],
                                    start=(c == 0), stop=(c == 7)), reads=[mxk, "wo"], writes=[pk])
                            P.op("dve", lambda xt=xt, pt=pt, half=half: nc.vector.tensor_tensor(
                                out=xt[:, half * 512:(half + 1) * 512], in0=pt[:], in1=xt[:, half * 512:(half + 1) * 512], op=ALU.add),
                                reads=[pk, xk], writes=[xk])
                        P.dma("sp", lambda xt=xt, b=b: nc.sync.dma_start(out=hA[b * 128:(b + 1) * 128, :], in_=xt[:]),
                              reads=[xk], writes=[("hA", b)])
                P.flush()

        def moe_pl_phase(l, hin, hout, final):
            with ExitStack() as st:
                def sb(name, shape, dt):
                    return st.enter_context(nc.sbuf_tensor(_un(name), shape, dt))
                NBS = 16
                hacc = sb("hacc", [128, NBS, 1024], F32)
                xT = sb("xT", [128, 8, NBS * 128], BF16)
                gBf = sb("gBf", [128, 1024], F32)
                gBp = sb("gBp", [128, 1024], F32)
                load_gB(P, gBf[:], "gBf", norm_ffn[l])
                load_gB(P, gBp[:], "gBp", norm_pl[l])
                if final:
                    gBo = sb("gBo", [128, 1024], F32)
                    load_gB(P, gBo[:], "gBo", final_norm)
                wr = sb("wr", [128, 8, 36], BF16)
                P.dma("pool", lambda: nc.gpsimd.dma_start(out=wr[:, :, 0:4], in_=router_c[l].rearrange("(k p) n -> p k n", p=128)),
                      writes=["wr"])
                P.dma("pool", lambda: nc.gpsimd.dma_start(out=wr[:, :, 4:36], in_=router_f[l].rearrange("(k p) n -> p k n", p=128)),
                      writes=["wr"])
                rb = sb("rb", [128, 36], F32)
                P.dma("sp", lambda: nc.sync.dma_start(out=rb[:, 0:4], in_=router_c_b[l].partition_broadcast(128)), writes=["rb"])
                P.dma("sp", lambda: nc.sync.dma_start(out=rb[:, 4:36], in_=router_f_b[l].partition_broadcast(128)), writes=["rb"])
                wpg = sb("wpg", [128, 8, 1024], BF16)
                wpe = sb("wpe", [128, 2, 1024], BF16)
                P.dma("pool", lambda: nc.gpsimd.dma_start(out=wpg[:], in_=w_pg[l].rearrange("(k p) n -> p k n", p=128)), writes=["wpg"])
                P.dma("pool", lambda: nc.gpsimd.dma_start(out=wpe[:], in_=w_pe[l].rearrange("(k p) n -> p k n", p=128)), writes=["wpe"])
                w13_rot = Rot([sb(f"w13_{i}", [128, 2, 8, 256], BF16) for i in range(2)], "w13")
                w2_rot = Rot([sb(f"w2_{i}", [128, 2, 1024], BF16) for i in range(2)], "w2")
                lg = sb("lg", [128, NBS, 36], F32)
                comb = sb("comb", [128, NBS, 32], F32)
                r_mx = sb("r_mx", [128, NBS], F32)
                r_gm = sb("r_gm", [128, NBS, 4], F32)
                r_ec = sb("r_ec", [128, NBS, 4], F32)
                r_pg = sb("r_pg", [128, NBS], F32)
                r_t = sb("r_t", [128, NBS, 4, 8], F32)
                r_lfs = sb("r_lfs", [128, NBS, 8], F32)
                r_t8 = sb("r_t8", [128, NBS, 8], F32)
                r_sel = sb("r_sel", [128, NBS, 8], F32)
                r_ex = sb("r_ex", [128, NBS, 8], F32)
                r_d = sb("r_d", [128, NBS], F32)
                s_rot = Rot([sb(f"s{i}", [128, 512], F32) for i in range(2)], "s")
                he_rot = Rot([sb(f"he{i}", [128, 2, 512], BF16) for i in range(2)], "he")
                pblk_rot = Rot([sb(f"pblk{i}", [128, 256], F32) for i in range(2)], "pblk")
                pb16_rot = Rot([sb(f"pb16{i}", [128, 256], BF16) for i in range(2)], "pb16")
                pT_rot = Rot([sb(f"pT{i}", [128, 2, 128], BF16) for i in range(2)], "pT")
                hnT_rot = Rot([sb(f"hnT{i}", [128, 8, 128], BF16) for i in range(2)], "hnT")
                sg_rot = Rot([sb(f"sg{i}", [128, 1024], F32) for i in range(1)], "sg")
                for sc in range(2):
                    hbank = Rot(pb[0:4], "pb")
                    ybank = Rot(pb[4:7], "pby")
                    for blk in range(NBS):
                        b = sc * NBS + blk
                        P.dma("sp", lambda blk=blk, b=b: nc.sync.dma_start(out=hacc[:, blk, :], in_=hin[b * 128:(b + 1) * 128, :]),
                              writes=hk(blk))
                        norm_T(P, hacc[:, blk, :], hk(blk), gBf[:], "gBf", xT[:, :, blk * 128:(blk + 1) * 128], ("xT", blk))
                        pt, pk = ybank.next()
                        for k in range(8):
                            P.op("pe", lambda pt=pt, k=k, blk=blk: nc.tensor.matmul(
                                pt[:, 0:36], lhsT=xT[:, k, blk * 128:(blk + 1) * 128], rhs=wr[:, k, :],
                                start=(k == 0), stop=(k == 7)), reads=[("xT", blk), "wr"], writes=[pk])
                        P.op("dve", lambda pt=pt, blk=blk: nc.vector.tensor_tensor(out=lg[:, blk, :], in0=pt[:, 0:36], in1=rb[:], op=ALU.add),
                             reads=[pk, "rb"], writes=["lg"])
                    lc = lg[:, :, 0:4]
                    lf = lg[:, :, 4:36].rearrange("p b (g e) -> p b g e", g=4)
                    P.op("dve", lambda: nc.vector.tensor_reduce(out=r_mx[:], in_=lc, axis=AX.X, op=ALU.max), reads=["lg"], writes=["r_mx"])
                    P.op("dve", lambda: nc.vector.tensor_tensor(out=r_gm[:], in0=lc, in1=r_mx[:].unsqueeze(2).to_broadcast([128, NBS, 4]),
                                                                op=ALU.is_ge), reads=["lg", "r_mx"], writes=["r_gm"])
                    P.op("dve", lambda: nc.vector.tensor_tensor(out=r_ec[:], in0=lc, in1=r_mx[:].unsqueeze(2).to_broadcast([128, NBS, 4]),
                                                                op=ALU.subtract), reads=["lg", "r_mx"], writes=["r_ec"])
                    P.op("act", lambda: nc.scalar.activation(out=r_ec[:], in_=r_ec[:], func=AF.Exp), reads=["r_ec"], writes=["r_ec"])
                    P.op("dve", lambda: nc.vector.tensor_reduce(out=r_pg[:], in_=r_ec[:], axis=AX.X, op=ALU.add), reads=["r_ec"], writes=["r_pg"])
                    P.op("dve", lambda: nc.vector.tensor_tensor(out=r_t[:], in0=lf, in1=r_gm[:].unsqueeze(3).to_broadcast([128, NBS, 4, 8]),
                                                                op=ALU.mult), reads=["lg", "r_gm"], writes=["r_t"])
                    P.op("dve", lambda: nc.vector.tensor_reduce(out=r_lfs[:], in_=r_t[:].rearrange("p b g e -> p b e g"), axis=AX.X, op=ALU.add),
                         reads=["r_t"], writes=["r_lfs"])
                    for blk in range(NBS):
                        P.op("dve", lambda blk=blk: nc.vector.max(out=r_t8[:, blk, :], in_=r_lfs[:, blk, :]), reads=["r_lfs"], writes=["r_t8"])
                    l1b = r_t8[:, :, 0:1].to_broadcast([128, NBS, 8])
                    l2b = r_t8[:, :, 1:2].to_broadcast([128, NBS, 8])
                    P.op("dve", lambda: nc.vector.tensor_tensor(out=r_sel[:], in0=r_lfs[:], in1=l2b, op=ALU.is_ge), reads=["r_lfs", "r_t8"], writes=["r_sel"])
                    P.op("dve", lambda: nc.vector.tensor_tensor(out=r_ex[:], in0=r_lfs[:], in1=l1b, op=ALU.subtract), reads=["r_lfs", "r_t8"], writes=["r_ex"])
                    P.op("act", lambda: nc.scalar.activation(out=r_ex[:], in_=r_ex[:], func=AF.Exp), reads=["r_ex"], writes=["r_ex"])
                    P.op("dve", lambda: nc.vector.tensor_tensor(out=r_ex[:], in0=r_ex[:], in1=r_sel[:], op=ALU.mult), reads=["r_ex", "r_sel"], writes=["r_ex"])
                    P.op("dve", lambda: nc.vector.tensor_reduce(out=r_d[:], in_=r_ex[:], axis=AX.X, op=ALU.add), reads=["r_ex"], writes=["r_d"])
                    P.op("dve", lambda: nc.vector.tensor_tensor(out=r_d[:], in0=r_d[:], in1=r_pg[:], op=ALU.mult), reads=["r_d", "r_pg"], writes=["r_d"])
                    P.op("dve", lambda: nc.vector.reciprocal(out=r_d[:], in_=r_d[:]), reads=["r_d"], writes=["r_d"])
                    P.op("dve", lambda: nc.vector.tensor_tensor(out=r_ex[:], in0=r_ex[:], in1=r_d[:].unsqueeze(2).to_broadcast([128, NBS, 8]),
                                                                op=ALU.mult), reads=["r_ex", "r_d"], writes=["r_ex"])
                    P.op("dve", lambda: nc.vector.tensor_tensor(
                        out=comb[:].rearrange("p b (g e) -> p b g e", g=4),
                        in0=r_gm[:].unsqueeze(3).to_broadcast([128, NBS, 4, 8]),
                        in1=r_ex[:].unsqueeze(2).to_broadcast([128, NBS, 4, 8]), op=ALU.mult),
                        reads=["r_gm", "r_ex"], writes=["comb"])
                    wts = {}

                    def load_w(e):
                        w13, w13k = w13_rot.next()
                        w2, w2k = w2_rot.next()
                        P.dma("pool", lambda: nc.gpsimd.dma_start(out=w13[:, 0, :, :], in_=moe_w1[l, e].rearrange("(k p) f -> p k f", p=128)),
                              writes=[(w13k, 0)])
                        P.dma("pool", lambda: nc.gpsimd.dma_start(out=w13[:, 1, :, :], in_=moe_w3[l, e].rearrange("(k p) f -> p k f", p=128)),
                              writes=[(w13k, 1)])
                        P.dma("pool", lambda: nc.gpsimd.dma_start(out=w2[:], in_=moe_w2[l, e].rearrange("(c p) d -> p c d", p=128)),
                              writes=[w2k])
                        wts[e] = (w13, w13k, w2, w2k)

                    units = [(e, tch) for e in range(32) for tch in range(4)]
                    ust = {}

                    def stage_h(u):
                        e, tch = units[u]
                        w13, w13k, w2, w2k = wts[e]
                        he, hek = he_rot.next()
                        ust[u] = (he, hek)
                        for fch in range(2):
                            p1, p1k = hbank.next()
                            p3, p3k = hbank.next()
                            for j, (pt, pk) in enumerate(((p1, p1k), (p3, p3k))):
                                for k in range(8):
                                    P.op("pe", lambda pt=pt, j=j, k=k, fch=fch: nc.tensor.matmul(
                                        pt[:], lhsT=w13[:, j, k, fch * 128:(fch + 1) * 128], rhs=xT[:, k, tch * 512:(tch + 1) * 512],
                                        start=(k == 0), stop=(k == 7)),
                                        reads=[(w13k, j)] + [("xT", 4 * tch + i) for i in range(4)], writes=[pk])
                            s, sk = s_rot.next()
                            P.op("act", lambda s=s, p1=p1: nc.scalar.activation(out=s[:], in_=p1[:], func=AF.Silu), reads=[p1k], writes=[sk])
                            P.op("dve", lambda s=s, p3=p3, fch=fch: nc.vector.tensor_tensor(out=he[:, fch, :], in0=p3[:], in1=s[:], op=ALU.mult),
                                 reads=[p3k, sk], writes=[(hek, fch)])

                    def stage_y(u):
                        e, tch = units[u]
                        w13, w13k, w2, w2k = wts[e]
                        he, hek = ust.pop(u)
                        for tb in range(4):
                            blk = tch * 4 + tb
                            for half in range(2):
                                py, pyk = ybank.next()
                                for fch in range(2):
                                    P.op("pe", lambda py=py, fch=fch, tb=tb, half=half: nc.tensor.matmul(
                                        py[:], lhsT=he[:, fch, tb * 128:(tb + 1) * 128], rhs=w2[:, fch, half * 512:(half + 1) * 512],
                                        start=(fch == 0), stop=(fch == 1)), reads=[(hek, fch), (w2k, fch)], writes=[pyk])
                                P.op("dve", lambda py=py, blk=blk, half=half: nc.vector.scalar_tensor_tensor(
                                    out=hacc[:, blk, half * 512:(half + 1) * 512], in0=py[:], scalar=comb[:, blk, e:e + 1],
                                    in1=hacc[:, blk, half * 512:(half + 1) * 512], op0=ALU.mult, op1=ALU.add),
                                    reads=[pyk, "comb", ("hacc", blk, half)], writes=[("hacc", blk, half)])
                        if tch == 3:
                            wts.pop(e)
                            if e + 2 < 32:
                                load_w(e + 2)

                    load_w(0)
                    load_w(1)
                    nu = len(units)
                    stage_h(0)
                    for u in range(nu):
                        if u + 1 < nu:
                            stage_h(u + 1)
                        stage_y(u)
                    gbank = Rot(pb[0:4], "pb")
                    for blk in range(NBS):
                        b = sc * NBS + blk
                        hnT, hnk = hnT_rot.next()
                        norm_T(P, hacc[:, blk, :], hk(blk), gBp[:], "gBp", hnT[:], hnk)
                        pblk, pblkk = pblk_rot.next()
                        p16, p16k = pb16_rot.next()
                        pT, pTk = pT_rot.next()
                        P.dma("sp", lambda pblk=pblk, b=b: nc.sync.dma_start(out=pblk[:], in_=p[l, b * 128:(b + 1) * 128, :]), writes=[pblkk])
                        P.op("pool", lambda p16=p16, pblk=pblk: nc.gpsimd.tensor_copy(out=p16[:], in_=pblk[:]), reads=[pblkk], writes=[p16k])
                        for k in range(2):
                            P.op("pe", lambda p16=p16, k=k: nc.tensor.transpose(out=ptb[:, k, :], in_=p16[:, k * 128:(k + 1) * 128], identity=ident[:]),
                                 reads=[p16k, "ident"], writes=["ptb"])
                        P.op("act", lambda pT=pT: nc.scalar.copy(out=pT[:], in_=ptb[:, 0:2, :]), reads=["ptb"], writes=[pTk])
                        sg, sgk = sg_rot.next()
                        for half in range(2):
                            pg_, pgk = gbank.next()
                            pe_, pek = gbank.next()
                            for k in range(8):
                                P.op("pe", lambda pg_=pg_, hnT=hnT, k=k, half=half: nc.tensor.matmul(
                                    pg_[:], lhsT=hnT[:, k, :], rhs=wpg[:, k, half * 512:(half + 1) * 512], start=(k == 0), stop=(k == 7)),
                                    reads=[hnk, "wpg"], writes=[pgk])
                            for k in range(2):
                                P.op("pe", lambda pe_=pe_, pT=pT, k=k, half=half: nc.tensor.matmul(
                                    pe_[:], lhsT=pT[:, k, :], rhs=wpe[:, k, half * 512:(half + 1) * 512], start=(k == 0), stop=(k == 1)),
                                    reads=[pTk, "wpe"], writes=[pek])
                            P.op("act", lambda sg=sg, pg_=pg_, half=half: nc.scalar.activation(out=sg[:, half * 512:(half + 1) * 512], in_=pg_[:], func=AF.Sigmoid),
                                 reads=[pgk], writes=[(sgk, half)])
                            P.op("dve", lambda sg=sg, pe_=pe_, half=half: nc.vector.tensor_tensor(
                                out=sg[:, half * 512:(half + 1) * 512], in0=pe_[:], in1=sg[:, half * 512:(half + 1) * 512], op=ALU.mult),
                                reads=[pek, (sgk, half)], writes=[(sgk, half)])
                        P.op("dve", lambda sg=sg, blk=blk: nc.vector.tensor_tensor(out=hacc[:, blk, :], in0=hacc[:, blk, :], in1=sg[:], op=ALU.add),
                             reads=[(sgk, 0), (sgk, 1)] + hk(blk), writes=hk(blk))
                        if final:
                            junk, jk = njunk.next()
                            ss, ssk = nss.next()
                            norm_rstd(P, hacc[:, blk, :], hk(blk), 1024, junk[:], jk, ss[:], ssk)
                            P.op("dve", lambda ss=ss, blk=blk: nc.vector.scalar_tensor_tensor(
                                out=hacc[:, blk, :], in0=hacc[:, blk, :], scalar=ss[:], in1=gBo[:], op0=ALU.mult, op1=ALU.mult),
                                reads=hk(blk) + [ssk, "gBo"], writes=hk(blk))
                        P.dma("sp", lambda blk=blk, b=b: nc.sync.dma_start(out=hout[b * 128:(b + 1) * 128, :], in_=hacc[:, blk, :]),
                              reads=hk(blk), writes=[("hout", b)])
                    P.flush()

        def sparse_moe_pl_phase(l, hin, hout, final):
            TS = 512
            NT = 48
            NA = TS // 128
            with ExitStack() as st:
                def sb(name, shape, dt):
                    return st.enter_context(nc.sbuf_tensor(_un(name), shape, dt))
                gBf = sb("gBf", [128, 1024], F32)
                gBp = sb("gBp", [128, 1024], F32)
                load_gB(P, gBf[:], "gBf", norm_ffn[l])
                load_gB(P, gBp[:], "gBp", norm_pl[l])
                if final:
                    gBo = sb("gBo", [128, 1024], F32)
                    load_gB(P, gBo[:], "gBo", final_norm)
                wr = sb("wr", [128, 8, 36], BF16)
                P.dma("pool", lambda: nc.gpsimd.dma_start(out=wr[:, :, 0:4], in_=router_c[l].rearrange("(k p) n -> p k n", p=128)),
                      writes=["wr"])
                P.dma("pool", lambda: nc.gpsimd.dma_start(out=wr[:, :, 4:36], in_=router_f[l].rearrange("(k p) n -> p k n", p=128)),
                      writes=["wr"])
                rb = sb("rb", [128, 36], F32)
                P.dma("sp", lambda: nc.sync.dma_start(out=rb[:, 0:4], in_=router_c_b[l].partition_broadcast(128)), writes=["rb"])
                P.dma("sp", lambda: nc.sync.dma_start(out=rb[:, 4:36], in_=router_f_b[l].partition_broadcast(128)), writes=["rb"])
                wpg = sb("wpg", [128, 8, 1024], BF16)
                wpe = sb("wpe", [128, 2, 1024], BF16)
                P.dma("pool", lambda: nc.gpsimd.dma_start(out=wpg[:], in_=w_pg[l].rearrange("(k p) n -> p k n", p=128)), writes=["wpg"])
                P.dma("pool", lambda: nc.gpsimd.dma_start(out=wpe[:], in_=w_pe[l].rearrange("(k p) n -> p k n", p=128)), writes=["wpe"])
                idx1_i = sb("idx1_i", [128, NB], I32)
                idx2_i = sb("idx2_i", [128, NB], I32)
                gate1 = sb("gate1", [128, NB], F32)
                gate2 = sb("gate2", [128, NB], F32)
                widx_i = sb("widx_i", [128, NT], I32)
                pio = sb("pio", [128, 1], F32)
                P.op("pool", lambda: nc.gpsimd.iota(pio[:], pattern=[[0, 1]], base=l * 32 * 128, channel_multiplier=1,
                                                    allow_small_or_imprecise_dtypes=True), writes=["pio"])
                with ExitStack() as st1:
                    def sb1(name, shape, dt):
                        return st1.enter_context(nc.sbuf_tensor(_un(name), shape, dt))
                    xnall = sb1("xnall", [128, NB, 1024], BF16)
                    ht_rot = Rot([sb1(f"ht{i}", [128, 1024], F32) for i in range(2)], "ht")
                    xTb_rot = Rot([sb1(f"xTb{i}", [128, 8, 128], BF16) for i in range(2)], "xTb")
                    lg = sb1("lg", [128, NB, 36], F32)
                    r_mx = sb1("r_mx", [128, NB], F32)
                    r_gm = sb1("r_gm", [128, NB, 4], F32)
                    r_ec = sb1("r_ec", [128, NB, 4], F32)
                    r_pg = sb1("r_pg", [128, NB], F32)
                    r_t = sb1("r_t", [128, NB, 4, 8], F32)
                    r_lfs = sb1("r_lfs", [128, NB, 8], F32)
                    r_t8 = sb1("r_t8", [128, NB, 8], F32)
                    r_sel = sb1("r_sel", [128, NB, 8], F32)
                    r_s1 = sb1("r_s1", [128, NB, 8], F32)
                    r_s2 = sb1("r_s2", [128, NB, 8], F32)
                    r_ex = sb1("r_ex", [128, NB, 8], F32)
                    r_d = sb1("r_d", [128, NB], F32)
                    r_tmp8 = sb1("r_tmp8", [128, NB, 8], F32)
                    M1 = sb1("M1", [128, NB, 32], F32)
                    M2 = sb1("M2", [128, NB, 32], F32)
                    Mb16 = sb1("Mb16", [128, NB, 32], BF16)
                    Rm16 = sb1("Rm16", [128, NB + 1, 32], BF16)
                    rank = sb1("rank", [128, NB, 32], F32)
                    cnt = sb1("cnt", [128, 32], F32)
                    thr16 = sb1("thr16", [128, 16], F32)
                    thr64 = sb1("thr64", [128, NT], F32)
                    cmpA = sb1("cmpA", [128, 32, 16], F32)
                    ntile = sb1("ntile", [128, 32], F32)
                    csA = sb1("csA", [128, 32], F32)
                    csB = sb1("csB", [128, 32], F32)
                    base = sb1("base", [128, 32], F32)
                    cmpB = sb1("cmpB", [128, NT, 32], F32)
                    te_f = sb1("te_f", [128, NT], F32)
                    tail = sb1("tail", [128, NT], F32)
                    tot = sb1("tot", [128, 1], F32)
                    idx_f = sb1("idx_f", [128, NB], F32)
                    tmp32 = sb1("tmp32", [128, NB, 32], F32)
                    ybank = Rot(pb[4:7], "pby")
                    for b in range(NB):
                        ht, htk = ht_rot.next()
                        P.dma("sp", lambda ht=ht, b=b: nc.sync.dma_start(out=ht[:], in_=hin[b * 128:(b + 1) * 128, :]), writes=[htk])
                        junk, jk = njunk.next()
                        ss, ssk = nss.next()
                        norm_rstd(P, ht[:], htk, 1024, junk[:], jk, ss[:], ssk)
                        P.op("dve", lambda ht=ht, ss=ss, b=b: nc.vector.scalar_tensor_tensor(
                            out=xnall[:, b, :], in0=ht[:], scalar=ss[:], in1=gBf[:], op0=ALU.mult, op1=ALU.mult),
                            reads=[htk, ssk, "gBf"], writes=[("xnall", b)])
                        for k in range(8):
                            P.op("pe", lambda k=k, b=b: nc.tensor.transpose(out=ptb[:, k, :], in_=xnall[:, b, k * 128:(k + 1) * 128], identity=ident[:]),
                                 reads=[("xnall", b), "ident"], writes=["ptb"])
                        xTb, xTbk = xTb_rot.next()
                        P.op("act", lambda xTb=xTb: nc.scalar.copy(out=xTb[:], in_=ptb[:]), reads=["ptb"], writes=[xTbk])
                        pt, pk = ybank.next()
                        for k in range(8):
                            P.op("pe", lambda pt=pt, k=k, xTb=xTb: nc.tensor.matmul(
                                pt[:, 0:36], lhsT=xTb[:, k, :], rhs=wr[:, k, :], start=(k == 0), stop=(k == 7)),
                                reads=[xTbk, "wr"], writes=[pk])
                        P.op("dve", lambda pt=pt, b=b: nc.vector.tensor_tensor(out=lg[:, b, :], in0=pt[:, 0:36], in1=rb[:], op=ALU.add),
                             reads=[pk, "rb"], writes=["lg"])
                    lc = lg[:, :, 0:4]
                    lf = lg[:, :, 4:36].rearrange("p b (g e) -> p b g e", g=4)

                    def bc(ap, shape):
                        return ap.to_broadcast(shape)

                    def dv(fn, reads, writes):
                        P.op("dve", fn, reads=reads, writes=writes)
                    dv(lambda: nc.vector.tensor_reduce(out=r_mx[:], in_=lc, axis=AX.X, op=ALU.max), ["lg"], ["r_mx"])
                    dv(lambda: nc.vector.tensor_tensor(out=r_gm[:], in0=lc, in1=bc(r_mx[:].unsqueeze(2), [128, NB, 4]), op=ALU.is_ge), ["lg", "r_mx"], ["r_gm"])
                    dv(lambda: nc.vector.tensor_tensor(out=r_ec[:], in0=lc, in1=bc(r_mx[:].unsqueeze(2), [128, NB, 4]), op=ALU.subtract), ["lg", "r_mx"], ["r_ec"])
                    P.op("act", lambda: nc.scalar.activation(out=r_ec[:], in_=r_ec[:], func=AF.Exp), reads=["r_ec"], writes=["r_ec"])
                    dv(lambda: nc.vector.tensor_reduce(out=r_pg[:], in_=r_ec[:], axis=AX.X, op=ALU.add), ["r_ec"], ["r_pg"])
                    dv(lambda: nc.vector.tensor_tensor(out=r_t[:], in0=lf, in1=bc(r_gm[:].unsqueeze(3), [128, NB, 4, 8]), op=ALU.mult), ["lg", "r_gm"], ["r_t"])
                    dv(lambda: nc.vector.tensor_reduce(out=r_lfs[:], in_=r_t[:].rearrange("p b g e -> p b e g"), axis=AX.X, op=ALU.add), ["r_t"], ["r_lfs"])
                    for b in range(NB):
                        dv(lambda b=b: nc.vector.max(out=r_t8[:, b, :], in_=r_lfs[:, b, :]), ["r_lfs"], ["r_t8"])
                    l1b = bc(r_t8[:, :, 0:1], [128, NB, 8])
                    l2b = bc(r_t8[:, :, 1:2], [128, NB, 8])
                    dv(lambda: nc.vector.tensor_tensor(out=r_sel[:], in0=r_lfs[:], in1=l2b, op=ALU.is_ge), ["r_lfs", "r_t8"], ["r_sel"])
                    dv(lambda: nc.vector.tensor_tensor(out=r_s1[:], in0=r_lfs[:], in1=l1b, op=ALU.is_ge), ["r_lfs", "r_t8"], ["r_s1"])
                    dv(lambda: nc.vector.tensor_tensor(out=r_s2[:], in0=r_sel[:], in1=r_s1[:], op=ALU.subtract), ["r_sel", "r_s1"], ["r_s2"])
                    dv(lambda: nc.vector.tensor_tensor(out=r_ex[:], in0=r_lfs[:], in1=l1b, op=ALU.subtract), ["r_lfs", "r_t8"], ["r_ex"])
                    P.op("act", lambda: nc.scalar.activation(out=r_ex[:], in_=r_ex[:], func=AF.Exp), reads=["r_ex"], writes=["r_ex"])
                    dv(lambda: nc.vector.tensor_tensor(out=r_ex[:], in0=r_ex[:], in1=r_sel[:], op=ALU.mult), ["r_ex", "r_sel"], ["r_ex"])
                    dv(lambda: nc.vector.tensor_reduce(out=r_d[:], in_=r_ex[:], axis=AX.X, op=ALU.add), ["r_ex"], ["r_d"])
                    dv(lambda: nc.vector.tensor_tensor(out=r_d[:], in0=r_d[:], in1=r_pg[:], op=ALU.mult), ["r_d", "r_pg"], ["r_d"])
                    dv(lambda: nc.vector.reciprocal(out=r_d[:], in_=r_d[:]), ["r_d"], ["r_d"])
                    dv(lambda: nc.vector.tensor_tensor(out=r_ex[:], in0=r_ex[:], in1=bc(r_d[:].unsqueeze(2), [128, NB, 8]), op=ALU.mult), ["r_ex", "r_d"], ["r_ex"])
                    dv(lambda: nc.vector.tensor_tensor(out=r_tmp8[:], in0=r_ex[:], in1=r_s1[:], op=ALU.mult), ["r_ex", "r_s1"], ["r_tmp8"])
                    dv(lambda: nc.vector.tensor_reduce(out=gate1[:], in_=r_tmp8[:], axis=AX.X, op=ALU.add), ["r_tmp8"], ["gate1"])
                    dv(lambda: nc.vector.tensor_tensor(out=r_tmp8[:], in0=r_ex[:], in1=r_s2[:], op=ALU.mult), ["r_ex", "r_s2", "gate1"], ["r_tmp8"])
                    dv(lambda: nc.vector.tensor_reduce(out=gate2[:], in_=r_tmp8[:], axis=AX.X, op=ALU.add), ["r_tmp8"], ["gate2"])
                    g4 = bc(r_gm[:].unsqueeze(3), [128, NB, 4, 8])
                    dv(lambda: nc.vector.tensor_tensor(out=M1[:].rearrange("p b (g e) -> p b g e", g=4), in0=g4,
                                                       in1=bc(r_s1[:].unsqueeze(2), [128, NB, 4, 8]), op=ALU.mult), ["r_gm", "r_s1"], ["M1"])
                    dv(lambda: nc.vector.tensor_tensor(out=M2[:].rearrange("p b (g e) -> p b g e", g=4), in0=g4,
                                                       in1=bc(r_s2[:].unsqueeze(2), [128, NB, 4, 8]), op=ALU.mult), ["r_gm", "r_s2"], ["M2"])
                    dv(lambda: nc.vector.tensor_tensor(out=Mb16[:], in0=M1[:], in1=M2[:], op=ALU.add), ["M1", "M2"], ["Mb16"])
                    P.op("pool", lambda: nc.gpsimd.memset(Rm16[:, 0, :], 0.0), writes=[("Rm", 0)])
                    for b in range(NB):
                        dv(lambda b=b: nc.vector.tensor_tensor(out=Rm16[:, b + 1, :], in0=Rm16[:, b, :], in1=Mb16[:, b, :], op=ALU.add),
                           [("Rm", b), "Mb16"], [("Rm", b + 1)])
                    rbank = [pb[0], pb[1]]
                    for b in range(NB):
                        pt = rbank[b // 16]
                        sl = slice((b % 16) * 32, (b % 16) * 32 + 32)
                        P.op("pe", lambda pt=pt, sl=sl, b=b: nc.tensor.matmul(pt[:, sl], lhsT=lstrict[:], rhs=Mb16[:, b, :], start=True, stop=False),
                             reads=["lstrict", "Mb16"], writes=[("pb", b // 16)])
                        P.op("pe", lambda pt=pt, sl=sl, b=b: nc.tensor.matmul(pt[:, sl], lhsT=pones[:], rhs=Rm16[:, b, :], start=False, stop=True),
                             reads=["pones", ("Rm", b)], writes=[("pb", b // 16)])
                    for hf in range(2):
                        dv(lambda hf=hf: nc.vector.tensor_copy(out=rank[:, hf * 16:(hf + 1) * 16, :],
                                                               in_=rbank[hf][:].rearrange("p (b e) -> p b e", b=16)),
                           [("pb", hf)], [("rank", hf)])
                    P.op("pe", lambda: nc.tensor.matmul(pb[2][:, 0:32], lhsT=pones[:], rhs=Rm16[:, NB, :], start=True, stop=True),
                         reads=["pones", ("Rm", NB)], writes=[("pb", 2)])
                    dv(lambda: nc.vector.tensor_copy(out=cnt[:], in_=pb[2][:, 0:32]), [("pb", 2)], ["cnt"])
                    P.op("pool", lambda: nc.gpsimd.iota(thr16[:], pattern=[[TS, 16]], base=0, channel_multiplier=0, allow_small_or_imprecise_dtypes=True), writes=["thr16"])
                    P.op("pool", lambda: nc.gpsimd.iota(thr64[:], pattern=[[TS, NT]], base=0, channel_multiplier=0, allow_small_or_imprecise_dtypes=True), writes=["thr64"])
                    dv(lambda: nc.vector.tensor_tensor(out=cmpA[:], in0=bc(cnt[:].unsqueeze(2), [128, 32, 16]), in1=bc(thr16[:].unsqueeze(1), [128, 32, 16]), op=ALU.is_gt),
                       ["cnt", "thr16"], ["cmpA"])
                    dv(lambda: nc.vector.tensor_reduce(out=ntile[:], in_=cmpA[:], axis=AX.X, op=ALU.add), ["cmpA"], ["ntile"])
                    src, srck = ntile, "ntile"
                    bufs = [(csA, "csA"), (csB, "csB")]
                    for si, sh in enumerate((1, 2, 4, 8, 16)):
                        dst, dstk = bufs[si % 2]
                        dv(lambda src=src, dst=dst, sh=sh: nc.vector.tensor_copy(out=dst[:, 0:sh], in_=src[:, 0:sh]), [srck], [(dstk, 0)])
                        dv(lambda src=src, dst=dst, sh=sh: nc.vector.tensor_tensor(out=dst[:, sh:32], in0=src[:, sh:32], in1=src[:, 0:32 - sh], op=ALU.add),
                           [srck], [(dstk, 1)])
                        src, srck = dst, dstk
                        srck_list = [(dstk, 0), (dstk, 1)]
                        srck = dstk
                        P.op("dve", lambda dst=dst: nc.vector.tensor_copy(out=dst[:, 0:1], in_=dst[:, 0:1]), reads=srck_list, writes=[dstk])
                    dv(lambda src=src: nc.vector.tensor_tensor(out=base[:], in0=src[:], in1=ntile[:], op=ALU.subtract), [srck, "ntile"], ["base"])
                    dv(lambda: nc.vector.tensor_scalar(out=base[:], in0=base[:], scalar1=float(TS), scalar2=None, op0=ALU.mult), ["base"], ["base"])
                    dv(lambda: nc.vector.tensor_tensor(out=cmpB[:], in0=bc(base[:].unsqueeze(1), [128, NT, 32]), in1=bc(thr64[:].unsqueeze(2), [128, NT, 32]), op=ALU.is_le),
                       ["base", "thr64"], ["cmpB"])
                    dv(lambda: nc.vector.tensor_reduce(out=te_f[:], in_=cmpB[:], axis=AX.X, op=ALU.add), ["cmpB"], ["te_f"])
                    dv(lambda: nc.vector.tensor_scalar(out=te_f[:], in0=te_f[:], scalar1=-1.0, scalar2=128.0, op0=ALU.add, op1=ALU.mult), ["te_f"], ["te_f"])
                    dv(lambda: nc.vector.tensor_scalar(out=te_f[:], in0=te_f[:], scalar1=pio[:, 0:1], scalar2=None, op0=ALU.add), ["te_f", "pio"], ["te_f"])
                    dv(lambda src=src: nc.vector.tensor_scalar(out=tot[:], in0=src[:, 31:32], scalar1=float(TS), scalar2=None, op0=ALU.mult), [srck], ["tot"])
                    dv(lambda: nc.vector.tensor_scalar(out=tail[:], in0=thr64[:], scalar1=tot[:, 0:1], scalar2=1.0e6, op0=ALU.is_ge, op1=ALU.mult),
                       ["thr64", "tot"], ["tail"])
                    dv(lambda: nc.vector.tensor_tensor(out=te_f[:], in0=te_f[:], in1=tail[:], op=ALU.add), ["te_f", "tail"], ["te_f"])
                    dv(lambda: nc.vector.tensor_copy(out=widx_i[:], in_=te_f[:]), ["te_f"], ["widx_i"])
                    dv(lambda: nc.vector.tensor_tensor(out=rank[:], in0=rank[:], in1=bc(base[:].unsqueeze(1), [128, NB, 32]), op=ALU.add),
                       [("rank", 0), ("rank", 1), "base"], ["rankp"])
                    for (Mk, Mkk, idxi, idxk) in ((M1, "M1", idx1_i, "idx1_i"), (M2, "M2", idx2_i, "idx2_i")):
                        dv(lambda Mk=Mk: nc.vector.tensor_tensor(out=tmp32[:], in0=rank[:], in1=Mk[:], op=ALU.mult), ["rankp", Mkk, "idx_f"], ["tmp32"])
                        dv(lambda: nc.vector.tensor_reduce(out=idx_f[:], in_=tmp32[:], axis=AX.X, op=ALU.add), ["tmp32"], ["idx_f"])
                        dv(lambda idxi=idxi: nc.vector.tensor_copy(out=idxi[:], in_=idx_f[:]), ["idx_f"], [idxk])
                    for b in range(NB):
                        for (idxi, idxk) in ((idx1_i, "idx1_i"), (idx2_i, "idx2_i")):
                            P.dma("pool", lambda b=b, idxi=idxi: nc.gpsimd.indirect_dma_start(
                                out=XS[:, :], out_offset=bass.IndirectOffsetOnAxis(ap=idxi[:, b:b + 1], axis=0),
                                in_=xnall[:, b, :], in_offset=None), reads=[("xnall", b), idxk], writes=["XS"])
                    P.flush()
                if sub < 2:
                    return
                with ExitStack() as st2_:
                    def sb2(name, shape, dt):
                        return st2_.enter_context(nc.sbuf_tensor(_un(name), shape, dt))
                    w13_rot = Rot([sb2(f"w13_{i}", [128, 2, 8, 256], BF16) for i in range(2)], "w13")
                    w2_rot = Rot([sb2(f"w2_{i}", [128, 2, 1024], BF16) for i in range(2)], "w2")
                    stg_rot = Rot([sb2(f"stg{i}", [128, 3, 2048], F32) for i in range(3)], "stg")
                    xs_rot = Rot([sb2(f"xs{i}", [128, NA, 1024], BF16) for i in range(2)], "xs")
                    xT_rot = Rot([sb2(f"xTt{i}", [128, 8, TS], BF16) for i in range(2)], "xTt")
                    s_rot = Rot([sb2(f"s{i}", [128, TS], F32) for i in range(2)], "s")
                    he_rot = Rot([sb2(f"he{i}", [128, 2, TS], BF16) for i in range(2)], "he")
                    yt_rot = Rot([sb2(f"yt{i}", [128, 1024], F32) for i in range(3)], "yt")
                    hbank = Rot(pb[0:4], "pb")
                    ybank = Rot(pb[4:7], "pby")
                    wts = {}
                    tst = {}
                    bcreg = {}

                    def mkreg():
                        bcreg["r"] = nc.gpsimd.to_reg(2 * 32 * 128 - 1)
                        return None
                    P.op("pool", mkreg, nosig=True)

                    wstg = {}

                    def cast_w(i):
                        stg, stgk = wstg.pop(i)
                        w13, w13k = w13_rot.next()
                        w2, w2k = w2_rot.next()
                        wts[i] = (w13, w13k, w2, w2k)
                        P.op("act", lambda: nc.scalar.copy(out=w13[:, 0, :, :].rearrange("p k f -> p (k f)"), in_=stg[:, 0, :]),
                             reads=[(stgk, 0)], writes=[(w13k, 0)])
                        P.op("dve", lambda: nc.vector.tensor_copy(out=w13[:, 1, :, :].rearrange("p k f -> p (k f)"), in_=stg[:, 1, :]),
                             reads=[(stgk, 1)], writes=[(w13k, 1)])
                        P.op("act", lambda: nc.scalar.copy(out=w2[:, 0, :], in_=stg[:, 2, 0:1024]),
                             reads=[(stgk, 2)], writes=[(w2k, 0)])
                        P.op("dve", lambda: nc.vector.tensor_copy(out=w2[:, 1, :], in_=stg[:, 2, 1024:2048]),
                             reads=[(stgk, 2)], writes=[(w2k, 1)])

                    def load_w(i):
                        stg, stgk = stg_rot.next()
                        ix = bass.IndirectOffsetOnAxis(ap=widx_i[:, i:i + 1], axis=0)
                        for j, (src_ap, pat, kw) in enumerate(((moe_w1, "l e (p k) f -> (l e p) (k f)", dict(k=8)),
                                                              (moe_w3, "l e (p k) f -> (l e p) (k f)", dict(k=8)),
                                                              (moe_w2, "l e (p c) d -> (l e p) (c d)", dict(c=2)))):
                            P.dma("pool", lambda j=j, src_ap=src_ap, pat=pat, kw=kw: nc.gpsimd.indirect_dma_start(
                                out=stg[:, j, :], out_offset=None, in_=src_ap.rearrange(pat, **kw), in_offset=ix,
                                bounds_check=bcreg["r"], oob_is_err=False),
                                reads=["widx_i"], writes=[(stgk, j)])
                        wstg[i] = (stg, stgk)

                    xsl = {}

                    def load_xs(i):
                        xs, xsk = xs_rot.next()
                        P.dma("sp", lambda: nc.sync.dma_start(out=xs[:], in_=XS[i * TS:(i + 1) * TS, :].rearrange("(a p) d -> p a d", p=128)),
                              writes=[xsk])
                        xsl[i] = (xs, xsk)

                    def stage_t(i):
                        cast_w(i)
                        if i + 3 < NT:
                            load_w(i + 3)
                        if i + 1 < NT:
                            load_xs(i + 1)
                        xs, xsk = xsl.pop(i)
                        xT, xTk = xT_rot.next()
                        for a in range(NA):
                            for k in range(8):
                                P.op("pe", lambda a=a, k=k: nc.tensor.transpose(out=ptb[:, k, :], in_=xs[:, a, k:1024:8], identity=ident[:]),
                                     reads=[xsk, "ident"], writes=["ptb"])
                            if a % 2 == 0:
                                P.op("act", lambda a=a: nc.scalar.copy(out=xT[:, :, a * 128:(a + 1) * 128], in_=ptb[:]), reads=["ptb"], writes=[(xTk, a)])
                            else:
                                P.op("dve", lambda a=a: nc.vector.tensor_copy(out=xT[:, :, a * 128:(a + 1) * 128], in_=ptb[:]), reads=["ptb"], writes=[(xTk, a)])
                        w13, w13k, w2, w2k = wts[i]
                        he, hek = he_rot.next()
                        tst[i] = (he, hek)
                        for fch in range(2):
                            p1, p1k = hbank.next()
                            p3, p3k = hbank.next()
                            for j, (pt, pk) in enumerate(((p1, p1k), (p3, p3k))):
                                for k in range(8):
                                    P.op("pe", lambda pt=pt, j=j, k=k, fch=fch: nc.tensor.matmul(
                                        pt[:, 0:TS], lhsT=w13[:, j, k, fch:256:2], rhs=xT[:, k, :],
                                        start=(k == 0), stop=(k == 7)),
                                        reads=[(w13k, j)] + [(xTk, a_) for a_ in range(NA)], writes=[pk])
                            s, sk = s_rot.next()
                            P.op("act", lambda s=s, p1=p1: nc.scalar.activation(out=s[:], in_=p1[:, 0:TS], func=AF.Silu), reads=[p1k], writes=[sk])
                            P.op("dve", lambda s=s, p3=p3, fch=fch: nc.vector.tensor_tensor(out=he[:, fch, :], in0=p3[:, 0:TS], in1=s[:], op=ALU.mult),
                                 reads=[p3k, sk], writes=[(hek, fch)])

                    def stage_y(i):
                        w13, w13k, w2, w2k = wts.pop(i)
                        he, hek = tst.pop(i)
                        for a in range(NA):
                            yt, ytk = yt_rot.next()
                            for half in range(2):
                                py, pyk = ybank.next()
                                for fch in range(2):
                                    P.op("pe", lambda py=py, fch=fch, a=a, half=half: nc.tensor.matmul(
                                        py[:], lhsT=he[:, fch, a * 128:(a + 1) * 128], rhs=w2[:, fch, half * 512:(half + 1) * 512],
                                        start=(fch == 0), stop=(fch == 1)), reads=[(hek, fch), (w2k, fch)], writes=[pyk])
                                if half == 0:
                                    P.op("act", lambda py=py, yt=yt, half=half: nc.scalar.copy(out=yt[:, half * 512:(half + 1) * 512], in_=py[:]),
                                         reads=[pyk], writes=[(ytk, half)])
                                else:
                                    P.op("dve", lambda py=py, yt=yt, half=half: nc.vector.tensor_copy(out=yt[:, half * 512:(half + 1) * 512], in_=py[:]),
                                         reads=[pyk], writes=[(ytk, half)])
                            r0 = i * TS + a * 128
                            P.dma("sp", lambda yt=yt, r0=r0: nc.sync.dma_start(out=YS[r0:r0 + 128, :], in_=yt[:]),
                                  reads=[(ytk, 0), (ytk, 1)], writes=[("YS", i, a)])

                    load_w(0)
                    load_w(1)
                    load_w(2)
                    load_xs(0)
                    stage_t(0)
                    for i in range(NT):
                        if i + 1 < NT:
                            stage_t(i + 1)
                        stage_y(i)
                    P.flush()
                if sub < 3:
                    return
                with ExitStack() as st3_:
                    def sb3(name, shape, dt):
                        return st3_.enter_context(nc.sbuf_tensor(_un(name), shape, dt))
                    ht_rot = Rot([sb3(f"htc{i}", [128, 1024], F32) for i in range(5)], "htc")
                    y1_rot = Rot([sb3(f"y1_{i}", [128, 1024], F32) for i in range(4)], "y1")
                    y2_rot = Rot([sb3(f"y2_{i}", [128, 1024], F32) for i in range(4)], "y2")
                    pblk_rot = Rot([sb3(f"pblk{i}", [128, 256], F32) for i in range(4)], "pblk")
                    pb16_rot = Rot([sb3(f"pb16{i}", [128, 256], BF16) for i in range(2)], "pb16")
                    pT_rot = Rot([sb3(f"pT{i}", [128, 2, 128], BF16) for i in range(2)], "pT")
                    hnT_rot = Rot([sb3(f"hnT{i}", [128, 8, 128], BF16) for i in range(2)], "hnT")
                    sg_rot = Rot([sb3(f"sg{i}", [128, 1024], F32) for i in range(2)], "sg")
                    gbank = Rot(pb[0:4], "pb")
                    ld = {}

                    def loads7(b):
                        ht, htk = ht_rot.next()
                        y1, y1k = y1_rot.next()
                        y2, y2k = y2_rot.next()
                        pblk, pblkk = pblk_rot.next()
                        P.dma("sp", lambda: nc.sync.dma_start(out=ht[:], in_=hin[b * 128:(b + 1) * 128, :]), writes=[htk])
                        P.dma("sp", lambda: nc.sync.dma_start(out=pblk[:], in_=p[l, b * 128:(b + 1) * 128, :]), writes=[pblkk])
                        P.dma("pool", lambda: nc.gpsimd.indirect_dma_start(
                            out=y1[:, :], out_offset=None, in_=YS[:, :], in_offset=bass.IndirectOffsetOnAxis(ap=idx1_i[:, b:b + 1], axis=0)),
                            reads=["idx1_i"], writes=[y1k])
                        P.dma("pool", lambda: nc.gpsimd.indirect_dma_start(
                            out=y2[:, :], out_offset=None, in_=YS[:, :], in_offset=bass.IndirectOffsetOnAxis(ap=idx2_i[:, b:b + 1], axis=0)),
                            reads=["idx2_i"], writes=[y2k])
                        ld[b] = (ht, htk, y1, y1k, y2, y2k, pblk, pblkk)

                    loads7(0)
                    loads7(1)
                    for b in range(NB):
                        if b + 2 < NB:
                            loads7(b + 2)
                        ht, htk, y1, y1k, y2, y2k, pblk, pblkk = ld.pop(b)
                        P.op("dve", lambda ht=ht, y1=y1, b=b: nc.vector.scalar_tensor_tensor(
                            out=ht[:], in0=y1[:], scalar=gate1[:, b:b + 1], in1=ht[:], op0=ALU.mult, op1=ALU.add),
                            reads=[htk, y1k, "gate1"], writes=[htk])
                        P.op("dve", lambda ht=ht, y2=y2, b=b: nc.vector.scalar_tensor_tensor(
                            out=ht[:], in0=y2[:], scalar=gate2[:, b:b + 1], in1=ht[:], op0=ALU.mult, op1=ALU.add),
                            reads=[htk, y2k, "gate2"], writes=[htk])
                        hnT, hnk = hnT_rot.next()
                        norm_T(P, ht[:], htk, gBp[:], "gBp", hnT[:], hnk)
                        p16, p16k = pb16_rot.next()
                        pT, pTk = pT_rot.next()
                        P.op("pool", lambda p16=p16, pblk=pblk: nc.gpsimd.tensor_copy(out=p16[:], in_=pblk[:]), reads=[pblkk], writes=[p16k])
                        for k in range(2):
                            P.op("pe", lambda p16=p16, k=k: nc.tensor.transpose(out=ptb[:, k, :], in_=p16[:, k * 128:(k + 1) * 128], identity=ident[:]),
                                 reads=[p16k, "ident"], writes=["ptb"])
                        P.op("act", lambda pT=pT: nc.scalar.copy(out=pT[:], in_=ptb[:, 0:2, :]), reads=["ptb"], writes=[pTk])
                        sg, sgk = sg_rot.next()
                        for half in range(2):
                            pg_, pgk = gbank.next()
                            pe_, pek = gbank.next()
                            for k in range(8):
                                P.op("pe", lambda pg_=pg_, hnT=hnT, k=k, half=half: nc.tensor.matmul(
                                    pg_[:], lhsT=hnT[:, k, :], rhs=wpg[:, k, half * 512:(half + 1) * 512], start=(k == 0), stop=(k == 7)),
                                    reads=[hnk, "wpg"], writes=[pgk])
                            for k in range(2):
                                P.op("pe", lambda pe_=pe_, pT=pT, k=k, half=half: nc.tensor.matmul(
                                    pe_[:], lhsT=pT[:, k, :], rhs=wpe[:, k, half * 512:(half + 1) * 512], start=(k == 0), stop=(k == 1)),
                                    reads=[pTk, "wpe"], writes=[pek])
                            P.op("act", lambda sg=sg, pg_=pg_, half=half: nc.scalar.activation(out=sg[:, half * 512:(half + 1) * 512], in_=pg_[:], func=AF.Sigmoid),
                                 reads=[pgk], writes=[(sgk, half)])
                            P.op("dve", lambda sg=sg, pe_=pe_, half=half: nc.vector.tensor_tensor(
                                out=sg[:, half * 512:(half + 1) * 512], in0=pe_[:], in1=sg[:, half * 512:(half + 1) * 512], op=ALU.mult),
                                reads=[pek, (sgk, half)], writes=[(sgk, half)])
                        P.op("pool", lambda sg=sg, ht=ht: nc.gpsimd.tensor_tensor(out=ht[:], in0=ht[:], in1=sg[:], op=ALU.add),
                             reads=[(sgk, 0), (sgk, 1), htk], writes=[htk])
                        if final:
                            junk, jk = njunk.next()
                            ss, ssk = nss.next()
                            norm_rstd(P, ht[:], htk, 1024, junk[:], jk, ss[:], ssk)
                            P.op("dve", lambda ss=ss, ht=ht: nc.vector.scalar_tensor_tensor(
                                out=ht[:], in0=ht[:], scalar=ss[:], in1=gBo[:], op0=ALU.mult, op1=ALU.mult),
                                reads=[htk, ssk, "gBo"], writes=[htk])
                        P.dma("sp", lambda ht=ht, b=b: nc.sync.dma_start(out=hout[b * 128:(b + 1) * 128, :], in_=ht[:]),
                              reads=[htk], writes=[("hout", b)])
                    P.flush()

        SPARSE = True
        mphase = sparse_moe_pl_phase if SPARSE else moe_pl_phase
        if stop_after >= 5:
            mphase(0, hA, hB, False)

        if stop_after >= 6:
            with ExitStack() as st:
                def sb(name, shape, dt):
                    return st.enter_context(nc.sbuf_tensor(_un(name), shape, dt))
                wi = sb("wi", [128, 8, 4096], BF16)
                wi_src = w_in_odd[0].rearrange("(k p) n -> p k n", p=128)
                for k in range(8):
                    P.dma("pool", lambda k=k: nc.gpsimd.dma_start(out=wi[:, k, :], in_=wi_src[:, k, :]), writes=[("wi", k)])
                wi_keys = [("wi", k) for k in range(8)]
                wo1 = sb("wo1", [128, 16, 1024], BF16)
                wo_src = w_out_odd[0].rearrange("(c p) n -> p c n", p=128)
                for c in range(0, 16, 4):
                    P.dma("pool", lambda c=c: nc.gpsimd.dma_start(out=wo1[:, c:c + 4, :], in_=wo_src[:, c:c + 4, :]), writes=[("wo1", c)])
                wo_keys = [("wo1", c) for c in range(0, 16, 4)]
                gB1 = sb("gB1", [128, 1024], F32)
                load_gB(P, gB1[:], "gB1", norm_mix[1])
                gvB = sb("gvB", [128, 2048], F32)
                P.dma("sp", lambda: nc.sync.dma_start(out=gvB[:], in_=g_v_odd[0].partition_broadcast(128)), writes=["gvB"])
                bsf = sb("bsf", [1, 8, 128], F32)
                bs16 = sb("bs16", [1, 8, 128], BF16)
                P.dma("sp", lambda: nc.sync.dma_start(out=bsf[:], in_=b_s_odd[0:1]), writes=["bsf"])
                P.op("dve", lambda: nc.vector.tensor_copy(out=bs16[:], in_=bsf[:]), reads=["bsf"], writes=["bs16"])
                wsT = sb("wsT", [128, 8, 128], BF16)
                st3 = ExitStack()
                st3.__enter__()
                wsf = st3.enter_context(nc.sbuf_tensor(_un("wsf"), [128, 8, 128], F32))
                ws16 = st3.enter_context(nc.sbuf_tensor(_un("ws16"), [128, 8, 128], BF16))
                P.dma("sp", lambda: nc.sync.dma_start(out=wsf[:], in_=w_s_odd[0].rearrange("g t s -> t g s")), writes=["wsf"])
                for g in range(8):
                    P.op("pool", lambda g=g: nc.gpsimd.affine_select(out=wsf[:, g, :], in_=wsf[:, g, :], pattern=[[-1, 128]],
                                                                      compare_op=ALU.is_ge, fill=0.0, base=0, channel_multiplier=1),
                         reads=["wsf"], writes=["wsf"])
                P.op("dve", lambda: nc.vector.tensor_copy(out=ws16[:], in_=wsf[:]), reads=["wsf"], writes=["ws16"])
                for g in range(8):
                    P.op("pe", lambda g=g: nc.tensor.transpose(out=ptb[:, g, :], in_=ws16[:, g, :], identity=ident[:]),
                         reads=["ws16", "ident"], writes=["ptb"])
                P.op("dve", lambda: nc.vector.tensor_copy(out=wsT[:], in_=ptb[:]), reads=["ptb"], writes=["wsT"])
                P.flush()
                st3.__exit__(None, None, None)
                ht_rot = Rot([sb(f"ht{i}", [128, 1024], F32) for i in range(5)], "ht")
                xg_rot = Rot([sb(f"xg{i}", [128, 8, 512], BF16) for i in range(2)], "xg")
                uT_rot = Rot([sb(f"uT{i}", [128, 16, 512], BF16) for i in range(1)], "uT")
                vt_rot = Rot([sb(f"vt{i}", [128, 2048], F32) for i in range(2)], "vt")
                vn_rot = Rot([sb(f"vn{i}", [128, 2048], BF16) for i in range(1)], "vn")
                yT_rot = Rot([sb(f"yT{i}", [128, 16, 128], BF16) for i in range(2)], "yT")
                abank = Rot(pb[0:3], "pb")
                gbank = Rot(pb[3:5], "pbg")
                obank = Rot(pb[5:7], "pbo")
                for tc in range(8):
                    xg, xgk = xg_rot.next()
                    hts = []
                    for tb in range(4):
                        b = tc * 4 + tb
                        ht, htk = ht_rot.next()
                        hts.append((ht, htk))
                        P.dma("sp", lambda ht=ht, b=b: nc.sync.dma_start(out=ht[:], in_=hB[b * 128:(b + 1) * 128, :]),
                              reads=[("hout", b)], writes=[htk])
                        norm_T(P, ht[:], htk, gB1[:], "gB1", xg[:, :, tb * 128:(tb + 1) * 128], (xgk, tb))
                    xgkeys = [(xgk, tb) for tb in range(4)]
                    uT, uTk = uT_rot.next()
                    for fcu in range(16):
                        pt, pk = abank.next()
                        for k in range(8):
                            P.op("pe", lambda pt=pt, k=k, fcu=fcu, xg=xg: nc.tensor.matmul(
                                pt[:], lhsT=wi[:, k, fcu * 128:(fcu + 1) * 128], rhs=xg[:, k, :], start=(k == 0), stop=(k == 7)),
                                reads=[("wi", k)] + xgkeys, writes=[pk])
                        P.op("act", lambda pt=pt, uT=uT, fcu=fcu: nc.scalar.activation(out=uT[:, fcu, :], in_=pt[:], func=AF.Gelu_apprx_tanh),
                             reads=[pk], writes=[(uTk, fcu)])
                    for tb in range(4):
                        b = tc * 4 + tb
                        ht, htk = hts[tb]
                        vt, vtk = vt_rot.next()
                        for vg in range(4):
                            pt, pk = abank.next()
                            for k in range(8):
                                P.op("pe", lambda pt=pt, k=k, vg=vg, xg=xg, tb=tb: nc.tensor.matmul(
                                    pt[:], lhsT=xg[:, k, tb * 128:(tb + 1) * 128], rhs=wi[:, k, 2048 + vg * 512: 2048 + (vg + 1) * 512],
                                    start=(k == 0), stop=(k == 7)), reads=[("wi", k), (xgk, tb)], writes=[pk])
                            P.op("act", lambda pt=pt, vt=vt, vg=vg: nc.scalar.activation(out=vt[:, vg * 512:(vg + 1) * 512], in_=pt[:], func=AF.Gelu_apprx_tanh),
                                 reads=[pk], writes=[(vtk, vg)])
                        ss, ssk = nss.next()
                        vtkeys = [(vtk, vg) for vg in range(4)]
                        vn, vnk = vn_rot.next()
                        P.op("act", lambda vt=vt, ss=ss, vn=vn: nc.scalar.activation(out=vn[:], in_=vt[:], func=AF.Square, accum_out=ss[:]),
                             reads=vtkeys, writes=[vnk, ssk])
                        P.op("act", lambda ss=ss: nc.scalar.activation(out=ss[:], in_=ss[:], func=AF.Ln, scale=1.0 / 2048, bias=epsb[:]),
                             reads=[ssk, "epsb"], writes=[ssk])
                        P.op("act", lambda ss=ss: nc.scalar.activation(out=ss[:], in_=ss[:], func=AF.Exp, scale=-0.5), reads=[ssk], writes=[ssk])
                        P.op("dve", lambda vn=vn, vt=vt, ss=ss: nc.vector.scalar_tensor_tensor(out=vn[:], in0=vt[:], scalar=ss[:], in1=gvB[:],
                                                                                              op0=ALU.mult, op1=ALU.mult),
                             reads=vtkeys + [ssk, "gvB"], writes=[vnk])
                        yT, yTk = yT_rot.next()
                        for q4 in range(4):
                            pg_, pgk = gbank.next()
                            for i4 in range(4):
                                fcu = q4 * 4 + i4
                                g = fcu // 2
                                P.op("pe", lambda pg_=pg_, i4=i4, fcu=fcu, g=g, vn=vn: nc.tensor.matmul(
                                    pg_[:, i4 * 128:(i4 + 1) * 128], lhsT=vn[:, fcu * 128:(fcu + 1) * 128], rhs=wsT[:, g, :], start=True, stop=False),
                                    reads=[vnk, "wsT"], writes=[pgk])
                                P.op("pe", lambda pg_=pg_, i4=i4, g=g: nc.tensor.matmul(
                                    pg_[:, i4 * 128:(i4 + 1) * 128], lhsT=ones1[0:1, :], rhs=bs16[0:1, g, :], start=False, stop=True),
                                    reads=["ones1", "bs16"], writes=[pgk])
                            P.op("dve", lambda pg_=pg_, yT=yT, uT=uT, q4=q4, tb=tb: nc.vector.tensor_tensor(
                                out=yT[:, q4 * 4:(q4 + 1) * 4, :], in0=pg_[:].rearrange("p (i t) -> p i t", i=4),
                                in1=uT[:, q4 * 4:(q4 + 1) * 4, tb * 128:(tb + 1) * 128], op=ALU.mult),
                                reads=[pgk] + [(uTk, q4 * 4 + i) for i in range(4)], writes=[(yTk, q4)])
                        for half in range(2):
                            po_, pok = obank.next()
                            for fcu in range(16):
                                P.op("pe", lambda po_=po_, yT=yT, fcu=fcu, half=half: nc.tensor.matmul(
                                    po_[:], lhsT=yT[:, fcu, :], rhs=wo1[:, fcu, half * 512:(half + 1) * 512], start=(fcu == 0), stop=(fcu == 15)),
                                    reads=[(yTk, fcu // 4), ("wo1", (fcu // 4) * 4)], writes=[pok])
                            P.op("dve", lambda po_=po_, ht=ht, half=half: nc.vector.tensor_tensor(
                                out=ht[:, half * 512:(half + 1) * 512], in0=po_[:], in1=ht[:, half * 512:(half + 1) * 512], op=ALU.add),
                                reads=[pok, htk], writes=[htk])
                        P.dma("sp", lambda ht=ht, b=b: nc.sync.dma_start(out=hA[b * 128:(b + 1) * 128, :], in_=ht[:]),
                              reads=[htk], writes=[("hA", b)])
                P.flush()

        if stop_after >= 7:
            mphase(1, hA, out, True)
        P.flush()
        nc._prog_stats = dict(nops=P.nops, ccount=dict(P.ccount), dcount=dict(P.dcount))
    return nc


_NC_CACHE = {}


def kernel(**inputs):
    n = 8
    if "nc" not in _NC_CACHE:
        _NC_CACHE["nc"] = build()
    nc = _NC_CACHE["nc"]
    in_maps = []
    for c in range(n):
        m = {}
        for k, v in inputs.items():
            v = np.asarray(v)
            if k == "x":
                m[k] = np.ascontiguousarray(v[c])
            elif k == "p":
                m[k] = np.ascontiguousarray(v[:, c])
            else:
                m[k] = np.ascontiguousarray(v)
        in_maps.append(m)
    res = run_bass_kernel_spmd(nc, in_maps, core_ids=list(range(n)))
    return np.stack([np.asarray(r["out"]) for r in res.results], axis=0).astype(np.float32)
```

```python
import numpy as np
from contextlib import ExitStack
import concourse.bass as bass
import concourse.mybir as mybir
from concourse.bass_utils import run_bass_kernel_spmd

F32 = mybir.dt.float32
BF16 = mybir.dt.bfloat16
AF = mybir.ActivationFunctionType
ALU = mybir.AluOpType
AX = mybir.AxisListType
I32 = mybir.dt.int32

EPOCH = 30000
NDMASEM = 12
S = 4096
D = 1024
NB = S // 128
EPS = 1e-6


class Prog:
    ENG = ("pe", "act", "dve", "pool", "sp")

    def __init__(self, nc, stack):
        self.nc = nc
        self.stack = stack
        self.ops = []
        self.ccount = {e: 0 for e in self.ENG}
        self.dcount = {e: 0 for e in self.ENG}
        self.csem = {e: [] for e in self.ENG}
        self.dsem = {e: [] for e in self.ENG}
        self.dma_hist = {e: [] for e in self.ENG}
        self.waited = {e: {} for e in self.ENG}
        self.nops = 0

    def eng_obj(self, e):
        nc = self.nc
        return {"pe": nc.tensor, "act": nc.scalar, "dve": nc.vector,
                "pool": nc.gpsimd, "sp": nc.sync}[e]

    def op(self, eng, fn, reads=(), writes=(), nosig=False):
        self.ops.append(dict(eng=eng, fn=fn, reads=tuple(reads), writes=tuple(writes), dma=False, nosig=nosig))

    def dma(self, eng, fn, reads=(), writes=()):
        self.ops.append(dict(eng=eng, fn=fn, reads=tuple(reads), writes=tuple(writes), dma=True))

    def _csem(self, e, ep):
        while len(self.csem[e]) <= ep:
            self.csem[e].append(self.stack.enter_context(self.nc.semaphore(f"c_{e}_{len(self.csem[e])}")))
        return self.csem[e][ep]

    def _dsem(self, e, k):
        while len(self.dsem[e]) <= k:
            self.dsem[e].append(self.stack.enter_context(self.nc.semaphore(f"d_{e}_{len(self.dsem[e])}")))
        return self.dsem[e][k]

    def _wait(self, e, key, sem, val):
        w = self.waited[e]
        if key[0] == "c":
            for kk, v in w.items():
                if kk[0] == "c" and kk[1] == key[1] and (kk[2] > key[2] or (kk[2] == key[2] and v >= val)):
                    return
        elif w.get(key, 0) >= val:
            return
        self.eng_obj(e).wait_ge(sem, val)
        w[key] = max(w.get(key, 0), val)

    def flush(self):
        ops = self.ops
        n = len(ops)
        self.nops += n
        lw, rdc, rdd = {}, {}, {}
        deps = [None] * n
        dlist = {e: [] for e in self.ENG}
        for i, o in enumerate(ops):
            d = set()
            for k in o["reads"]:
                w = lw.get(k)
                if w is not None:
                    d.add(w)
            for k in o["writes"]:
                w = lw.get(k)
                if w is not None:
                    d.add(w)
                d.update(rdc.get(k, {}).values())
                d.update(rdd.get(k, ()))
            for k in o["reads"]:
                if o["dma"]:
                    rdd.setdefault(k, []).append(i)
                else:
                    rdc.setdefault(k, {})[o["eng"]] = i
            for k in o["writes"]:
                lw[k] = i
                rdc[k] = {}
                rdd[k] = []
            if o["dma"]:
                q = o["eng"]
                o["dn"] = self.dcount[q]
                self.dcount[q] += 1
                lst = dlist[q]
                if len(lst) >= NDMASEM:
                    d.add(lst[len(lst) - NDMASEM])
                lst.append(i)
            d.discard(i)
            if o["eng"] == "pe" and not o["dma"]:
                d = {j for j in d if not (ops[j]["eng"] == "pe" and not ops[j]["dma"])}
            deps[i] = d
        signaling = [False] * n
        for i in range(n):
            for j in deps[i]:
                signaling[j] = True
        last = {}
        for i, o in enumerate(ops):
            if not o["dma"] and not o.get("nosig"):
                last[o["eng"]] = i
        for e, i in last.items():
            signaling[i] = True
        for i, o in enumerate(ops):
            if not o["dma"] and signaling[i]:
                c = self.ccount[o["eng"]]
                o["sig"] = (c // EPOCH, c % EPOCH + 1)
                self.ccount[o["eng"]] = c + 1
        for i, o in enumerate(ops):
            e = o["eng"]
            need = {}
            for j in deps[i]:
                p = ops[j]
                if p["dma"]:
                    key = ("d", p["eng"], p["dn"] % NDMASEM)
                    val = 16 * (p["dn"] // NDMASEM + 1)
                    sem = self._dsem(p["eng"], p["dn"] % NDMASEM)
                else:
                    ep, cv = p["sig"]
                    key = ("c", p["eng"], ep)
                    val = cv
                    sem = self._csem(p["eng"], ep)
                if need.get(key, (None, 0))[1] < val:
                    need[key] = (sem, val)
            for key, (sem, val) in need.items():
                self._wait(e, key, sem, val)
            ins = o["fn"]()
            if o["dma"]:
                ins.then_inc(self._dsem(e, o["dn"] % NDMASEM), 16)
            elif signaling[i]:
                ep, cv = o["sig"]
                ins.then_inc(self._csem(e, ep), 1)
        for e in self.ENG:
            for e2, i in last.items():
                ep, cv = ops[i]["sig"]
                self._wait(e, ("c", e2, ep), self._csem(e2, ep), cv)
            for q in self.ENG:
                dc = self.dcount[q]
                for k in range(min(NDMASEM, dc)):
                    lastdn = ((dc - 1 - k) // NDMASEM) * NDMASEM + k
                    self._wait(e, ("d", q, k), self._dsem(q, k), 16 * (lastdn // NDMASEM + 1))
        self.ops = []


_UN = [0]


def _un(name):
    _UN[0] += 1
    return f"{name}_{_UN[0]}"


def _kl(k):
    return list(k) if isinstance(k, list) else [k]


def hk(blk):
    return [("hacc", blk, 0), ("hacc", blk, 1)]


class Rot:
    def __init__(self, tiles, name):
        self.tiles = tiles
        self.name = name
        self.i = 0

    def next(self):
        j = self.i % len(self.tiles)
        self.i += 1
        return self.tiles[j], (self.name, j)


class RotK:
    def __init__(self, tiles, keys):
        self.tiles = tiles
        self.keys = keys
        self.i = 0

    def next(self):
        j = self.i % len(self.tiles)
        self.i += 1
        return self.tiles[j], self.keys[j]


def build(stop_after=99, dbg=False, sub=9):
    nc = bass.Bass("TRN2", target_bir_lowering=False)

    def din(name, shape):
        return nc.dram_tensor(name, list(shape), F32, kind="ExternalInput").ap()

    x = din("x", [S, D])
    p = din("p", [2, S, 256])
    norm_mix = din("norm_mix", [2, D])
    norm_ffn = din("norm_ffn", [2, D])
    norm_pl = din("norm_pl", [2, D])
    final_norm = din("final_norm", [D])
    w_in_even = din("w_in_even", [1, D, 3072])
    conv_w_even = din("conv_w_even", [1, 3, 512])
    w_out_even = din("w_out_even", [1, 1024, D])
    w_in_odd = din("w_in_odd", [1, D, 4096])
    g_v_odd = din("g_v_odd", [1, 2048])
    w_s_odd = din("w_s_odd", [1, 8, 128, 128])
    b_s_odd = din("b_s_odd", [1, 8, 128])
    w_out_odd = din("w_out_odd", [1, 2048, D])
    router_c = din("router_c", [2, D, 4])
    router_c_b = din("router_c_b", [2, 4])
    router_f = din("router_f", [2, D, 32])
    router_f_b = din("router_f_b", [2, 32])
    moe_w1 = din("moe_w1", [2, 32, D, 256])
    moe_w3 = din("moe_w3", [2, 32, D, 256])
    moe_w2 = din("moe_w2", [2, 32, 256, D])
    w_pe = din("w_pe", [2, 256, D])
    w_pg = din("w_pg", [2, D, D])
    out = nc.dram_tensor("out", [S, D], F32, kind="ExternalOutput").ap()
    mixT = nc.dram_tensor("mixT", [8, 128, S], BF16, kind="ExternalOutput" if dbg else "Internal").ap()
    XS = nc.dram_tensor("XS", [24576, 1024], BF16, kind="Internal").ap()
    YS = nc.dram_tensor("YS", [24576, 1024], F32, kind="Internal").ap()
    hA = nc.dram_tensor("hA", [S, D], F32, kind="ExternalOutput" if dbg else "Internal").ap()
    hB = nc.dram_tensor("hB", [S, D], F32, kind="ExternalOutput" if dbg else "Internal").ap()

    with ExitStack() as gst:
        P = Prog(nc, gst)

        def gsb(name, shape, dt):
            return gst.enter_context(nc.sbuf_tensor(_un(name), shape, dt))

        pb = [gst.enter_context(nc.psum_tensor(f"pb{i}", [128, 512], F32)) for i in range(7)]
        ptb = gst.enter_context(nc.psum_tensor("ptb", [128, 8, 128], BF16))

        identf = gsb("identf", [128, 128], F32)
        ident = gsb("ident", [128, 128], BF16)
        ntri = gsb("ntri", [128, 128], BF16)
        nones = gsb("nones", [128, 128], BF16)
        zeros = gsb("zeros", [128, 128], BF16)
        nmask = gsb("nmask", [128, 128], BF16)
        ones1 = gsb("ones1", [1, 128], BF16)
        pones = gsb("pones", [128, 128], BF16)
        lstrict = gsb("lstrict", [128, 128], BF16)
        lsf = gsb("lsf", [128, 128], F32)
        tmpf = gsb("tmpf", [128, 128], F32)
        P.op("pool", lambda: nc.gpsimd.memset(identf[:], 1.0), writes=["identf"])
        P.op("pool", lambda: nc.gpsimd.affine_select(out=identf[:], in_=identf[:], pattern=[[-1, 128]],
                                                      compare_op=ALU.is_equal, fill=0.0, base=0, channel_multiplier=1),
             reads=["identf"], writes=["identf"])
        P.op("dve", lambda: nc.vector.tensor_copy(out=ident[:], in_=identf[:]), reads=["identf"], writes=["ident"])
        P.op("pool", lambda: nc.gpsimd.memset(tmpf[:], -1.0), writes=["tmpf"])
        P.op("dve", lambda: nc.vector.tensor_copy(out=nones[:], in_=tmpf[:]), reads=["tmpf"], writes=["nones"])
        P.op("pool", lambda: nc.gpsimd.affine_select(out=tmpf[:], in_=tmpf[:], pattern=[[-1, 128]],
                                                      compare_op=ALU.is_ge, fill=0.0, base=0, channel_multiplier=1),
             reads=["tmpf", "nones"], writes=["tmpf"])
        P.op("dve", lambda: nc.vector.tensor_copy(out=ntri[:], in_=tmpf[:]), reads=["tmpf"], writes=["ntri"])
        P.op("dve", lambda: nc.vector.tensor_scalar(out=nmask[:], in0=tmpf[:], scalar1=30000.0, scalar2=None, op0=ALU.mult),
             reads=["tmpf"], writes=["nmask"])
        P.op("pool", lambda: nc.gpsimd.memset(zeros[:], 0.0), writes=["zeros"])
        P.op("pool", lambda: nc.gpsimd.memset(ones1[:], 1.0), writes=["ones1"])
        P.op("pool", lambda: nc.gpsimd.memset(pones[:], 1.0), writes=["pones"])
        P.op("pool", lambda: nc.gpsimd.memset(lsf[:], 1.0), writes=["lsf"])
        P.op("pool", lambda: nc.gpsimd.affine_select(out=lsf[:], in_=lsf[:], pattern=[[1, 128]],
                                                      compare_op=ALU.is_gt, fill=0.0, base=0, channel_multiplier=-1),
             reads=["lsf"], writes=["lsf"])
        P.op("dve", lambda: nc.vector.tensor_copy(out=lstrict[:], in_=lsf[:]), reads=["lsf"], writes=["lstrict"])
        P.flush()

        def norm_rstd(P, src, skey, width, junk, jkey, ss, sskey):
            P.op("act", lambda: nc.scalar.activation(out=junk, in_=src, func=AF.Square, accum_out=ss),
                 reads=_kl(skey), writes=[jkey, sskey])
            P.op("act", lambda: nc.scalar.activation(out=ss, in_=ss, func=AF.Ln, scale=1.0 / width, bias=epsb[:]),
                 reads=[sskey, "epsb"], writes=[sskey])
            P.op("act", lambda: nc.scalar.activation(out=ss, in_=ss, func=AF.Exp, scale=-0.5),
                 reads=[sskey], writes=[sskey])

        epsb = gsb("epsb", [128, 1], F32)
        P.op("pool", lambda: nc.gpsimd.memset(epsb[:], EPS), writes=["epsb"])

        njunk = Rot([gsb(f"njunk{i}", [128, 1024], BF16) for i in range(1)], "njunk")
        nss = Rot([gsb(f"nss{i}", [128, 1], F32) for i in range(4)], "nss")
        nxn = Rot([gsb(f"nxn{i}", [128, 1024], BF16) for i in range(2)], "nxn")

        def norm_T(P, src, skey, gB, gkey, dstT, dkey, cp_eng="act"):
            junk, jk = njunk.next()
            ss, ssk = nss.next()
            xn, xnk = nxn.next()
            norm_rstd(P, src, skey, 1024, junk[:], jk, ss[:], ssk)
            P.op("dve", lambda: nc.vector.scalar_tensor_tensor(out=xn[:], in0=src, scalar=ss[:], in1=gB,
                                                                 op0=ALU.mult, op1=ALU.mult),
                 reads=_kl(skey) + [ssk, gkey], writes=[xnk])
            for k in range(8):
                P.op("pe", lambda k=k: nc.tensor.transpose(out=ptb[:, k, :], in_=xn[:, k * 128:(k + 1) * 128], identity=ident[:]),
                     reads=[xnk, "ident"], writes=["ptb"])
            if cp_eng == "act":
                P.op("act", lambda: nc.scalar.copy(out=dstT, in_=ptb[:]), reads=["ptb"], writes=[dkey])
            else:
                P.op("dve", lambda: nc.vector.tensor_copy(out=dstT, in_=ptb[:]), reads=["ptb"], writes=[dkey])
            return ss, ssk

        def load_gB(P, dst, key, src_row):
            P.dma("sp", lambda: nc.sync.dma_start(out=dst, in_=src_row.partition_broadcast(128)), writes=[key])

        with ExitStack() as st:
            def sb(name, shape, dt):
                return st.enter_context(nc.sbuf_tensor(_un(name), shape, dt))
            xnT = sb("xnT", [128, 8, S], BF16)
            zrow = sb("zrow", [128, 4, 1024], BF16)
            P.op("pool", lambda: nc.gpsimd.memset(zrow[:], 0.0), writes=["zrow"])
            for zi in range(24576 // 512):
                P.dma("pool", lambda zi=zi: nc.gpsimd.dma_start(out=XS[zi * 512:(zi + 1) * 512, :].rearrange("(p a) d -> p a d", a=4), in_=zrow[:]),
                      reads=["zrow"], writes=["XS"])
            gB0 = sb("gB0", [128, 1024], F32)
            load_gB(P, gB0[:], "gB0", norm_mix[0])
            xt_rot = Rot([sb(f"xt{i}", [128, 1024], F32) for i in range(2)], "xt")
            for b in range(NB):
                xt, xk = xt_rot.next()
                P.dma("sp", lambda xt=xt, b=b: nc.sync.dma_start(out=xt[:], in_=x[b * 128:(b + 1) * 128, :]), writes=[xk])
                norm_T(P, xt[:], xk, gB0[:], "gB0", xnT[:, :, b * 128:(b + 1) * 128], ("xnT", b))
            xnT_keys = lambda tc: [("xnT", 4 * tc + i) for i in range(4)]

            P.flush()
            st2 = ExitStack()
            st2.__enter__()
            sb_outer = sb

            def sb(name, shape, dt):
                return st2.enter_context(nc.sbuf_tensor(_un(name), shape, dt))
            cw = sb("cw", [128, 4, 3], F32)
            for fc in range(4):
                P.dma("sp", lambda fc=fc: nc.sync.dma_start(
                    out=cw[:, fc, :], in_=conv_w_even[0, :, fc * 128:(fc + 1) * 128].rearrange("w f -> f w"),
                    allow_slow_non_contiguous=True), writes=["cw"])
            wc_rot = Rot([sb(f"wc{i}", [128, 3, 8, 128], BF16) for i in range(2)], "wc")
            z_rot = Rot([sb(f"z{i}", [128, S + 2], F32) for i in range(2)], "z")
            hs_rot = Rot([sb(f"hs{i}", [128, 512], F32) for i in range(2)], "hs")
            acc_rot = Rot([sb(f"acc{i}", [128, 512], F32) for i in range(2)], "acc")
            ya_rot = Rot([sb(f"ya{i}", [128, 512], BF16) for i in range(2)], "ya")
            w_in0 = w_in_even[0].rearrange("(k p) n -> p k n", p=128)
            bank = Rot(pb[0:6], "pb")
            for fc in range(4):
                wc, wck = wc_rot.next()
                for j in range(3):
                    P.dma("pool", lambda wc=wc, j=j, fc=fc: nc.gpsimd.dma_start(
                        out=wc[:, j, :, :], in_=w_in0[:, :, j * 512 + fc * 128: j * 512 + (fc + 1) * 128]),
                        writes=[(wck, j)])
                z, zk = z_rot.next()
                P.op("pool", lambda z=z: nc.gpsimd.memset(z[:, 0:2], 0.0), writes=[(zk, -1)])
                for tc in range(8):
                    pp = []
                    for j in range(3):
                        pt, pk = bank.next()
                        for k in range(8):
                            P.op("pe", lambda pt=pt, wc=wc, j=j, k=k, tc=tc: nc.tensor.matmul(
                                pt[:], lhsT=wc[:, j, k, :], rhs=xnT[:, k, tc * 512:(tc + 1) * 512],
                                start=(k == 0), stop=(k == 7)),
                                reads=[(wck, j)] + xnT_keys(tc), writes=[pk])
                        pp.append((pt, pk))
                    (ph, phk), (pgb, pgbk), (pgc, pgck) = pp
                    hs, hsk = hs_rot.next()
                    acc, acck = acc_rot.next()
                    ya, yak = ya_rot.next()
                    P.op("act", lambda hs=hs, ph=ph: nc.scalar.copy(out=hs[:], in_=ph[:]), reads=[phk], writes=[hsk])
                    P.op("dve", lambda z=z, tc=tc, pgc=pgc, hs=hs: nc.vector.tensor_tensor(
                        out=z[:, 2 + tc * 512: 2 + (tc + 1) * 512], in0=pgc[:], in1=hs[:], op=ALU.mult),
                        reads=[pgck, hsk], writes=[(zk, tc)])
                    zr = [(zk, tc), (zk, tc - 1)]
                    P.op("dve", lambda acc=acc, z=z, tc=tc, fc=fc: nc.vector.tensor_scalar(
                        out=acc[:], in0=z[:, tc * 512: tc * 512 + 512], scalar1=cw[:, fc, 0:1], scalar2=None, op0=ALU.mult),
                        reads=zr + ["cw"], writes=[acck])
                    for wv in (1, 2):
                        P.op("dve", lambda acc=acc, z=z, tc=tc, fc=fc, wv=wv: nc.vector.scalar_tensor_tensor(
                            out=acc[:], in0=z[:, tc * 512 + wv: tc * 512 + wv + 512], scalar=cw[:, fc, wv:wv + 1],
                            in1=acc[:], op0=ALU.mult, op1=ALU.add),
                            reads=zr + ["cw", acck], writes=[acck])
                    P.op("dve", lambda ya=ya, acc=acc, pgb=pgb: nc.vector.tensor_tensor(
                        out=ya[:], in0=pgb[:], in1=acc[:], op=ALU.mult), reads=[pgbk, acck], writes=[yak])
                    P.dma("sp", lambda ya=ya, fc=fc, tc=tc: nc.sync.dma_start(
                        out=mixT[fc, :, tc * 512:(tc + 1) * 512], in_=ya[:]), reads=[yak], writes=[("mixT", fc, tc)])
            P.flush()
            st2.__exit__(None, None, None)
            sb = sb_outer
            if stop_after >= 2:
                wq_rot = Rot([sb(f"wq{i}", [128, 3, 8, 128], BF16) for i in range(2)], "wq")
                qT_rot = Rot([sb(f"qT{i}", [128, S], BF16) for i in range(2)], "qT")
                kT_rot = Rot([sb(f"kT{i}", [128, S], BF16) for i in range(2)], "kT")
                v_rot = Rot([sb(f"v{i}", [128, NB, 128], BF16) for i in range(2)], "v")
                yb_rot = Rot([sb(f"yb{i}", [128, S], BF16) for i in range(2)], "yb")
                e_rot = Rot([sb(f"e{i}", [128, 512], F32) for i in range(3)], "e")
                sp_rot = Rot([sb(f"sp{i}", [128, 512], BF16) for i in range(3)], "sp")
                a_rot = Rot([sb(f"a{i}", [128, 512], BF16) for i in range(3)], "a")
                r32_rot = Rot([sb(f"r32{i}", [128, 512], F32) for i in range(2)], "r32")
                r16_rot = Rot([sb(f"r16{i}", [128, 512], BF16) for i in range(3)], "r16")
                bk = [("bk", j) for j in range(7)]
                zb = RotK(pb[0:2], bk[0:2])
                lb = RotK(pb[2:4], bk[2:4])
                ob = RotK(pb[4:7], bk[4:7])
                pjb = RotK(pb[3:7], bk[3:7])

                def proj(hp):
                    wq, wqk = wq_rot.next()
                    for j in range(3):
                        P.dma("pool", lambda wq=wq, j=j, hp=hp: nc.gpsimd.dma_start(
                            out=wq[:, j, :, :], in_=w_in0[:, :, 1536 + j * 512 + hp * 128: 1536 + j * 512 + (hp + 1) * 128]),
                            writes=[(wqk, j)])
                    qT, qk = qT_rot.next()
                    kT, kk = kT_rot.next()
                    v, vk = v_rot.next()
                    for tc in range(8):
                        for j, (dst, dk, sc) in enumerate(((qT, qk, 0.125), (kT, kk, 1.0))):
                            pt, pk = pjb.next()
                            for k in range(8):
                                P.op("pe", lambda pt=pt, wq=wq, j=j, k=k, tc=tc: nc.tensor.matmul(
                                    pt[:], lhsT=wq[:, j, k, :], rhs=xnT[:, k, tc * 512:(tc + 1) * 512],
                                    start=(k == 0), stop=(k == 7)),
                                    reads=[(wqk, j)] + xnT_keys(tc), writes=[pk])
                            P.op("dve", lambda dst=dst, pt=pt, tc=tc, sc=sc: nc.vector.tensor_scalar(
                                out=dst[:, tc * 512:(tc + 1) * 512], in0=pt[:], scalar1=sc, scalar2=None, op0=ALU.mult),
                                reads=[pk], writes=[(dk, tc)])
                        pt, pk = pjb.next()
                        for tb in range(4):
                            b = tc * 4 + tb
                            for k in range(8):
                                P.op("pe", lambda pt=pt, wq=wq, k=k, b=b, tb=tb: nc.tensor.matmul(
                                    pt[:, tb * 128:(tb + 1) * 128], lhsT=xnT[:, k, b * 128:(b + 1) * 128], rhs=wq[:, 2, k, :],
                                    start=(k == 0), stop=(k == 7)),
                                    reads=[(wqk, 2), ("xnT", b)], writes=[pk])
                        P.op("dve", lambda v=v, pt=pt, tc=tc: nc.vector.tensor_copy(
                            out=v[:, tc * 4:(tc + 1) * 4, :], in_=pt[:].rearrange("p (b d) -> p b d", b=4)),
                            reads=[pk], writes=[(vk, tc)])
                    return (qT, qk, kT, kk, v, vk)

                def attention(hp, qkv):
                    qT, qk, kT, kk, v, vk = qkv
                    yb, ybk = yb_rot.next()
                    tiles = []
                    for hh in range(2):
                        for qc in range(8):
                            nkb = 4 * qc + 4
                            for ii, kb in enumerate(range(nkb - 1, -1, -1)):
                                jd = kb - 4 * qc
                                c0 = jd * 128 if jd >= 0 else 0
                                tiles.append(dict(hh=hh, qc=qc, kb=kb, c0=c0, diag=(jd >= 0), first=(ii == 0),
                                                  last=(kb == 0)))
                    nt = len(tiles)
                    st_ = [dict() for _ in range(nt)]

                    def stage_qk(i):
                        t = tiles[i]
                        po = t["hh"] * 64
                        zt, zk_ = zb.next()
                        st_[i]["z"] = (zt, zk_)
                        c0 = t["c0"]
                        qc, kb = t["qc"], t["kb"]
                        P.op("pe", lambda: nc.tensor.matmul(
                            zt[:, c0:512], lhsT=kT[po:po + 64, kb * 128:(kb + 1) * 128],
                            rhs=qT[po:po + 64, qc * 512 + c0:(qc + 1) * 512], start=True, stop=not t["diag"]),
                            reads=[(kk, kb // 4), (qk, qc)], writes=[zk_])
                        if t["diag"]:
                            P.op("pe", lambda: nc.tensor.matmul(
                                zt[:, c0:c0 + 128], lhsT=ident[:], rhs=nmask[:], start=False, stop=True),
                                reads=["ident", "nmask"], writes=[zk_])
                        et, ek = e_rot.next()
                        spt, spk = sp_rot.next()
                        st_[i]["sp"] = (spt, spk)
                        P.op("act", lambda: nc.scalar.activation(out=et[:, c0:512], in_=zt[:, c0:512], func=AF.Exp),
                             reads=[zk_], writes=[ek])
                        P.op("act", lambda: nc.scalar.activation(out=spt[:, c0:512], in_=et[:, c0:512], func=AF.Ln, bias=1.0),
                             reads=[ek], writes=[spk])

                    def stage_cum(i):
                        t = tiles[i]
                        zt, zk_ = st_[i]["z"]
                        spt, spk = st_[i]["sp"]
                        c0 = t["c0"]
                        if t["first"]:
                            r32, r32k = r32_rot.next()
                            st_[i]["r32"] = (r32, r32k)
                            P.op("pool", lambda: nc.gpsimd.memset(r32[:], 0.0), writes=[r32k])
                        else:
                            st_[i]["r32"] = st_[i - 1]["r32"]
                            r32, r32k = st_[i]["r32"]
                        po = t["hh"] * 64
                        qc, kb = t["qc"], t["kb"]
                        zt, zk_ = lb.next()
                        P.op("pe", lambda: nc.tensor.matmul(
                            zt[:, c0:512], lhsT=kT[po:po + 64, kb * 128:(kb + 1) * 128],
                            rhs=qT[po:po + 64, qc * 512 + c0:(qc + 1) * 512], start=True, stop=False),
                            reads=[(kk, kb // 4), (qk, qc)], writes=[zk_])
                        if t["diag"]:
                            P.op("pe", lambda: nc.tensor.matmul(
                                zt[:, c0:c0 + 128], lhsT=ident[:], rhs=nmask[:], start=False, stop=False),
                                reads=["ident", "nmask"], writes=[zk_])
                        P.op("pe", lambda: nc.tensor.matmul(zt[:, c0:512], lhsT=ntri[:], rhs=spt[:, c0:512],
                                                            start=False, stop=t["first"]),
                             reads=["ntri", spk], writes=[zk_])
                        if not t["first"]:
                            r16, r16k = st_[i]["r16"]
                            P.op("pe", lambda: nc.tensor.matmul(zt[:, c0:512], lhsT=nones[:], rhs=r16[:, c0:512],
                                                                start=False, stop=True),
                                 reads=["nones"] + st_[i]["r16keys"], writes=[zk_])
                        if not t["last"]:
                            c1 = tiles[i + 1]["c0"]
                            r16n, r16nk = r16_rot.next()
                            st_[i + 1]["r16"] = (r16n, r16nk)
                            wk = [r16nk]
                            if c1 < c0:
                                P.op("pool", lambda: nc.gpsimd.memset(r16n[:, c1:c0], 0.0), writes=[r16nk])
                            P.op("dve", lambda: nc.vector.tensor_tensor(out=r16n[:, c0:512], in0=r32[:, c0:512],
                                                                       in1=spt[:, c0:512], op=ALU.add),
                                 reads=[r32k, spk], writes=[r16nk])
                            P.op("dve", lambda: nc.vector.tensor_tensor(out=r32[:, c0:512], in0=r32[:, c0:512],
                                                                       in1=spt[:, c0:512], op=ALU.add),
                                 reads=[r32k, spk], writes=[r32k])
                            st_[i + 1]["r16keys"] = wk
                        at, ak = a_rot.next()
                        st_[i]["a"] = (at, ak)
                        P.op("act", lambda: nc.scalar.activation(out=at[:, c0:512], in_=zt[:, c0:512], func=AF.Exp),
                             reads=[zk_], writes=[ak])

                    def stage_av(i):
                        t = tiles[i]
                        at, ak = st_[i]["a"]
                        c0 = t["c0"]
                        kb = t["kb"]
                        po = t["hh"] * 64
                        if t["first"]:
                            ot, ok = ob.next()
                            st_[i]["o"] = (ot, ok)
                            P.op("pe", lambda: nc.tensor.matmul(ot[:], lhsT=zeros[:], rhs=qT[:, 0:512], start=True, stop=False),
                                 reads=["zeros", (qk, 0)], writes=[ok])
                        else:
                            st_[i]["o"] = st_[i - 1]["o"]
                            ot, ok = st_[i]["o"]
                        P.op("pe", lambda: nc.tensor.matmul(ot[:, c0:512], lhsT=v[:, kb, :], rhs=at[:, c0:512],
                                                            start=False, stop=t["last"]),
                             reads=[(vk, kb // 4), ak], writes=[ok])
                        if t["last"]:
                            qc = t["qc"]
                            P.op("dve", lambda: nc.vector.tensor_copy(out=yb[po:po + 64, qc * 512:(qc + 1) * 512],
                                                                      in_=ot[po:po + 64, :]),
                                 reads=[ok], writes=[(ybk, qc, t["hh"])])
                        st_[i].pop("z", None)

                    stage_qk(0)
                    for i in range(nt):
                        if i + 1 < nt:
                            stage_qk(i + 1)
                        stage_cum(i)
                        if i >= 1:
                            stage_av(i - 1)
                    stage_av(nt - 1)
                    for qc in range(8):
                        P.dma("sp", lambda qc=qc: nc.sync.dma_start(out=mixT[4 + hp, :, qc * 512:(qc + 1) * 512],
                                                                    in_=yb[:, qc * 512:(qc + 1) * 512]),
                              reads=[(ybk, qc, 0), (ybk, qc, 1)], writes=[("mixT", 4 + hp, qc)])

                nhp = 4 if stop_after >= 3 else 1
                qkvs = {0: proj(0)}
                for hp in range(nhp):
                    if hp + 1 < nhp:
                        qkvs[hp + 1] = proj(hp + 1)
                    attention(hp, qkvs.pop(hp))
            P.flush()

        if stop_after >= 4:
            with ExitStack() as st:
                def sb(name, shape, dt):
                    return st.enter_context(nc.sbuf_tensor(_un(name), shape, dt))
                wo = sb("wo", [128, 8, 1024], BF16)
                P.dma("pool", lambda: nc.gpsimd.dma_start(out=wo[:], in_=w_out_even[0].rearrange("(k p) n -> p k n", p=128)),
                      writes=["wo"])
                mx_rot = Rot([sb(f"mx{i}", [128, 8, 512], BF16) for i in range(2)], "mx")
                xt_rot = Rot([sb(f"xt{i}", [128, 1024], F32) for i in range(3)], "xt")
                bank = Rot(pb[0:6], "pb")
                for tc in range(8):
                    mx, mxk = mx_rot.next()
                    P.dma("sp", lambda mx=mx, tc=tc: nc.sync.dma_start(
                        out=mx[:], in_=mixT[:, :, tc * 512:(tc + 1) * 512].rearrange("c p t -> p c t")), writes=[mxk])
                    for tb in range(4):
                        b = tc * 4 + tb
                        xt, xk = xt_rot.next()
                        P.dma("sp", lambda xt=xt, b=b: nc.sync.dma_start(out=xt[:], in_=x[b * 128:(b + 1) * 128, :]), writes=[xk])
                        for half in range(2):
                            pt, pk = bank.next()
                            for c in range(8):
                                P.op("pe", lambda pt=pt, mx=mx, c=c, tb=tb, half=half: nc.tensor.matmul(
                                    pt[:], lhsT=mx[:, c, tb * 128:(tb + 1) * 128], rhs=wo[:, c, half * 512:(half + 1) * 512],
                                    start=(c == 0), stop=(c == 7)), reads=[mxk, "wo"], writes=[pk])
                            P.op("dve", lambda xt=xt, pt=pt, half=half: nc.vector.tensor_tensor(
                                out=xt[:, half * 512:(half + 1) * 512], in0=pt[:], in1=xt[:, half * 512:(half + 1) * 512], op=ALU.add),
                                reads=[pk, xk], writes=[xk])
                        P.dma("sp", lambda xt=xt, b=b: nc.sync.dma_start(out=hA[b * 128:(b + 1) * 128, :], in_=xt[:]),
                              reads=[xk], writes=[("hA", b)])
                P.flush()

        def moe_pl_phase(l, hin, hout, final):
            with ExitStack() as st:
                def sb(name, shape, dt):
                    return st.enter_context(nc.sbuf_tensor(_un(name), shape, dt))
                NBS = 16
                hacc = sb("hacc", [128, NBS, 1024], F32)
                xT = sb("xT", [128, 8, NBS * 128], BF16)
                gBf = sb("gBf", [128, 1024], F32)
                gBp = sb("gBp", [128, 1024], F32)
                load_gB(P, gBf[:], "gBf", norm_ffn[l])
                load_gB(P, gBp[:], "gBp", norm_pl[l])
                if final:
                    gBo = sb("gBo", [128, 1024], F32)
                    load_gB(P, gBo[:], "gBo", final_norm)
                wr = sb("wr", [128, 8, 36], BF16)
                P.dma("pool", lambda: nc.gpsimd.dma_start(out=wr[:, :, 0:4], in_=router_c[l].rearrange("(k p) n -> p k n", p=128)),
                      writes=["wr"])
                P.dma("pool", lambda: nc.gpsimd.dma_start(out=wr[:, :, 4:36], in_=router_f[l].rearrange("(k p) n -> p k n", p=128)),
                      writes=["wr"])
                rb = sb("rb", [128, 36], F32)
                P.dma("sp", lambda: nc.sync.dma_start(out=rb[:, 0:4], in_=router_c_b[l].partition_broadcast(128)), writes=["rb"])
                P.dma("sp", lambda: nc.sync.dma_start(out=rb[:, 4:36], in_=router_f_b[l].partition_broadcast(128)), writes=["rb"])
                wpg = sb("wpg", [128, 8, 1024], BF16)
                wpe = sb("wpe", [128, 2, 1024], BF16)
                P.dma("pool", lambda: nc.gpsimd.dma_start(out=wpg[:], in_=w_pg[l].rearrange("(k p) n -> p k n", p=128)), writes=["wpg"])
                P.dma("pool", lambda: nc.gpsimd.dma_start(out=wpe[:], in_=w_pe[l].rearrange("(k p) n -> p k n", p=128)), writes=["wpe"])
                w13_rot = Rot([sb(f"w13_{i}", [128, 2, 8, 256], BF16) for i in range(2)], "w13")
                w2_rot = Rot([sb(f"w2_{i}", [128, 2, 1024], BF16) for i in range(2)], "w2")
                lg = sb("lg", [128, NBS, 36], F32)
                comb = sb("comb", [128, NBS, 32], F32)
                r_mx = sb("r_mx", [128, NBS], F32)
                r_gm = sb("r_gm", [128, NBS, 4], F32)
                r_ec = sb("r_ec", [128, NBS, 4], F32)
                r_pg = sb("r_pg", [128, NBS], F32)
                r_t = sb("r_t", [128, NBS, 4, 8], F32)
                r_lfs = sb("r_lfs", [128, NBS, 8], F32)
                r_t8 = sb("r_t8", [128, NBS, 8], F32)
                r_sel = sb("r_sel", [128, NBS, 8], F32)
                r_ex = sb("r_ex", [128, NBS, 8], F32)
                r_d = sb("r_d", [128, NBS], F32)
                s_rot = Rot([sb(f"s{i}", [128, 512], F32) for i in range(2)], "s")
                he_rot = Rot([sb(f"he{i}", [128, 2, 512], BF16) for i in range(2)], "he")
                pblk_rot = Rot([sb(f"pblk{i}", [128, 256], F32) for i in range(2)], "pblk")
                pb16_rot = Rot([sb(f"pb16{i}", [128, 256], BF16) for i in range(2)], "pb16")
                pT_rot = Rot([sb(f"pT{i}", [128, 2, 128], BF16) for i in range(2)], "pT")
                hnT_rot = Rot([sb(f"hnT{i}", [128, 8, 128], BF16) for i in range(2)], "hnT")
                sg_rot = Rot([sb(f"sg{i}", [128, 1024], F32) for i in range(1)], "sg")
                for sc in range(2):
                    hbank = Rot(pb[0:4], "pb")
                    ybank = Rot(pb[4:7], "pby")
                    for blk in range(NBS):
                        b = sc * NBS + blk
                        P.dma("sp", lambda blk=blk, b=b: nc.sync.dma_start(out=hacc[:, blk, :], in_=hin[b * 128:(b + 1) * 128, :]),
                              writes=hk(blk))
                        norm_T(P, hacc[:, blk, :], hk(blk), gBf[:], "gBf", xT[:, :, blk * 128:(blk + 1) * 128], ("xT", blk))
                        pt, pk = ybank.next()
                        for k in range(8):
                            P.op("pe", lambda pt=pt, k=k, blk=blk: nc.tensor.matmul(
                                pt[:, 0:36], lhsT=xT[:, k, blk * 128:(blk + 1) * 128], rhs=wr[:, k, :],
                                start=(k == 0), stop=(k == 7)), reads=[("xT", blk), "wr"], writes=[pk])
                        P.op("dve", lambda pt=pt, blk=blk: nc.vector.tensor_tensor(out=lg[:, blk, :], in0=pt[:, 0:36], in1=rb[:], op=ALU.add),
                             reads=[pk, "rb"], writes=["lg"])
                    lc = lg[:, :, 0:4]
                    lf = lg[:, :, 4:36].rearrange("p b (g e) -> p b g e", g=4)
                    P.op("dve", lambda: nc.vector.tensor_reduce(out=r_mx[:], in_=lc, axis=AX.X, op=ALU.max), reads=["lg"], writes=["r_mx"])
                    P.op("dve", lambda: nc.vector.tensor_tensor(out=r_gm[:], in0=lc, in1=r_mx[:].unsqueeze(2).to_broadcast([128, NBS, 4]),
                                                                op=ALU.is_ge), reads=["lg", "r_mx"], writes=["r_gm"])
                    P.op("dve", lambda: nc.vector.tensor_tensor(out=r_ec[:], in0=lc, in1=r_mx[:].unsqueeze(2).to_broadcast([128, NBS, 4]),
                                                                op=ALU.subtract), reads=["lg", "r_mx"], writes=["r_ec"])
                    P.op("act", lambda: nc.scalar.activation(out=r_ec[:], in_=r_ec[:], func=AF.Exp), reads=["r_ec"], writes=["r_ec"])
                    P.op("dve", lambda: nc.vector.tensor_reduce(out=r_pg[:], in_=r_ec[:], axis=AX.X, op=ALU.add), reads=["r_ec"], writes=["r_pg"])
                    P.op("dve", lambda: nc.vector.tensor_tensor(out=r_t[:], in0=lf, in1=r_gm[:].unsqueeze(3).to_broadcast([128, NBS, 4, 8]),
                                                                op=ALU.mult), reads=["lg", "r_gm"], writes=["r_t"])
                    P.op("dve", lambda: nc.vector.tensor_reduce(out=r_lfs[:], in_=r_t[:].rearrange("p b g e -> p b e g"), axis=AX.X, op=ALU.add),
                         reads=["r_t"], writes=["r_lfs"])
                    for blk in range(NBS):
                        P.op("dve", lambda blk=blk: nc.vector.max(out=r_t8[:, blk, :], in_=r_lfs[:, blk, :]), reads=["r_lfs"], writes=["r_t8"])
                    l1b = r_t8[:, :, 0:1].to_broadcast([128, NBS, 8])
                    l2b = r_t8[:, :, 1:2].to_broadcast([128, NBS, 8])
                    P.op("dve", lambda: nc.vector.tensor_tensor(out=r_sel[:], in0=r_lfs[:], in1=l2b, op=ALU.is_ge), reads=["r_lfs", "r_t8"], writes=["r_sel"])
                    P.op("dve", lambda: nc.vector.tensor_tensor(out=r_ex[:], in0=r_lfs[:], in1=l1b, op=ALU.subtract), reads=["r_lfs", "r_t8"], writes=["r_ex"])
                    P.op("act", lambda: nc.scalar.activation(out=r_ex[:], in_=r_ex[:], func=AF.Exp), reads=["r_ex"], writes=["r_ex"])
                    P.op("dve", lambda: nc.vector.tensor_tensor(out=r_ex[:], in0=r_ex[:], in1=r_sel[:], op=ALU.mult), reads=["r_ex", "r_sel"], writes=["r_ex"])
                    P.op("dve", lambda: nc.vector.tensor_reduce(out=r_d[:], in_=r_ex[:], axis=AX.X, op=ALU.add), reads=["r_ex"], writes=["r_d"])
                    P.op("dve", lambda: nc.vector.tensor_tensor(out=r_d[:], in0=r_d[:], in1=r_pg[:], op=ALU.mult), reads=["r_d", "r_pg"], writes=["r_d"])
                    P.op("dve", lambda: nc.vector.reciprocal(out=r_d[:], in_=r_d[:]), reads=["r_d"], writes=["r_d"])
                    P.op("dve", lambda: nc.vector.tensor_tensor(out=r_ex[:], in0=r_ex[:], in1=r_d[:].unsqueeze(2).to_broadcast([128, NBS, 8]),
                                                                op=ALU.mult), reads=["r_ex", "r_d"], writes=["r_ex"])
                    P.op("dve", lambda: nc.vector.tensor_tensor(
                        out=comb[:].rearrange("p b (g e) -> p b g e", g=4),
                        in0=r_gm[:].unsqueeze(3).to_broadcast([128, NBS, 4, 8]),
                        in1=r_ex[:].unsqueeze(2).to_broadcast([128, NBS, 4, 8]), op=ALU.mult),
                        reads=["r_gm", "r_ex"], writes=["comb"])
                    wts = {}

                    def load_w(e):
                        w13, w13k = w13_rot.next()
                        w2, w2k = w2_rot.next()
                        P.dma("pool", lambda: nc.gpsimd.dma_start(out=w13[:, 0, :, :], in_=moe_w1[l, e].rearrange("(k p) f -> p k f", p=128)),
                              writes=[(w13k, 0)])
                        P.dma("pool", lambda: nc.gpsimd.dma_start(out=w13[:, 1, :, :], in_=moe_w3[l, e].rearrange("(k p) f -> p k f", p=128)),
                              writes=[(w13k, 1)])
                        P.dma("pool", lambda: nc.gpsimd.dma_start(out=w2[:], in_=moe_w2[l, e].rearrange("(c p) d -> p c d", p=128)),
                              writes=[w2k])
                        wts[e] = (w13, w13k, w2, w2k)

                    units = [(e, tch) for e in range(32) for tch in range(4)]
                    ust = {}

                    def stage_h(u):
                        e, tch = units[u]
                        w13, w13k, w2, w2k = wts[e]
                        he, hek = he_rot.next()
                        ust[u] = (he, hek)
                        for fch in range(2):
                            p1, p1k = hbank.next()
                            p3, p3k = hbank.next()
                            for j, (pt, pk) in enumerate(((p1, p1k), (p3, p3k))):
                                for k in range(8):
                                    P.op("pe", lambda pt=pt, j=j, k=k, fch=fch: nc.tensor.matmul(
                                        pt[:], lhsT=w13[:, j, k, fch * 128:(fch + 1) * 128], rhs=xT[:, k, tch * 512:(tch + 1) * 512],
                                        start=(k == 0), stop=(k == 7)),
                                        reads=[(w13k, j)] + [("xT", 4 * tch + i) for i in range(4)], writes=[pk])
                            s, sk = s_rot.next()
                            P.op("act", lambda s=s, p1=p1: nc.scalar.activation(out=s[:], in_=p1[:], func=AF.Silu), reads=[p1k], writes=[sk])
                            P.op("dve", lambda s=s, p3=p3, fch=fch: nc.vector.tensor_tensor(out=he[:, fch, :], in0=p3[:], in1=s[:], op=ALU.mult),
                                 reads=[p3k, sk], writes=[(hek, fch)])

                    def stage_y(u):
                        e, tch = units[u]
                        w13, w13k, w2, w2k = wts[e]
                        he, hek = ust.pop(u)
                        for tb in range(4):
                            blk = tch * 4 + tb
                            for half in range(2):
                                py, pyk = ybank.next()
                                for fch in range(2):
                                    P.op("pe", lambda py=py, fch=fch, tb=tb, half=half: nc.tensor.matmul(
                                        py[:], lhsT=he[:, fch, tb * 128:(tb + 1) * 128], rhs=w2[:, fch, half * 512:(half + 1) * 512],
                                        start=(fch == 0), stop=(fch == 1)), reads=[(hek, fch), (w2k, fch)], writes=[pyk])
                                P.op("dve", lambda py=py, blk=blk, half=half: nc.vector.scalar_tensor_tensor(
                                    out=hacc[:, blk, half * 512:(half + 1) * 512], in0=py[:], scalar=comb[:, blk, e:e + 1],
                                    in1=hacc[:, blk, half * 512:(half + 1) * 512], op0=ALU.mult, op1=ALU.add),
                                    reads=[pyk, "comb", ("hacc", blk, half)], writes=[("hacc", blk, half)])
                        if tch == 3:
                            wts.pop(e)
                            if e + 2 < 32:
                                load_w(e + 2)

                    load_w(0)
                    load_w(1)
                    nu = len(units)
                    stage_h(0)
                    for u in range(nu):
                        if u + 1 < nu:
                            stage_h(u + 1)
                        stage_y(u)
                    gbank = Rot(pb[0:4], "pb")
                    for blk in range(NBS):
                        b = sc * NBS + blk
                        hnT, hnk = hnT_rot.next()
                        norm_T(P, hacc[:, blk, :], hk(blk), gBp[:], "gBp", hnT[:], hnk)
                        pblk, pblkk = pblk_rot.next()
                        p16, p16k = pb16_rot.next()
                        pT, pTk = pT_rot.next()
                        P.dma("sp", lambda pblk=pblk, b=b: nc.sync.dma_start(out=pblk[:], in_=p[l, b * 128:(b + 1) * 128, :]), writes=[pblkk])
                        P.op("pool", lambda p16=p16, pblk=pblk: nc.gpsimd.tensor_copy(out=p16[:], in_=pblk[:]), reads=[pblkk], writes=[p16k])
                        for k in range(2):
                            P.op("pe", lambda p16=p16, k=k: nc.tensor.transpose(out=ptb[:, k, :], in_=p16[:, k * 128:(k + 1) * 128], identity=ident[:]),
                                 reads=[p16k, "ident"], writes=["ptb"])
                        P.op("act", lambda pT=pT: nc.scalar.copy(out=pT[:], in_=ptb[:, 0:2, :]), reads=["ptb"], writes=[pTk])
                        sg, sgk = sg_rot.next()
                        for half in range(2):
                            pg_, pgk = gbank.next()
                            pe_, pek = gbank.next()
                            for k in range(8):
                                P.op("pe", lambda pg_=pg_, hnT=hnT, k=k, half=half: nc.tensor.matmul(
                                    pg_[:], lhsT=hnT[:, k, :], rhs=wpg[:, k, half * 512:(half + 1) * 512], start=(k == 0), stop=(k == 7)),
                                    reads=[hnk, "wpg"], writes=[pgk])
                            for k in range(2):
                                P.op("pe", lambda pe_=pe_, pT=pT, k=k, half=half: nc.tensor.matmul(
                                    pe_[:], lhsT=pT[:, k, :], rhs=wpe[:, k, half * 512:(half + 1) * 512], start=(k == 0), stop=(k == 1)),
                                    reads=[pTk, "wpe"], writes=[pek])
                            P.op("act", lambda sg=sg, pg_=pg_, half=half: nc.scalar.activation(out=sg[:, half * 512:(half + 1) * 512], in_=pg_[:], func=AF.Sigmoid),
                                 reads=[pgk], writes=[(sgk, half)])
                            P.op("dve", lambda sg=sg, pe_=pe_, half=half: nc.vector.tensor_tensor(
                                out=sg[:, half * 512:(half + 1) * 512], in0=pe_[:], in1=sg[:, half * 512:(half + 1) * 512], op=ALU.mult),
                                reads=[pek, (sgk, half)], writes=[(sgk, half)])
                        P.op("dve", lambda sg=sg, blk=blk: nc.vector.tensor_tensor(out=hacc[:, blk, :], in0=hacc[:, blk, :], in1=sg[:], op=ALU.add),
                             reads=[(sgk, 0), (sgk, 1)] + hk(blk), writes=hk(blk))
                        if final:
                            junk, jk = njunk.next()
                            ss, ssk = nss.next()
                            norm_rstd(P, hacc[:, blk, :], hk(blk), 1024, junk[:], jk, ss[:], ssk)
                            P.op("dve", lambda ss=ss, blk=blk: nc.vector.scalar_tensor_tensor(
                                out=hacc[:, blk, :], in0=hacc[:, blk, :], scalar=ss[:], in1=gBo[:], op0=ALU.mult, op1=ALU.mult),
                                reads=hk(blk) + [ssk, "gBo"], writes=hk(blk))
                        P.dma("sp", lambda blk=blk, b=b: nc.sync.dma_start(out=hout[b * 128:(b + 1) * 128, :], in_=hacc[:, blk, :]),
                              reads=hk(blk), writes=[("hout", b)])
                    P.flush()

        def sparse_moe_pl_phase(l, hin, hout, final):
            TS = 512
            NT = 48
            NA = TS // 128
            with ExitStack() as st:
                def sb(name, shape, dt):
                    return st.enter_context(nc.sbuf_tensor(_un(name), shape, dt))
                gBf = sb("gBf", [128, 1024], F32)
                gBp = sb("gBp", [128, 1024], F32)
                load_gB(P, gBf[:], "gBf", norm_ffn[l])
                load_gB(P, gBp[:], "gBp", norm_pl[l])
                if final:
                    gBo = sb("gBo", [128, 1024], F32)
                    load_gB(P, gBo[:], "gBo", final_norm)
                wr = sb("wr", [128, 8, 36], BF16)
                P.dma("pool", lambda: nc.gpsimd.dma_start(out=wr[:, :, 0:4], in_=router_c[l].rearrange("(k p) n -> p k n", p=128)),
                      writes=["wr"])
                P.dma("pool", lambda: nc.gpsimd.dma_start(out=wr[:, :, 4:36], in_=router_f[l].rearrange("(k p) n -> p k n", p=128)),
                      writes=["wr"])
                rb = sb("rb", [128, 36], F32)
                P.dma("sp", lambda: nc.sync.dma_start(out=rb[:, 0:4], in_=router_c_b[l].partition_broadcast(128)), writes=["rb"])
                P.dma("sp", lambda: nc.sync.dma_start(out=rb[:, 4:36], in_=router_f_b[l].partition_broadcast(128)), writes=["rb"])
                wpg = sb("wpg", [128, 8, 1024], BF16)
                wpe = sb("wpe", [128, 2, 1024], BF16)
                P.dma("pool", lambda: nc.gpsimd.dma_start(out=wpg[:], in_=w_pg[l].rearrange("(k p) n -> p k n", p=128)), writes=["wpg"])
                P.dma("pool", lambda: nc.gpsimd.dma_start(out=wpe[:], in_=w_pe[l].rearrange("(k p) n -> p k n", p=128)), writes=["wpe"])
                idx1_i = sb("idx1_i", [128, NB], I32)
                idx2_i = sb("idx2_i", [128, NB], I32)
                gate1 = sb("gate1", [128, NB], F32)
                gate2 = sb("gate2", [128, NB], F32)
                widx_i = sb("widx_i", [128, NT], I32)
                pio = sb("pio", [128, 1], F32)
                P.op("pool", lambda: nc.gpsimd.iota(pio[:], pattern=[[0, 1]], base=l * 32 * 128, channel_multiplier=1,
                                                    allow_small_or_imprecise_dtypes=True), writes=["pio"])
                with ExitStack() as st1:
                    def sb1(name, shape, dt):
                        return st1.enter_context(nc.sbuf_tensor(_un(name), shape, dt))
                    xnall = sb1("xnall", [128, NB, 1024], BF16)
                    ht_rot = Rot([sb1(f"ht{i}", [128, 1024], F32) for i in range(2)], "ht")
                    xTb_rot = Rot([sb1(f"xTb{i}", [128, 8, 128], BF16) for i in range(2)], "xTb")
                    lg = sb1("lg", [128, NB, 36], F32)
                    r_mx = sb1("r_mx", [128, NB], F32)
                    r_gm = sb1("r_gm", [128, NB, 4], F32)
                    r_ec = sb1("r_ec", [128, NB, 4], F32)
                    r_pg = sb1("r_pg", [128, NB], F32)
                    r_t = sb1("r_t", [128, NB, 4, 8], F32)
                    r_lfs = sb1("r_lfs", [128, NB, 8], F32)
                    r_t8 = sb1("r_t8", [128, NB, 8], F32)
                    r_sel = sb1("r_sel", [128, NB, 8], F32)
                    r_s1 = sb1("r_s1", [128, NB, 8], F32)
                    r_s2 = sb1("r_s2", [128, NB, 8], F32)
                    r_ex = sb1("r_ex", [128, NB, 8], F32)
                    r_d = sb1("r_d", [128, NB], F32)
                    r_tmp8 = sb1("r_tmp8", [128, NB, 8], F32)
                    M1 = sb1("M1", [128, NB, 32], F32)
                    M2 = sb1("M2", [128, NB, 32], F32)
                    Mb16 = sb1("Mb16", [128, NB, 32], BF16)
                    Rm16 = sb1("Rm16", [128, NB + 1, 32], BF16)
                    rank = sb1("rank", [128, NB, 32], F32)
                    cnt = sb1("cnt", [128, 32], F32)
                    thr16 = sb1("thr16", [128, 16], F32)
                    thr64 = sb1("thr64", [128, NT], F32)
                    cmpA = sb1("cmpA", [128, 32, 16], F32)
                    ntile = sb1("ntile", [128, 32], F32)
                    csA = sb1("csA", [128, 32], F32)
                    csB = sb1("csB", [128, 32], F32)
                    base = sb1("base", [128, 32], F32)
                    cmpB = sb1("cmpB", [128, NT, 32], F32)
                    te_f = sb1("te_f", [128, NT], F32)
                    tail = sb1("tail", [128, NT], F32)
                    tot = sb1("tot", [128, 1], F32)
                    idx_f = sb1("idx_f", [128, NB], F32)
                    tmp32 = sb1("tmp32", [128, NB, 32], F32)
                    ybank = Rot(pb[4:7], "pby")
                    for b in range(NB):
                        ht, htk = ht_rot.next()
                        P.dma("sp", lambda ht=ht, b=b: nc.sync.dma_start(out=ht[:], in_=hin[b * 128:(b + 1) * 128, :]), writes=[htk])
                        junk, jk = njunk.next()
                        ss, ssk = nss.next()
                        norm_rstd(P, ht[:], htk, 1024, junk[:], jk, ss[:], ssk)
                        P.op("dve", lambda ht=ht, ss=ss, b=b: nc.vector.scalar_tensor_tensor(
                            out=xnall[:, b, :], in0=ht[:], scalar=ss[:], in1=gBf[:], op0=ALU.mult, op1=ALU.mult),
                            reads=[htk, ssk, "gBf"], writes=[("xnall", b)])
                        for k in range(8):
                            P.op("pe", lambda k=k, b=b: nc.tensor.transpose(out=ptb[:, k, :], in_=xnall[:, b, k * 128:(k + 1) * 128], identity=ident[:]),
                                 reads=[("xnall", b), "ident"], writes=["ptb"])
                        xTb, xTbk = xTb_rot.next()
                        P.op("act", lambda xTb=xTb: nc.scalar.copy(out=xTb[:], in_=ptb[:]), reads=["ptb"], writes=[xTbk])
                        pt, pk = ybank.next()
                        for k in range(8):
                            P.op("pe", lambda pt=pt, k=k, xTb=xTb: nc.tensor.matmul(
                                pt[:, 0:36], lhsT=xTb[:, k, :], rhs=wr[:, k, :], start=(k == 0), stop=(k == 7)),
                                reads=[xTbk, "wr"], writes=[pk])
                        P.op("dve", lambda pt=pt, b=b: nc.vector.tensor_tensor(out=lg[:, b, :], in0=pt[:, 0:36], in1=rb[:], op=ALU.add),
                             reads=[pk, "rb"], writes=["lg"])
                    lc = lg[:, :, 0:4]
                    lf = lg[:, :, 4:36].rearrange("p b (g e) -> p b g e", g=4)

                    def bc(ap, shape):
                        return ap.to_broadcast(shape)

                    def dv(fn, reads, writes):
                        P.op("dve", fn, reads=reads, writes=writes)
                    dv(lambda: nc.vector.tensor_reduce(out=r_mx[:], in_=lc, axis=AX.X, op=ALU.max), ["lg"], ["r_mx"])
                    dv(lambda: nc.vector.tensor_tensor(out=r_gm[:], in0=lc, in1=bc(r_mx[:].unsqueeze(2), [128, NB, 4]), op=ALU.is_ge), ["lg", "r_mx"], ["r_gm"])
                    dv(lambda: nc.vector.tensor_tensor(out=r_ec[:], in0=lc, in1=bc(r_mx[:].unsqueeze(2), [128, NB, 4]), op=ALU.subtract), ["lg", "r_mx"], ["r_ec"])
                    P.op("act", lambda: nc.scalar.activation(out=r_ec[:], in_=r_ec[:], func=AF.Exp), reads=["r_ec"], writes=["r_ec"])
                    dv(lambda: nc.vector.tensor_reduce(out=r_pg[:], in_=r_ec[:], axis=AX.X, op=ALU.add), ["r_ec"], ["r_pg"])
                    dv(lambda: nc.vector.tensor_tensor(out=r_t[:], in0=lf, in1=bc(r_gm[:].unsqueeze(3), [128, NB, 4, 8]), op=ALU.mult), ["lg", "r_gm"], ["r_t"])
                    dv(lambda: nc.vector.tensor_reduce(out=r_lfs[:], in_=r_t[:].rearrange("p b g e -> p b e g"), axis=AX.X, op=ALU.add), ["r_t"], ["r_lfs"])
                    for b in range(NB):
                        dv(lambda b=b: nc.vector.max(out=r_t8[:, b, :], in_=r_lfs[:, b, :]), ["r_lfs"], ["r_t8"])
                    l1b = bc(r_t8[:, :, 0:1], [128, NB, 8])
                    l2b = bc(r_t8[:, :, 1:2], [128, NB, 8])
                    dv(lambda: nc.vector.tensor_tensor(out=r_sel[:], in0=r_lfs[:], in1=l2b, op=ALU.is_ge), ["r_lfs", "r_t8"], ["r_sel"])
                    dv(lambda: nc.vector.tensor_tensor(out=r_s1[:], in0=r_lfs[:], in1=l1b, op=ALU.is_ge), ["r_lfs", "r_t8"], ["r_s1"])
                    dv(lambda: nc.vector.tensor_tensor(out=r_s2[:], in0=r_sel[:], in1=r_s1[:], op=ALU.subtract), ["r_sel", "r_s1"], ["r_s2"])
                    dv(lambda: nc.vector.tensor_tensor(out=r_ex[:], in0=r_lfs[:], in1=l1b, op=ALU.subtract), ["r_lfs", "r_t8"], ["r_ex"])
                    P.op("act", lambda: nc.scalar.activation(out=r_ex[:], in_=r_ex[:], func=AF.Exp), reads=["r_ex"], writes=["r_ex"])
                    dv(lambda: nc.vector.tensor_tensor(out=r_ex[:], in0=r_ex[:], in1=r_sel[:], op=ALU.mult), ["r_ex", "r_sel"], ["r_ex"])
                    dv(lambda: nc.vector.tensor_reduce(out=r_d[:], in_=r_ex[:], axis=AX.X, op=ALU.add), ["r_ex"], ["r_d"])
                    dv(lambda: nc.vector.tensor_tensor(out=r_d[:], in0=r_d[:], in1=r_pg[:], op=ALU.mult), ["r_d", "r_pg"], ["r_d"])
                    dv(lambda: nc.vector.reciprocal(out=r_d[:], in_=r_d[:]), ["r_d"], ["r_d"])
                    dv(lambda: nc.vector.tensor_tensor(out=r_ex[:], in0=r_ex[:], in1=bc(r_d[:].unsqueeze(2), [128, NB, 8]), op=ALU.mult), ["r_ex", "r_d"], ["r_ex"])
                    dv(lambda: nc.vector.tensor_tensor(out=r_tmp8[:], in0=r_ex[:], in1=r_s1[:], op=ALU.mult), ["r_ex", "r_s1"], ["r_tmp8"])
                    dv(lambda: nc.vector.tensor_reduce(out=gate1[:], in_=r_tmp8[:], axis=AX.X, op=ALU.add), ["r_tmp8"], ["gate1"])
                    dv(lambda: nc.vector.tensor_tensor(out=r_tmp8[:], in0=r_ex[:], in1=r_s2[:], op=ALU.mult), ["r_ex", "r_s2", "gate1"], ["r_tmp8"])
                    dv(lambda: nc.vector.tensor_reduce(out=gate2[:], in_=r_tmp8[:], axis=AX.X, op=ALU.add), ["r_tmp8"], ["gate2"])
                    g4 = bc(r_gm[:].unsqueeze(3), [128, NB, 4, 8])
                    dv(lambda: nc.vector.tensor_tensor(out=M1[:].rearrange("p b (g e) -> p b g e", g=4), in0=g4,
                                                       in1=bc(r_s1[:].unsqueeze(2), [128, NB, 4, 8]), op=ALU.mult), ["r_gm", "r_s1"], ["M1"])
                    dv(lambda: nc.vector.tensor_tensor(out=M2[:].rearrange("p b (g e) -> p b g e", g=4), in0=g4,
                                                       in1=bc(r_s2[:].unsqueeze(2), [128, NB, 4, 8]), op=ALU.mult), ["r_gm", "r_s2"], ["M2"])
                    dv(lambda: nc.vector.tensor_tensor(out=Mb16[:], in0=M1[:], in1=M2[:], op=ALU.add), ["M1", "M2"], ["Mb16"])
                    P.op("pool", lambda: nc.gpsimd.memset(Rm16[:, 0, :], 0.0), writes=[("Rm", 0)])
                    for b in range(NB):
                        dv(lambda b=b: nc.vector.tensor_tensor(out=Rm16[:, b + 1, :], in0=Rm16[:, b, :], in1=Mb16[:, b, :], op=ALU.add),
                           [("Rm", b), "Mb16"], [("Rm", b + 1)])
                    rbank = [pb[0], pb[1]]
                    for b in range(NB):
                        pt = rbank[b // 16]
                        sl = slice((b % 16) * 32, (b % 16) * 32 + 32)
                        P.op("pe", lambda pt=pt, sl=sl, b=b: nc.tensor.matmul(pt[:, sl], lhsT=lstrict[:], rhs=Mb16[:, b, :], start=True, stop=False),
                             reads=["lstrict", "Mb16"], writes=[("pb", b // 16)])
                        P.op("pe", lambda pt=pt, sl=sl, b=b: nc.tensor.matmul(pt[:, sl], lhsT=pones[:], rhs=Rm16[:, b, :], start=False, stop=True),
                             reads=["pones", ("Rm", b)], writes=[("pb", b // 16)])
                    for hf in range(2):
                        dv(lambda hf=hf: nc.vector.tensor_copy(out=rank[:, hf * 16:(hf + 1) * 16, :],
                                                               in_=rbank[hf][:].rearrange("p (b e) -> p b e", b=16)),
                           [("pb", hf)], [("rank", hf)])
                    P.op("pe", lambda: nc.tensor.matmul(pb[2][:, 0:32], lhsT=pones[:], rhs=Rm16[:, NB, :], start=True, stop=True),
                         reads=["pones", ("Rm", NB)], writes=[("pb", 2)])
                    dv(lambda: nc.vector.tensor_copy(out=cnt[:], in_=pb[2][:, 0:32]), [("pb", 2)], ["cnt"])
                    P.op("pool", lambda: nc.gpsimd.iota(thr16[:], pattern=[[TS, 16]], base=0, channel_multiplier=0, allow_small_or_imprecise_dtypes=True), writes=["thr16"])
                    P.op("pool", lambda: nc.gpsimd.iota(thr64[:], pattern=[[TS, NT]], base=0, channel_multiplier=0, allow_small_or_imprecise_dtypes=True), writes=["thr64"])
                    dv(lambda: nc.vector.tensor_tensor(out=cmpA[:], in0=bc(cnt[:].unsqueeze(2), [128, 32, 16]), in1=bc(thr16[:].unsqueeze(1), [128, 32, 16]), op=ALU.is_gt),
                       ["cnt", "thr16"], ["cmpA"])
                    dv(lambda: nc.vector.tensor_reduce(out=ntile[:], in_=cmpA[:], axis=AX.X, op=ALU.add), ["cmpA"], ["ntile"])
                    src, srck = ntile, "ntile"
                    bufs = [(csA, "csA"), (csB, "csB")]
                    for si, sh in enumerate((1, 2, 4, 8, 16)):
                        dst, dstk = bufs[si % 2]
                        dv(lambda src=src, dst=dst, sh=sh: nc.vector.tensor_copy(out=dst[:, 0:sh], in_=src[:, 0:sh]), [srck], [(dstk, 0)])
                        dv(lambda src=src, dst=dst, sh=sh: nc.vector.tensor_tensor(out=dst[:, sh:32], in0=src[:, sh:32], in1=src[:, 0:32 - sh], op=ALU.add),
                           [srck], [(dstk, 1)])
                        src, srck = dst, dstk
                        srck_list = [(dstk, 0), (dstk, 1)]
                        srck = dstk
                        P.op("dve", lambda dst=dst: nc.vector.tensor_copy(out=dst[:, 0:1], in_=dst[:, 0:1]), reads=srck_list, writes=[dstk])
                    dv(lambda src=src: nc.vector.tensor_tensor(out=base[:], in0=src[:], in1=ntile[:], op=ALU.subtract), [srck, "ntile"], ["base"])
                    dv(lambda: nc.vector.tensor_scalar(out=base[:], in0=base[:], scalar1=float(TS), scalar2=None, op0=ALU.mult), ["base"], ["base"])
                    dv(lambda: nc.vector.tensor_tensor(out=cmpB[:], in0=bc(base[:].unsqueeze(1), [128, NT, 32]), in1=bc(thr64[:].unsqueeze(2), [128, NT, 32]), op=ALU.is_le),
                       ["base", "thr64"], ["cmpB"])
                    dv(lambda: nc.vector.tensor_reduce(out=te_f[:], in_=cmpB[:], axis=AX.X, op=ALU.add), ["cmpB"], ["te_f"])
                    dv(lambda: nc.vector.tensor_scalar(out=te_f[:], in0=te_f[:], scalar1=-1.0, scalar2=128.0, op0=ALU.add, op1=ALU.mult), ["te_f"], ["te_f"])
                    dv(lambda: nc.vector.tensor_scalar(out=te_f[:], in0=te_f[:], scalar1=pio[:, 0:1], scalar2=None, op0=ALU.add), ["te_f", "pio"], ["te_f"])
                    dv(lambda src=src: nc.vector.tensor_scalar(out=tot[:], in0=src[:, 31:32], scalar1=float(TS), scalar2=None, op0=ALU.mult), [srck], ["tot"])
                    dv(lambda: nc.vector.tensor_scalar(out=tail[:], in0=thr64[:], scalar1=tot[:, 0:1], scalar2=1.0e6, op0=ALU.is_ge, op1=ALU.mult),
                       ["thr64", "tot"], ["tail"])
                    dv(lambda: nc.vector.tensor_tensor(out=te_f[:], in0=te_f[:], in1=tail[:], op=ALU.add), ["te_f", "tail"], ["te_f"])
                    dv(lambda: nc.vector.tensor_copy(out=widx_i[:], in_=te_f[:]), ["te_f"], ["widx_i"])
                    dv(lambda: nc.vector.tensor_tensor(out=rank[:], in0=rank[:], in1=bc(base[:].unsqueeze(1), [128, NB, 32]), op=ALU.add),
                       [("rank", 0), ("rank", 1), "base"], ["rankp"])
                    for (Mk, Mkk, idxi, idxk) in ((M1, "M1", idx1_i, "idx1_i"), (M2, "M2", idx2_i, "idx2_i")):
                        dv(lambda Mk=Mk: nc.vector.tensor_tensor(out=tmp32[:], in0=rank[:], in1=Mk[:], op=ALU.mult), ["rankp", Mkk, "idx_f"], ["tmp32"])
                        dv(lambda: nc.vector.tensor_reduce(out=idx_f[:], in_=tmp32[:], axis=AX.X, op=ALU.add), ["tmp32"], ["idx_f"])
                        dv(lambda idxi=idxi: nc.vector.tensor_copy(out=idxi[:], in_=idx_f[:]), ["idx_f"], [idxk])
                    for b in range(NB):
                        for (idxi, idxk) in ((idx1_i, "idx1_i"), (idx2_i, "idx2_i")):
                            P.dma("pool", lambda b=b, idxi=idxi: nc.gpsimd.indirect_dma_start(
                                out=XS[:, :], out_offset=bass.IndirectOffsetOnAxis(ap=idxi[:, b:b + 1], axis=0),
                                in_=xnall[:, b, :], in_offset=None), reads=[("xnall", b), idxk], writes=["XS"])
                    P.flush()
                if sub < 2:
                    return
                with ExitStack() as st2_:
                    def sb2(name, shape, dt):
                        return st2_.enter_context(nc.sbuf_tensor(_un(name), shape, dt))
                    w13_rot = Rot([sb2(f"w13_{i}", [128, 2, 8, 256], BF16) for i in range(2)], "w13")
                    w2_rot = Rot([sb2(f"w2_{i}", [128, 2, 1024], BF16) for i in range(2)], "w2")
                    stg_rot = Rot([sb2(f"stg{i}", [128, 3, 2048], F32) for i in range(3)], "stg")
                    xs_rot = Rot([sb2(f"xs{i}", [128, NA, 1024], BF16) for i in range(2)], "xs")
                    xT_rot = Rot([sb2(f"xTt{i}", [128, 8, TS], BF16) for i in range(2)], "xTt")
                    s_rot = Rot([sb2(f"s{i}", [128, TS], F32) for i in range(2)], "s")
                    he_rot = Rot([sb2(f"he{i}", [128, 2, TS], BF16) for i in range(2)], "he")
                    yt_rot = Rot([sb2(f"yt{i}", [128, 1024], F32) for i in range(3)], "yt")
                    hbank = Rot(pb[0:4], "pb")
                    ybank = Rot(pb[4:7], "pby")
                    wts = {}
                    tst = {}
                    bcreg = {}

                    def mkreg():
                        bcreg["r"] = nc.gpsimd.to_reg(2 * 32 * 128 - 1)
                        return None
                    P.op("pool", mkreg, nosig=True)

                    wstg = {}

                    def cast_w(i):
                        stg, stgk = wstg.pop(i)
                        w13, w13k = w13_rot.next()
                        w2, w2k = w2_rot.next()
                        wts[i] = (w13, w13k, w2, w2k)
                        P.op("act", lambda: nc.scalar.copy(out=w13[:, 0, :, :].rearrange("p k f -> p (k f)"), in_=stg[:, 0, :]),
                             reads=[(stgk, 0)], writes=[(w13k, 0)])
                        P.op("dve", lambda: nc.vector.tensor_copy(out=w13[:, 1, :, :].rearrange("p k f -> p (k f)"), in_=stg[:, 1, :]),
                             reads=[(stgk, 1)], writes=[(w13k, 1)])
                        P.op("act", lambda: nc.scalar.copy(out=w2[:, 0, :], in_=stg[:, 2, 0:1024]),
                             reads=[(stgk, 2)], writes=[(w2k, 0)])
                        P.op("dve", lambda: nc.vector.tensor_copy(out=w2[:, 1, :], in_=stg[:, 2, 1024:2048]),
                             reads=[(stgk, 2)], writes=[(w2k, 1)])

                    def load_w(i):
                        stg, stgk = stg_rot.next()
                        ix = bass.IndirectOffsetOnAxis(ap=widx_i[:, i:i + 1], axis=0)
                        for j, (src_ap, pat, kw) in enumerate(((moe_w1, "l e (p k) f -> (l e p) (k f)", dict(k=8)),
                                                              (moe_w3, "l e (p k) f -> (l e p) (k f)", dict(k=8)),
                                                              (moe_w2, "l e (p c) d -> (l e p) (c d)", dict(c=2)))):
                            P.dma("pool", lambda j=j, src_ap=src_ap, pat=pat, kw=kw: nc.gpsimd.indirect_dma_start(
                                out=stg[:, j, :], out_offset=None, in_=src_ap.rearrange(pat, **kw), in_offset=ix,
                                bounds_check=bcreg["r"], oob_is_err=False),
                                reads=["widx_i"], writes=[(stgk, j)])
                        wstg[i] = (stg, stgk)

                    xsl = {}

                    def load_xs(i):
                        xs, xsk = xs_rot.next()
                        P.dma("sp", lambda: nc.sync.dma_start(out=xs[:], in_=XS[i * TS:(i + 1) * TS, :].rearrange("(a p) d -> p a d", p=128)),
                              writes=[xsk])
                        xsl[i] = (xs, xsk)

                    def stage_t(i):
                        cast_w(i)
                        if i + 3 < NT:
                            load_w(i + 3)
                        if i + 1 < NT:
                            load_xs(i + 1)
                        xs, xsk = xsl.pop(i)
                        xT, xTk = xT_rot.next()
                        for a in range(NA):
                            for k in range(8):
                                P.op("pe", lambda a=a, k=k: nc.tensor.transpose(out=ptb[:, k, :], in_=xs[:, a, k:1024:8], identity=ident[:]),
                                     reads=[xsk, "ident"], writes=["ptb"])
                            if a % 2 == 0:
                                P.op("act", lambda a=a: nc.scalar.copy(out=xT[:, :, a * 128:(a + 1) * 128], in_=ptb[:]), reads=["ptb"], writes=[(xTk, a)])
                            else:
                                P.op("dve", lambda a=a: nc.vector.tensor_copy(out=xT[:, :, a * 128:(a + 1) * 128], in_=ptb[:]), reads=["ptb"], writes=[(xTk, a)])
                        w13, w13k, w2, w2k = wts[i]
                        he, hek = he_rot.next()
                        tst[i] = (he, hek)
                        for fch in range(2):
                            p1, p1k = hbank.next()
                            p3, p3k = hbank.next()
                            for j, (pt, pk) in enumerate(((p1, p1k), (p3, p3k))):
                                for k in range(8):
                                    P.op("pe", lambda pt=pt, j=j, k=k, fch=fch: nc.tensor.matmul(
                                        pt[:, 0:TS], lhsT=w13[:, j, k, fch:256:2], rhs=xT[:, k, :],
                                        start=(k == 0), stop=(k == 7)),
                                        reads=[(w13k, j)] + [(xTk, a_) for a_ in range(NA)], writes=[pk])
                            s, sk = s_rot.next()
                            P.op("act", lambda s=s, p1=p1: nc.scalar.activation(out=s[:], in_=p1[:, 0:TS], func=AF.Silu), reads=[p1k], writes=[sk])
                            P.op("dve", lambda s=s, p3=p3, fch=fch: nc.vector.tensor_tensor(out=he[:, fch, :], in0=p3[:, 0:TS], in1=s[:], op=ALU.mult),
                                 reads=[p3k, sk], writes=[(hek, fch)])

                    def stage_y(i):
                        w13, w13k, w2, w2k = wts.pop(i)
                        he, hek = tst.pop(i)
                        for a in range(NA):
                            yt, ytk = yt_rot.next()
                            for half in range(2):
                                py, pyk = ybank.next()
                                for fch in range(2):
                                    P.op("pe", lambda py=py, fch=fch, a=a, half=half: nc.tensor.matmul(
                                        py[:], lhsT=he[:, fch, a * 128:(a + 1) * 128], rhs=w2[:, fch, half * 512:(half + 1) * 512],
                                        start=(fch == 0), stop=(fch == 1)), reads=[(hek, fch), (w2k, fch)], writes=[pyk])
                                if half == 0:
                                    P.op("act", lambda py=py, yt=yt, half=half: nc.scalar.copy(out=yt[:, half * 512:(half + 1) * 512], in_=py[:]),
                                         reads=[pyk], writes=[(ytk, half)])
                                else:
                                    P.op("dve", lambda py=py, yt=yt, half=half: nc.vector.tensor_copy(out=yt[:, half * 512:(half + 1) * 512], in_=py[:]),
                                         reads=[pyk], writes=[(ytk, half)])
                            r0 = i * TS + a * 128
                            P.dma("sp", lambda yt=yt, r0=r0: nc.sync.dma_start(out=YS[r0:r0 + 128, :], in_=yt[:]),
                                  reads=[(ytk, 0), (ytk, 1)], writes=[("YS", i, a)])

                    load_w(0)
                    load_w(1)
                    load_w(2)
                    load_xs(0)
                    stage_t(0)
                    for i in range(NT):
                        if i + 1 < NT:
                            stage_t(i + 1)
                        stage_y(i)
                    P.flush()
                if sub < 3:
                    return
                with ExitStack() as st3_:
                    def sb3(name, shape, dt):
                        return st3_.enter_context(nc.sbuf_tensor(_un(name), shape, dt))
                    ht_rot = Rot([sb3(f"htc{i}", [128, 1024], F32) for i in range(5)], "htc")
                    y1_rot = Rot([sb3(f"y1_{i}", [128, 1024], F32) for i in range(4)], "y1")
                    y2_rot = Rot([sb3(f"y2_{i}", [128, 1024], F32) for i in range(4)], "y2")
                    pblk_rot = Rot([sb3(f"pblk{i}", [128, 256], F32) for i in range(4)], "pblk")
                    pb16_rot = Rot([sb3(f"pb16{i}", [128, 256], BF16) for i in range(2)], "pb16")
                    pT_rot = Rot([sb3(f"pT{i}", [128, 2, 128], BF16) for i in range(2)], "pT")
                    hnT_rot = Rot([sb3(f"hnT{i}", [128, 8, 128], BF16) for i in range(2)], "hnT")
                    sg_rot = Rot([sb3(f"sg{i}", [128, 1024], F32) for i in range(2)], "sg")
                    gbank = Rot(pb[0:4], "pb")
                    ld = {}

                    def loads7(b):
                        ht, htk = ht_rot.next()
                        y1, y1k = y1_rot.next()
                        y2, y2k = y2_rot.next()
                        pblk, pblkk = pblk_rot.next()
                        P.dma("sp", lambda: nc.sync.dma_start(out=ht[:], in_=hin[b * 128:(b + 1) * 128, :]), writes=[htk])
                        P.dma("sp", lambda: nc.sync.dma_start(out=pblk[:], in_=p[l, b * 128:(b + 1) * 128, :]), writes=[pblkk])
                        P.dma("pool", lambda: nc.gpsimd.indirect_dma_start(
                            out=y1[:, :], out_offset=None, in_=YS[:, :], in_offset=bass.IndirectOffsetOnAxis(ap=idx1_i[:, b:b + 1], axis=0)),
                            reads=["idx1_i"], writes=[y1k])
                        P.dma("pool", lambda: nc.gpsimd.indirect_dma_start(
                            out=y2[:, :], out_offset=None, in_=YS[:, :], in_offset=bass.IndirectOffsetOnAxis(ap=idx2_i[:, b:b + 1], axis=0)),
                            reads=["idx2_i"], writes=[y2k])
                        ld[b] = (ht, htk, y1, y1k, y2, y2k, pblk, pblkk)

                    loads7(0)
                    loads7(1)
                    for b in range(NB):
                        if b + 2 < NB:
                            loads7(b + 2)
                        ht, htk, y1, y1k, y2, y2k, pblk, pblkk = ld.pop(b)
                        P.op("dve", lambda ht=ht, y1=y1, b=b: nc.vector.scalar_tensor_tensor(
                            out=ht[:], in0=y1[:], scalar=gate1[:, b:b + 1], in1=ht[:], op0=ALU.mult, op1=ALU.add),
                            reads=[htk, y1k, "gate1"], writes=[htk])
                        P.op("dve", lambda ht=ht, y2=y2, b=b: nc.vector.scalar_tensor_tensor(
                            out=ht[:], in0=y2[:], scalar=gate2[:, b:b + 1], in1=ht[:], op0=ALU.mult, op1=ALU.add),
                            reads=[htk, y2k, "gate2"], writes=[htk])
                        hnT, hnk = hnT_rot.next()
                        norm_T(P, ht[:], htk, gBp[:], "gBp", hnT[:], hnk)
                        p16, p16k = pb16_rot.next()
                        pT, pTk = pT_rot.next()
                        P.op("pool", lambda p16=p16, pblk=pblk: nc.gpsimd.tensor_copy(out=p16[:], in_=pblk[:]), reads=[pblkk], writes=[p16k])
                        for k in range(2):
                            P.op("pe", lambda p16=p16, k=k: nc.tensor.transpose(out=ptb[:, k, :], in_=p16[:, k * 128:(k + 1) * 128], identity=ident[:]),
                                 reads=[p16k, "ident"], writes=["ptb"])
                        P.op("act", lambda pT=pT: nc.scalar.copy(out=pT[:], in_=ptb[:, 0:2, :]), reads=["ptb"], writes=[pTk])
                        sg, sgk = sg_rot.next()
                        for half in range(2):
                            pg_, pgk = gbank.next()
                            pe_, pek = gbank.next()
                            for k in range(8):
                                P.op("pe", lambda pg_=pg_, hnT=hnT, k=k, half=half: nc.tensor.matmul(
                                    pg_[:], lhsT=hnT[:, k, :], rhs=wpg[:, k, half * 512:(half + 1) * 512], start=(k == 0), stop=(k == 7)),
                                    reads=[hnk, "wpg"], writes=[pgk])
                            for k in range(2):
                                P.op("pe", lambda pe_=pe_, pT=pT, k=k, half=half: nc.tensor.matmul(
                                    pe_[:], lhsT=pT[:, k, :], rhs=wpe[:, k, half * 512:(half + 1) * 512], start=(k == 0), stop=(k == 1)),
                                    reads=[pTk, "wpe"], writes=[pek])
                            P.op("act", lambda sg=sg, pg_=pg_, half=half: nc.scalar.activation(out=sg[:, half * 512:(half + 1) * 512], in_=pg_[:], func=AF.Sigmoid),
                                 reads=[pgk], writes=[(sgk, half)])
                            P.op("dve", lambda sg=sg, pe_=pe_, half=half: nc.vector.tensor_tensor(
                                out=sg[:, half * 512:(half + 1) * 512], in0=pe_[:], in1=sg[:, half * 512:(half + 1) * 512], op=ALU.mult),
                                reads=[pek, (sgk, half)], writes=[(sgk, half)])
                        P.op("pool", lambda sg=sg, ht=ht: nc.gpsimd.tensor_tensor(out=ht[:], in0=ht[:], in1=sg[:], op=ALU.add),
                             reads=[(sgk, 0), (sgk, 1), htk], writes=[htk])
                        if final:
                            junk, jk = njunk.next()
                            ss, ssk = nss.next()
                            norm_rstd(P, ht[:], htk, 1024, junk[:], jk, ss[:], ssk)
                            P.op("dve", lambda ss=ss, ht=ht: nc.vector.scalar_tensor_tensor(
                                out=ht[:], in0=ht[:], scalar=ss[:], in1=gBo[:], op0=ALU.mult, op1=ALU.mult),
                                reads=[htk, ssk, "gBo"], writes=[htk])
                        P.dma("sp", lambda ht=ht, b=b: nc.sync.dma_start(out=hout[b * 128:(b + 1) * 128, :], in_=ht[:]),
                              reads=[htk], writes=[("hout", b)])
                    P.flush()

        SPARSE = True
        mphase = sparse_moe_pl_phase if SPARSE else moe_pl_phase
        if stop_after >= 5:
            mphase(0, hA, hB, False)

        if stop_after >= 6:
            with ExitStack() as st:
                def sb(name, shape, dt):
                    return st.enter_context(nc.sbuf_tensor(_un(name), shape, dt))
                wi = sb("wi", [128, 8, 4096], BF16)
                wi_src = w_in_odd[0].rearrange("(k p) n -> p k n", p=128)
                for k in range(8):
                    P.dma("pool", lambda k=k: nc.gpsimd.dma_start(out=wi[:, k, :], in_=wi_src[:, k, :]), writes=[("wi", k)])
                wi_keys = [("wi", k) for k in range(8)]
                wo1 = sb("wo1", [128, 16, 1024], BF16)
                wo_src = w_out_odd[0].rearrange("(c p) n -> p c n", p=128)
                for c in range(0, 16, 4):
                    P.dma("pool", lambda c=c: nc.gpsimd.dma_start(out=wo1[:, c:c + 4, :], in_=wo_src[:, c:c + 4, :]), writes=[("wo1", c)])
                wo_keys = [("wo1", c) for c in range(0, 16, 4)]
                gB1 = sb("gB1", [128, 1024], F32)
                load_gB(P, gB1[:], "gB1", norm_mix[1])
                gvB = sb("gvB", [128, 2048], F32)
                P.dma("sp", lambda: nc.sync.dma_start(out=gvB[:], in_=g_v_odd[0].partition_broadcast(128)), writes=["gvB"])
                bsf = sb("bsf", [1, 8, 128], F32)
                bs16 = sb("bs16", [1, 8, 128], BF16)
                P.dma("sp", lambda: nc.sync.dma_start(out=bsf[:], in_=b_s_odd[0:1]), writes=["bsf"])
                P.op("dve", lambda: nc.vector.tensor_copy(out=bs16[:], in_=bsf[:]), reads=["bsf"], writes=["bs16"])
                wsT = sb("wsT", [128, 8, 128], BF16)
                st3 = ExitStack()
                st3.__enter__()
                wsf = st3.enter_context(nc.sbuf_tensor(_un("wsf"), [128, 8, 128], F32))
                ws16 = st3.enter_context(nc.sbuf_tensor(_un("ws16"), [128, 8, 128], BF16))
                P.dma("sp", lambda: nc.sync.dma_start(out=wsf[:], in_=w_s_odd[0].rearrange("g t s -> t g s")), writes=["wsf"])
                for g in range(8):
                    P.op("pool", lambda g=g: nc.gpsimd.affine_select(out=wsf[:, g, :], in_=wsf[:, g, :], pattern=[[-1, 128]],
                                                                      compare_op=ALU.is_ge, fill=0.0, base=0, channel_multiplier=1),
                         reads=["wsf"], writes=["wsf"])
                P.op("dve", lambda: nc.vector.tensor_copy(out=ws16[:], in_=wsf[:]), reads=["wsf"], writes=["ws16"])
                for g in range(8):
                    P.op("pe", lambda g=g: nc.tensor.transpose(out=ptb[:, g, :], in_=ws16[:, g, :], identity=ident[:]),
                         reads=["ws16", "ident"], writes=["ptb"])
                P.op("dve", lambda: nc.vector.tensor_copy(out=wsT[:], in_=ptb[:]), reads=["ptb"], writes=["wsT"])
                P.flush()
                st3.__exit__(None, None, None)
                ht_rot = Rot([sb(f"ht{i}", [128, 1024], F32) for i in range(5)], "ht")
                xg_rot = Rot([sb(f"xg{i}", [128, 8, 512], BF16) for i in range(2)], "xg")
                uT_rot = Rot([sb(f"uT{i}", [128, 16, 512], BF16) for i in range(1)], "uT")
                vt_rot = Rot([sb(f"vt{i}", [128, 2048], F32) for i in range(2)], "vt")
                vn_rot = Rot([sb(f"vn{i}", [128, 2048], BF16) for i in range(1)], "vn")
                yT_rot = Rot([sb(f"yT{i}", [128, 16, 128], BF16) for i in range(2)], "yT")
                abank = Rot(pb[0:3], "pb")
                gbank = Rot(pb[3:5], "pbg")
                obank = Rot(pb[5:7], "pbo")
                for tc in range(8):
                    xg, xgk = xg_rot.next()
                    hts = []
                    for tb in range(4):
                        b = tc * 4 + tb
                        ht, htk = ht_rot.next()
                        hts.append((ht, htk))
                        P.dma("sp", lambda ht=ht, b=b: nc.sync.dma_start(out=ht[:], in_=hB[b * 128:(b + 1) * 128, :]),
                              reads=[("hout", b)], writes=[htk])
                        norm_T(P, ht[:], htk, gB1[:], "gB1", xg[:, :, tb * 128:(tb + 1) * 128], (xgk, tb))
                    xgkeys = [(xgk, tb) for tb in range(4)]
                    uT, uTk = uT_rot.next()
                    for fcu in range(16):
                        pt, pk = abank.next()
                        for k in range(8):
                            P.op("pe", lambda pt=pt, k=k, fcu=fcu, xg=xg: nc.tensor.matmul(
                                pt[:], lhsT=wi[:, k, fcu * 128:(fcu + 1) * 128], rhs=xg[:, k, :], start=(k == 0), stop=(k == 7)),
                                reads=[("wi", k)] + xgkeys, writes=[pk])
                        P.op("act", lambda pt=pt, uT=uT, fcu=fcu: nc.scalar.activation(out=uT[:, fcu, :], in_=pt[:], func=AF.Gelu_apprx_tanh),
                             reads=[pk], writes=[(uTk, fcu)])
                    for tb in range(4):
                        b = tc * 4 + tb
                        ht, htk = hts[tb]
                        vt, vtk = vt_rot.next()
                        for vg in range(4):
                            pt, pk = abank.next()
                            for k in range(8):
                                P.op("pe", lambda pt=pt, k=k, vg=vg, xg=xg, tb=tb: nc.tensor.matmul(
                                    pt[:], lhsT=xg[:, k, tb * 128:(tb + 1) * 128], rhs=wi[:, k, 2048 + vg * 512: 2048 + (vg + 1) * 512],
                                    start=(k == 0), stop=(k == 7)), reads=[("wi", k), (xgk, tb)], writes=[pk])
                            P.op("act", lambda pt=pt, vt=vt, vg=vg: nc.scalar.activation(out=vt[:, vg * 512:(vg + 1) * 512], in_=pt[:], func=AF.Gelu_apprx_tanh),
                                 reads=[pk], writes=[(vtk, vg)])
                        ss, ssk = nss.next()
                        vtkeys = [(vtk, vg) for vg in range(4)]
                        vn, vnk = vn_rot.next()
                        P.op("act", lambda vt=vt, ss=ss, vn=vn: nc.scalar.activation(out=vn[:], in_=vt[:], func=AF.Square, accum_out=ss[:]),
                             reads=vtkeys, writes=[vnk, ssk])
                        P.op("act", lambda ss=ss: nc.scalar.activation(out=ss[:], in_=ss[:], func=AF.Ln, scale=1.0 / 2048, bias=epsb[:]),
                             reads=[ssk, "epsb"], writes=[ssk])
                        P.op("act", lambda ss=ss: nc.scalar.activation(out=ss[:], in_=ss[:], func=AF.Exp, scale=-0.5), reads=[ssk], writes=[ssk])
                        P.op("dve", lambda vn=vn, vt=vt, ss=ss: nc.vector.scalar_tensor_tensor(out=vn[:], in0=vt[:], scalar=ss[:], in1=gvB[:],
                                                                                              op0=ALU.mult, op1=ALU.mult),
                             reads=vtkeys + [ssk, "gvB"], writes=[vnk])
                        yT, yTk = yT_rot.next()
                        for q4 in range(4):
                            pg_, pgk = gbank.next()
                            for i4 in range(4):
                                fcu = q4 * 4 + i4
                                g = fcu // 2
                                P.op("pe", lambda pg_=pg_, i4=i4, fcu=fcu, g=g, vn=vn: nc.tensor.matmul(
                                    pg_[:, i4 * 128:(i4 + 1) * 128], lhsT=vn[:, fcu * 128:(fcu + 1) * 128], rhs=wsT[:, g, :], start=True, stop=False),
                                    reads=[vnk, "wsT"], writes=[pgk])
                                P.op("pe", lambda pg_=pg_, i4=i4, g=g: nc.tensor.matmul(
                                    pg_[:, i4 * 128:(i4 + 1) * 128], lhsT=ones1[0:1, :], rhs=bs16[0:1, g, :], start=False, stop=True),
                                    reads=["ones1", "bs16"], writes=[pgk])
                            P.op("dve", lambda pg_=pg_, yT=yT, uT=uT, q4=q4, tb=tb: nc.vector.tensor_tensor(
                                out=yT[:, q4 * 4:(q4 + 1) * 4, :], in0=pg_[:].rearrange("p (i t) -> p i t", i=4),
                                in1=uT[:, q4 * 4:(q4 + 1) * 4, tb * 128:(tb + 1) * 128], op=ALU.mult),
                                reads=[pgk] + [(uTk, q4 * 4 + i) for i in range(4)], writes=[(yTk, q4)])
                        for half in range(2):
                            po_, pok = obank.next()
                            for fcu in range(16):
                                P.op("pe", lambda po_=po_, yT=yT, fcu=fcu, half=half: nc.tensor.matmul(
                                    po_[:], lhsT=yT[:, fcu, :], rhs=wo1[:, fcu, half * 512:(half + 1) * 512], start=(fcu == 0), stop=(fcu == 15)),
                                    reads=[(yTk, fcu // 4), ("wo1", (fcu // 4) * 4)], writes=[pok])
                            P.op("dve", lambda po_=po_, ht=ht, half=half: nc.vector.tensor_tensor(
                                out=ht[:, half * 512:(half + 1) * 512], in0=po_[:], in1=ht[:, half * 512:(half + 1) * 512], op=ALU.add),
                                reads=[pok, htk], writes=[htk])
                        P.dma("sp", lambda ht=ht, b=b: nc.sync.dma_start(out=hA[b * 128:(b + 1) * 128, :], in_=ht[:]),
                              reads=[htk], writes=[("hA", b)])
                P.flush()

        if stop_after >= 7:
            mphase(1, hA, out, True)
        P.flush()
        nc._prog_stats = dict(nops=P.nops, ccount=dict(P.ccount), dcount=dict(P.dcount))
    return nc


_NC_CACHE = {}


def kernel(**inputs):
    n = 8
    if "nc" not in _NC_CACHE:
        _NC_CACHE["nc"] = build()
    nc = _NC_CACHE["nc"]
    in_maps = []
    for c in range(n):
        m = {}
        for k, v in inputs.items():
            v = np.asarray(v)
            if k == "x":
                m[k] = np.ascontiguousarray(v[c])
            elif k == "p":
                m[k] = np.ascontiguousarray(v[:, c])
            else:
                m[k] = np.ascontiguousarray(v)
        in_maps.append(m)
    res = run_bass_kernel_spmd(nc, in_maps, core_ids=list(range(n)))
    return np.stack([np.asarray(r["out"]) for r in res.results], axis=0).astype(np.float32)
```
